# Optimizing a Trainium2 kernel written in Bass

```python
import jax, jax.numpy as jnp
from jax import lax
import numpy as np

D_MODEL = 1024
BATCH = 8
SEQ = 4096
DEPTH = 1

HEAD_DIM = 64
MOBA_HEADS = D_MODEL // 128
NSA_HEADS = D_MODEL // 128
NSA_KV_GROUPS = 2
NSA_HPG = NSA_HEADS // NSA_KV_GROUPS
MOBA_BLOCK = 256
MOBA_TOPK = 3
CMP_LEN = 32
CMP_STRIDE = 16
CMP_HIDDEN = 128
SLC_BLOCK = 64
SLC_TOPN = 16
WINDOW = 512
GATHER_Q_CHUNK = 16
BAND_Q_CHUNK = 128
RMS_EPS = 1e-6
NEG_INF = -1e30
FORCED_SCORE = 1e9

MOBA_W = MOBA_HEADS * HEAD_DIM
NSA_W = NSA_HEADS * HEAD_DIM
NSA_KV_W = NSA_KV_GROUPS * HEAD_DIM
MIX_W = MOBA_W + NSA_W
IN_SPLITS = [MOBA_W, MOBA_W, MOBA_W, MOBA_W, NSA_W,
             NSA_KV_W, NSA_KV_W, NSA_KV_W, NSA_KV_W, NSA_KV_W, NSA_KV_W,
             3 * NSA_HEADS, NSA_W]
D_IN = sum(IN_SPLITS)

kernel_name = "hymba_moba_nsa_sandwich_alibi"


def _rmsnorm(x, g):
    xf = x.astype(jnp.float32)
    r = lax.rsqrt(jnp.mean(xf * xf, axis=-1, keepdims=True) + RMS_EPS)
    return (xf * r * g.astype(jnp.float32)).astype(x.dtype)


def _alibi_slopes(n):
    return jnp.asarray(2.0 ** (-8.0 * np.arange(1, n + 1) / n), dtype=jnp.float32)


def _masked_softmax(s, mask):
    s = jnp.where(mask, s.astype(jnp.float32), NEG_INF)
    m = jnp.max(s, axis=-1, keepdims=True)
    p = jnp.where(mask, jnp.exp(s - m), 0.0)
    return p / jnp.maximum(jnp.sum(p, axis=-1, keepdims=True), 1e-30)


def _moba(q, k, v, slopes):
    B, H, S, dh = q.shape
    nb = -(-S // MOBA_BLOCK)
    pad = nb * MOBA_BLOCK - S
    kb = jnp.pad(k, ((0, 0), (0, 0), (0, pad), (0, 0))).reshape(B, H, nb, MOBA_BLOCK, dh)
    vb = jnp.pad(v, ((0, 0), (0, 0), (0, pad), (0, 0))).reshape(B, H, nb, MOBA_BLOCK, dh)
    kmean = jnp.mean(kb, axis=3)
    pos = jnp.arange(S)
    own = pos // MOBA_BLOCK
    gate = jnp.einsum('bhsd,bhnd->bhsn', q, kmean).astype(jnp.float32)
    past = jnp.arange(nb)[None, :] < own[:, None]
    gate = jnp.where(past, gate, NEG_INF)
    _, top_idx = lax.top_k(gate, min(MOBA_TOPK, nb))
    top_valid = top_idx < own[:, None]
    own_b = jnp.broadcast_to(own[:, None], (B, H, S, 1)).astype(top_idx.dtype)
    idx = jnp.concatenate([top_idx, own_b], axis=-1)
    valid = jnp.concatenate([top_valid, jnp.ones((B, H, S, 1), bool)], axis=-1)
    scale = dh ** -0.5
    bi = jnp.arange(B)[:, None, None, None]
    hi = jnp.arange(H)[None, :, None, None]
    offs = jnp.arange(MOBA_BLOCK)
    n_chunks = S // GATHER_Q_CHUNK

    def chunk(q0):
        qc = lax.dynamic_slice_in_dim(q, q0, GATHER_Q_CHUNK, axis=2)
        ic = lax.dynamic_slice_in_dim(idx, q0, GATHER_Q_CHUNK, axis=2)
        vm = lax.dynamic_slice_in_dim(valid, q0, GATHER_Q_CHUNK, axis=2)
        kg = kb[bi, hi, ic]
        vg = vb[bi, hi, ic]
        s = jnp.einsum('bhqd,bhqjkd->bhqjk', qc, kg).astype(jnp.float32) * scale
        tq = q0 + jnp.arange(GATHER_Q_CHUNK)
        dist = tq[:, None, None] - (ic[..., None] * MOBA_BLOCK + offs)
        mask = vm[..., None] & (dist >= 0)
        s = s - slopes[None, :, None, None, None] * dist.astype(jnp.float32)
        shp = s.shape
        p = _masked_softmax(s.reshape(B, H, GATHER_Q_CHUNK, -1),
                            mask.reshape(B, H, GATHER_Q_CHUNK, -1)).reshape(shp)
        return jnp.einsum('bhqjk,bhqjkd->bhqd', p.astype(v.dtype), vg)

    outs = lax.map(chunk, jnp.arange(n_chunks, dtype=jnp.int32) * GATHER_Q_CHUNK)
    return outs.transpose(1, 2, 0, 3, 4).reshape(B, H, S, dh)


def _compress(x, pe, w1, w2):
    B, G, S, dh = x.shape
    n_cmp = (S - CMP_LEN) // CMP_STRIDE + 1
    cidx = jnp.arange(n_cmp)[:, None] * CMP_STRIDE + jnp.arange(CMP_LEN)[None, :]
    blocks = x[:, :, cidx] + pe
    flat = blocks.reshape(B, G, n_cmp, CMP_LEN * dh)
    return jax.nn.silu(flat @ w1) @ w2


def _nsa(q, k_cmp, v_cmp, k_slc, v_slc, k_win, v_win, gates, slopes,
         pos_k, pos_v, w_ck1, w_ck2, w_cv1, w_cv2):
    B, G, HPG, S, dh = q.shape
    scale = dh ** -0.5
    t = jnp.arange(S)
    sl = slopes.reshape(G, HPG)

    kc = _compress(k_cmp, pos_k, w_ck1, w_ck2)
    vc = _compress(v_cmp, pos_v, w_cv1, w_cv2)
    n_cmp = kc.shape[2]
    cstart = jnp.arange(n_cmp) * CMP_STRIDE
    cmask = (cstart + CMP_LEN - 1)[None, :] <= t[:, None]
    sc = jnp.einsum('bghtd,bgcd->bghtc', q, kc).astype(jnp.float32) * scale
    p_cmp = _masked_softmax(sc, cmask)
    o_cmp = jnp.einsum('bghtc,bgcd->bghtd', p_cmp.astype(vc.dtype), vc)

    n_slc = S // SLC_BLOCK
    jstart = jnp.arange(n_slc) * SLC_BLOCK
    overlap = ((cstart[:, None] < jstart[None, :] + SLC_BLOCK) &
               (cstart[:, None] + CMP_LEN > jstart[None, :])).astype(jnp.float32)
    imp = jnp.einsum('bghtc,cj->bgtj', p_cmp, overlap)
    own = t // SLC_BLOCK
    jb = jnp.arange(n_slc)
    forced = (jb[None, :] == 0) | (jb[None, :] == own[:, None]) | (jb[None, :] == own[:, None] - 1)
    causal_blk = jb[None, :] <= own[:, None]
    imp = jnp.where(forced, FORCED_SCORE, jnp.where(causal_blk, imp, NEG_INF))
    _, sidx = lax.top_k(imp, min(SLC_TOPN, n_slc))
    svalid = sidx <= own[:, None]

    ksb = k_slc.reshape(B, G, n_slc, SLC_BLOCK, dh)
    vsb = v_slc.reshape(B, G, n_slc, SLC_BLOCK, dh)
    bi = jnp.arange(B)[:, None, None, None]
    gi = jnp.arange(G)[None, :, None, None]
    offs = jnp.arange(SLC_BLOCK)

    def slc_chunk(q0):
        qc = lax.dynamic_slice_in_dim(q, q0, GATHER_Q_CHUNK, axis=3)
        ic = lax.dynamic_slice_in_dim(sidx, q0, GATHER_Q_CHUNK, axis=2)
        vm = lax.dynamic_slice_in_dim(svalid, q0, GATHER_Q_CHUNK, axis=2)
        kg = ksb[bi, gi, ic]
        vg = vsb[bi, gi, ic]
        s = jnp.einsum('bghqd,bgqnkd->bghqnk', qc, kg).astype(jnp.float32) * scale
        tq = q0 + jnp.arange(GATHER_Q_CHUNK)
        dist = tq[:, None, None] - (ic[..., None] * SLC_BLOCK + offs)
        mask = (vm[..., None] & (dist >= 0))[:, :, None]
        s = s - sl[None, :, :, None, None, None] * dist[:, :, None].astype(jnp.float32)
        shp = s.shape
        mask = jnp.broadcast_to(mask, shp)
        p = _masked_softmax(s.reshape(B, G, HPG, GATHER_Q_CHUNK, -1),
                            mask.reshape(B, G, HPG, GATHER_Q_CHUNK, -1)).reshape(shp)
        return jnp.einsum('bghqnk,bgqnkd->bghqd', p.astype(v_slc.dtype), vg)

    o_slc = lax.map(slc_chunk, jnp.arange(S // GATHER_Q_CHUNK, dtype=jnp.int32) * GATHER_Q_CHUNK)
    o_slc = o_slc.transpose(1, 2, 3, 0, 4, 5).reshape(B, G, HPG, S, dh)

    kwp = jnp.pad(k_win, ((0, 0), (0, 0), (WINDOW, 0), (0, 0)))
    vwp = jnp.pad(v_win, ((0, 0), (0, 0), (WINDOW, 0), (0, 0)))
    span = WINDOW + BAND_Q_CHUNK

    def win_chunk(q0):
        qc = lax.dynamic_slice_in_dim(q, q0, BAND_Q_CHUNK, axis=3)
        kw = lax.dynamic_slice_in_dim(kwp, q0, span, axis=2)
        vw = lax.dynamic_slice_in_dim(vwp, q0, span, axis=2)
        s = jnp.einsum('bghqd,bgkd->bghqk', qc, kw).astype(jnp.float32) * scale
        tq = q0 + jnp.arange(BAND_Q_CHUNK)
        sp = q0 - WINDOW + jnp.arange(span)
        dist = tq[:, None] - sp[None, :]
        mask = (dist >= 0) & (dist < WINDOW) & (sp[None, :] >= 0)
        s = s - sl[None, :, :, None, None] * dist.astype(jnp.float32)
        p = _masked_softmax(s, mask)
        return jnp.einsum('bghqk,bgkd->bghqd', p.astype(v_win.dtype), vw)

    o_win = lax.map(win_chunk, jnp.arange(S // BAND_Q_CHUNK, dtype=jnp.int32) * BAND_Q_CHUNK)
    o_win = o_win.transpose(1, 2, 3, 0, 4, 5).reshape(B, G, HPG, S, dh)

    return gates[..., 0:1] * o_cmp + gates[..., 1:2] * o_slc + gates[..., 2:3] * o_win


def setup_inputs(seed: int = 0) -> dict:
    key = jax.random.key(seed)
    ks = jax.random.split(key, 12)
    f32 = jnp.float32
    x = jax.random.normal(ks[0], (BATCH, SEQ, D_MODEL), f32)
    pre_norm_g = 1.0 + 0.05 * jax.random.normal(ks[1], (DEPTH, D_MODEL), f32)
    post_norm_g = 1.0 + 0.05 * jax.random.normal(ks[2], (DEPTH, D_MODEL), f32)
    w_in = jax.random.normal(ks[3], (DEPTH, D_MODEL, D_IN), f32) * D_MODEL ** -0.5
    cmp_pos_k = 0.1 * jax.random.normal(ks[4], (DEPTH, CMP_LEN, HEAD_DIM), f32)
    cmp_pos_v = 0.1 * jax.random.normal(ks[5], (DEPTH, CMP_LEN, HEAD_DIM), f32)
    w_cmp_k1 = jax.random.normal(ks[6], (DEPTH, CMP_LEN * HEAD_DIM, CMP_HIDDEN), f32) * (CMP_LEN * HEAD_DIM) ** -0.5
    w_cmp_k2 = jax.random.normal(ks[7], (DEPTH, CMP_HIDDEN, HEAD_DIM), f32) * CMP_HIDDEN ** -0.5
    w_cmp_v1 = jax.random.normal(ks[8], (DEPTH, CMP_LEN * HEAD_DIM, CMP_HIDDEN), f32) * (CMP_LEN * HEAD_DIM) ** -0.5
    w_cmp_v2 = jax.random.normal(ks[9], (DEPTH, CMP_HIDDEN, HEAD_DIM), f32) * CMP_HIDDEN ** -0.5
    w_out = jax.random.normal(ks[10], (DEPTH, MIX_W, D_MODEL), f32) * MIX_W ** -0.5
    return {"x": x, "pre_norm_g": pre_norm_g, "post_norm_g": post_norm_g, "w_in": w_in,
            "cmp_pos_k": cmp_pos_k, "cmp_pos_v": cmp_pos_v,
            "w_cmp_k1": w_cmp_k1, "w_cmp_k2": w_cmp_k2,
            "w_cmp_v1": w_cmp_v1, "w_cmp_v2": w_cmp_v2, "w_out": w_out}


def reference(x, pre_norm_g, post_norm_g, w_in, cmp_pos_k, cmp_pos_v,
              w_cmp_k1, w_cmp_k2, w_cmp_v1, w_cmp_v2, w_out):
    B, S, _ = x.shape
    split_points = np.cumsum(IN_SPLITS)[:-1].tolist()
    slopes_a = _alibi_slopes(MOBA_HEADS)
    slopes_b = _alibi_slopes(NSA_HEADS)
    for l in range(DEPTH):
        h = _rmsnorm(x, pre_norm_g[l])
        proj = h @ w_in[l]
        (qa, ka, va, za, qb, kcm, vcm, ksl, vsl, kwi, vwi, g_logit, zb) = jnp.split(proj, split_points, axis=-1)

        def heads_a(t):
            return t.reshape(B, S, MOBA_HEADS, HEAD_DIM).transpose(0, 2, 1, 3)
        oa = _moba(heads_a(qa), heads_a(ka), heads_a(va), slopes_a)
        oa = oa.transpose(0, 2, 1, 3).reshape(B, S, MOBA_W) * jax.nn.silu(za)

        def heads_kv(t):
            return t.reshape(B, S, NSA_KV_GROUPS, HEAD_DIM).transpose(0, 2, 1, 3)
        qn = qb.reshape(B, S, NSA_KV_GROUPS, NSA_HPG, HEAD_DIM).transpose(0, 2, 3, 1, 4)
        gates = jax.nn.sigmoid(g_logit.astype(jnp.float32)).astype(qb.dtype)
        gates = gates.reshape(B, S, NSA_KV_GROUPS, NSA_HPG, 3).transpose(0, 2, 3, 1, 4)
        ob = _nsa(qn, heads_kv(kcm), heads_kv(vcm), heads_kv(ksl), heads_kv(vsl),
                  heads_kv(kwi), heads_kv(vwi), gates, slopes_b,
                  cmp_pos_k[l], cmp_pos_v[l], w_cmp_k1[l], w_cmp_k2[l], w_cmp_v1[l], w_cmp_v2[l])
        ob = ob.transpose(0, 3, 1, 2, 4).reshape(B, S, NSA_W) * jax.nn.silu(zb)

        y = jnp.concatenate([oa, ob], axis=-1) @ w_out[l]
        x = x + _rmsnorm(y.astype(x.dtype), post_norm_g[l])
    return x
```

```python
import numpy as np
from contextlib import ExitStack
import concourse.bass as bass
import concourse.mybir as mybir
from concourse.bass_utils import run_bass_kernel_spmd

F32 = mybir.dt.float32
BF16 = mybir.dt.bfloat16
ALU = mybir.AluOpType
AF = mybir.ActivationFunctionType
AX = mybir.AxisListType

S = 4096
D = 1024
NT = 32
DIN = 3864
NEGM = -30000.0
ENG = ("sync", "scalar", "vector", "gpsimd", "tensor")


class Sem:
    def __init__(self, h):
        self.h = h
        self.n = 0


class SemPool:
    def __init__(self, nc, es, ndma=24):
        self.eng = {e: Sem(es.enter_context(nc.semaphore(f"pool_{e}"))) for e in ENG}
        self.dma = {"gpsimd": [Sem(es.enter_context(nc.semaphore(f"pool_g{i}"))) for i in range(ndma)],
                    "sync": [Sem(es.enter_context(nc.semaphore(f"pool_s{i}"))) for i in range(ndma)],
                    "scalar": [Sem(es.enter_context(nc.semaphore(f"pool_a{i}"))) for i in range(4)]}

    def all(self):
        return list(self.eng.values()) + self.dma["gpsimd"] + self.dma["sync"] + self.dma["scalar"]


POOL = [None]


class Blk:
    def __init__(self, nc, name):
        self.nc = nc
        self.name = name
        self.ops = {e: [] for e in ENG}
        self.esem = POOL[0].eng
        self.k = {"gpsimd": 0, "sync": 0, "scalar": 0}

    def newsem(self, kind="gpsimd"):
        lst = POOL[0].dma[kind]
        s_ = lst[self.k[kind] % len(lst)]
        self.k[kind] += 1
        s_.kind = kind
        return s_

    def op(self, eng, fn, waits=(), sig=False):
        tok = None
        s = None
        if sig:
            s = self.esem[eng]
            s.n += 1
            tok = (s.h, s.n)
        self.ops[eng].append((fn, tuple(w for w in waits if w is not None), s, 1))
        return tok

    def dma(self, eng, out, in_, sem, waits=()):
        assert sem.kind == eng, (sem.kind, eng)
        sem.n += 16
        tok = (sem.h, sem.n)
        self.ops[eng].append((lambda e: e.dma_start(out=out, in_=in_), tuple(w for w in waits if w is not None), sem, 16))
        return tok

    def last(self, eng):
        s = self.esem[eng]
        return (s.h, s.n) if s.n > 0 else None

    def run(self):
        with self.nc.Block() as block:
            for e in ENG:
                ops = self.ops[e]

                def body(eng, ops=ops):
                    seen = {}
                    for fn, waits, s, amt in ops:
                        for (h, v) in waits:
                            key = id(h)
                            if seen.get(key, 0) >= v:
                                continue
                            seen[key] = v
                            eng.wait_ge(h, v)
                        ins = fn(eng)
                        if s is not None:
                            ins.then_inc(s.h, amt)

                getattr(block, e)(body)


def _slopes():
    return [2.0 ** (-(i + 1)) for i in range(8)]


def host_consts():
    c = {}
    c["c_ident"] = np.eye(128, dtype=np.float32)
    k = np.arange(S)
    c["c_E16"] = (k[None, :] // 256 == np.arange(16)[:, None]).astype(np.float32)
    c["c_E64"] = (NEGM * (k[None, :] // 64 == np.arange(64)[:, None])).astype(np.float32)
    am = np.zeros((128, NT, 16), np.float32)
    for tt in range(NT):
        own = tt // 2
        am[:, tt, own] = 1e9
        am[:, tt, own + 1:] = -1e9
    c["c_addm_moba"] = am
    a2 = np.zeros((128, NT, 64), np.float32)
    t = (np.arange(NT)[None, :] * 128 + np.arange(128)[:, None])
    own = t // 64
    j = np.arange(64)[None, None, :]
    a2 = np.where(j > own[:, :, None], -1e9, 0.0).astype(np.float32)
    a2 = np.where(j == 0, 1e9, a2)
    a2 = np.where(j == own[:, :, None] - 1, 2e9, a2)
    a2 = np.where(j == own[:, :, None], 3e9, a2)
    c["c_addm_slc"] = a2.astype(np.float32)
    cr = np.arange(128)[:, None, None]
    r = np.arange(4)[None, :, None]
    tr = np.arange(512)[None, None, :]
    c["c_cmask"] = (16 * cr + 31 - 512 * r <= tr).astype(np.float32)
    kk = np.arange(128)[:, None]
    tq = np.arange(128)[None, :]
    c["c_tri"] = np.stack([(kk <= tq), (kk > tq)], axis=1).astype(np.float32)
    sl = np.array(_slopes(), np.float32)
    dl = np.arange(32)[None, None, :]
    c["c_btab"] = (sl[None, :, None] * (np.arange(128)[:, None, None] - 128.0 * dl - 64.0)).astype(np.float32)
    tmod = (np.arange(S) % 128).astype(np.float32)
    qrow = np.zeros((8, 2, S), np.float32)
    krow = np.zeros((8, 2, S), np.float32)
    for h in range(8):
        qrow[h, 0] = -sl[h] * tmod
        qrow[h, 1] = 1.0
        krow[h, 0] = 1.0
        krow[h, 1] = sl[h] * tmod
    c["c_qrow"] = qrow
    c["c_krow"] = krow
    rc = np.zeros((128, 2, 65), np.float32)
    cidx = np.arange(2)[None, :] * 128 + np.arange(128)[:, None]
    valid = cidx < 255
    rc[:, :, 0] = valid
    cst = cidx * 16
    js = np.arange(64)[None, None, :] * 64
    ov = (cst[:, :, None] < js + 64) & (cst[:, :, None] + 32 > js) & valid[:, :, None]
    rc[:, :, 1:] = ov
    c["c_rc"] = rc
    return c


def host_weights(pre_norm_g, post_norm_g, w_in, cmp_pos_k, cmp_pos_v, w_cmp_k1, w_cmp_k2, w_cmp_v1, w_cmp_v2, w_out):
    w = {}
    W = np.ascontiguousarray(w_in[0].reshape(8, 128, DIN).transpose(1, 0, 2))

    def cols(a, n=64):
        return W[:, :, a:a + n]

    w["wA"] = np.ascontiguousarray(np.stack(
        [np.stack([cols(0 + h * 64), cols(512 + h * 64), cols(1024 + h * 64)], axis=1) for h in range(8)], 0))
    zc = [1536 + p * 128 for p in range(4)] + [3352 + p * 128 for p in range(4)]
    w["wZ"] = np.ascontiguousarray(np.stack([cols(a, 128) for a in zc], 0))
    w["wQB"] = np.ascontiguousarray(np.stack([cols(2048 + h * 64) for h in range(8)], 0))
    w["wG"] = np.ascontiguousarray(cols(3328, 24))
    kv = []
    for g in range(2):
        kv.append(np.concatenate([cols(2560 + g * 64), cols(2688 + g * 64), cols(2816 + g * 64),
                                  cols(3072 + g * 64), cols(2944 + g * 64), cols(3200 + g * 64)], axis=2))
    w["wKV"] = np.ascontiguousarray(np.stack(kv, 0))
    k1 = w_cmp_k1[0].reshape(32, 64, 128).transpose(1, 0, 2)
    v1 = w_cmp_v1[0].reshape(32, 64, 128).transpose(1, 0, 2)
    w["w1kv"] = np.ascontiguousarray(np.concatenate([k1, v1], 0))
    w["w2kv"] = np.ascontiguousarray(np.stack([w_cmp_k2[0], w_cmp_v2[0]], 1))
    w["peT"] = np.ascontiguousarray(np.concatenate([cmp_pos_k[0].T, cmp_pos_v[0].T], 0))
    w["wO"] = np.ascontiguousarray(w_out[0].reshape(8, 128, D).transpose(1, 0, 2))
    w["gpre"] = np.ascontiguousarray(pre_norm_g[0].reshape(8, 128).T)
    w["gpost"] = np.ascontiguousarray(np.broadcast_to(post_norm_g[0][None, :], (128, D)))
    return {k: np.asarray(v, np.float32) for k, v in w.items()}


def emit_attention(B, T, groups, single_acc_key=None, filler=None, nS=2, nPT=3):
    psS, PT = T.psS, T.PT
    flat = []
    for gi, g in enumerate(groups):
        for ti, t in enumerate(g["tiles"]):
            flat.append((gi, ti, t))
    n = len(flat)
    act_done = [None] * n
    rdy = [None] * n
    s_done = [None] * n
    pv_done = [None] * n
    epi_done = [None] * len(groups)
    last_pv_of_group = [None] * len(groups)

    def emit_S(i):
        gi, ti, t = flat[i]
        w = [act_done[i - nS] if i >= nS else None]
        ew = t.get("extra_wait")
        if ew is not None:
            w += list(ew) if isinstance(ew, (list, tuple)) and not (len(ew) == 2 and not isinstance(ew[0], tuple)) else [ew]
        m = len(t["smm"])
        for j, (o, l, r) in enumerate(t["smm"]):
            tk = B.op("tensor", lambda e, o=o, l=l, r=r: e.matmul(o, l, r, start=True, stop=True),
                      waits=w if j == 0 else (), sig=(j == m - 1))
        s_done[i] = tk

    def emit_act(i):
        gi, ti, t = flat[i]
        in_, o, bias = t["act"]
        w = [s_done[i], pv_done[i - nPT] if i >= nPT else None]
        if bias is None:
            act_done[i] = B.op("scalar", lambda e, in_=in_, o=o: e.activation(out=o, in_=in_, func=AF.Exp), waits=w, sig=True)
        else:
            act_done[i] = B.op("scalar", lambda e, in_=in_, o=o, bias=bias: e.activation(out=o, in_=in_, func=AF.Exp, bias=bias),
                               waits=w, sig=True)
        rdy[i] = act_done[i]
        if t.get("mask") is not None:
            ap, mk = t["mask"]
            rdy[i] = B.op("vector", lambda e, ap=ap, mk=mk: e.tensor_tensor(out=ap, in0=ap, in1=mk, op=ALU.mult),
                          waits=[act_done[i]], sig=True)

    def emit_PV(i):
        gi, ti, t = flat[i]
        if filler is not None:
            filler(B)
        w = [rdy[i]]
        if ti == 0 and gi >= 2:
            w.append(epi_done[gi - 2])
        if single_acc_key and t.get(single_acc_key) and gi >= 1:
            w.append(epi_done[gi - 1])
        m = len(t["pv"])
        for j, (o, l, r, st) in enumerate(t["pv"]):
            tk = B.op("tensor", lambda e, o=o, l=l, r=r, st=st: e.matmul(o, l, r, start=st, stop=False),
                      waits=w if j == 0 else (), sig=(j == m - 1))
        pv_done[i] = tk
        if ti == len(groups[gi]["tiles"]) - 1:
            last_pv_of_group[gi] = tk
            if gi >= 1 and groups[gi - 1].get("post"):
                groups[gi - 1]["post"](B)
            epi_done[gi] = groups[gi]["epi"](B, tk)

    dD = nS - 1
    for i in range(n + dD):
        if i < n:
            emit_S(i)
            emit_act(i)
        if i >= dD:
            emit_PV(i - dD)
    if groups and groups[-1].get("post"):
        groups[-1]["post"](B)


def _build_rest(nc, L, debug, dbg, stop):
    import types
    T = types.SimpleNamespace(**{k: v for k, v in L.items() if k not in ("es", "B")})
    hT, OT, QA, KA, KW, VA, VW, PT, Ost = T.hT, T.OT, T.QA, T.KA, T.KW, T.VA, T.VW, T.PT, T.Ost
    psS, psO, psO2, psM, psT = T.psS, T.psO, T.psO2, T.psM, T.psT
    ident, btab, tri, cmask, rden = T.ident, T.btab, T.tri, T.cmask, T.rden
    dram = T.dram
    banks4 = [psS[0], psS[1], psO[0], psO[1]]
    SB4 = [psS[0], psS[1], psO2[0], psO2[1]]

    def v3(ap2d, a):
        return ap2d.rearrange("p (a b) -> p a b", a=a)

    def gating_block(name, p, wz, zs):
        B = Blk(nc, name)
        s1 = B.newsem()
        t_w = B.dma("gpsimd", wz[:], dram["wZ"][p], s1)
        z_free = [None, None]
        ps_free = [None, None]
        for qt in range(8):
            b = qt % 2
            for c in range(8):
                t_mm = B.op("tensor", lambda e, b=b, c=c, qt=qt: e.matmul(
                    psS[b][:], wz[:, c, :], hT[:, c, qt * 512:(qt + 1) * 512], start=(c == 0), stop=(c == 7)),
                    waits=[t_w, ps_free[b]] if c == 0 else (), sig=(c == 7))
            t_s = B.op("scalar", lambda e, b=b: e.activation(out=zs[b][:], in_=psS[b][:], func=AF.Silu),
                       waits=[t_mm, z_free[b]], sig=True)
            ps_free[b] = t_s
            z_free[b] = B.op("vector", lambda e, b=b, qt=qt: e.tensor_tensor(
                out=OT[:, p, qt * 512:(qt + 1) * 512], in0=OT[:, p, qt * 512:(qt + 1) * 512], in1=zs[b][:], op=ALU.mult),
                waits=[t_s], sig=True)
        B.run()

    with ExitStack() as esM:
        def sbm(name, shape, dt):
            return esM.enter_context(nc.sbuf_tensor("m_" + name, list(shape), dt))
        Wqkv = sbm("Wqkv", [128, 3, 8, 64], BF16)
        maskpad = sbm("maskpad", [128, NT, 80], BF16)
        scg = sbm("scg", [128, NT, 16], F32)
        m8 = sbm("m8", [128, NT, 8], F32)
        tmpf = sbm("tmpf", [128, NT, 16], F32)
        kmf = sbm("kmf", [64, 16], F32)
        kmb = sbm("kmb", [64, 16], BF16)
        wz = sbm("wz", [128, 8, 128], BF16)
        zs = [sbm(f"zs{i}", [128, 512], BF16) for i in range(2)]

        B = Blk(nc, "bM0")
        s1 = B.newsem()
        t1 = B.op("vector", lambda e: e.memset(maskpad[:], 0.0), sig=True)
        t2 = B.dma("gpsimd", KA[64:80, :], dram["c_E16"][:], s1)
        B.op("sync", lambda e: e.nop(), waits=[t1, t2])
        B.run()

        nheads = 8 if stop is None else int(stop.get("moba_heads", 8)) if isinstance(stop, dict) else 8
        for h in range(nheads):
            pair, off = h // 2, (h % 2) * 64
            B = Blk(nc, f"bP{h}")
            s1 = B.newsem()
            t_w = B.dma("gpsimd", Wqkv[:], dram["wA"][h], s1)
            t_qr = B.dma("gpsimd", QA[80:82, :], dram["c_qrow"][h], B.newsem())
            t_kr = B.dma("gpsimd", KA[80:82, :], dram["c_krow"][h], B.newsem())
            bank_free = [None] * 4
            bi = 0
            q_ev = []
            k_ev = []
            for qt in range(8):
                for which in (0, 1):
                    bk = banks4[bi % 4]
                    for c in range(8):
                        t_mm = B.op("tensor", lambda e, bk=bk, which=which, c=c, qt=qt: e.matmul(
                            bk[0:64, :], Wqkv[:, which, c, :], hT[:, c, qt * 512:(qt + 1) * 512], start=(c == 0), stop=(c == 7)),
                            waits=[t_w, bank_free[bi % 4]] if c == 0 else (), sig=(c == 7))
                    if which == 0:
                        t_e = B.op("scalar", lambda e, bk=bk, qt=qt: e.activation(
                            out=QA[0:64, qt * 512:(qt + 1) * 512], in_=bk[0:64, :], func=AF.Copy, scale=0.125), waits=[t_mm], sig=True)
                        q_ev.append(t_e)
                    else:
                        t_e = B.op("vector", lambda e, bk=bk, qt=qt: e.tensor_copy(
                            out=KA[0:64, qt * 512:(qt + 1) * 512], in_=bk[0:64, :]), waits=[t_mm], sig=True)
                        k_ev.append(t_e)
                    bank_free[bi % 4] = t_e
                    bi += 1
            for tg in range(8):
                bk = banks4[bi % 4]
                for j in range(4):
                    tt = tg * 4 + j
                    for c in range(8):
                        t_mm = B.op("tensor", lambda e, bk=bk, j=j, c=c, tt=tt: e.matmul(
                            bk[:, j * 64:(j + 1) * 64], hT[:, c, tt * 128:(tt + 1) * 128], Wqkv[:, 2, c, :], start=(c == 0), stop=(c == 7)),
                            waits=[bank_free[bi % 4]] if (c == 0 and j == 0) else (), sig=(c == 7 and j == 3))
                t_e = B.op("scalar", lambda e, bk=bk, tg=tg: e.copy(
                    out=VA[:, tg * 4:(tg + 1) * 4, 0:64], in_=v3(bk[:, 0:256], 4)), waits=[t_mm], sig=True)
                bank_free[bi % 4] = t_e
                bi += 1
            t_km = B.op("vector", lambda e: e.tensor_reduce(out=kmf[:], in_=KA[0:64, :].rearrange("p (j k) -> p j k", k=256),
                                                          axis=AX.X, op=ALU.add), waits=[k_ev[-1], t_qr, t_kr], sig=True)
            t_kb = B.op("vector", lambda e: e.tensor_scalar(out=kmb[:], in0=kmf[:], scalar1=1.0 / 256, scalar2=None, op0=ALU.mult),
                        waits=[t_km], sig=True)
            for tt in range(NT):
                t_g = B.op("tensor", lambda e, tt=tt: e.matmul(psM[:, tt * 16:(tt + 1) * 16], QA[0:64, tt * 128:(tt + 1) * 128], kmb[:],
                                                              start=True, stop=True),
                           waits=[t_kb, q_ev[-1]] if tt == 0 else (), sig=(tt == NT - 1))
            t_sc = B.op("vector", lambda e: e.tensor_tensor(out=scg[:], in0=v3(psM[:], NT), in1=T.addm_moba[:], op=ALU.add),
                        waits=[t_g], sig=True)
            for tt in range(NT):
                t_m8 = B.op("vector", lambda e, tt=tt: e.max(out=m8[:, tt, :], in_=scg[:, tt, :]),
                            waits=[t_sc] if tt == 0 else (), sig=(tt == NT - 1))
            t_c = B.op("vector", lambda e: e.tensor_tensor(out=tmpf[:], in0=scg[:], in1=m8[:, :, 3:4].to_broadcast([128, NT, 16]),
                                                         op=ALU.is_lt), waits=[t_m8], sig=True)
            t_mp = B.op("vector", lambda e: e.tensor_scalar(out=maskpad[:, :, 64:80], in0=tmpf[:], scalar1=NEGM, scalar2=None,
                                                          op0=ALU.mult), waits=[t_c], sig=True)
            tb_free = [bank_free[0], bank_free[1]]
            for g8 in range(8):
                bk = psS[g8 % 2]
                for j in range(4):
                    tt = g8 * 4 + j
                    t_tr = B.op("tensor", lambda e, bk=bk, j=j, tt=tt: e.matmul(
                        bk[0:80, j * 128:(j + 1) * 128], maskpad[:, tt, :], ident[:], start=True, stop=True),
                        waits=[t_mp, tb_free[g8 % 2]] if j == 0 else (), sig=(j == 3))
                tb_free[g8 % 2] = B.op("vector", lambda e, bk=bk, g8=g8: e.tensor_copy(
                    out=QA[64:80, g8 * 512:(g8 + 1) * 512], in_=bk[64:80, :]), waits=[t_tr], sig=True)
            B.run()

            B = Blk(nc, f"bT{h}")
            groups = []
            post_state = {"psT_free": None}
            for qt in range(8):
                aset = qt % 2
                acc = v3(psO[aset][:], 4)
                tiles = []
                first = True
                for dl in range(4 * qt + 3, -1, -1):
                    smm, pv = [], []
                    qs_valid = [qs for qs in range(4) if 4 * qt + qs - dl >= 0]
                    i_tile = len(tiles)
                    for qs in qs_valid:
                        kt = 4 * qt + qs - dl
                        tq = 4 * qt + qs
                        smm.append(("S", qs, KA[0:82, kt * 128:(kt + 1) * 128], QA[0:82, tq * 128:(tq + 1) * 128]))
                        pv.append((acc[:, qs, 0:65], qs, VA[:, kt, :], first))
                        first = False
                    tiles.append(dict(qs=qs_valid, smm=smm, pv=pv, bias=float(-_slopes()[h] * 128.0 * dl), mask=(dl == 0)))
                groups.append(dict(qt=qt, aset=aset, tiles=tiles))
            gi_tile = 0
            for g in groups:
                for t in g["tiles"]:
                    sb_ = SB4[gi_tile % 4]
                    pt_ = PT[gi_tile % 4]
                    q0, q1 = t["qs"][0], t["qs"][-1] + 1
                    t["smm"] = [(sb_[:, qs * 128:(qs + 1) * 128], l, r) for (_, qs, l, r) in t["smm"]]
                    t["act"] = (sb_[:, q0 * 128:q1 * 128], pt_[:, q0 * 128:q1 * 128], t["bias"])
                    t["pv"] = [(o, pt_[:, qs * 128:(qs + 1) * 128], r, st) for (o, qs, r, st) in t["pv"]]
                    if t["mask"]:
                        t["mask"] = (v3(pt_[:], 4), tri[:, 0:1, :].to_broadcast([128, 4, 128]))
                    else:
                        t["mask"] = None
                    gi_tile += 1

            def mk_epi(qt, aset):
                acc = v3(psO[aset][:], 4)

                def epi(B, tk):
                    rd = rden[:, aset * 4:aset * 4 + 4].unsqueeze(2)
                    t_r = B.op("vector", lambda e: e.reciprocal(out=rd, in_=acc[:, :, 64:65]),
                               waits=[tk, st_ost[aset]], sig=True)
                    t_n = B.op("vector", lambda e: e.tensor_tensor(out=Ost[aset][:, :, off:off + 64], in0=acc[:, :, 0:64],
                                                                 in1=rd.to_broadcast([128, 4, 64]), op=ALU.mult),
                               waits=[t_r], sig=True)
                    epi_tok[qt] = t_n
                    return t_n
                return epi

            def mk_post(qt, aset):
                def post(B):
                    for qs in range(4):
                        t_tr = B.op("tensor", lambda e, qs=qs: e.matmul(psT[:, qs * 128:(qs + 1) * 128], Ost[aset][:, qs, :], ident[:],
                                                                       start=True, stop=True),
                                    waits=[epi_tok[qt], post_state["psT_free"]] if qs == 0 else (), sig=(qs == 3))
                    st_ost[aset] = t_tr
                    post_state["psT_free"] = B.op("vector", lambda e: e.tensor_copy(
                        out=OT[off:off + 64, pair, qt * 512:(qt + 1) * 512], in_=psT[off:off + 64, :]), waits=[t_tr], sig=True)
                return post

            epi_tok = {}
            st_ost = [None, None]
            for g in groups:
                g["epi"] = mk_epi(g["qt"], g["aset"])
                g["post"] = mk_post(g["qt"], g["aset"])
            emit_attention(B, T, groups, filler=_mk_filler(lambda: psM[:, 0:128], ident, True), nS=4, nPT=4)
            B.run()

            if h % 2 == 1:
                gating_block(f"bZ{pair}", pair, wz, zs)


    only = stop.get("only") if isinstance(stop, dict) else None
    if only != "moba":
        _build_nsa(nc, T, gating_block, v3, stop)

    if debug and "OT" in debug:
        with ExitStack() as esD:
            tmp = esD.enter_context(nc.sbuf_tensor("dbgtmp2", [128, 8, 512], F32))
            B = Blk(nc, "bD2")
            s1 = B.newsem("sync")
            tk = None
            for q in range(8):
                t1 = B.op("vector", lambda e, q=q: e.tensor_copy(out=tmp[:], in_=OT[:, :, q * 512:(q + 1) * 512]),
                          waits=[tk], sig=True)
                tk = B.dma("sync", dbg["OT"][:, :, q * 512:(q + 1) * 512], tmp[:], s1, waits=[t1])
            B.op("sync", lambda e: e.nop(), waits=[tk])
            B.run()


def _build_nsa(nc, T, gating_block, v3, stop):
    hT, OT, QA, KA, KW, VA, VW, PT, Ost = T.hT, T.OT, T.QA, T.KA, T.KW, T.VA, T.VW, T.PT, T.Ost
    psS, psO, psO2, psM, psT = T.psS, T.psO, T.psO2, T.psM, T.psT
    ident, btab, tri, cmask, rden, gates = T.ident, T.btab, T.tri, T.cmask, T.rden, T.gates
    dram = T.dram
    banks4 = [psS[0], psS[1], psO[0], psO[1]]
    SB4 = [psS[0], psS[1], psO2[0], psO2[1]]
    SB3 = [psS[0], psS[1], psO2[1]]
    ngroups = int(stop.get("nsa_groups", 2)) if isinstance(stop, dict) else 2
    stage = int(stop.get("nsa_stage", 9)) if isinstance(stop, dict) else 9
    with ExitStack() as esN:
        def sbn(name, shape, dt):
            return esN.enter_context(nc.sbuf_tensor("n_" + name, list(shape), dt))
        kcT = sbn("kcT", [64, 256], BF16)
        rcmp = sbn("rcmp", [128, 2, 129], BF16)
        Wq = sbn("Wq", [128, 8, 64], BF16)
        wz = sbn("wz", [128, 8, 128], BF16)
        m16 = sbn("m16", [128, NT, 16], F32)
        wk = sbn("wk", [128, 64], F32)
        maskp = [sbn(f"maskp{i}", [128, 4, 128], BF16) for i in range(2)]
        zs = [maskp[i][:].rearrange("p a b -> p (a b)") for i in range(2)]
        hs = [sbn(f"hs{i}", [128, 256], BF16) for i in range(2)]
        d3 = [sbn(f"d3{i}", [128, 4, 3], F32) for i in range(2)]
        tA = sbn("tA", [128, 4, 64], F32)
        tB = sbn("tB", [128, 4, 64], F32)
        bcol = sbn("bcol", [128, 2], F32)

        with ExitStack() as es1:
            Wg = es1.enter_context(nc.sbuf_tensor("n_Wg", [128, 8, 24], BF16))
            B = Blk(nc, "bG")
            s1, s2, s3 = B.newsem(), B.newsem(), B.newsem()
            t_w = B.dma("gpsimd", Wg[:], dram["wG"][:], s1)
            t_e = B.dma("gpsimd", KA[64:128, :], dram["c_E64"][:], s2)
            t_rc = B.dma("gpsimd", rcmp[:, :, 64:129], dram["c_rc"][:], s3)
            t_z0 = B.op("vector", lambda e: e.memset(maskp[0][:], 0.0))
            t_z1 = B.op("vector", lambda e: e.memset(maskp[1][:], 0.0))
            t_z2 = B.op("vector", lambda e: e.memset(hs[0][:], 0.0))
            t_z3 = B.op("vector", lambda e: e.memset(hs[1][:], 0.0))
            t_z4 = B.op("vector", lambda e: e.memset(kcT[:], 0.0), sig=True)
            gT = es1.enter_context(nc.sbuf_tensor("n_gT", [24, S], BF16))
            gfree = [None, None]
            for qt in range(8):
                bk = psS[qt % 2]
                for c in range(8):
                    t_mm = B.op("tensor", lambda e, bk=bk, c=c, qt=qt: e.matmul(
                        bk[0:24, :], Wg[:, c, :], hT[:, c, qt * 512:(qt + 1) * 512], start=(c == 0), stop=(c == 7)),
                        waits=[t_w, gfree[qt % 2]] if c == 0 else (), sig=(c == 7))
                gfree[qt % 2] = B.op("scalar", lambda e, bk=bk, qt=qt: e.activation(
                    out=gT[:, qt * 512:(qt + 1) * 512], in_=bk[0:24, :], func=AF.Sigmoid), waits=[t_mm], sig=True)
            for half in range(2):
                bk = (psM, psT)[half]
                for j in range(16):
                    tt = half * 16 + j
                    t_mm = B.op("tensor", lambda e, bk=bk, j=j, tt=tt: e.matmul(
                        bk[:, j * 24:(j + 1) * 24], gT[:, tt * 128:(tt + 1) * 128], ident[0:24, 0:24], start=True, stop=True),
                        waits=[gfree[0], gfree[1]] if j == 0 else (), sig=(j == 15))
                B.op("scalar", lambda e, bk=bk, half=half: e.copy(
                    out=gates[:, half * 16:(half + 1) * 16, :], in_=bk[:, 0:384].rearrange("p (a b) -> p a b", a=16)),
                    waits=[t_mm], sig=True)
            B.op("sync", lambda e: e.nop(), waits=[t_e, t_rc, t_z4, B.last("scalar")])
            B.run()

        for g in range(ngroups if stage >= 2 else 0):
            with ExitStack() as es1:
                Wkv = es1.enter_context(nc.sbuf_tensor(f"n_Wkv{g}", [128, 8, 384], BF16))
                w1kv = es1.enter_context(nc.sbuf_tensor(f"n_w1kv{g}", [128, 32, 128], BF16))
                w2kv = es1.enter_context(nc.sbuf_tensor(f"n_w2kv{g}", [128, 2, 64], BF16))
                peT = es1.enter_context(nc.sbuf_tensor(f"n_peT{g}", [128, 32], BF16))
                B = Blk(nc, f"bK{g}")
                s1, s2, s3, s4 = B.newsem(), B.newsem(), B.newsem(), B.newsem()
                B.dma("gpsimd", Wkv[:, 0:4, :], dram["wKV"][g][:, 0:4, :], s1)
                t_w = B.dma("gpsimd", Wkv[:, 4:8, :], dram["wKV"][g][:, 4:8, :], s1)
                import os
                skip = os.environ.get("BK_SKIP", "")
                for i4 in range(0 if "w" in skip else 3):
                    B.dma("gpsimd", w1kv[:, i4 * 8:(i4 + 1) * 8, :], dram["w1kv"][:, i4 * 8:(i4 + 1) * 8, :], s2)
                t_w1a = B.dma("gpsimd", w1kv[:, 24:32, :], dram["w1kv"][:, 24:32, :], s2)
                t_w1b = t_w1a
                t_w2 = B.dma("gpsimd", w2kv[:], dram["w2kv"][:], s3)
                t_pe = B.dma("gpsimd", peT[:], dram["peT"][:], s4)
                bank_free = [None] * 4
                bi = 0
                evs = {"scalar": None, "vector": None}
                specs = [(0, 128, QA, 128), (128, 64, KA, 64), (192, 64, KW, 64)]
                if "a" in skip:
                    specs = specs[0:1]
                if "b" in skip:
                    specs = specs[1:2]
                if "c" in skip:
                    specs = specs[2:3]
                for qt in range(0 if "q" in skip else 8):
                    for si, (c0, m, dst, rows) in enumerate(specs):
                        bk = banks4[bi % 4]
                        for c in range(8):
                            t_mm = B.op("tensor", lambda e, bk=bk, c=c, qt=qt, c0=c0, m=m, rows=rows: e.matmul(
                                bk[0:rows, :], Wkv[:, c, c0:c0 + m], hT[:, c, qt * 512:(qt + 1) * 512], start=(c == 0), stop=(c == 7)),
                                waits=[t_w, bank_free[bi % 4]] if c == 0 else (), sig=(c == 7))
                        eng = "scalar" if si != 1 else "vector"
                        if eng == "scalar":
                            t_e = B.op("scalar", lambda e, bk=bk, dst=dst, rows=rows, qt=qt: e.copy(
                                out=dst[0:rows, qt * 512:(qt + 1) * 512], in_=bk[0:rows, :]), waits=[t_mm], sig=True)
                        else:
                            t_e = B.op("vector", lambda e, bk=bk, dst=dst, rows=rows, qt=qt: e.tensor_copy(
                                out=dst[0:rows, qt * 512:(qt + 1) * 512], in_=bk[0:rows, :]), waits=[t_mm], sig=True)
                        evs[eng] = t_e
                        bank_free[bi % 4] = t_e
                        bi += 1
                for tg in range(0 if "v" in skip else 8):
                    bk = banks4[bi % 4]
                    for j in range(4):
                        tt = tg * 4 + j
                        for c in range(8):
                            t_mm = B.op("tensor", lambda e, bk=bk, j=j, c=c, tt=tt: e.matmul(
                                bk[:, j * 128:(j + 1) * 128], hT[:, c, tt * 128:(tt + 1) * 128], Wkv[:, c, 256:384], start=(c == 0), stop=(c == 7)),
                                waits=[bank_free[bi % 4]] if (c == 0 and j == 0) else (), sig=(c == 7 and j == 3))
                    B.op("scalar", lambda e, bk=bk, tg=tg: e.copy(
                        out=VA[:, tg * 4:(tg + 1) * 4, 0:64], in_=v3(bk[:], 4)[:, :, 0:64]), waits=[t_mm])
                    t_e2 = B.op("scalar", lambda e, bk=bk, tg=tg: e.copy(
                        out=VW[:, tg * 4:(tg + 1) * 4, 0:64], in_=v3(bk[:], 4)[:, :, 64:128]), waits=[t_mm], sig=True)
                    bank_free[bi % 4] = t_e2
                    bi += 1
                kvv = QA[:, :].rearrange("p (c s) -> p c s", s=16)
                import os
                sub = int(os.environ.get("BK_SUB", "9"))
                for kv in range(2 if sub >= 2 else 0):
                    r0 = kv * 64
                    bkH = banks4[bi % 4]
                    bi += 1
                    for l in range(32):
                        a, b_ = l // 16, l % 16
                        t_h = B.op("tensor", lambda e, bkH=bkH, l=l, a=a, b_=b_, r0=r0: e.matmul(
                            bkH[:, 0:255], w1kv[r0:r0 + 64, l, :], kvv[r0:r0 + 64, a:a + 255, b_], start=(l == 0), stop=(l == 31)),
                            waits=[t_w1a, t_w1b, evs["scalar"], evs["vector"], bank_free[(bi - 1) % 4]] if l == 0 else (), sig=(l == 31))
                    if sub == 2:
                        continue
                    if kv == 1 and sub == 3:
                        continue
                    for l in range(32):
                        t_b = B.op("tensor", lambda e, bkH=bkH, l=l, r0=r0: e.matmul(
                            bkH[:, 256:257], w1kv[r0:r0 + 64, l, :], peT[r0:r0 + 64, l:l + 1], start=(l == 0), stop=(l == 31)),
                            waits=[t_pe] if l == 0 else (), sig=(l == 31))
                    t_bc = B.op("vector", lambda e, bkH=bkH, kv=kv: e.tensor_copy(out=bcol[:, kv:kv + 1], in_=bkH[:, 256:257]),
                                waits=[t_b], sig=True)
                    t_s = B.op("scalar", lambda e, bkH=bkH, kv=kv: e.activation(
                        out=hs[kv][:, 0:255], in_=bkH[:, 0:255], func=AF.Silu, bias=bcol[:, kv:kv + 1]), waits=[t_bc, t_h], sig=True)
                    bank_free[(bi - 1) % 4] = t_s
                    if kv == 0:
                        t_k = B.op("tensor", lambda e: e.matmul(psM[0:64, 0:255], w2kv[:, 0, :], hs[0][:, 0:255], start=True, stop=True),
                                   waits=[t_s, t_w2], sig=True)
                        B.op("vector", lambda e: e.tensor_copy(out=kcT[:, 0:255], in_=psM[0:64, 0:255]), waits=[t_k], sig=True)
                    else:
                        for ct in range(2):
                            t_v = B.op("tensor", lambda e, ct=ct: e.matmul(psT[:, ct * 64:(ct + 1) * 64], hs[1][:, ct * 128:(ct + 1) * 128],
                                                                          w2kv[:, 1, :], start=True, stop=True),
                                       waits=[t_s, t_w2] if ct == 0 else (), sig=(ct == 1))
                        B.op("vector", lambda e: e.tensor_copy(out=rcmp[:, :, 0:64], in_=v3(psT[:, 0:128], 2)), waits=[t_v], sig=True)
                B.run()

            with ExitStack() as es2:
                imp = es2.enter_context(nc.sbuf_tensor(f"n_imp{g}", [128, NT, 64], F32))
                for hh in range(4 if stage >= 3 else 0):
                    hB = g * 4 + hh
                    B = Blk(nc, f"bC{hB}")
                    s1 = B.newsem()
                    t_w = B.dma("gpsimd", Wq[:], dram["wQB"][hB], s1)
                    q_ev = _proj_q(B, T, Wq, t_w)
                    groups = []
                    gi_tile = 0
                    for qt in range(8):
                        aset = qt % 2
                        acc = v3(psO[aset][:], 4)
                        tiles = []
                        first = True
                        for ct in range(qt // 4 + 1):
                            sb_ = SB4[gi_tile % 4]
                            pt_ = PT[gi_tile % 4]
                            pv = []
                            for qs in range(4):
                                pv.append((acc[:, qs, 0:65], pt_[:, qs * 128:(qs + 1) * 128], rcmp[:, ct, 64:129], first))
                                first = False
                            tiles.append(dict(
                                smm=[(sb_[:], kcT[:, ct * 128:(ct + 1) * 128], QA[0:64, qt * 512:(qt + 1) * 512])],
                                act=(sb_[:], pt_[:], None),
                                mask=(pt_[:], cmask[:, qt % 4, :]) if ct == qt // 4 else None,
                                pv=pv))
                            gi_tile += 1

                        def mk_epi(qt, aset, acc):
                            def epi(B, tk):
                                rd = rden[:, aset * 4:aset * 4 + 4].unsqueeze(2)
                                t1 = B.op("vector", lambda e: e.tensor_scalar(out=rd, in0=acc[:, :, 0:1], scalar1=1e-30, scalar2=None,
                                                                            op0=ALU.max), waits=[tk], sig=True)
                                t2 = B.op("vector", lambda e: e.reciprocal(out=rd, in_=rd), waits=[t1], sig=True)
                                dst = imp[:, 4 * qt:4 * qt + 4, :]
                                if hh == 0:
                                    t3 = B.op("vector", lambda e: e.tensor_tensor(out=dst, in0=acc[:, :, 1:65], in1=rd.to_broadcast([128, 4, 64]),
                                                                                op=ALU.mult), waits=[t2], sig=True)
                                else:
                                    t3 = B.op("vector", lambda e: e.tensor_tensor(out=tA[:], in0=acc[:, :, 1:65], in1=rd.to_broadcast([128, 4, 64]),
                                                                                op=ALU.mult), waits=[t2], sig=True)
                                    B.op("vector", lambda e: e.tensor_tensor(out=dst, in0=dst, in1=tA[:], op=ALU.add), waits=[t3], sig=True)
                                return t3
                            return epi
                        groups.append(dict(tiles=tiles, epi=mk_epi(qt, aset, acc), post=None))
                    groups[0]["tiles"][0]["extra_wait"] = q_ev
                    emit_attention(B, T, groups, filler=_mk_filler(lambda: psM[:, 0:128], ident, True), nS=4, nPT=4)
                    B.run()

                if stage < 4:
                    continue
                B = Blk(nc, f"bS{g}")
                B.op("vector", lambda e: e.memset(maskp[0][:], 0.0))
                B.op("vector", lambda e: e.memset(maskp[1][:], 0.0))
                t_a = B.op("vector", lambda e: e.tensor_tensor(out=imp[:], in0=imp[:], in1=T.addm_slc[:], op=ALU.add), sig=True)
                tk = t_a
                for tt in range(NT):
                    t1 = B.op("vector", lambda e, tt=tt: e.max(out=m16[:, tt, 0:8], in_=imp[:, tt, :]), waits=[tk], sig=True)
                    t2 = B.op("vector", lambda e, tt=tt: e.match_replace(out=wk[:], in_to_replace=m16[:, tt, 0:8], in_values=imp[:, tt, :],
                                                                       imm_value=-1e30), waits=[t1], sig=True)
                    tk = B.op("vector", lambda e, tt=tt: e.max(out=m16[:, tt, 8:16], in_=wk[:]), waits=[t2], sig=True)
                mp_free = [None, None]
                ps_free = [None, None]
                for c4 in range(8):
                    b = c4 % 2
                    t_m = B.op("vector", lambda e, c4=c4, b=b: e.tensor_tensor(
                        out=maskp[b][:, :, 64:128], in0=imp[:, 4 * c4:4 * c4 + 4, :],
                        in1=m16[:, 4 * c4:4 * c4 + 4, 15:16].to_broadcast([128, 4, 64]), op=ALU.is_lt), waits=[tk, mp_free[b]], sig=True)
                    for j in range(4):
                        t_tr = B.op("tensor", lambda e, b=b, j=j: e.matmul(psS[b][:, j * 128:(j + 1) * 128], maskp[b][:, j, :], ident[:],
                                                                         start=True, stop=True),
                                    waits=[t_m, ps_free[b]] if j == 0 else (), sig=(j == 3))
                    mp_free[b] = t_tr
                    ps_free[b] = B.op("scalar", lambda e, b=b, c4=c4: e.copy(out=KW[64:128, c4 * 512:(c4 + 1) * 512], in_=psS[b][64:128, :]),
                                      waits=[t_tr], sig=True)
                B.run()

            for hh in range(4 if stage >= 5 else 0):
                hB = g * 4 + hh
                pair, off = 4 + hB // 2, (hB % 2) * 64
                B = Blk(nc, f"bN{hB}")
                s1 = B.newsem()
                t_w = B.dma("gpsimd", Wq[:], dram["wQB"][hB], s1)
                q_ev = _proj_q(B, T, Wq, t_w)
                t_mc = B.op("vector", lambda e: e.tensor_copy(out=QA[64:128, :], in_=KW[64:128, :]), sig=True)
                groups = []
                gi_tile = 0
                for qt in range(8):
                    aset = qt % 2
                    accS = v3(psO[aset][:], 4)
                    accW = v3(psO2[0][:], 4)
                    accC = v3(psM[:], 4)
                    tiles = []
                    firstS, firstW, firstC = True, True, True
                    for dl in range(4 * qt + 3, -1, -1):
                        sb_ = SB3[gi_tile % 3]
                        pt_ = PT[gi_tile % 4]
                        qsv = [qs for qs in range(4) if 4 * qt + qs - dl >= 0]
                        smm, pv = [], []
                        for qs in qsv:
                            kt, tq = 4 * qt + qs - dl, 4 * qt + qs
                            smm.append((sb_[:, qs * 128:(qs + 1) * 128], KA[:, kt * 128:(kt + 1) * 128], QA[:, tq * 128:(tq + 1) * 128]))
                            pv.append((accS[:, qs, 0:65], pt_[:, qs * 128:(qs + 1) * 128], VA[:, kt, :], firstS))
                            firstS = False
                        q0, q1 = qsv[0], qsv[-1] + 1
                        tiles.append(dict(smm=smm, act=(sb_[:, q0 * 128:q1 * 128], pt_[:, q0 * 128:q1 * 128], btab[:, hB, dl:dl + 1]),
                                          mask=(v3(pt_[:], 4), tri[:, 0:1, :].to_broadcast([128, 4, 128])) if dl == 0 else None, pv=pv))
                        gi_tile += 1
                    for dl in range(4, -1, -1):
                        qsv = [qs for qs in range(4) if 4 * qt + qs - dl >= 0]
                        if not qsv:
                            continue
                        sb_ = SB3[gi_tile % 3]
                        pt_ = PT[gi_tile % 4]
                        smm, pv = [], []
                        for qs in qsv:
                            kt, tq = 4 * qt + qs - dl, 4 * qt + qs
                            smm.append((sb_[:, qs * 128:(qs + 1) * 128], KW[0:64, kt * 128:(kt + 1) * 128], QA[0:64, tq * 128:(tq + 1) * 128]))
                            pv.append((accW[:, qs, 0:65], pt_[:, qs * 128:(qs + 1) * 128], VW[:, kt, :], firstW))
                            firstW = False
                        q0, q1 = qsv[0], qsv[-1] + 1
                        mk = None
                        if dl == 0 or dl == 4:
                            mi = 0 if dl == 0 else 1
                            mk = (v3(pt_[:], 4)[:, q0:q1, :], tri[:, mi:mi + 1, :].to_broadcast([128, q1 - q0, 128]))
                        tiles.append(dict(smm=smm, act=(sb_[:, q0 * 128:q1 * 128], pt_[:, q0 * 128:q1 * 128], btab[:, hB, dl:dl + 1]),
                                          mask=mk, pv=pv, cmp_first=(dl == 0)))
                        gi_tile += 1
                    for ct in range(qt // 4 + 1):
                        sb_ = SB3[gi_tile % 3]
                        pt_ = PT[gi_tile % 4]
                        pv = []
                        for qs in range(4):
                            pv.append((accC[:, qs, 0:65], pt_[:, qs * 128:(qs + 1) * 128], rcmp[:, ct, 0:65], firstC))
                            firstC = False
                        tiles.append(dict(
                            smm=[(sb_[:], kcT[:, ct * 128:(ct + 1) * 128], QA[0:64, qt * 512:(qt + 1) * 512])],
                            act=(sb_[:], pt_[:], None),
                            mask=(pt_[:], cmask[:, qt % 4, :]) if ct == qt // 4 else None,
                            pv=pv, cmp_first=(ct == 0)))
                        gi_tile += 1
                    groups.append(dict(qt=qt, aset=aset, tiles=tiles, accs=(accC, accS, accW)))

                epi_tok = {}
                st_ost = [None, None]
                post_state = {"psT_free": None}

                def mk_epi(qt, aset, accs):
                    def epi(B, tk):
                        dd = d3[aset]
                        for br in range(3):
                            t1 = B.op("vector", lambda e, br=br: e.tensor_scalar(out=dd[:, :, br:br + 1], in0=accs[br][:, :, 64:65], scalar1=1e-30,
                                                                               scalar2=None, op0=ALU.max), waits=[tk], sig=(br == 2))
                        t2 = B.op("vector", lambda e: e.reciprocal(out=dd[:], in_=dd[:]), waits=[t1], sig=True)
                        t3 = B.op("vector", lambda e: e.tensor_tensor(out=dd[:], in0=dd[:], in1=gates[:, 4 * qt:4 * qt + 4, 3 * hB:3 * hB + 3],
                                                                    op=ALU.mult), waits=[t2], sig=True)
                        t4 = B.op("vector", lambda e: e.tensor_tensor(out=tA[:], in0=accs[0][:, :, 0:64],
                                                                    in1=dd[:, :, 0:1].to_broadcast([128, 4, 64]), op=ALU.mult), waits=[t3], sig=True)
                        t5 = B.op("vector", lambda e: e.tensor_tensor(out=tB[:], in0=accs[1][:, :, 0:64],
                                                                    in1=dd[:, :, 1:2].to_broadcast([128, 4, 64]), op=ALU.mult), waits=[t3], sig=True)
                        t6 = B.op("vector", lambda e: e.tensor_tensor(out=tA[:], in0=tA[:], in1=tB[:], op=ALU.add), waits=[t4, t5], sig=True)
                        t7 = B.op("vector", lambda e: e.tensor_tensor(out=tB[:], in0=accs[2][:, :, 0:64],
                                                                    in1=dd[:, :, 2:3].to_broadcast([128, 4, 64]), op=ALU.mult), waits=[t6], sig=True)
                        t8 = B.op("vector", lambda e: e.tensor_tensor(out=Ost[aset][:, :, off:off + 64], in0=tA[:], in1=tB[:], op=ALU.add),
                                  waits=[t7, st_ost[aset]], sig=True)
                        epi_tok[qt] = t8
                        return t7
                    return epi

                def mk_post(qt, aset):
                    def post(B):
                        for qs in range(4):
                            t_tr = B.op("tensor", lambda e, qs=qs: e.matmul(psT[:, qs * 128:(qs + 1) * 128], Ost[aset][:, qs, :], ident[:],
                                                                           start=True, stop=True),
                                        waits=[epi_tok[qt], post_state["psT_free"]] if qs == 0 else (), sig=(qs == 3))
                        st_ost[aset] = t_tr
                        post_state["psT_free"] = B.op("vector", lambda e: e.tensor_copy(
                            out=OT[off:off + 64, pair, qt * 512:(qt + 1) * 512], in_=psT[off:off + 64, :]), waits=[t_tr], sig=True)
                    return post

                for gr in groups:
                    gr["epi"] = mk_epi(gr["qt"], gr["aset"], gr["accs"])
                    gr["post"] = mk_post(gr["qt"], gr["aset"])
                groups[0]["tiles"][0]["extra_wait"] = [q_ev, t_mc]
                emit_attention(B, T, groups, single_acc_key="cmp_first", nS=3, nPT=4,
                               filler=_mk_filler(lambda: psT[:, 0:128], ident, True, nf=NFILL2,
                                                 wait_fn=lambda: post_state["psT_free"]))
                B.run()
                if hB % 2 == 1:
                    gating_block(f"bZ{pair}", pair, wz, zs)


NFILL = [4]


NFILL2 = "NFILL2"


def _mk_filler(dst_fn, ident, start, m=128, nf=None, wait_fn=None):
    import os
    nfill = int(os.environ.get("NFILL", NFILL[0]))
    if nf == NFILL2:
        nfill = int(os.environ.get("NFILL2", 3))
    if nfill <= 0:
        return None

    def filler(B):
        dst = dst_fn()
        n = dst.shape[-1]
        for k in range(nfill):
            w = [wait_fn()] if (wait_fn is not None and k == 0) else ()
            B.op("tensor", lambda e: e.matmul(dst, ident[:, 0:m], ident[:, 0:n], start=start, stop=start), waits=w)
    return filler


def _proj_q(B, T, Wq, t_w):
    banks4 = [T.psS[0], T.psS[1], T.psO[0], T.psO[1]]
    free = [None] * 4
    t_e = None
    for qt in range(8):
        bk = banks4[qt % 4]
        for c in range(8):
            t_mm = B.op("tensor", lambda e, bk=bk, c=c, qt=qt: e.matmul(
                bk[0:64, :], Wq[:, c, :], T.hT[:, c, qt * 512:(qt + 1) * 512], start=(c == 0), stop=(c == 7)),
                waits=[t_w, free[qt % 4]] if c == 0 else (), sig=(c == 7))
        t_e = B.op("scalar", lambda e, bk=bk, qt=qt: e.activation(
            out=T.QA[0:64, qt * 512:(qt + 1) * 512], in_=bk[0:64, :], func=AF.Copy, scale=0.125), waits=[t_mm], sig=True)
        free[qt % 4] = t_e
    return t_e


def _build_final(nc, hT, OT, psS, psO, dram, x, out, epsc, psO2=None, psM=None, psT=None):
    with ExitStack() as esF:
        def sbf(name, shape, dt):
            return esF.enter_context(nc.sbuf_tensor("f_" + name, list(shape), dt))
        wO = sbf("wO", [128, 8, D], BF16)
        gpost = sbf("gpost", [128, D], F32)
        xt = [sbf(f"xt{i}", [128, 2, D], F32) for i in range(3)]
        yt = [sbf(f"yt{i}", [128, 2, D], F32) for i in range(2)]
        junk = sbf("junk", [128, 512], BF16)
        ssq = sbf("ssq", [128, NT, 2], F32)
        rs = sbf("rs", [128, NT], F32)
        B = Blk(nc, "bF")
        sw, sg = B.newsem(), B.newsem("sync")
        t_w = [B.dma("gpsimd", wO[:, c, :], dram["wO"][:, c, :], sw) for c in range(8)]
        t_g = B.dma("sync", gpost[:], dram["gpost"][:], sg)
        xs = [B.newsem("sync") for _ in range(3)]
        os_ = [B.newsem("sync") for _ in range(2)]
        pY = [(psS[0], psS[1]), (psO[0], psO[1]), (psO2[0], psO2[1]), (psM, psT)]
        ps_free = [None] * 4
        xt_free = [None, None, None]
        yt_free = [None, None]
        t_xs = [None] * (NT // 2)
        for tp in range(NT // 2):
            b3 = tp % 3
            by = tp % 2
            xv = x[tp * 256:(tp + 1) * 256, :].rearrange("(n p) d -> p n d", p=128)
            ov = out[tp * 256:(tp + 1) * 256, :].rearrange("(n p) d -> p n d", p=128)
            if tp == 0:
                for t2 in range(2):
                    xv2 = x[t2 * 256:(t2 + 1) * 256, :].rearrange("(n p) d -> p n d", p=128)
                    t_xs[t2] = B.dma("sync", xt[t2 % 3][:], xv2, xs[t2 % 3])
            t_x = t_xs[tp]
            for j in range(2):
                tt = tp * 2 + j
                b = tt % 4
                for half in range(2):
                    pk = pY[b][half]
                    for c in range(8):
                        t_mm = B.op("tensor", lambda e, pk=pk, c=c, tt=tt, half=half: e.matmul(
                            pk[:], OT[:, c, tt * 128:(tt + 1) * 128], wO[:, c, half * 512:(half + 1) * 512], start=(c == 0), stop=(c == 7)),
                            waits=(t_w + [ps_free[b]]) if (c == 0 and half == 0) else (), sig=(c == 7 and half == 1))
                for half in range(2):
                    t_sq = B.op("scalar", lambda e, half=half, b=b, tt=tt: e.activation(
                        out=junk[:], in_=pY[b][half][:], func=AF.Square, accum_out=ssq[:, tt, half:half + 1]), waits=[t_mm], sig=True)
                t_a = B.op("vector", lambda e, tt=tt: e.tensor_tensor(out=rs[:, tt:tt + 1], in0=ssq[:, tt, 0:1], in1=ssq[:, tt, 1:2], op=ALU.add),
                           waits=[t_sq], sig=True)
                t_r1 = B.op("scalar", lambda e, tt=tt: e.activation(out=rs[:, tt:tt + 1], in_=rs[:, tt:tt + 1], func=AF.Sqrt,
                                                                  bias=epsc[:, 0:1], scale=1.0 / D), waits=[t_a], sig=True)
                t_r2 = B.op("vector", lambda e, tt=tt: e.reciprocal(out=rs[:, tt:tt + 1], in_=rs[:, tt:tt + 1]), waits=[t_r1], sig=True)
                for half in range(2):
                    t_y = B.op("vector", lambda e, half=half, b=b, tt=tt, by=by, j=j: e.scalar_tensor_tensor(
                        out=yt[by][:, j, half * 512:(half + 1) * 512], in0=pY[b][half][:], scalar=rs[:, tt:tt + 1],
                        in1=gpost[:, half * 512:(half + 1) * 512], op0=ALU.mult, op1=ALU.mult),
                        waits=[t_r2, t_g, yt_free[by]] if half == 0 else (), sig=True)
                ps_free[b] = t_y
            t_o = B.op("vector", lambda e, by=by, b3=b3: e.tensor_tensor(out=yt[by][:], in0=yt[by][:], in1=xt[b3][:], op=ALU.add),
                       waits=[t_y, t_x], sig=True)
            xt_free[b3] = t_o
            yt_free[by] = B.dma("sync", ov, yt[by][:], os_[by], waits=[t_o])
            if tp + 2 < NT // 2:
                t3 = tp + 2
                xv2 = x[t3 * 256:(t3 + 1) * 256, :].rearrange("(n p) d -> p n d", p=128)
                t_xs[t3] = B.dma("sync", xt[t3 % 3][:], xv2, xs[t3 % 3], waits=[xt_free[t3 % 3]])
        B.op("sync", lambda e: e.nop(), waits=[yt_free[0], yt_free[1]])
        B.run()


def build(debug=None, stop=None):
    nc = bass.Bass("TRN2", target_bir_lowering=False)
    dram = {}

    def din(name, shape):
        dram[name] = nc.dram_tensor(name, list(shape), F32, kind="ExternalInput").ap()
        return dram[name]

    x = din("x", [S, D])
    cshapes = {k: v.shape for k, v in host_consts().items()}
    for k, shp in cshapes.items():
        din(k, shp)
    wshapes = dict(wA=[8, 128, 3, 8, 64], wZ=[8, 128, 8, 128], wQB=[8, 128, 8, 64], wG=[128, 8, 24],
                   wKV=[2, 128, 8, 384], w1kv=[128, 32, 128], w2kv=[128, 2, 64], peT=[128, 32],
                   wO=[128, 8, 1024], gpre=[128, 8], gpost=[128, D])
    for k, shp in wshapes.items():
        din(k, shp)
    out = nc.dram_tensor("out", [S, D], F32, kind="ExternalOutput").ap()
    dbg = {}
    if debug:
        for name, shp in debug.items():
            dbg[name] = nc.dram_tensor("dbg_" + name, list(shp), F32, kind="ExternalOutput").ap()

    es = ExitStack()

    def sb(name, shape, dt):
        return es.enter_context(nc.sbuf_tensor("s_" + name, list(shape), dt))

    def ps(name, shape, dt=F32):
        return es.enter_context(nc.psum_tensor(name, list(shape), dt))

    with es:
        hT = sb("hT", [128, 8, S], BF16)
        OT = sb("OT", [128, 8, S], BF16)
        ident = sb("ident", [128, 128], BF16)
        addm_moba = sb("addm_moba", [128, NT, 16], F32)
        addm_slc = sb("addm_slc", [128, NT, 64], BF16)
        cmask = sb("cmask", [128, 4, 512], BF16)
        tri = sb("tri", [128, 2, 128], BF16)
        btab = sb("btab", [128, 8, 32], F32)
        gpre = sb("gpre", [128, 8], F32)
        epsc = sb("epsc", [128, 1], F32)
        gates = sb("gates", [128, NT, 24], F32)
        esW = ExitStack()

        def sbw(name, shape, dt):
            return esW.enter_context(nc.sbuf_tensor("w_" + name, list(shape), dt))
        QA = sbw("QA", [128, S], BF16)
        KA = sbw("KA", [128, S], BF16)
        KW = sbw("KW", [128, S], BF16)
        VA = sbw("VA", [128, NT, 65], BF16)
        VW = sbw("VW", [128, NT, 65], BF16)
        PT = [sbw(f"PT{i}", [128, 512], BF16) for i in range(4)]
        Ost = [sbw(f"Ost{i}", [128, 4, 128], BF16) for i in range(2)]
        rden = sbw("rden", [128, 16], F32)
        psS = [ps(f"psS{i}", [128, 512]) for i in range(2)]
        psO = [ps(f"psO{i}", [128, 512]) for i in range(2)]
        psO2 = [ps(f"psO2{i}", [128, 512]) for i in range(2)]
        psM = ps("psM", [128, 512])
        psT = ps("psT", [128, 512])

        POOL[0] = SemPool(nc, es)
        with nc.Block() as blk_clr:
            @blk_clr.gpsimd
            def _(g):
                for sm in POOL[0].all():
                    g.sem_clear(sm.h)

        B = Blk(nc, "b0")
        toks = []
        toks.append(B.dma("gpsimd", ident[:], dram["c_ident"][:], B.newsem()))
        toks.append(B.dma("sync", addm_moba[:], dram["c_addm_moba"][:], B.newsem("sync")))
        toks.append(B.dma("gpsimd", addm_slc[:], dram["c_addm_slc"][:], B.newsem()))
        toks.append(B.dma("gpsimd", cmask[:], dram["c_cmask"][:], B.newsem()))
        toks.append(B.dma("gpsimd", tri[:], dram["c_tri"][:], B.newsem()))
        toks.append(B.dma("sync", btab[:], dram["c_btab"][:], B.newsem("sync")))
        toks.append(B.dma("sync", gpre[:], dram["gpre"][:], B.newsem("sync")))
        B.op("vector", lambda e: e.memset(VA[:, :, 64:65], 1.0))
        B.op("vector", lambda e: e.memset(epsc[:], 1e-6))
        B.op("vector", lambda e: e.memset(VW[:, :, 64:65], 1.0))
        B.op("vector", lambda e: e.memset(QA[64:128, :], 0.0))
        t_ms = B.op("vector", lambda e: e.memset(KA[64:128, :], 0.0), sig=True)
        B.op("gpsimd", lambda e: e.memset(Ost[0][:], 0.0))
        B.op("gpsimd", lambda e: e.memset(Ost[1][:], 0.0))
        B.op("sync", lambda e: e.nop(), waits=toks)
        B.run()

        with ExitStack() as esA:
            xt = [esA.enter_context(nc.sbuf_tensor(f"xt{i}", [128, D], F32)) for i in range(4)]
            junk = esA.enter_context(nc.sbuf_tensor("junkA", [128, D], BF16))
            hb = [esA.enter_context(nc.sbuf_tensor(f"hb{i}", [128, D], BF16)) for i in range(2)]
            ss = esA.enter_context(nc.sbuf_tensor("ssA", [128, NT], F32))
            rs = esA.enter_context(nc.sbuf_tensor("rsA", [128, NT], F32))
            B = Blk(nc, "bA")
            xs = [B.newsem("sync") for _ in range(4)]
            xtok = [None] * NT
            hb_free = [None, None]
            xt_free = [None] * 4
            ps_free = [None, None]
            pA = [(psS[0], psS[1]), (psO[0], psO[1])]
            tr2 = [None] * NT
            tsq = [None] * NT

            def stage0(tt):
                b3 = tt % 4
                xtok[tt] = B.dma("sync", xt[b3][:], x[tt * 128:(tt + 1) * 128, :], xs[b3], waits=[xt_free[b3]])

            def stage1(tt):
                b3 = tt % 4
                t_sq = B.op("scalar", lambda e, b3=b3, tt=tt: e.activation(out=junk[:], in_=xt[b3][:], func=AF.Square,
                                                                     accum_out=ss[:, tt:tt + 1]),
                            waits=[xtok[tt]], sig=True)
                t_r1 = B.op("scalar", lambda e, tt=tt: e.activation(out=rs[:, tt:tt + 1], in_=ss[:, tt:tt + 1], func=AF.Sqrt,
                                                                  bias=epsc[:, 0:1], scale=1.0 / D),
                            waits=[t_sq], sig=True)
                tr2[tt] = B.op("vector", lambda e, tt=tt: e.reciprocal(out=rs[:, tt:tt + 1], in_=rs[:, tt:tt + 1]),
                               waits=[t_r1], sig=True)
                tsq[tt] = t_sq

            def stage2(tt):
                b3 = tt % 4
                b2 = tt % 2
                t_h = B.op("scalar", lambda e, tt=tt, b3=b3, b2=b2: e.activation(
                    out=hb[b2][:], in_=xt[b3][:], func=AF.Copy, scale=rs[:, tt:tt + 1]),
                    waits=[tr2[tt], hb_free[b2], tsq[tt]], sig=True)
                xt_free[b3] = t_h
                pa, pb = pA[b2]
                for c in range(8):
                    dst = (pa if c < 4 else pb)[:, (c % 4) * 128:(c % 4 + 1) * 128]
                    t_tr = B.op("tensor", lambda e, dst=dst, b2=b2, c=c: e.matmul(
                        dst, hb[b2][:, c * 128:(c + 1) * 128], ident[:], start=True, stop=True),
                        waits=[t_h, ps_free[b2]], sig=(c == 7))
                hb_free[b2] = t_tr
                B.op("vector", lambda e, tt=tt, pa=pa: e.tensor_tensor(
                    out=hT[:, 0:4, tt * 128:(tt + 1) * 128], in0=pa[:].rearrange("p (c t) -> p c t", c=4),
                    in1=gpre[:, 0:4].unsqueeze(2).to_broadcast([128, 4, 128]), op=ALU.mult), waits=[t_tr])
                ps_free[b2] = B.op("vector", lambda e, tt=tt, pb=pb: e.tensor_tensor(
                    out=hT[:, 4:8, tt * 128:(tt + 1) * 128], in0=pb[:].rearrange("p (c t) -> p c t", c=4),
                    in1=gpre[:, 4:8].unsqueeze(2).to_broadcast([128, 4, 128]), op=ALU.mult), waits=[t_tr], sig=True)

            for t0 in range(3):
                stage0(t0)
            stage1(0)
            stage1(1)
            for tt in range(NT):
                if tt + 3 < NT:
                    stage0(tt + 3)
                if tt + 2 < NT:
                    stage1(tt + 2)
                stage2(tt)
            B.run()


        if stop == "A":
            pass
        else:
            _build_rest(nc, locals(), debug, dbg, stop)
        esW.close()
        if stop is None or (isinstance(stop, dict) and stop.get("final")):
            _build_final(nc, hT, OT, psS, psO, dram, x, out, epsc, psO2, psM, psT)

        if debug and "hT" in debug:
            with ExitStack() as esD:
                tmp = esD.enter_context(nc.sbuf_tensor("dbgtmp", [128, 8, 512], F32))
                B = Blk(nc, "bD")
                s1 = B.newsem("sync")
                tk = None
                for q in range(8):
                    t1 = B.op("vector", lambda e, q=q: e.tensor_copy(out=tmp[:], in_=hT[:, :, q * 512:(q + 1) * 512]),
                              waits=[tk], sig=True)
                    tk = B.dma("sync", dbg["hT"][:, :, q * 512:(q + 1) * 512], tmp[:], s1, waits=[t1])
                B.op("sync", lambda e: e.nop(), waits=[tk])
                B.run()
    return nc


_CACHE = {}


def kernel(x, pre_norm_g, post_norm_g, w_in, cmp_pos_k, cmp_pos_v, w_cmp_k1, w_cmp_k2, w_cmp_v1, w_cmp_v2, w_out):
    x = np.asarray(x, np.float32)
    consts = host_consts()
    wts = host_weights(*(np.asarray(a, np.float32) for a in (pre_norm_g, post_norm_g, w_in, cmp_pos_k, cmp_pos_v,
                                                            w_cmp_k1, w_cmp_k2, w_cmp_v1, w_cmp_v2, w_out)))
    nc = build()
    in_maps = []
    for b in range(8):
        m = {"x": np.ascontiguousarray(x[b])}
        m.update(consts)
        m.update(wts)
        in_maps.append(m)
    res = run_bass_kernel_spmd(nc, in_maps, core_ids=list(range(8)))
    return np.stack([r["out"] for r in res.results], 0).astype(np.float32)
```

```python
import numpy as np
from contextlib import ExitStack
import concourse.bass as bass
import concourse.mybir as mybir
from concourse.bass_utils import run_bass_kernel_spmd

F32 = mybir.dt.float32
BF16 = mybir.dt.bfloat16
ALU = mybir.AluOpType
AF = mybir.ActivationFunctionType
AX = mybir.AxisListType

S = 4096
D = 1024
NT = 32
DIN = 3864
NEGM = -30000.0
ENG = ("sync", "scalar", "vector", "gpsimd", "tensor")


class Sem:
    def __init__(self, h):
        self.h = h
        self.n = 0


class SemPool:
    def __init__(self, nc, es, ndma=24):
        self.eng = {e: Sem(es.enter_context(nc.semaphore(f"pool_{e}"))) for e in ENG}
        self.dma = {"gpsimd": [Sem(es.enter_context(nc.semaphore(f"pool_g{i}"))) for i in range(ndma)],
                    "sync": [Sem(es.enter_context(nc.semaphore(f"pool_s{i}"))) for i in range(ndma)],
                    "scalar": [Sem(es.enter_context(nc.semaphore(f"pool_a{i}"))) for i in range(4)]}

    def all(self):
        return list(self.eng.values()) + self.dma["gpsimd"] + self.dma["sync"] + self.dma["scalar"]


POOL = [None]


class Blk:
    def __init__(self, nc, name):
        self.nc = nc
        self.name = name
        self.ops = {e: [] for e in ENG}
        self.esem = POOL[0].eng
        self.k = {"gpsimd": 0, "sync": 0, "scalar": 0}

    def newsem(self, kind="gpsimd"):
        lst = POOL[0].dma[kind]
        s_ = lst[self.k[kind] % len(lst)]
        self.k[kind] += 1
        s_.kind = kind
        return s_

    def op(self, eng, fn, waits=(), sig=False):
        tok = None
        s = None
        if sig:
            s = self.esem[eng]
            s.n += 1
            tok = (s.h, s.n)
        self.ops[eng].append((fn, tuple(w for w in waits if w is not None), s, 1))
        return tok

    def dma(self, eng, out, in_, sem, waits=()):
        assert sem.kind == eng, (sem.kind, eng)
        sem.n += 16
        tok = (sem.h, sem.n)
        self.ops[eng].append((lambda e: e.dma_start(out=out, in_=in_), tuple(w for w in waits if w is not None), sem, 16))
        return tok

    def last(self, eng):
        s = self.esem[eng]
        return (s.h, s.n) if s.n > 0 else None

    def run(self):
        with self.nc.Block() as block:
            for e in ENG:
                ops = self.ops[e]

                def body(eng, ops=ops):
                    seen = {}
                    for fn, waits, s, amt in ops:
                        for (h, v) in waits:
                            key = id(h)
                            if seen.get(key, 0) >= v:
                                continue
                            seen[key] = v
                            eng.wait_ge(h, v)
                        ins = fn(eng)
                        if s is not None:
                            ins.then_inc(s.h, amt)

                getattr(block, e)(body)


def _slopes():
    return [2.0 ** (-(i + 1)) for i in range(8)]


def host_consts():
    c = {}
    c["c_ident"] = np.eye(128, dtype=np.float32)
    k = np.arange(S)
    c["c_E16"] = (k[None, :] // 256 == np.arange(16)[:, None]).astype(np.float32)
    c["c_E64"] = (NEGM * (k[None, :] // 64 == np.arange(64)[:, None])).astype(np.float32)
    am = np.zeros((128, NT, 16), np.float32)
    for tt in range(NT):
        own = tt // 2
        am[:, tt, own] = 1e9
        am[:, tt, own + 1:] = -1e9
    c["c_addm_moba"] = am
    a2 = np.zeros((128, NT, 64), np.float32)
    t = (np.arange(NT)[None, :] * 128 + np.arange(128)[:, None])
    own = t // 64
    j = np.arange(64)[None, None, :]
    a2 = np.where(j > own[:, :, None], -1e9, 0.0).astype(np.float32)
    a2 = np.where(j == 0, 1e9, a2)
    a2 = np.where(j == own[:, :, None] - 1, 2e9, a2)
    a2 = np.where(j == own[:, :, None], 3e9, a2)
    c["c_addm_slc"] = a2.astype(np.float32)
    cr = np.arange(128)[:, None, None]
    r = np.arange(4)[None, :, None]
    tr = np.arange(512)[None, None, :]
    c["c_cmask"] = (16 * cr + 31 - 512 * r <= tr).astype(np.float32)
    kk = np.arange(128)[:, None]
    tq = np.arange(128)[None, :]
    c["c_tri"] = np.stack([(kk <= tq), (kk > tq)], axis=1).astype(np.float32)
    sl = np.array(_slopes(), np.float32)
    dl = np.arange(32)[None, None, :]
    c["c_btab"] = (sl[None, :, None] * (np.arange(128)[:, None, None] - 128.0 * dl - 64.0)).astype(np.float32)
    tmod = (np.arange(S) % 128).astype(np.float32)
    qrow = np.zeros((8, 2, S), np.float32)
    krow = np.zeros((8, 2, S), np.float32)
    for h in range(8):
        qrow[h, 0] = -sl[h] * tmod
        qrow[h, 1] = 1.0
        krow[h, 0] = 1.0
        krow[h, 1] = sl[h] * tmod
    c["c_qrow"] = qrow
    c["c_krow"] = krow
    rc = np.zeros((128, 2, 65), np.float32)
    cidx = np.arange(2)[None, :] * 128 + np.arange(128)[:, None]
    valid = cidx < 255
    rc[:, :, 0] = valid
    cst = cidx * 16
    js = np.arange(64)[None, None, :] * 64
    ov = (cst[:, :, None] < js + 64) & (cst[:, :, None] + 32 > js) & valid[:, :, None]
    rc[:, :, 1:] = ov
    c["c_rc"] = rc
    return c


def host_weights(pre_norm_g, post_norm_g, w_in, cmp_pos_k, cmp_pos_v, w_cmp_k1, w_cmp_k2, w_cmp_v1, w_cmp_v2, w_out):
    w = {}
    W = np.ascontiguousarray(w_in[0].reshape(8, 128, DIN).transpose(1, 0, 2))

    def cols(a, n=64):
        return W[:, :, a:a + n]

    w["wA"] = np.ascontiguousarray(np.stack(
        [np.stack([cols(0 + h * 64), cols(512 + h * 64), cols(1024 + h * 64)], axis=1) for h in range(8)], 0))
    zc = [1536 + p * 128 for p in range(4)] + [3352 + p * 128 for p in range(4)]
    w["wZ"] = np.ascontiguousarray(np.stack([cols(a, 128) for a in zc], 0))
    w["wQB"] = np.ascontiguousarray(np.stack([cols(2048 + h * 64) for h in range(8)], 0))
    w["wG"] = np.ascontiguousarray(cols(3328, 24))
    kv = []
    for g in range(2):
        kv.append(np.concatenate([cols(2560 + g * 64), cols(2688 + g * 64), cols(2816 + g * 64),
                                  cols(3072 + g * 64), cols(2944 + g * 64), cols(3200 + g * 64)], axis=2))
    w["wKV"] = np.ascontiguousarray(np.stack(kv, 0))
    k1 = w_cmp_k1[0].reshape(32, 64, 128).transpose(1, 0, 2)
    v1 = w_cmp_v1[0].reshape(32, 64, 128).transpose(1, 0, 2)
    w["w1kv"] = np.ascontiguousarray(np.concatenate([k1, v1], 0))
    w["w2kv"] = np.ascontiguousarray(np.stack([w_cmp_k2[0], w_cmp_v2[0]], 1))
    w["peT"] = np.ascontiguousarray(np.concatenate([cmp_pos_k[0].T, cmp_pos_v[0].T], 0))
    w["wO"] = np.ascontiguousarray(w_out[0].reshape(8, 128, D).transpose(1, 0, 2))
    w["gpre"] = np.ascontiguousarray(pre_norm_g[0].reshape(8, 128).T)
    w["gpost"] = np.ascontiguousarray(np.broadcast_to(post_norm_g[0][None, :], (128, D)))
    return {k: np.asarray(v, np.float32) for k, v in w.items()}


def emit_attention(B, T, groups, single_acc_key=None, filler=None, nS=2, nPT=3):
    psS, PT = T.psS, T.PT
    flat = []
    for gi, g in enumerate(groups):
        for ti, t in enumerate(g["tiles"]):
            flat.append((gi, ti, t))
    n = len(flat)
    act_done = [None] * n
    rdy = [None] * n
    s_done = [None] * n
    pv_done = [None] * n
    epi_done = [None] * len(groups)
    last_pv_of_group = [None] * len(groups)

    def emit_S(i):
        gi, ti, t = flat[i]
        w = [act_done[i - nS] if i >= nS else None]
        ew = t.get("extra_wait")
        if ew is not None:
            w += list(ew) if isinstance(ew, (list, tuple)) and not (len(ew) == 2 and not isinstance(ew[0], tuple)) else [ew]
        m = len(t["smm"])
        for j, (o, l, r) in enumerate(t["smm"]):
            tk = B.op("tensor", lambda e, o=o, l=l, r=r: e.matmul(o, l, r, start=True, stop=True),
                      waits=w if j == 0 else (), sig=(j == m - 1))
        s_done[i] = tk

    def emit_act(i):
        gi, ti, t = flat[i]
        in_, o, bias = t["act"]
        w = [s_done[i], pv_done[i - nPT] if i >= nPT else None]
        if bias is None:
            act_done[i] = B.op("scalar", lambda e, in_=in_, o=o: e.activation(out=o, in_=in_, func=AF.Exp), waits=w, sig=True)
        else:
            act_done[i] = B.op("scalar", lambda e, in_=in_, o=o, bias=bias: e.activation(out=o, in_=in_, func=AF.Exp, bias=bias),
                               waits=w, sig=True)
        rdy[i] = act_done[i]
        if t.get("mask") is not None:
            ap, mk = t["mask"]
            rdy[i] = B.op("vector", lambda e, ap=ap, mk=mk: e.tensor_tensor(out=ap, in0=ap, in1=mk, op=ALU.mult),
                          waits=[act_done[i]], sig=True)

    def emit_PV(i):
        gi, ti, t = flat[i]
        if filler is not None:
            filler(B)
        w = [rdy[i]]
        if ti == 0 and gi >= 2:
            w.append(epi_done[gi - 2])
        if single_acc_key and t.get(single_acc_key) and gi >= 1:
            w.append(epi_done[gi - 1])
        m = len(t["pv"])
        for j, (o, l, r, st) in enumerate(t["pv"]):
            tk = B.op("tensor", lambda e, o=o, l=l, r=r, st=st: e.matmul(o, l, r, start=st, stop=False),
                      waits=w if j == 0 else (), sig=(j == m - 1))
        pv_done[i] = tk
        if ti == len(groups[gi]["tiles"]) - 1:
            last_pv_of_group[gi] = tk
            if gi >= 1 and groups[gi - 1].get("post"):
                groups[gi - 1]["post"](B)
            epi_done[gi] = groups[gi]["epi"](B, tk)

    dD = nS - 1
    for i in range(n + dD):
        if i < n:
            emit_S(i)
            emit_act(i)
        if i >= dD:
            emit_PV(i - dD)
    if groups and groups[-1].get("post"):
        groups[-1]["post"](B)


def _build_rest(nc, L, debug, dbg, stop):
    import types
    T = types.SimpleNamespace(**{k: v for k, v in L.items() if k not in ("es", "B")})
    hT, OT, QA, KA, KW, VA, VW, PT, Ost = T.hT, T.OT, T.QA, T.KA, T.KW, T.VA, T.VW, T.PT, T.Ost
    psS, psO, psO2, psM, psT = T.psS, T.psO, T.psO2, T.psM, T.psT
    ident, btab, tri, cmask, rden = T.ident, T.btab, T.tri, T.cmask, T.rden
    dram = T.dram
    banks4 = [psS[0], psS[1], psO[0], psO[1]]
    SB4 = [psS[0], psS[1], psO2[0], psO2[1]]

    def v3(ap2d, a):
        return ap2d.rearrange("p (a b) -> p a b", a=a)

    def gating_block(name, p, wz, zs):
        B = Blk(nc, name)
        s1 = B.newsem()
        t_w = B.dma("gpsimd", wz[:], dram["wZ"][p], s1)
        z_free = [None, None]
        ps_free = [None, None]
        for qt in range(8):
            b = qt % 2
            for c in range(8):
                t_mm = B.op("tensor", lambda e, b=b, c=c, qt=qt: e.matmul(
                    psS[b][:], wz[:, c, :], hT[:, c, qt * 512:(qt + 1) * 512], start=(c == 0), stop=(c == 7)),
                    waits=[t_w, ps_free[b]] if c == 0 else (), sig=(c == 7))
            t_s = B.op("scalar", lambda e, b=b: e.activation(out=zs[b][:], in_=psS[b][:], func=AF.Silu),
                       waits=[t_mm, z_free[b]], sig=True)
            ps_free[b] = t_s
            z_free[b] = B.op("vector", lambda e, b=b, qt=qt: e.tensor_tensor(
                out=OT[:, p, qt * 512:(qt + 1) * 512], in0=OT[:, p, qt * 512:(qt + 1) * 512], in1=zs[b][:], op=ALU.mult),
                waits=[t_s], sig=True)
        B.run()

    with ExitStack() as esM:
        def sbm(name, shape, dt):
            return esM.enter_context(nc.sbuf_tensor("m_" + name, list(shape), dt))
        QA2 = sbm("QA2", [128, S], BF16)
        Wq2 = [sbm(f"Wqkv{i}", [128, 3, 8, 64], BF16) for i in range(2)]
        maskpad = sbm("maskpad", [128, NT, 80], BF16)
        scg = sbm("scg", [128, NT, 16], F32)
        m8 = sbm("m8", [128, NT, 8], F32)
        tmpf = sbm("tmpf", [128, NT, 16], F32)
        kmf = sbm("kmf", [64, 16], F32)
        kmb = sbm("kmb", [64, 16], BF16)
        wz = sbm("wz", [128, 8, 128], BF16)
        zs = [scg[:].rearrange("p a b -> p (a b)").bitcast(BF16)[:, 0:512], tmpf[:].rearrange("p a b -> p (a b)").bitcast(BF16)[:, 0:512]]
        QS = [QA, QA2]
        KS_ = [KA, KW]
        VS_ = [VA, VW]

        B = Blk(nc, "bM0")
        t1 = B.op("vector", lambda e: e.memset(maskpad[:], 0.0), sig=True)
        B.op("vector", lambda e: e.memset(QA2[64:128, :], 0.0))
        t1b = B.op("vector", lambda e: e.memset(KW[64:128, :], 0.0), sig=True)
        t2 = B.dma("gpsimd", KA[64:80, :], dram["c_E16"][:], B.newsem())
        t3 = B.dma("gpsimd", KW[64:80, :], dram["c_E16"][:], B.newsem(), waits=[t1b])
        B.op("sync", lambda e: e.nop(), waits=[t1, t2, t3])
        B.run()

        nheads = 8 if stop is None else int(stop.get("moba_heads", 8)) if isinstance(stop, dict) else 8

        def proj_ops(B, h, t_w, banks, bank_free):
            st = h % 2
            Wt, Qd, Kd, Vd = Wq2[st], QS[st], KS_[st], VS_[st]
            ops = []
            state = {"bi": 0, "first": True}

            def mm_qk(which, qt, c):
                def f():
                    bi = state["bi"]
                    bk = banks[bi % len(banks)]
                    w = ()
                    if c == 0:
                        w = [bank_free[bi % len(banks)]] + ([t_w] if state["first"] else [])
                        state["first"] = False
                    tk = B.op("tensor", lambda e: e.matmul(bk[0:64, :], Wt[:, which, c, :], hT[:, c, qt * 512:(qt + 1) * 512],
                                                          start=(c == 0), stop=(c == 7)), waits=w, sig=(c == 7))
                    if c == 7:
                        if which == 0:
                            t_e = B.op("vector", lambda e: e.tensor_scalar(out=Qd[0:64, qt * 512:(qt + 1) * 512], in0=bk[0:64, :],
                                                                         scalar1=0.125, scalar2=None, op0=ALU.mult), waits=[tk], sig=True)
                        else:
                            t_e = B.op("vector", lambda e: e.tensor_copy(out=Kd[0:64, qt * 512:(qt + 1) * 512], in_=bk[0:64, :]),
                                       waits=[tk], sig=True)
                        bank_free[bi % len(banks)] = t_e
                        state["last_ev"] = t_e
                        state["bi"] += 1
                return f

            def mm_v(tg, j, c):
                def f():
                    bi = state["bi"]
                    bk = banks[bi % len(banks)]
                    tt = tg * 4 + j
                    w = [bank_free[bi % len(banks)]] if (c == 0 and j == 0) else ()
                    tk = B.op("tensor", lambda e: e.matmul(bk[:, j * 64:(j + 1) * 64], hT[:, c, tt * 128:(tt + 1) * 128], Wt[:, 2, c, :],
                                                          start=(c == 0), stop=(c == 7)), waits=w, sig=(c == 7 and j == 3))
                    if c == 7 and j == 3:
                        t_e = B.op("vector", lambda e: e.tensor_copy(out=Vd[:, tg * 4:(tg + 1) * 4, 0:64], in_=v3(bk[:, 0:256], 4)),
                                   waits=[tk], sig=True)
                        bank_free[bi % len(banks)] = t_e
                        state["last_ev"] = t_e
                        state["bi"] += 1
                return f

            qk_groups = [[(mm_qk(which, qt, c), 1.0) for c in range(8)] for qt in range(8) for which in (0, 1)]
            v_groups = [[(mm_v(tg, j, c), 0.15) for j in range(4) for c in range(8)] for tg in range(8)]
            gi = 0
            for k in range(8):
                ops += qk_groups[2 * k]
                ops += v_groups[k]
                ops += qk_groups[2 * k + 1]
            return ops, state

        def load_head_consts(B, h):
            st = h % 2
            t_w = B.dma("gpsimd", Wq2[st][:], dram["wA"][h], B.newsem())
            t_qr = B.dma("gpsimd", QS[st][80:82, :], dram["c_qrow"][h], B.newsem())
            t_kr = B.dma("gpsimd", KS_[st][80:82, :], dram["c_krow"][h], B.newsem())
            return t_w, t_qr, t_kr

        def selection_block(h, extra_waits=()):
            st = h % 2
            Qd, Kd = QS[st], KS_[st]
            B = Blk(nc, f"bS{h}")
            t_km = B.op("vector", lambda e: e.tensor_reduce(out=kmf[:], in_=Kd[0:64, :].rearrange("p (j k) -> p j k", k=256),
                                                          axis=AX.X, op=ALU.add), waits=list(extra_waits), sig=True)
            t_kb = B.op("vector", lambda e: e.tensor_scalar(out=kmb[:], in0=kmf[:], scalar1=1.0 / 256, scalar2=None, op0=ALU.mult),
                        waits=[t_km], sig=True)
            for tt in range(NT):
                t_g = B.op("tensor", lambda e, tt=tt: e.matmul(psM[:, tt * 16:(tt + 1) * 16], Qd[0:64, tt * 128:(tt + 1) * 128], kmb[:],
                                                              start=True, stop=True),
                           waits=[t_kb] if tt == 0 else (), sig=(tt == NT - 1))
            t_sc = B.op("vector", lambda e: e.tensor_tensor(out=scg[:], in0=v3(psM[:], NT), in1=T.addm_moba[:], op=ALU.add),
                        waits=[t_g], sig=True)
            for tt in range(NT):
                t_m8 = B.op("vector", lambda e, tt=tt: e.max(out=m8[:, tt, :], in_=scg[:, tt, :]),
                            waits=[t_sc] if tt == 0 else (), sig=(tt == NT - 1))
            t_c = B.op("vector", lambda e: e.tensor_tensor(out=tmpf[:], in0=scg[:], in1=m8[:, :, 3:4].to_broadcast([128, NT, 16]),
                                                         op=ALU.is_lt), waits=[t_m8], sig=True)
            t_mp = B.op("vector", lambda e: e.tensor_scalar(out=maskpad[:, :, 64:80], in0=tmpf[:], scalar1=NEGM, scalar2=None,
                                                          op0=ALU.mult), waits=[t_c], sig=True)
            tb_free = [None, None]
            for g8 in range(8):
                bk = psS[g8 % 2]
                for j in range(4):
                    tt = g8 * 4 + j
                    t_tr = B.op("tensor", lambda e, bk=bk, j=j, tt=tt: e.matmul(
                        bk[0:80, j * 128:(j + 1) * 128], maskpad[:, tt, :], ident[:], start=True, stop=True),
                        waits=[t_mp, tb_free[g8 % 2]] if j == 0 else (), sig=(j == 3))
                tb_free[g8 % 2] = B.op("vector", lambda e, bk=bk, g8=g8: e.tensor_copy(
                    out=Qd[64:80, g8 * 512:(g8 + 1) * 512], in_=bk[64:80, :]), waits=[t_tr], sig=True)
            B.run()

        if nheads > 0:
            B = Blk(nc, "bP0")
            t_w, t_qr, t_kr = load_head_consts(B, 0)
            bfree = [None] * 4
            ops, stt = proj_ops(B, 0, t_w, banks4, bfree)
            for f, _c in ops:
                f()
            B.op("sync", lambda e: e.nop(), waits=[t_qr, t_kr, stt["last_ev"]])
            B.run()
            selection_block(0)

        for h in range(nheads):
            pair, off = h // 2, (h % 2) * 64
            st = h % 2
            Qd, Kd, Vd = QS[st], KS_[st], VS_[st]
            B = Blk(nc, f"bT{h}")
            nxt = h + 1 < nheads
            if nxt:
                t_w, t_qr, t_kr = load_head_consts(B, h + 1)
                pbfree = [None, None]
                pops, pstate = proj_ops(B, h + 1, t_w, [psO2[1], psM], pbfree)
            SBk = [psS[0], psS[1], psO2[0]] if nxt else SB4
            groups = []
            for qt in range(8):
                aset = qt % 2
                acc = v3(psO[aset][:], 4)
                tiles = []
                first = True
                for dl in range(4 * qt + 3, -1, -1):
                    smm, pv = [], []
                    qs_valid = [qs for qs in range(4) if 4 * qt + qs - dl >= 0]
                    for qs in qs_valid:
                        kt = 4 * qt + qs - dl
                        tq = 4 * qt + qs
                        smm.append(("S", qs, Kd[0:82, kt * 128:(kt + 1) * 128], Qd[0:82, tq * 128:(tq + 1) * 128]))
                        pv.append((acc[:, qs, 0:65], qs, Vd[:, kt, :], first))
                        first = False
                    tiles.append(dict(qs=qs_valid, smm=smm, pv=pv, bias=float(-_slopes()[h] * 128.0 * dl), mask=(dl == 0)))
                groups.append(dict(qt=qt, aset=aset, tiles=tiles))
            gi_tile = 0
            ntile = sum(len(g["tiles"]) for g in groups)
            for g in groups:
                for t in g["tiles"]:
                    sb_ = SBk[gi_tile % len(SBk)]
                    pt_ = PT[gi_tile % 4]
                    q0, q1 = t["qs"][0], t["qs"][-1] + 1
                    t["smm"] = [(sb_[:, qs * 128:(qs + 1) * 128], l, r) for (_, qs, l, r) in t["smm"]]
                    t["act"] = (sb_[:, q0 * 128:q1 * 128], pt_[:, q0 * 128:q1 * 128], t["bias"])
                    t["pv"] = [(o, pt_[:, qs * 128:(qs + 1) * 128], r, st_) for (o, qs, r, st_) in t["pv"]]
                    if t["mask"]:
                        t["mask"] = (v3(pt_[:], 4), tri[:, 0:1, :].to_broadcast([128, 4, 128]))
                    else:
                        t["mask"] = None
                    gi_tile += 1

            def mk_epi(qt, aset):
                acc = v3(psO[aset][:], 4)

                def epi(B, tk):
                    rd = rden[:, aset * 4:aset * 4 + 4].unsqueeze(2)
                    t_r = B.op("vector", lambda e: e.reciprocal(out=rd, in_=acc[:, :, 64:65]),
                               waits=[tk, st_ost[aset]], sig=True)
                    t_n = B.op("vector", lambda e: e.tensor_tensor(out=Ost[aset][:, :, off:off + 64], in0=acc[:, :, 0:64],
                                                                 in1=rd.to_broadcast([128, 4, 64]), op=ALU.mult),
                               waits=[t_r], sig=True)
                    epi_tok[qt] = t_n
                    return t_n
                return epi

            def mk_post(qt, aset):
                def post(B):
                    for qs in range(4):
                        t_tr = B.op("tensor", lambda e, qs=qs: e.matmul(psT[:, qs * 128:(qs + 1) * 128], Ost[aset][:, qs, :], ident[:],
                                                                       start=True, stop=True),
                                    waits=[epi_tok[qt], post_state["psT_free"]] if qs == 0 else (), sig=(qs == 3))
                    st_ost[aset] = t_tr
                    post_state["psT_free"] = B.op("vector", lambda e: e.tensor_copy(
                        out=OT[off:off + 64, pair, qt * 512:(qt + 1) * 512], in_=psT[off:off + 64, :]), waits=[t_tr], sig=True)
                return post

            epi_tok = {}
            st_ost = [None, None]
            post_state = {"psT_free": None}
            for g in groups:
                g["epi"] = mk_epi(g["qt"], g["aset"])
                g["post"] = mk_post(g["qt"], g["aset"])
            if nxt:
                budget = sum(c_ for _f, c_ in pops) / max(1, ntile - 8)
                pq = list(pops)
                fstate = {"acc": 0.0}

                def filler(B):
                    fstate["acc"] += budget
                    while pq and fstate["acc"] > 0:
                        f, c_ = pq.pop(0)
                        f()
                        fstate["acc"] -= c_
                emit_attention(B, T, groups, filler=filler, nS=3, nPT=4)
                while pq:
                    pq.pop(0)[0]()
                B.op("sync", lambda e: e.nop(), waits=[t_qr, t_kr, pstate["last_ev"]])
            else:
                emit_attention(B, T, groups, filler=_mk_filler(lambda: psM[:, 0:128], ident, True), nS=4, nPT=4)
            B.run()
            if nxt:
                selection_block(h + 1)
            if h % 2 == 1:
                gating_block(f"bZ{pair}", pair, wz, zs)

    only = stop.get("only") if isinstance(stop, dict) else None
    if only != "moba":
        _build_nsa(nc, T, gating_block, v3, stop)

    if debug and "OT" in debug:
        with ExitStack() as esD:
            tmp = esD.enter_context(nc.sbuf_tensor("dbgtmp2", [128, 8, 512], F32))
            B = Blk(nc, "bD2")
            s1 = B.newsem("sync")
            tk = None
            for q in range(8):
                t1 = B.op("vector", lambda e, q=q: e.tensor_copy(out=tmp[:], in_=OT[:, :, q * 512:(q + 1) * 512]),
                          waits=[tk], sig=True)
                tk = B.dma("sync", dbg["OT"][:, :, q * 512:(q + 1) * 512], tmp[:], s1, waits=[t1])
            B.op("sync", lambda e: e.nop(), waits=[tk])
            B.run()


def _build_nsa(nc, T, gating_block, v3, stop):
    hT, OT, QA, KA, KW, VA, VW, PT, Ost = T.hT, T.OT, T.QA, T.KA, T.KW, T.VA, T.VW, T.PT, T.Ost
    psS, psO, psO2, psM, psT = T.psS, T.psO, T.psO2, T.psM, T.psT
    ident, btab, tri, cmask, rden, gates = T.ident, T.btab, T.tri, T.cmask, T.rden, T.gates
    dram = T.dram
    banks4 = [psS[0], psS[1], psO[0], psO[1]]
    SB4 = [psS[0], psS[1], psO2[0], psO2[1]]
    SB3 = [psS[0], psS[1], psO2[1]]
    ngroups = int(stop.get("nsa_groups", 2)) if isinstance(stop, dict) else 2
    stage = int(stop.get("nsa_stage", 9)) if isinstance(stop, dict) else 9
    with ExitStack() as esN:
        def sbn(name, shape, dt):
            return esN.enter_context(nc.sbuf_tensor("n_" + name, list(shape), dt))
        kcT = sbn("kcT", [64, 256], BF16)
        rcmp = sbn("rcmp", [128, 2, 129], BF16)
        Wq = sbn("Wq", [128, 8, 64], BF16)
        wz = sbn("wz", [128, 8, 128], BF16)
        m16 = sbn("m16", [128, NT, 16], F32)
        wk = sbn("wk", [128, 64], F32)
        maskp = [sbn(f"maskp{i}", [128, 4, 128], BF16) for i in range(2)]
        zs = [maskp[i][:].rearrange("p a b -> p (a b)") for i in range(2)]
        hs = [sbn(f"hs{i}", [128, 256], BF16) for i in range(2)]
        d3 = [sbn(f"d3{i}", [128, 4, 3], F32) for i in range(2)]
        tA = sbn("tA", [128, 4, 64], F32)
        tB = sbn("tB", [128, 4, 64], F32)
        bcol = sbn("bcol", [128, 2], F32)

        with ExitStack() as es1:
            Wg = es1.enter_context(nc.sbuf_tensor("n_Wg", [128, 8, 24], BF16))
            B = Blk(nc, "bG")
            s1, s2, s3 = B.newsem(), B.newsem(), B.newsem()
            t_w = B.dma("gpsimd", Wg[:], dram["wG"][:], s1)
            t_e = B.dma("gpsimd", KA[64:128, :], dram["c_E64"][:], s2)
            t_rc = B.dma("gpsimd", rcmp[:, :, 64:129], dram["c_rc"][:], s3)
            t_z0 = B.op("vector", lambda e: e.memset(maskp[0][:], 0.0))
            t_z1 = B.op("vector", lambda e: e.memset(maskp[1][:], 0.0))
            t_z2 = B.op("vector", lambda e: e.memset(hs[0][:], 0.0))
            t_z3 = B.op("vector", lambda e: e.memset(hs[1][:], 0.0))
            t_z4 = B.op("vector", lambda e: e.memset(kcT[:], 0.0), sig=True)
            gT = es1.enter_context(nc.sbuf_tensor("n_gT", [24, S], BF16))
            gfree = [None, None]
            for qt in range(8):
                bk = psS[qt % 2]
                for c in range(8):
                    t_mm = B.op("tensor", lambda e, bk=bk, c=c, qt=qt: e.matmul(
                        bk[0:24, :], Wg[:, c, :], hT[:, c, qt * 512:(qt + 1) * 512], start=(c == 0), stop=(c == 7)),
                        waits=[t_w, gfree[qt % 2]] if c == 0 else (), sig=(c == 7))
                gfree[qt % 2] = B.op("scalar", lambda e, bk=bk, qt=qt: e.activation(
                    out=gT[:, qt * 512:(qt + 1) * 512], in_=bk[0:24, :], func=AF.Sigmoid), waits=[t_mm], sig=True)
            for half in range(2):
                bk = (psM, psT)[half]
                for j in range(16):
                    tt = half * 16 + j
                    t_mm = B.op("tensor", lambda e, bk=bk, j=j, tt=tt: e.matmul(
                        bk[:, j * 24:(j + 1) * 24], gT[:, tt * 128:(tt + 1) * 128], ident[0:24, 0:24], start=True, stop=True),
                        waits=[gfree[0], gfree[1]] if j == 0 else (), sig=(j == 15))
                B.op("scalar", lambda e, bk=bk, half=half: e.copy(
                    out=gates[:, half * 16:(half + 1) * 16, :], in_=bk[:, 0:384].rearrange("p (a b) -> p a b", a=16)),
                    waits=[t_mm], sig=True)
            B.op("sync", lambda e: e.nop(), waits=[t_e, t_rc, t_z4, B.last("scalar")])
            B.run()

        for g in range(ngroups if stage >= 2 else 0):
            with ExitStack() as es1:
                Wkv = es1.enter_context(nc.sbuf_tensor(f"n_Wkv{g}", [128, 8, 384], BF16))
                w1kv = es1.enter_context(nc.sbuf_tensor(f"n_w1kv{g}", [128, 32, 128], BF16))
                w2kv = es1.enter_context(nc.sbuf_tensor(f"n_w2kv{g}", [128, 2, 64], BF16))
                peT = es1.enter_context(nc.sbuf_tensor(f"n_peT{g}", [128, 32], BF16))
                B = Blk(nc, f"bK{g}")
                s1, s2, s3, s4 = B.newsem(), B.newsem(), B.newsem(), B.newsem()
                B.dma("gpsimd", Wkv[:, 0:4, :], dram["wKV"][g][:, 0:4, :], s1)
                t_w = B.dma("gpsimd", Wkv[:, 4:8, :], dram["wKV"][g][:, 4:8, :], s1)
                import os
                skip = os.environ.get("BK_SKIP", "")
                for i4 in range(0 if "w" in skip else 3):
                    B.dma("gpsimd", w1kv[:, i4 * 8:(i4 + 1) * 8, :], dram["w1kv"][:, i4 * 8:(i4 + 1) * 8, :], s2)
                t_w1a = B.dma("gpsimd", w1kv[:, 24:32, :], dram["w1kv"][:, 24:32, :], s2)
                t_w1b = t_w1a
                t_w2 = B.dma("gpsimd", w2kv[:], dram["w2kv"][:], s3)
                t_pe = B.dma("gpsimd", peT[:], dram["peT"][:], s4)
                bank_free = [None] * 4
                bi = 0
                evs = {"scalar": None, "vector": None}
                specs = [(0, 128, QA, 128), (128, 64, KA, 64), (192, 64, KW, 64)]
                if "a" in skip:
                    specs = specs[0:1]
                if "b" in skip:
                    specs = specs[1:2]
                if "c" in skip:
                    specs = specs[2:3]
                for qt in range(0 if "q" in skip else 8):
                    for si, (c0, m, dst, rows) in enumerate(specs):
                        bk = banks4[bi % 4]
                        for c in range(8):
                            t_mm = B.op("tensor", lambda e, bk=bk, c=c, qt=qt, c0=c0, m=m, rows=rows: e.matmul(
                                bk[0:rows, :], Wkv[:, c, c0:c0 + m], hT[:, c, qt * 512:(qt + 1) * 512], start=(c == 0), stop=(c == 7)),
                                waits=[t_w, bank_free[bi % 4]] if c == 0 else (), sig=(c == 7))
                        eng = "scalar" if si != 1 else "vector"
                        if eng == "scalar":
                            t_e = B.op("scalar", lambda e, bk=bk, dst=dst, rows=rows, qt=qt: e.copy(
                                out=dst[0:rows, qt * 512:(qt + 1) * 512], in_=bk[0:rows, :]), waits=[t_mm], sig=True)
                        else:
                            t_e = B.op("vector", lambda e, bk=bk, dst=dst, rows=rows, qt=qt: e.tensor_copy(
                                out=dst[0:rows, qt * 512:(qt + 1) * 512], in_=bk[0:rows, :]), waits=[t_mm], sig=True)
                        evs[eng] = t_e
                        bank_free[bi % 4] = t_e
                        bi += 1
                for tg in range(0 if "v" in skip else 8):
                    bk = banks4[bi % 4]
                    for j in range(4):
                        tt = tg * 4 + j
                        for c in range(8):
                            t_mm = B.op("tensor", lambda e, bk=bk, j=j, c=c, tt=tt: e.matmul(
                                bk[:, j * 128:(j + 1) * 128], hT[:, c, tt * 128:(tt + 1) * 128], Wkv[:, c, 256:384], start=(c == 0), stop=(c == 7)),
                                waits=[bank_free[bi % 4]] if (c == 0 and j == 0) else (), sig=(c == 7 and j == 3))
                    B.op("scalar", lambda e, bk=bk, tg=tg: e.copy(
                        out=VA[:, tg * 4:(tg + 1) * 4, 0:64], in_=v3(bk[:], 4)[:, :, 0:64]), waits=[t_mm])
                    t_e2 = B.op("scalar", lambda e, bk=bk, tg=tg: e.copy(
                        out=VW[:, tg * 4:(tg + 1) * 4, 0:64], in_=v3(bk[:], 4)[:, :, 64:128]), waits=[t_mm], sig=True)
                    bank_free[bi % 4] = t_e2
                    bi += 1
                kvv = QA[:, :].rearrange("p (c s) -> p c s", s=16)
                import os
                sub = int(os.environ.get("BK_SUB", "9"))
                for kv in range(2 if sub >= 2 else 0):
                    r0 = kv * 64
                    bkH = banks4[bi % 4]
                    bi += 1
                    for l in range(32):
                        a, b_ = l // 16, l % 16
                        t_h = B.op("tensor", lambda e, bkH=bkH, l=l, a=a, b_=b_, r0=r0: e.matmul(
                            bkH[:, 0:255], w1kv[r0:r0 + 64, l, :], kvv[r0:r0 + 64, a:a + 255, b_], start=(l == 0), stop=(l == 31)),
                            waits=[t_w1a, t_w1b, evs["scalar"], evs["vector"], bank_free[(bi - 1) % 4]] if l == 0 else (), sig=(l == 31))
                    if sub == 2:
                        continue
                    if kv == 1 and sub == 3:
                        continue
                    for l in range(32):
                        t_b = B.op("tensor", lambda e, bkH=bkH, l=l, r0=r0: e.matmul(
                            bkH[:, 256:257], w1kv[r0:r0 + 64, l, :], peT[r0:r0 + 64, l:l + 1], start=(l == 0), stop=(l == 31)),
                            waits=[t_pe] if l == 0 else (), sig=(l == 31))
                    t_bc = B.op("vector", lambda e, bkH=bkH, kv=kv: e.tensor_copy(out=bcol[:, kv:kv + 1], in_=bkH[:, 256:257]),
                                waits=[t_b], sig=True)
                    t_s = B.op("scalar", lambda e, bkH=bkH, kv=kv: e.activation(
                        out=hs[kv][:, 0:255], in_=bkH[:, 0:255], func=AF.Silu, bias=bcol[:, kv:kv + 1]), waits=[t_bc, t_h], sig=True)
                    bank_free[(bi - 1) % 4] = t_s
                    if kv == 0:
                        t_k = B.op("tensor", lambda e: e.matmul(psM[0:64, 0:255], w2kv[:, 0, :], hs[0][:, 0:255], start=True, stop=True),
                                   waits=[t_s, t_w2], sig=True)
                        B.op("vector", lambda e: e.tensor_copy(out=kcT[:, 0:255], in_=psM[0:64, 0:255]), waits=[t_k], sig=True)
                    else:
                        for ct in range(2):
                            t_v = B.op("tensor", lambda e, ct=ct: e.matmul(psT[:, ct * 64:(ct + 1) * 64], hs[1][:, ct * 128:(ct + 1) * 128],
                                                                          w2kv[:, 1, :], start=True, stop=True),
                                       waits=[t_s, t_w2] if ct == 0 else (), sig=(ct == 1))
                        B.op("vector", lambda e: e.tensor_copy(out=rcmp[:, :, 0:64], in_=v3(psT[:, 0:128], 2)), waits=[t_v], sig=True)
                B.run()

            with ExitStack() as es2:
                imp = es2.enter_context(nc.sbuf_tensor(f"n_imp{g}", [128, NT, 64], F32))
                for hh in range(4 if stage >= 3 else 0):
                    hB = g * 4 + hh
                    B = Blk(nc, f"bC{hB}")
                    s1 = B.newsem()
                    t_w = B.dma("gpsimd", Wq[:], dram["wQB"][hB], s1)
                    q_ev = _proj_q(B, T, Wq, t_w)
                    groups = []
                    gi_tile = 0
                    for qt in range(8):
                        aset = qt % 2
                        acc = v3(psO[aset][:], 4)
                        tiles = []
                        first = True
                        for ct in range(qt // 4 + 1):
                            sb_ = SB4[gi_tile % 4]
                            pt_ = PT[gi_tile % 4]
                            pv = []
                            for qs in range(4):
                                pv.append((acc[:, qs, 0:65], pt_[:, qs * 128:(qs + 1) * 128], rcmp[:, ct, 64:129], first))
                                first = False
                            tiles.append(dict(
                                smm=[(sb_[:], kcT[:, ct * 128:(ct + 1) * 128], QA[0:64, qt * 512:(qt + 1) * 512])],
                                act=(sb_[:], pt_[:], None),
                                mask=(pt_[:], cmask[:, qt % 4, :]) if ct == qt // 4 else None,
                                pv=pv))
                            gi_tile += 1

                        def mk_epi(qt, aset, acc):
                            def epi(B, tk):
                                rd = rden[:, aset * 4:aset * 4 + 4].unsqueeze(2)
                                t1 = B.op("vector", lambda e: e.tensor_scalar(out=rd, in0=acc[:, :, 0:1], scalar1=1e-30, scalar2=None,
                                                                            op0=ALU.max), waits=[tk], sig=True)
                                t2 = B.op("vector", lambda e: e.reciprocal(out=rd, in_=rd), waits=[t1], sig=True)
                                dst = imp[:, 4 * qt:4 * qt + 4, :]
                                if hh == 0:
                                    t3 = B.op("vector", lambda e: e.tensor_tensor(out=dst, in0=acc[:, :, 1:65], in1=rd.to_broadcast([128, 4, 64]),
                                                                                op=ALU.mult), waits=[t2], sig=True)
                                else:
                                    t3 = B.op("vector", lambda e: e.tensor_tensor(out=tA[:], in0=acc[:, :, 1:65], in1=rd.to_broadcast([128, 4, 64]),
                                                                                op=ALU.mult), waits=[t2], sig=True)
                                    B.op("vector", lambda e: e.tensor_tensor(out=dst, in0=dst, in1=tA[:], op=ALU.add), waits=[t3], sig=True)
                                return t3
                            return epi
                        groups.append(dict(tiles=tiles, epi=mk_epi(qt, aset, acc), post=None))
                    groups[0]["tiles"][0]["extra_wait"] = q_ev
                    emit_attention(B, T, groups, filler=_mk_filler(lambda: psM[:, 0:128], ident, True), nS=4, nPT=4)
                    B.run()

                if stage < 4:
                    continue
                B = Blk(nc, f"bS{g}")
                B.op("vector", lambda e: e.memset(maskp[0][:], 0.0))
                B.op("vector", lambda e: e.memset(maskp[1][:], 0.0))
                t_a = B.op("vector", lambda e: e.tensor_tensor(out=imp[:], in0=imp[:], in1=T.addm_slc[:], op=ALU.add), sig=True)
                tk = t_a
                for tt in range(NT):
                    t1 = B.op("vector", lambda e, tt=tt: e.max(out=m16[:, tt, 0:8], in_=imp[:, tt, :]), waits=[tk], sig=True)
                    t2 = B.op("vector", lambda e, tt=tt: e.match_replace(out=wk[:], in_to_replace=m16[:, tt, 0:8], in_values=imp[:, tt, :],
                                                                       imm_value=-1e30), waits=[t1], sig=True)
                    tk = B.op("vector", lambda e, tt=tt: e.max(out=m16[:, tt, 8:16], in_=wk[:]), waits=[t2], sig=True)
                mp_free = [None, None]
                ps_free = [None, None]
                for c4 in range(8):
                    b = c4 % 2
                    t_m = B.op("vector", lambda e, c4=c4, b=b: e.tensor_tensor(
                        out=maskp[b][:, :, 64:128], in0=imp[:, 4 * c4:4 * c4 + 4, :],
                        in1=m16[:, 4 * c4:4 * c4 + 4, 15:16].to_broadcast([128, 4, 64]), op=ALU.is_lt), waits=[tk, mp_free[b]], sig=True)
                    for j in range(4):
                        t_tr = B.op("tensor", lambda e, b=b, j=j: e.matmul(psS[b][:, j * 128:(j + 1) * 128], maskp[b][:, j, :], ident[:],
                                                                         start=True, stop=True),
                                    waits=[t_m, ps_free[b]] if j == 0 else (), sig=(j == 3))
                    mp_free[b] = t_tr
                    ps_free[b] = B.op("scalar", lambda e, b=b, c4=c4: e.copy(out=KW[64:128, c4 * 512:(c4 + 1) * 512], in_=psS[b][64:128, :]),
                                      waits=[t_tr], sig=True)
                B.run()

            for hh in range(4 if stage >= 5 else 0):
                hB = g * 4 + hh
                pair, off = 4 + hB // 2, (hB % 2) * 64
                B = Blk(nc, f"bN{hB}")
                s1 = B.newsem()
                t_w = B.dma("gpsimd", Wq[:], dram["wQB"][hB], s1)
                q_ev = _proj_q(B, T, Wq, t_w)
                t_mc = B.op("vector", lambda e: e.tensor_copy(out=QA[64:128, :], in_=KW[64:128, :]), sig=True)
                groups = []
                gi_tile = 0
                for qt in range(8):
                    aset = qt % 2
                    accS = v3(psO[aset][:], 4)
                    accW = v3(psO2[0][:], 4)
                    accC = v3(psM[:], 4)
                    tiles = []
                    firstS, firstW, firstC = True, True, True
                    for dl in range(4 * qt + 3, -1, -1):
                        sb_ = SB3[gi_tile % 3]
                        pt_ = PT[gi_tile % 4]
                        qsv = [qs for qs in range(4) if 4 * qt + qs - dl >= 0]
                        smm, pv = [], []
                        for qs in qsv:
                            kt, tq = 4 * qt + qs - dl, 4 * qt + qs
                            smm.append((sb_[:, qs * 128:(qs + 1) * 128], KA[:, kt * 128:(kt + 1) * 128], QA[:, tq * 128:(tq + 1) * 128]))
                            pv.append((accS[:, qs, 0:65], pt_[:, qs * 128:(qs + 1) * 128], VA[:, kt, :], firstS))
                            firstS = False
                        q0, q1 = qsv[0], qsv[-1] + 1
                        tiles.append(dict(smm=smm, act=(sb_[:, q0 * 128:q1 * 128], pt_[:, q0 * 128:q1 * 128], btab[:, hB, dl:dl + 1]),
                                          mask=(v3(pt_[:], 4), tri[:, 0:1, :].to_broadcast([128, 4, 128])) if dl == 0 else None, pv=pv))
                        gi_tile += 1
                    for dl in range(4, -1, -1):
                        qsv = [qs for qs in range(4) if 4 * qt + qs - dl >= 0]
                        if not qsv:
                            continue
                        sb_ = SB3[gi_tile % 3]
                        pt_ = PT[gi_tile % 4]
                        smm, pv = [], []
                        for qs in qsv:
                            kt, tq = 4 * qt + qs - dl, 4 * qt + qs
                            smm.append((sb_[:, qs * 128:(qs + 1) * 128], KW[0:64, kt * 128:(kt + 1) * 128], QA[0:64, tq * 128:(tq + 1) * 128]))
                            pv.append((accW[:, qs, 0:65], pt_[:, qs * 128:(qs + 1) * 128], VW[:, kt, :], firstW))
                            firstW = False
                        q0, q1 = qsv[0], qsv[-1] + 1
                        mk = None
                        if dl == 0 or dl == 4:
                            mi = 0 if dl == 0 else 1
                            mk = (v3(pt_[:], 4)[:, q0:q1, :], tri[:, mi:mi + 1, :].to_broadcast([128, q1 - q0, 128]))
                        tiles.append(dict(smm=smm, act=(sb_[:, q0 * 128:q1 * 128], pt_[:, q0 * 128:q1 * 128], btab[:, hB, dl:dl + 1]),
                                          mask=mk, pv=pv, cmp_first=(dl == 0)))
                        gi_tile += 1
                    for ct in range(qt // 4 + 1):
                        sb_ = SB3[gi_tile % 3]
                        pt_ = PT[gi_tile % 4]
                        pv = []
                        for qs in range(4):
                            pv.append((accC[:, qs, 0:65], pt_[:, qs * 128:(qs + 1) * 128], rcmp[:, ct, 0:65], firstC))
                            firstC = False
                        tiles.append(dict(
                            smm=[(sb_[:], kcT[:, ct * 128:(ct + 1) * 128], QA[0:64, qt * 512:(qt + 1) * 512])],
                            act=(sb_[:], pt_[:], None),
                            mask=(pt_[:], cmask[:, qt % 4, :]) if ct == qt // 4 else None,
                            pv=pv, cmp_first=(ct == 0)))
                        gi_tile += 1
                    groups.append(dict(qt=qt, aset=aset, tiles=tiles, accs=(accC, accS, accW)))

                epi_tok = {}
                st_ost = [None, None]
                post_state = {"psT_free": None}

                def mk_epi(qt, aset, accs):
                    def epi(B, tk):
                        dd = d3[aset]
                        for br in range(3):
                            t1 = B.op("vector", lambda e, br=br: e.tensor_scalar(out=dd[:, :, br:br + 1], in0=accs[br][:, :, 64:65], scalar1=1e-30,
                                                                               scalar2=None, op0=ALU.max), waits=[tk], sig=(br == 2))
                        t2 = B.op("vector", lambda e: e.reciprocal(out=dd[:], in_=dd[:]), waits=[t1], sig=True)
                        t3 = B.op("vector", lambda e: e.tensor_tensor(out=dd[:], in0=dd[:], in1=gates[:, 4 * qt:4 * qt + 4, 3 * hB:3 * hB + 3],
                                                                    op=ALU.mult), waits=[t2], sig=True)
                        t4 = B.op("vector", lambda e: e.tensor_tensor(out=tA[:], in0=accs[0][:, :, 0:64],
                                                                    in1=dd[:, :, 0:1].to_broadcast([128, 4, 64]), op=ALU.mult), waits=[t3], sig=True)
                        t5 = B.op("vector", lambda e: e.tensor_tensor(out=tB[:], in0=accs[1][:, :, 0:64],
                                                                    in1=dd[:, :, 1:2].to_broadcast([128, 4, 64]), op=ALU.mult), waits=[t3], sig=True)
                        t6 = B.op("vector", lambda e: e.tensor_tensor(out=tA[:], in0=tA[:], in1=tB[:], op=ALU.add), waits=[t4, t5], sig=True)
                        t7 = B.op("vector", lambda e: e.tensor_tensor(out=tB[:], in0=accs[2][:, :, 0:64],
                                                                    in1=dd[:, :, 2:3].to_broadcast([128, 4, 64]), op=ALU.mult), waits=[t6], sig=True)
                        t8 = B.op("vector", lambda e: e.tensor_tensor(out=Ost[aset][:, :, off:off + 64], in0=tA[:], in1=tB[:], op=ALU.add),
                                  waits=[t7, st_ost[aset]], sig=True)
                        epi_tok[qt] = t8
                        return t7
                    return epi

                def mk_post(qt, aset):
                    def post(B):
                        for qs in range(4):
                            t_tr = B.op("tensor", lambda e, qs=qs: e.matmul(psT[:, qs * 128:(qs + 1) * 128], Ost[aset][:, qs, :], ident[:],
                                                                           start=True, stop=True),
                                        waits=[epi_tok[qt], post_state["psT_free"]] if qs == 0 else (), sig=(qs == 3))
                        st_ost[aset] = t_tr
                        post_state["psT_free"] = B.op("vector", lambda e: e.tensor_copy(
                            out=OT[off:off + 64, pair, qt * 512:(qt + 1) * 512], in_=psT[off:off + 64, :]), waits=[t_tr], sig=True)
                    return post

                for gr in groups:
                    gr["epi"] = mk_epi(gr["qt"], gr["aset"], gr["accs"])
                    gr["post"] = mk_post(gr["qt"], gr["aset"])
                groups[0]["tiles"][0]["extra_wait"] = [q_ev, t_mc]
                emit_attention(B, T, groups, single_acc_key="cmp_first", nS=3, nPT=4,
                               filler=_mk_filler(lambda: psT[:, 0:128], ident, True, nf=NFILL2,
                                                 wait_fn=lambda: post_state["psT_free"]))
                B.run()
                if hB % 2 == 1:
                    gating_block(f"bZ{pair}", pair, wz, zs)


NFILL = [4]


NFILL2 = "NFILL2"


def _mk_filler(dst_fn, ident, start, m=128, nf=None, wait_fn=None):
    import os
    nfill = int(os.environ.get("NFILL", NFILL[0]))
    if nf == NFILL2:
        nfill = int(os.environ.get("NFILL2", 3))
    if nfill <= 0:
        return None

    def filler(B):
        dst = dst_fn()
        n = dst.shape[-1]
        for k in range(nfill):
            w = [wait_fn()] if (wait_fn is not None and k == 0) else ()
            B.op("tensor", lambda e: e.matmul(dst, ident[:, 0:m], ident[:, 0:n], start=start, stop=start), waits=w)
    return filler


def _proj_q(B, T, Wq, t_w):
    banks4 = [T.psS[0], T.psS[1], T.psO[0], T.psO[1]]
    free = [None] * 4
    t_e = None
    for qt in range(8):
        bk = banks4[qt % 4]
        for c in range(8):
            t_mm = B.op("tensor", lambda e, bk=bk, c=c, qt=qt: e.matmul(
                bk[0:64, :], Wq[:, c, :], T.hT[:, c, qt * 512:(qt + 1) * 512], start=(c == 0), stop=(c == 7)),
                waits=[t_w, free[qt % 4]] if c == 0 else (), sig=(c == 7))
        t_e = B.op("scalar", lambda e, bk=bk, qt=qt: e.activation(
            out=T.QA[0:64, qt * 512:(qt + 1) * 512], in_=bk[0:64, :], func=AF.Copy, scale=0.125), waits=[t_mm], sig=True)
        free[qt % 4] = t_e
    return t_e


def _build_final(nc, hT, OT, psS, psO, dram, x, out, epsc, psO2=None, psM=None, psT=None):
    with ExitStack() as esF:
        def sbf(name, shape, dt):
            return esF.enter_context(nc.sbuf_tensor("f_" + name, list(shape), dt))
        wO = sbf("wO", [128, 8, D], BF16)
        gpost = sbf("gpost", [128, D], F32)
        xt = [sbf(f"xt{i}", [128, 2, D], F32) for i in range(3)]
        yt = [sbf(f"yt{i}", [128, 2, D], F32) for i in range(2)]
        junk = sbf("junk", [128, 512], BF16)
        ssq = sbf("ssq", [128, NT, 2], F32)
        rs = sbf("rs", [128, NT], F32)
        B = Blk(nc, "bF")
        sw, sg = B.newsem(), B.newsem("sync")
        t_w = [B.dma("gpsimd", wO[:, c, :], dram["wO"][:, c, :], sw) for c in range(8)]
        t_g = B.dma("sync", gpost[:], dram["gpost"][:], sg)
        xs = [B.newsem("sync") for _ in range(3)]
        os_ = [B.newsem("sync") for _ in range(2)]
        pY = [(psS[0], psS[1]), (psO[0], psO[1]), (psO2[0], psO2[1]), (psM, psT)]
        ps_free = [None] * 4
        xt_free = [None, None, None]
        yt_free = [None, None]
        t_xs = [None] * (NT // 2)
        for tp in range(NT // 2):
            b3 = tp % 3
            by = tp % 2
            xv = x[tp * 256:(tp + 1) * 256, :].rearrange("(n p) d -> p n d", p=128)
            ov = out[tp * 256:(tp + 1) * 256, :].rearrange("(n p) d -> p n d", p=128)
            if tp == 0:
                for t2 in range(2):
                    xv2 = x[t2 * 256:(t2 + 1) * 256, :].rearrange("(n p) d -> p n d", p=128)
                    t_xs[t2] = B.dma("sync", xt[t2 % 3][:], xv2, xs[t2 % 3])
            t_x = t_xs[tp]
            for j in range(2):
                tt = tp * 2 + j
                b = tt % 4
                for half in range(2):
                    pk = pY[b][half]
                    for c in range(8):
                        t_mm = B.op("tensor", lambda e, pk=pk, c=c, tt=tt, half=half: e.matmul(
                            pk[:], OT[:, c, tt * 128:(tt + 1) * 128], wO[:, c, half * 512:(half + 1) * 512], start=(c == 0), stop=(c == 7)),
                            waits=(t_w + [ps_free[b]]) if (c == 0 and half == 0) else (), sig=(c == 7 and half == 1))
                for half in range(2):
                    t_sq = B.op("scalar", lambda e, half=half, b=b, tt=tt: e.activation(
                        out=junk[:], in_=pY[b][half][:], func=AF.Square, accum_out=ssq[:, tt, half:half + 1]), waits=[t_mm], sig=True)
                t_a = B.op("vector", lambda e, tt=tt: e.tensor_tensor(out=rs[:, tt:tt + 1], in0=ssq[:, tt, 0:1], in1=ssq[:, tt, 1:2], op=ALU.add),
                           waits=[t_sq], sig=True)
                t_r1 = B.op("scalar", lambda e, tt=tt: e.activation(out=rs[:, tt:tt + 1], in_=rs[:, tt:tt + 1], func=AF.Sqrt,
                                                                  bias=epsc[:, 0:1], scale=1.0 / D), waits=[t_a], sig=True)
                t_r2 = B.op("vector", lambda e, tt=tt: e.reciprocal(out=rs[:, tt:tt + 1], in_=rs[:, tt:tt + 1]), waits=[t_r1], sig=True)
                for half in range(2):
                    t_y = B.op("vector", lambda e, half=half, b=b, tt=tt, by=by, j=j: e.scalar_tensor_tensor(
                        out=yt[by][:, j, half * 512:(half + 1) * 512], in0=pY[b][half][:], scalar=rs[:, tt:tt + 1],
                        in1=gpost[:, half * 512:(half + 1) * 512], op0=ALU.mult, op1=ALU.mult),
                        waits=[t_r2, t_g, yt_free[by]] if half == 0 else (), sig=True)
                ps_free[b] = t_y
            t_o = B.op("vector", lambda e, by=by, b3=b3: e.tensor_tensor(out=yt[by][:], in0=yt[by][:], in1=xt[b3][:], op=ALU.add),
                       waits=[t_y, t_x], sig=True)
            xt_free[b3] = t_o
            yt_free[by] = B.dma("sync", ov, yt[by][:], os_[by], waits=[t_o])
            if tp + 2 < NT // 2:
                t3 = tp + 2
                xv2 = x[t3 * 256:(t3 + 1) * 256, :].rearrange("(n p) d -> p n d", p=128)
                t_xs[t3] = B.dma("sync", xt[t3 % 3][:], xv2, xs[t3 % 3], waits=[xt_free[t3 % 3]])
        B.op("sync", lambda e: e.nop(), waits=[yt_free[0], yt_free[1]])
        B.run()


def build(debug=None, stop=None):
    nc = bass.Bass("TRN2", target_bir_lowering=False)
    dram = {}

    def din(name, shape):
        dram[name] = nc.dram_tensor(name, list(shape), F32, kind="ExternalInput").ap()
        return dram[name]

    x = din("x", [S, D])
    cshapes = {k: v.shape for k, v in host_consts().items()}
    for k, shp in cshapes.items():
        din(k, shp)
    wshapes = dict(wA=[8, 128, 3, 8, 64], wZ=[8, 128, 8, 128], wQB=[8, 128, 8, 64], wG=[128, 8, 24],
                   wKV=[2, 128, 8, 384], w1kv=[128, 32, 128], w2kv=[128, 2, 64], peT=[128, 32],
                   wO=[128, 8, 1024], gpre=[128, 8], gpost=[128, D])
    for k, shp in wshapes.items():
        din(k, shp)
    out = nc.dram_tensor("out", [S, D], F32, kind="ExternalOutput").ap()
    dbg = {}
    if debug:
        for name, shp in debug.items():
            dbg[name] = nc.dram_tensor("dbg_" + name, list(shp), F32, kind="ExternalOutput").ap()

    es = ExitStack()

    def sb(name, shape, dt):
        return es.enter_context(nc.sbuf_tensor("s_" + name, list(shape), dt))

    def ps(name, shape, dt=F32):
        return es.enter_context(nc.psum_tensor(name, list(shape), dt))

    with es:
        hT = sb("hT", [128, 8, S], BF16)
        OT = sb("OT", [128, 8, S], BF16)
        ident = sb("ident", [128, 128], BF16)
        addm_moba = sb("addm_moba", [128, NT, 16], F32)
        addm_slc = sb("addm_slc", [128, NT, 64], BF16)
        cmask = sb("cmask", [128, 4, 512], BF16)
        tri = sb("tri", [128, 2, 128], BF16)
        btab = sb("btab", [128, 8, 32], F32)
        gpre = sb("gpre", [128, 8], F32)
        epsc = sb("epsc", [128, 1], F32)
        gates = sb("gates", [128, NT, 24], F32)
        esW = ExitStack()

        def sbw(name, shape, dt):
            return esW.enter_context(nc.sbuf_tensor("w_" + name, list(shape), dt))
        QA = sbw("QA", [128, S], BF16)
        KA = sbw("KA", [128, S], BF16)
        KW = sbw("KW", [128, S], BF16)
        VA = sbw("VA", [128, NT, 65], BF16)
        VW = sbw("VW", [128, NT, 65], BF16)
        PT = [sbw(f"PT{i}", [128, 512], BF16) for i in range(4)]
        Ost = [sbw(f"Ost{i}", [128, 4, 128], BF16) for i in range(2)]
        rden = sbw("rden", [128, 16], F32)
        psS = [ps(f"psS{i}", [128, 512]) for i in range(2)]
        psO = [ps(f"psO{i}", [128, 512]) for i in range(2)]
        psO2 = [ps(f"psO2{i}", [128, 512]) for i in range(2)]
        psM = ps("psM", [128, 512])
        psT = ps("psT", [128, 512])

        POOL[0] = SemPool(nc, es)
        with nc.Block() as blk_clr:
            @blk_clr.gpsimd
            def _(g):
                for sm in POOL[0].all():
                    g.sem_clear(sm.h)

        B = Blk(nc, "b0")
        toks = []
        toks.append(B.dma("gpsimd", ident[:], dram["c_ident"][:], B.newsem()))
        toks.append(B.dma("sync", addm_moba[:], dram["c_addm_moba"][:], B.newsem("sync")))
        toks.append(B.dma("gpsimd", addm_slc[:], dram["c_addm_slc"][:], B.newsem()))
        toks.append(B.dma("gpsimd", cmask[:], dram["c_cmask"][:], B.newsem()))
        toks.append(B.dma("gpsimd", tri[:], dram["c_tri"][:], B.newsem()))
        toks.append(B.dma("sync", btab[:], dram["c_btab"][:], B.newsem("sync")))
        toks.append(B.dma("sync", gpre[:], dram["gpre"][:], B.newsem("sync")))
        B.op("vector", lambda e: e.memset(VA[:, :, 64:65], 1.0))
        B.op("vector", lambda e: e.memset(epsc[:], 1e-6))
        B.op("vector", lambda e: e.memset(VW[:, :, 64:65], 1.0))
        B.op("vector", lambda e: e.memset(QA[64:128, :], 0.0))
        t_ms = B.op("vector", lambda e: e.memset(KA[64:128, :], 0.0), sig=True)
        B.op("gpsimd", lambda e: e.memset(Ost[0][:], 0.0))
        B.op("gpsimd", lambda e: e.memset(Ost[1][:], 0.0))
        B.op("sync", lambda e: e.nop(), waits=toks)
        B.run()

        with ExitStack() as esA:
            xt = [esA.enter_context(nc.sbuf_tensor(f"xt{i}", [128, D], F32)) for i in range(4)]
            junk = esA.enter_context(nc.sbuf_tensor("junkA", [128, D], BF16))
            hb = [esA.enter_context(nc.sbuf_tensor(f"hb{i}", [128, D], BF16)) for i in range(2)]
            ss = esA.enter_context(nc.sbuf_tensor("ssA", [128, NT], F32))
            rs = esA.enter_context(nc.sbuf_tensor("rsA", [128, NT], F32))
            B = Blk(nc, "bA")
            xs = [B.newsem("sync") for _ in range(4)]
            xtok = [None] * NT
            hb_free = [None, None]
            xt_free = [None] * 4
            ps_free = [None, None]
            pA = [(psS[0], psS[1]), (psO[0], psO[1])]
            tr2 = [None] * NT
            tsq = [None] * NT

            def stage0(tt):
                b3 = tt % 4
                xtok[tt] = B.dma("sync", xt[b3][:], x[tt * 128:(tt + 1) * 128, :], xs[b3], waits=[xt_free[b3]])

            def stage1(tt):
                b3 = tt % 4
                t_sq = B.op("scalar", lambda e, b3=b3, tt=tt: e.activation(out=junk[:], in_=xt[b3][:], func=AF.Square,
                                                                     accum_out=ss[:, tt:tt + 1]),
                            waits=[xtok[tt]], sig=True)
                t_r1 = B.op("scalar", lambda e, tt=tt: e.activation(out=rs[:, tt:tt + 1], in_=ss[:, tt:tt + 1], func=AF.Sqrt,
                                                                  bias=epsc[:, 0:1], scale=1.0 / D),
                            waits=[t_sq], sig=True)
                tr2[tt] = B.op("vector", lambda e, tt=tt: e.reciprocal(out=rs[:, tt:tt + 1], in_=rs[:, tt:tt + 1]),
                               waits=[t_r1], sig=True)
                tsq[tt] = t_sq

            def stage2(tt):
                b3 = tt % 4
                b2 = tt % 2
                t_h = B.op("scalar", lambda e, tt=tt, b3=b3, b2=b2: e.activation(
                    out=hb[b2][:], in_=xt[b3][:], func=AF.Copy, scale=rs[:, tt:tt + 1]),
                    waits=[tr2[tt], hb_free[b2], tsq[tt]], sig=True)
                xt_free[b3] = t_h
                pa, pb = pA[b2]
                for c in range(8):
                    dst = (pa if c < 4 else pb)[:, (c % 4) * 128:(c % 4 + 1) * 128]
                    t_tr = B.op("tensor", lambda e, dst=dst, b2=b2, c=c: e.matmul(
                        dst, hb[b2][:, c * 128:(c + 1) * 128], ident[:], start=True, stop=True),
                        waits=[t_h, ps_free[b2]], sig=(c == 7))
                hb_free[b2] = t_tr
                B.op("vector", lambda e, tt=tt, pa=pa: e.tensor_tensor(
                    out=hT[:, 0:4, tt * 128:(tt + 1) * 128], in0=pa[:].rearrange("p (c t) -> p c t", c=4),
                    in1=gpre[:, 0:4].unsqueeze(2).to_broadcast([128, 4, 128]), op=ALU.mult), waits=[t_tr])
                ps_free[b2] = B.op("vector", lambda e, tt=tt, pb=pb: e.tensor_tensor(
                    out=hT[:, 4:8, tt * 128:(tt + 1) * 128], in0=pb[:].rearrange("p (c t) -> p c t", c=4),
                    in1=gpre[:, 4:8].unsqueeze(2).to_broadcast([128, 4, 128]), op=ALU.mult), waits=[t_tr], sig=True)

            for t0 in range(3):
                stage0(t0)
            stage1(0)
            stage1(1)
            for tt in range(NT):
                if tt + 3 < NT:
                    stage0(tt + 3)
                if tt + 2 < NT:
                    stage1(tt + 2)
                stage2(tt)
            B.run()


        if stop == "A":
            pass
        else:
            _build_rest(nc, locals(), debug, dbg, stop)
        esW.close()
        if stop is None or (isinstance(stop, dict) and stop.get("final")):
            _build_final(nc, hT, OT, psS, psO, dram, x, out, epsc, psO2, psM, psT)

        if debug and "hT" in debug:
            with ExitStack() as esD:
                tmp = esD.enter_context(nc.sbuf_tensor("dbgtmp", [128, 8, 512], F32))
                B = Blk(nc, "bD")
                s1 = B.newsem("sync")
                tk = None
                for q in range(8):
                    t1 = B.op("vector", lambda e, q=q: e.tensor_copy(out=tmp[:], in_=hT[:, :, q * 512:(q + 1) * 512]),
                              waits=[tk], sig=True)
                    tk = B.dma("sync", dbg["hT"][:, :, q * 512:(q + 1) * 512], tmp[:], s1, waits=[t1])
                B.op("sync", lambda e: e.nop(), waits=[tk])
                B.run()
    return nc


_CACHE = {}


def kernel(x, pre_norm_g, post_norm_g, w_in, cmp_pos_k, cmp_pos_v, w_cmp_k1, w_cmp_k2, w_cmp_v1, w_cmp_v2, w_out):
    x = np.asarray(x, np.float32)
    consts = host_consts()
    wts = host_weights(*(np.asarray(a, np.float32) for a in (pre_norm_g, post_norm_g, w_in, cmp_pos_k, cmp_pos_v,
                                                            w_cmp_k1, w_cmp_k2, w_cmp_v1, w_cmp_v2, w_out)))
    nc = build()
    in_maps = []
    for b in range(8):
        m = {"x": np.ascontiguousarray(x[b])}
        m.update(consts)
        m.update(wts)
        in_maps.append(m)
    res = run_bass_kernel_spmd(nc, in_maps, core_ids=list(range(8)))
    return np.stack([r["out"] for r in res.results], 0).astype(np.float32)
```

```python
import numpy as np
from contextlib import ExitStack
import concourse.bass as bass
import concourse.mybir as mybir
from concourse.bass_utils import run_bass_kernel_spmd

F32 = mybir.dt.float32
BF16 = mybir.dt.bfloat16
ALU = mybir.AluOpType
AF = mybir.ActivationFunctionType
AX = mybir.AxisListType

S = 4096
D = 1024
NT = 32
DIN = 3864
NEGM = -30000.0
ENG = ("sync", "scalar", "vector", "gpsimd", "tensor")


class Sem:
    def __init__(self, h):
        self.h = h
        self.n = 0


class SemPool:
    def __init__(self, nc, es, ndma=24):
        self.eng = {e: Sem(es.enter_context(nc.semaphore(f"pool_{e}"))) for e in ENG}
        self.dma = {"gpsimd": [Sem(es.enter_context(nc.semaphore(f"pool_g{i}"))) for i in range(ndma)],
                    "sync": [Sem(es.enter_context(nc.semaphore(f"pool_s{i}"))) for i in range(ndma)],
                    "scalar": [Sem(es.enter_context(nc.semaphore(f"pool_a{i}"))) for i in range(4)]}

    def all(self):
        return list(self.eng.values()) + self.dma["gpsimd"] + self.dma["sync"] + self.dma["scalar"]


POOL = [None]


class Blk:
    def __init__(self, nc, name):
        self.nc = nc
        self.name = name
        self.ops = {e: [] for e in ENG}
        self.esem = POOL[0].eng
        self.k = {"gpsimd": 0, "sync": 0, "scalar": 0}

    def newsem(self, kind="gpsimd"):
        lst = POOL[0].dma[kind]
        s_ = lst[self.k[kind] % len(lst)]
        self.k[kind] += 1
        s_.kind = kind
        return s_

    def op(self, eng, fn, waits=(), sig=False):
        tok = None
        s = None
        if sig:
            s = self.esem[eng]
            s.n += 1
            tok = (s.h, s.n)
        self.ops[eng].append((fn, tuple(w for w in waits if w is not None), s, 1))
        return tok

    def dma(self, eng, out, in_, sem, waits=()):
        assert sem.kind == eng, (sem.kind, eng)
        sem.n += 16
        tok = (sem.h, sem.n)
        self.ops[eng].append((lambda e: e.dma_start(out=out, in_=in_), tuple(w for w in waits if w is not None), sem, 16))
        return tok

    def last(self, eng):
        s = self.esem[eng]
        return (s.h, s.n) if s.n > 0 else None

    def run(self):
        with self.nc.Block() as block:
            for e in ENG:
                ops = self.ops[e]

                def body(eng, ops=ops):
                    seen = {}
                    for fn, waits, s, amt in ops:
                        for (h, v) in waits:
                            key = id(h)
                            if seen.get(key, 0) >= v:
                                continue
                            seen[key] = v
                            eng.wait_ge(h, v)
                        ins = fn(eng)
                        if s is not None:
                            ins.then_inc(s.h, amt)

                getattr(block, e)(body)


def _slopes():
    return [2.0 ** (-(i + 1)) for i in range(8)]


def host_consts():
    c = {}
    c["c_ident"] = np.eye(128, dtype=np.float32)
    k = np.arange(S)
    c["c_E16"] = (k[None, :] // 256 == np.arange(16)[:, None]).astype(np.float32)
    c["c_E64"] = (NEGM * (k[None, :] // 64 == np.arange(64)[:, None])).astype(np.float32)
    am = np.zeros((128, NT, 16), np.float32)
    for tt in range(NT):
        own = tt // 2
        am[:, tt, own] = 1e9
        am[:, tt, own + 1:] = -1e9
    c["c_addm_moba"] = am
    a2 = np.zeros((128, NT, 64), np.float32)
    t = (np.arange(NT)[None, :] * 128 + np.arange(128)[:, None])
    own = t // 64
    j = np.arange(64)[None, None, :]
    a2 = np.where(j > own[:, :, None], -1e9, 0.0).astype(np.float32)
    a2 = np.where(j == 0, 1e9, a2)
    a2 = np.where(j == own[:, :, None] - 1, 2e9, a2)
    a2 = np.where(j == own[:, :, None], 3e9, a2)
    c["c_addm_slc"] = a2.astype(np.float32)
    cr = np.arange(128)[:, None, None]
    r = np.arange(4)[None, :, None]
    tr = np.arange(512)[None, None, :]
    c["c_cmask"] = (16 * cr + 31 - 512 * r <= tr).astype(np.float32)
    kk = np.arange(128)[:, None]
    tq = np.arange(128)[None, :]
    c["c_tri"] = np.stack([(kk <= tq), (kk > tq)], axis=1).astype(np.float32)
    sl = np.array(_slopes(), np.float32)
    dl = np.arange(32)[None, None, :]
    c["c_btab"] = (sl[None, :, None] * (np.arange(128)[:, None, None] - 128.0 * dl - 64.0)).astype(np.float32)
    tmod = (np.arange(S) % 128).astype(np.float32)
    qrow = np.zeros((8, 2, S), np.float32)
    krow = np.zeros((8, 2, S), np.float32)
    for h in range(8):
        qrow[h, 0] = -sl[h] * tmod
        qrow[h, 1] = 1.0
        krow[h, 0] = 1.0
        krow[h, 1] = sl[h] * tmod
    c["c_qrow"] = qrow
    c["c_krow"] = krow
    rc = np.zeros((128, 2, 65), np.float32)
    cidx = np.arange(2)[None, :] * 128 + np.arange(128)[:, None]
    valid = cidx < 255
    rc[:, :, 0] = valid
    cst = cidx * 16
    js = np.arange(64)[None, None, :] * 64
    ov = (cst[:, :, None] < js + 64) & (cst[:, :, None] + 32 > js) & valid[:, :, None]
    rc[:, :, 1:] = ov
    c["c_rc"] = rc
    return c


def host_weights(pre_norm_g, post_norm_g, w_in, cmp_pos_k, cmp_pos_v, w_cmp_k1, w_cmp_k2, w_cmp_v1, w_cmp_v2, w_out):
    w = {}
    W = np.ascontiguousarray(w_in[0].reshape(8, 128, DIN).transpose(1, 0, 2))

    def cols(a, n=64):
        return W[:, :, a:a + n]

    w["wA"] = np.ascontiguousarray(np.stack(
        [np.stack([cols(0 + h * 64), cols(512 + h * 64), cols(1024 + h * 64)], axis=1) for h in range(8)], 0))
    zc = [1536 + p * 128 for p in range(4)] + [3352 + p * 128 for p in range(4)]
    w["wZ"] = np.ascontiguousarray(np.stack([cols(a, 128) for a in zc], 0))
    w["wQB"] = np.ascontiguousarray(np.stack([cols(2048 + h * 64) for h in range(8)], 0))
    w["wG"] = np.ascontiguousarray(cols(3328, 24))
    kv = []
    for g in range(2):
        kv.append(np.concatenate([cols(2560 + g * 64), cols(2688 + g * 64), cols(2816 + g * 64),
                                  cols(3072 + g * 64), cols(2944 + g * 64), cols(3200 + g * 64)], axis=2))
    w["wKV"] = np.ascontiguousarray(np.stack(kv, 0))
    k1 = w_cmp_k1[0].reshape(32, 64, 128).transpose(1, 0, 2)
    v1 = w_cmp_v1[0].reshape(32, 64, 128).transpose(1, 0, 2)
    w["w1kv"] = np.ascontiguousarray(np.concatenate([k1, v1], 0))
    w["w2kv"] = np.ascontiguousarray(np.stack([w_cmp_k2[0], w_cmp_v2[0]], 1))
    w["peT"] = np.ascontiguousarray(np.concatenate([cmp_pos_k[0].T, cmp_pos_v[0].T], 0))
    w["wO"] = np.ascontiguousarray(w_out[0].reshape(8, 128, D).transpose(1, 0, 2))
    w["gpre"] = np.ascontiguousarray(pre_norm_g[0].reshape(8, 128).T)
    w["gpost"] = np.ascontiguousarray(np.broadcast_to(post_norm_g[0][None, :], (128, D)))
    return {k: np.asarray(v, np.float32) for k, v in w.items()}


def emit_attention(B, T, groups, single_acc_key=None, filler=None, nS=2, nPT=3):
    psS, PT = T.psS, T.PT
    flat = []
    for gi, g in enumerate(groups):
        for ti, t in enumerate(g["tiles"]):
            flat.append((gi, ti, t))
    n = len(flat)
    act_done = [None] * n
    rdy = [None] * n
    s_done = [None] * n
    pv_done = [None] * n
    epi_done = [None] * len(groups)
    last_pv_of_group = [None] * len(groups)

    def emit_S(i):
        gi, ti, t = flat[i]
        w = [act_done[i - nS] if i >= nS else None]
        ew = t.get("extra_wait")
        if ew is not None:
            w += list(ew) if isinstance(ew, (list, tuple)) and not (len(ew) == 2 and not isinstance(ew[0], tuple)) else [ew]
        m = len(t["smm"])
        for j, (o, l, r) in enumerate(t["smm"]):
            tk = B.op("tensor", lambda e, o=o, l=l, r=r: e.matmul(o, l, r, start=True, stop=True),
                      waits=w if j == 0 else (), sig=(j == m - 1))
        s_done[i] = tk

    def emit_act(i):
        gi, ti, t = flat[i]
        in_, o, bias = t["act"]
        w = [s_done[i], pv_done[i - nPT] if i >= nPT else None]
        if bias is None:
            act_done[i] = B.op("scalar", lambda e, in_=in_, o=o: e.activation(out=o, in_=in_, func=AF.Exp), waits=w, sig=True)
        else:
            act_done[i] = B.op("scalar", lambda e, in_=in_, o=o, bias=bias: e.activation(out=o, in_=in_, func=AF.Exp, bias=bias),
                               waits=w, sig=True)
        rdy[i] = act_done[i]
        if t.get("mask") is not None:
            ap, mk = t["mask"]
            rdy[i] = B.op("vector", lambda e, ap=ap, mk=mk: e.tensor_tensor(out=ap, in0=ap, in1=mk, op=ALU.mult),
                          waits=[act_done[i]], sig=True)

    def emit_PV(i):
        gi, ti, t = flat[i]
        if filler is not None:
            filler(B)
        w = [rdy[i]]
        if ti == 0 and gi >= 2:
            w.append(epi_done[gi - 2])
        if single_acc_key and t.get(single_acc_key) and gi >= 1:
            w.append(epi_done[gi - 1])
        m = len(t["pv"])
        for j, (o, l, r, st) in enumerate(t["pv"]):
            tk = B.op("tensor", lambda e, o=o, l=l, r=r, st=st: e.matmul(o, l, r, start=st, stop=False),
                      waits=w if j == 0 else (), sig=(j == m - 1))
        pv_done[i] = tk
        if ti == len(groups[gi]["tiles"]) - 1:
            last_pv_of_group[gi] = tk
            if gi >= 1 and groups[gi - 1].get("post"):
                groups[gi - 1]["post"](B)
            epi_done[gi] = groups[gi]["epi"](B, tk)

    dD = nS - 1
    for i in range(n + dD):
        if i < n:
            emit_S(i)
            emit_act(i)
        if i >= dD:
            emit_PV(i - dD)
    if groups and groups[-1].get("post"):
        groups[-1]["post"](B)


def _build_rest(nc, L, debug, dbg, stop):
    import types
    T = types.SimpleNamespace(**{k: v for k, v in L.items() if k not in ("es", "B")})
    hT, OT, QA, KA, KW, VA, VW, PT, Ost = T.hT, T.OT, T.QA, T.KA, T.KW, T.VA, T.VW, T.PT, T.Ost
    psS, psO, psO2, psM, psT = T.psS, T.psO, T.psO2, T.psM, T.psT
    ident, btab, tri, cmask, rden = T.ident, T.btab, T.tri, T.cmask, T.rden
    dram = T.dram
    banks4 = [psS[0], psS[1], psO[0], psO[1]]
    SB4 = [psS[0], psS[1], psO2[0], psO2[1]]

    def v3(ap2d, a):
        return ap2d.rearrange("p (a b) -> p a b", a=a)

    def gating_block(name, p, wz, zs):
        B = Blk(nc, name)
        s1 = B.newsem()
        t_w = B.dma("gpsimd", wz[:], dram["wZ"][p], s1)
        z_free = [None, None]
        ps_free = [None, None]
        for qt in range(8):
            b = qt % 2
            for c in range(8):
                t_mm = B.op("tensor", lambda e, b=b, c=c, qt=qt: e.matmul(
                    psS[b][:], wz[:, c, :], hT[:, c, qt * 512:(qt + 1) * 512], start=(c == 0), stop=(c == 7)),
                    waits=[t_w, ps_free[b]] if c == 0 else (), sig=(c == 7))
            t_s = B.op("scalar", lambda e, b=b: e.activation(out=zs[b][:], in_=psS[b][:], func=AF.Silu),
                       waits=[t_mm, z_free[b]], sig=True)
            ps_free[b] = t_s
            z_free[b] = B.op("vector", lambda e, b=b, qt=qt: e.tensor_tensor(
                out=OT[:, p, qt * 512:(qt + 1) * 512], in0=OT[:, p, qt * 512:(qt + 1) * 512], in1=zs[b][:], op=ALU.mult),
                waits=[t_s], sig=True)
        B.run()

    with ExitStack() as esM:
        def sbm(name, shape, dt):
            return esM.enter_context(nc.sbuf_tensor("m_" + name, list(shape), dt))
        QA2 = sbm("QA2", [128, S], BF16)
        Wq2 = [sbm(f"Wqk{i}", [128, 2, 8, 128], BF16) for i in range(2)]
        Wv2 = [sbm(f"Wv{i}", [128, 8, 64], BF16) for i in range(2)]
        maskpad = sbm("maskpad", [128, NT, 16], BF16)
        scg = sbm("scg", [128, NT, 16], F32)
        m8 = sbm("m8", [128, NT, 8], F32)
        tmpf = sbm("tmpf", [128, NT, 16], F32)
        kmf = sbm("kmf", [64, 16], F32)
        kmb = sbm("kmb", [64, 16], BF16)
        wz = sbm("wz", [128, 8, 128], BF16)
        zs = [scg[:].rearrange("p a b -> p (a b)").bitcast(BF16)[:, 0:512], tmpf[:].rearrange("p a b -> p (a b)").bitcast(BF16)[:, 0:512]]
        QS = [QA, QA2]
        KS_ = [KA, KW]
        VS_ = [VA, VW]

        B = Blk(nc, "bM0")
        t1 = B.op("vector", lambda e: e.memset(maskpad[:], 0.0), sig=True)
        B.op("vector", lambda e: e.memset(QA2[64:128, :], 0.0))
        t1b = B.op("vector", lambda e: e.memset(KW[64:128, :], 0.0), sig=True)
        t2 = B.dma("gpsimd", KA[64:80, :], dram["c_E16"][:], B.newsem())
        t3 = B.dma("gpsimd", KW[64:80, :], dram["c_E16"][:], B.newsem(), waits=[t1b])
        B.op("sync", lambda e: e.nop(), waits=[t1, t2, t3])
        B.run()

        nheads = 8 if stop is None else int(stop.get("moba_heads", 8)) if isinstance(stop, dict) else 8

        def proj_ops(B, h, t_w, banks, bank_free):
            st = h % 2
            Wt, Wv, Qd, Kd, Vd = Wq2[st], Wv2[st], QS[st], KS_[st], VS_[st]
            ops = []
            state = {"bi": 0, "first": True}

            def mm_qk(which, qt, c):
                def f():
                    bi = state["bi"]
                    bk = banks[bi % len(banks)]
                    w = ()
                    if c == 0:
                        w = [bank_free[bi % len(banks)]] + ([t_w] if state["first"] else [])
                        state["first"] = False
                    tk = B.op("tensor", lambda e: e.matmul(bk[:, :], Wt[:, which, c, :], hT[:, c, qt * 512:(qt + 1) * 512],
                                                          start=(c == 0), stop=(c == 7)), waits=w, sig=(c == 7))
                    if c == 7:
                        if which == 0:
                            t_e = B.op("vector", lambda e: e.tensor_scalar(out=Qd[0:64, qt * 512:(qt + 1) * 512], in0=bk[0:64, :],
                                                                         scalar1=0.125, scalar2=None, op0=ALU.mult), waits=[tk], sig=True)
                        else:
                            t_e = B.op("vector", lambda e: e.tensor_copy(out=Kd[0:64, qt * 512:(qt + 1) * 512], in_=bk[0:64, :]),
                                       waits=[tk], sig=True)
                        bank_free[bi % len(banks)] = t_e
                        state["last_ev"] = t_e
                        state["bi"] += 1
                return f

            def mm_v(tg, j, c):
                def f():
                    bi = state["bi"]
                    bk = banks[bi % len(banks)]
                    tt = tg * 4 + j
                    w = [bank_free[bi % len(banks)]] if (c == 0 and j == 0) else ()
                    tk = B.op("tensor", lambda e: e.matmul(bk[:, j * 64:(j + 1) * 64], hT[:, c, tt * 128:(tt + 1) * 128], Wv[:, c, :],
                                                          start=(c == 0), stop=(c == 7)), waits=w, sig=(c == 7 and j == 3))
                    if c == 7 and j == 3:
                        t_e = B.op("vector", lambda e: e.tensor_copy(out=Vd[:, tg * 4:(tg + 1) * 4, 0:64], in_=v3(bk[:, 0:256], 4)),
                                   waits=[tk], sig=True)
                        bank_free[bi % len(banks)] = t_e
                        state["last_ev"] = t_e
                        state["bi"] += 1
                return f

            qk_groups = [[(mm_qk(which, qt, c), 1.0) for c in range(8)] for qt in range(8) for which in (0, 1)]
            v_groups = [[(mm_v(tg, j, c), 0.15) for j in range(4) for c in range(8)] for tg in range(8)]
            gi = 0
            for k in range(8):
                ops += qk_groups[2 * k]
                ops += v_groups[k]
                ops += qk_groups[2 * k + 1]
            return ops, state

        def load_head_consts(B, h):
            st = h % 2
            sw_ = B.newsem()
            for which in range(2):
                for half in range(2):
                    B.dma("gpsimd", Wq2[st][:, which, :, half * 64:(half + 1) * 64], dram["wA"][h][:, which], sw_)
            t_w = B.dma("gpsimd", Wv2[st][:], dram["wA"][h][:, 2], sw_)
            t_qr = B.dma("gpsimd", QS[st][80:82, :], dram["c_qrow"][h], B.newsem())
            t_kr = B.dma("gpsimd", KS_[st][80:82, :], dram["c_krow"][h], B.newsem())
            return t_w, t_qr, t_kr

        def selection_block(h, extra_waits=()):
            st = h % 2
            Qd, Kd = QS[st], KS_[st]
            B = Blk(nc, f"bS{h}")
            t_km = B.op("vector", lambda e: e.tensor_reduce(out=kmf[:], in_=Kd[0:64, :].rearrange("p (j k) -> p j k", k=256),
                                                          axis=AX.X, op=ALU.add), waits=list(extra_waits), sig=True)
            t_kb = B.op("vector", lambda e: e.tensor_scalar(out=kmb[:], in0=kmf[:], scalar1=1.0 / 256, scalar2=None, op0=ALU.mult),
                        waits=[t_km], sig=True)
            for tt in range(NT):
                t_g = B.op("tensor", lambda e, tt=tt: e.matmul(psM[:, tt * 16:(tt + 1) * 16], Qd[0:64, tt * 128:(tt + 1) * 128], kmb[:],
                                                              start=True, stop=True),
                           waits=[t_kb] if tt == 0 else (), sig=(tt == NT - 1))
            t_sc = B.op("vector", lambda e: e.tensor_tensor(out=scg[:], in0=v3(psM[:], NT), in1=T.addm_moba[:], op=ALU.add),
                        waits=[t_g], sig=True)
            for tt in range(NT):
                t_m8 = B.op("vector", lambda e, tt=tt: e.max(out=m8[:, tt, :], in_=scg[:, tt, :]),
                            waits=[t_sc] if tt == 0 else (), sig=(tt == NT - 1))
            t_c = B.op("vector", lambda e: e.tensor_tensor(out=tmpf[:], in0=scg[:], in1=m8[:, :, 3:4].to_broadcast([128, NT, 16]),
                                                         op=ALU.is_lt), waits=[t_m8], sig=True)
            t_mp = B.op("vector", lambda e: e.tensor_scalar(out=maskpad[:], in0=tmpf[:], scalar1=NEGM, scalar2=None,
                                                          op0=ALU.mult), waits=[t_c], sig=True)
            tb_free = [None, None]
            for g8 in range(8):
                bk = psS[g8 % 2]
                for j in range(4):
                    tt = g8 * 4 + j
                    t_tr = B.op("tensor", lambda e, bk=bk, j=j, tt=tt: e.matmul(
                        bk[64:80, j * 128:(j + 1) * 128], maskpad[:, tt, :], ident[:], start=True, stop=True),
                        waits=[t_mp, tb_free[g8 % 2]] if j == 0 else (), sig=(j == 3))
                tb_free[g8 % 2] = B.op("vector", lambda e, bk=bk, g8=g8: e.tensor_copy(
                    out=Qd[64:80, g8 * 512:(g8 + 1) * 512], in_=bk[64:80, :]), waits=[t_tr], sig=True)
            B.run()

        if nheads > 0:
            B = Blk(nc, "bP0")
            t_w, t_qr, t_kr = load_head_consts(B, 0)
            bfree = [None] * 4
            ops, stt = proj_ops(B, 0, t_w, banks4, bfree)
            for f, _c in ops:
                f()
            B.op("sync", lambda e: e.nop(), waits=[t_qr, t_kr, stt["last_ev"]])
            B.run()
            selection_block(0)

        for h in range(nheads):
            pair, off = h // 2, (h % 2) * 64
            st = h % 2
            Qd, Kd, Vd = QS[st], KS_[st], VS_[st]
            B = Blk(nc, f"bT{h}")
            nxt = h + 1 < nheads
            if nxt:
                t_w, t_qr, t_kr = load_head_consts(B, h + 1)
                pbfree = [None, None]
                pops, pstate = proj_ops(B, h + 1, t_w, [psO2[1], psM], pbfree)
            SBk = [psS[0], psS[1], psO2[0]] if nxt else SB4
            groups = []
            for qt in range(8):
                aset = qt % 2
                acc = v3(psO[aset][:], 4)
                tiles = []
                first = True
                for dl in range(4 * qt + 3, -1, -1):
                    smm, pv = [], []
                    qs_valid = [qs for qs in range(4) if 4 * qt + qs - dl >= 0]
                    for qs in qs_valid:
                        kt = 4 * qt + qs - dl
                        tq = 4 * qt + qs
                        smm.append(("S", qs, Kd[0:82, kt * 128:(kt + 1) * 128], Qd[0:82, tq * 128:(tq + 1) * 128]))
                        pv.append((acc[:, qs, 0:65], qs, Vd[:, kt, :], first))
                        first = False
                    tiles.append(dict(qs=qs_valid, smm=smm, pv=pv, bias=float(-_slopes()[h] * 128.0 * dl), mask=(dl == 0)))
                groups.append(dict(qt=qt, aset=aset, tiles=tiles))
            gi_tile = 0
            ntile = sum(len(g["tiles"]) for g in groups)
            for g in groups:
                for t in g["tiles"]:
                    sb_ = SBk[gi_tile % len(SBk)]
                    pt_ = PT[gi_tile % 4]
                    q0, q1 = t["qs"][0], t["qs"][-1] + 1
                    t["smm"] = [(sb_[:, qs * 128:(qs + 1) * 128], l, r) for (_, qs, l, r) in t["smm"]]
                    t["act"] = (sb_[:, q0 * 128:q1 * 128], pt_[:, q0 * 128:q1 * 128], t["bias"])
                    t["pv"] = [(o, pt_[:, qs * 128:(qs + 1) * 128], r, st_) for (o, qs, r, st_) in t["pv"]]
                    if t["mask"]:
                        t["mask"] = (v3(pt_[:], 4), tri[:, 0:1, :].to_broadcast([128, 4, 128]))
                    else:
                        t["mask"] = None
                    gi_tile += 1

            def mk_epi(qt, aset):
                acc = v3(psO[aset][:], 4)

                def epi(B, tk):
                    rd = rden[:, aset * 4:aset * 4 + 4].unsqueeze(2)
                    t_r = B.op("vector", lambda e: e.reciprocal(out=rd, in_=acc[:, :, 64:65]),
                               waits=[tk, st_ost[aset]], sig=True)
                    t_n = B.op("vector", lambda e: e.tensor_tensor(out=Ost[aset][:, :, off:off + 64], in0=acc[:, :, 0:64],
                                                                 in1=rd.to_broadcast([128, 4, 64]), op=ALU.mult),
                               waits=[t_r], sig=True)
                    epi_tok[qt] = t_n
                    return t_n
                return epi

            def mk_post(qt, aset):
                def post(B):
                    for qs in range(4):
                        t_tr = B.op("tensor", lambda e, qs=qs: e.matmul(psT[:, qs * 128:(qs + 1) * 128], Ost[aset][:, qs, :], ident[:],
                                                                       start=True, stop=True),
                                    waits=[epi_tok[qt], post_state["psT_free"]] if qs == 0 else (), sig=(qs == 3))
                    st_ost[aset] = t_tr
                    post_state["psT_free"] = B.op("vector", lambda e: e.tensor_copy(
                        out=OT[off:off + 64, pair, qt * 512:(qt + 1) * 512], in_=psT[off:off + 64, :]), waits=[t_tr], sig=True)
                return post

            epi_tok = {}
            st_ost = [None, None]
            post_state = {"psT_free": None}
            for g in groups:
                g["epi"] = mk_epi(g["qt"], g["aset"])
                g["post"] = mk_post(g["qt"], g["aset"])
            if nxt:
                budget = sum(c_ for _f, c_ in pops) / max(1, ntile - 8)
                pq = list(pops)
                fstate = {"acc": 0.0}

                def filler(B):
                    fstate["acc"] += budget
                    while pq and fstate["acc"] > 0:
                        f, c_ = pq.pop(0)
                        f()
                        fstate["acc"] -= c_
                emit_attention(B, T, groups, filler=filler, nS=3, nPT=4)
                while pq:
                    pq.pop(0)[0]()
                B.op("sync", lambda e: e.nop(), waits=[t_qr, t_kr, pstate["last_ev"]])
            else:
                emit_attention(B, T, groups, filler=_mk_filler(lambda: psM[:, 0:128], ident, True), nS=4, nPT=4)
            B.run()
            if nxt:
                selection_block(h + 1)
            if h % 2 == 1:
                gating_block(f"bZ{pair}", pair, wz, zs)

    only = stop.get("only") if isinstance(stop, dict) else None
    if only != "moba":
        _build_nsa(nc, T, gating_block, v3, stop)

    if debug and "OT" in debug:
        with ExitStack() as esD:
            tmp = esD.enter_context(nc.sbuf_tensor("dbgtmp2", [128, 8, 512], F32))
            B = Blk(nc, "bD2")
            s1 = B.newsem("sync")
            tk = None
            for q in range(8):
                t1 = B.op("vector", lambda e, q=q: e.tensor_copy(out=tmp[:], in_=OT[:, :, q * 512:(q + 1) * 512]),
                          waits=[tk], sig=True)
                tk = B.dma("sync", dbg["OT"][:, :, q * 512:(q + 1) * 512], tmp[:], s1, waits=[t1])
            B.op("sync", lambda e: e.nop(), waits=[tk])
            B.run()


def _build_nsa(nc, T, gating_block, v3, stop):
    hT, OT, QA, KA, KW, VA, VW, PT, Ost = T.hT, T.OT, T.QA, T.KA, T.KW, T.VA, T.VW, T.PT, T.Ost
    psS, psO, psO2, psM, psT = T.psS, T.psO, T.psO2, T.psM, T.psT
    ident, btab, tri, cmask, rden, gates = T.ident, T.btab, T.tri, T.cmask, T.rden, T.gates
    dram = T.dram
    banks4 = [psS[0], psS[1], psO[0], psO[1]]
    SB4 = [psS[0], psS[1], psO2[0], psO2[1]]
    SB3 = [psS[0], psS[1], psO2[1]]
    ngroups = int(stop.get("nsa_groups", 2)) if isinstance(stop, dict) else 2
    stage = int(stop.get("nsa_stage", 9)) if isinstance(stop, dict) else 9
    with ExitStack() as esN:
        def sbn(name, shape, dt):
            return esN.enter_context(nc.sbuf_tensor("n_" + name, list(shape), dt))
        kcT = sbn("kcT", [64, 256], BF16)
        rcmp = sbn("rcmp", [128, 2, 129], BF16)
        Wq = sbn("Wq", [128, 8, 64], BF16)
        wz = sbn("wz", [128, 8, 128], BF16)
        m16 = sbn("m16", [128, NT, 16], F32)
        wk = sbn("wk", [128, 64], F32)
        maskp = [sbn(f"maskp{i}", [128, 4, 128], BF16) for i in range(2)]
        zs = [maskp[i][:].rearrange("p a b -> p (a b)") for i in range(2)]
        hs = [sbn(f"hs{i}", [128, 256], BF16) for i in range(2)]
        d3 = [sbn(f"d3{i}", [128, 4, 3], F32) for i in range(2)]
        tA = sbn("tA", [128, 4, 64], F32)
        tB = sbn("tB", [128, 4, 64], F32)
        bcol = sbn("bcol", [128, 2], F32)

        with ExitStack() as es1:
            Wg = es1.enter_context(nc.sbuf_tensor("n_Wg", [128, 8, 24], BF16))
            B = Blk(nc, "bG")
            s1, s2, s3 = B.newsem(), B.newsem(), B.newsem()
            t_w = B.dma("gpsimd", Wg[:], dram["wG"][:], s1)
            t_e = B.dma("gpsimd", KA[64:128, :], dram["c_E64"][:], s2)
            t_rc = B.dma("gpsimd", rcmp[:, :, 64:129], dram["c_rc"][:], s3)
            t_z0 = B.op("vector", lambda e: e.memset(maskp[0][:], 0.0))
            t_z1 = B.op("vector", lambda e: e.memset(maskp[1][:], 0.0))
            t_z2 = B.op("vector", lambda e: e.memset(hs[0][:], 0.0))
            t_z3 = B.op("vector", lambda e: e.memset(hs[1][:], 0.0))
            t_z4 = B.op("vector", lambda e: e.memset(kcT[:], 0.0), sig=True)
            gT = es1.enter_context(nc.sbuf_tensor("n_gT", [24, S], BF16))
            gfree = [None, None]
            for qt in range(8):
                bk = psS[qt % 2]
                for c in range(8):
                    t_mm = B.op("tensor", lambda e, bk=bk, c=c, qt=qt: e.matmul(
                        bk[0:24, :], Wg[:, c, :], hT[:, c, qt * 512:(qt + 1) * 512], start=(c == 0), stop=(c == 7)),
                        waits=[t_w, gfree[qt % 2]] if c == 0 else (), sig=(c == 7))
                gfree[qt % 2] = B.op("scalar", lambda e, bk=bk, qt=qt: e.activation(
                    out=gT[:, qt * 512:(qt + 1) * 512], in_=bk[0:24, :], func=AF.Sigmoid), waits=[t_mm], sig=True)
            for half in range(2):
                bk = (psM, psT)[half]
                for j in range(16):
                    tt = half * 16 + j
                    t_mm = B.op("tensor", lambda e, bk=bk, j=j, tt=tt: e.matmul(
                        bk[:, j * 24:(j + 1) * 24], gT[:, tt * 128:(tt + 1) * 128], ident[0:24, 0:24], start=True, stop=True),
                        waits=[gfree[0], gfree[1]] if j == 0 else (), sig=(j == 15))
                B.op("scalar", lambda e, bk=bk, half=half: e.copy(
                    out=gates[:, half * 16:(half + 1) * 16, :], in_=bk[:, 0:384].rearrange("p (a b) -> p a b", a=16)),
                    waits=[t_mm], sig=True)
            B.op("sync", lambda e: e.nop(), waits=[t_e, t_rc, t_z4, B.last("scalar")])
            B.run()

        for g in range(ngroups if stage >= 2 else 0):
            with ExitStack() as es1:
                Wkv = es1.enter_context(nc.sbuf_tensor(f"n_Wkv{g}", [128, 8, 384], BF16))
                w1kv = es1.enter_context(nc.sbuf_tensor(f"n_w1kv{g}", [128, 32, 128], BF16))
                w2kv = es1.enter_context(nc.sbuf_tensor(f"n_w2kv{g}", [128, 2, 64], BF16))
                peT = es1.enter_context(nc.sbuf_tensor(f"n_peT{g}", [128, 32], BF16))
                B = Blk(nc, f"bK{g}")
                s1, s2, s3, s4 = B.newsem(), B.newsem(), B.newsem(), B.newsem()
                B.dma("gpsimd", Wkv[:, 0:4, :], dram["wKV"][g][:, 0:4, :], s1)
                t_w = B.dma("gpsimd", Wkv[:, 4:8, :], dram["wKV"][g][:, 4:8, :], s1)
                import os
                skip = os.environ.get("BK_SKIP", "")
                for i4 in range(0 if "w" in skip else 3):
                    B.dma("gpsimd", w1kv[:, i4 * 8:(i4 + 1) * 8, :], dram["w1kv"][:, i4 * 8:(i4 + 1) * 8, :], s2)
                t_w1a = B.dma("gpsimd", w1kv[:, 24:32, :], dram["w1kv"][:, 24:32, :], s2)
                t_w1b = t_w1a
                t_w2 = B.dma("gpsimd", w2kv[:], dram["w2kv"][:], s3)
                t_pe = B.dma("gpsimd", peT[:], dram["peT"][:], s4)
                bank_free = [None] * 4
                bi = 0
                evs = {"scalar": None, "vector": None}
                specs = [(0, 128, QA, 128), (128, 64, KA, 64), (192, 64, KW, 64)]
                if "a" in skip:
                    specs = specs[0:1]
                if "b" in skip:
                    specs = specs[1:2]
                if "c" in skip:
                    specs = specs[2:3]
                for qt in range(0 if "q" in skip else 8):
                    for si, (c0, m, dst, rows) in enumerate(specs):
                        bk = banks4[bi % 4]
                        for c in range(8):
                            t_mm = B.op("tensor", lambda e, bk=bk, c=c, qt=qt, c0=c0, m=m, rows=rows: e.matmul(
                                bk[0:rows, :], Wkv[:, c, c0:c0 + m], hT[:, c, qt * 512:(qt + 1) * 512], start=(c == 0), stop=(c == 7)),
                                waits=[t_w, bank_free[bi % 4]] if c == 0 else (), sig=(c == 7))
                        eng = "scalar" if si != 1 else "vector"
                        if eng == "scalar":
                            t_e = B.op("scalar", lambda e, bk=bk, dst=dst, rows=rows, qt=qt: e.copy(
                                out=dst[0:rows, qt * 512:(qt + 1) * 512], in_=bk[0:rows, :]), waits=[t_mm], sig=True)
                        else:
                            t_e = B.op("vector", lambda e, bk=bk, dst=dst, rows=rows, qt=qt: e.tensor_copy(
                                out=dst[0:rows, qt * 512:(qt + 1) * 512], in_=bk[0:rows, :]), waits=[t_mm], sig=True)
                        evs[eng] = t_e
                        bank_free[bi % 4] = t_e
                        bi += 1
                for tg in range(0 if "v" in skip else 8):
                    bk = banks4[bi % 4]
                    for j in range(4):
                        tt = tg * 4 + j
                        for c in range(8):
                            t_mm = B.op("tensor", lambda e, bk=bk, j=j, c=c, tt=tt: e.matmul(
                                bk[:, j * 128:(j + 1) * 128], hT[:, c, tt * 128:(tt + 1) * 128], Wkv[:, c, 256:384], start=(c == 0), stop=(c == 7)),
                                waits=[bank_free[bi % 4]] if (c == 0 and j == 0) else (), sig=(c == 7 and j == 3))
                    B.op("scalar", lambda e, bk=bk, tg=tg: e.copy(
                        out=VA[:, tg * 4:(tg + 1) * 4, 0:64], in_=v3(bk[:], 4)[:, :, 0:64]), waits=[t_mm])
                    t_e2 = B.op("scalar", lambda e, bk=bk, tg=tg: e.copy(
                        out=VW[:, tg * 4:(tg + 1) * 4, 0:64], in_=v3(bk[:], 4)[:, :, 64:128]), waits=[t_mm], sig=True)
                    bank_free[bi % 4] = t_e2
                    bi += 1
                kvv = QA[:, :].rearrange("p (c s) -> p c s", s=16)
                import os
                sub = int(os.environ.get("BK_SUB", "9"))
                for kv in range(2 if sub >= 2 else 0):
                    r0 = kv * 64
                    bkH = banks4[bi % 4]
                    bi += 1
                    for l in range(32):
                        a, b_ = l // 16, l % 16
                        t_h = B.op("tensor", lambda e, bkH=bkH, l=l, a=a, b_=b_, r0=r0: e.matmul(
                            bkH[:, 0:255], w1kv[r0:r0 + 64, l, :], kvv[r0:r0 + 64, a:a + 255, b_], start=(l == 0), stop=(l == 31)),
                            waits=[t_w1a, t_w1b, evs["scalar"], evs["vector"], bank_free[(bi - 1) % 4]] if l == 0 else (), sig=(l == 31))
                    if sub == 2:
                        continue
                    if kv == 1 and sub == 3:
                        continue
                    for l in range(32):
                        t_b = B.op("tensor", lambda e, bkH=bkH, l=l, r0=r0: e.matmul(
                            bkH[:, 256:257], w1kv[r0:r0 + 64, l, :], peT[r0:r0 + 64, l:l + 1], start=(l == 0), stop=(l == 31)),
                            waits=[t_pe] if l == 0 else (), sig=(l == 31))
                    t_bc = B.op("vector", lambda e, bkH=bkH, kv=kv: e.tensor_copy(out=bcol[:, kv:kv + 1], in_=bkH[:, 256:257]),
                                waits=[t_b], sig=True)
                    t_s = B.op("scalar", lambda e, bkH=bkH, kv=kv: e.activation(
                        out=hs[kv][:, 0:255], in_=bkH[:, 0:255], func=AF.Silu, bias=bcol[:, kv:kv + 1]), waits=[t_bc, t_h], sig=True)
                    bank_free[(bi - 1) % 4] = t_s
                    if kv == 0:
                        t_k = B.op("tensor", lambda e: e.matmul(psM[0:64, 0:255], w2kv[:, 0, :], hs[0][:, 0:255], start=True, stop=True),
                                   waits=[t_s, t_w2], sig=True)
                        B.op("vector", lambda e: e.tensor_copy(out=kcT[:, 0:255], in_=psM[0:64, 0:255]), waits=[t_k], sig=True)
                    else:
                        for ct in range(2):
                            t_v = B.op("tensor", lambda e, ct=ct: e.matmul(psT[:, ct * 64:(ct + 1) * 64], hs[1][:, ct * 128:(ct + 1) * 128],
                                                                          w2kv[:, 1, :], start=True, stop=True),
                                       waits=[t_s, t_w2] if ct == 0 else (), sig=(ct == 1))
                        B.op("vector", lambda e: e.tensor_copy(out=rcmp[:, :, 0:64], in_=v3(psT[:, 0:128], 2)), waits=[t_v], sig=True)
                B.run()

            with ExitStack() as es2:
                imp = es2.enter_context(nc.sbuf_tensor(f"n_imp{g}", [128, NT, 64], F32))
                for hh in range(4 if stage >= 3 else 0):
                    hB = g * 4 + hh
                    B = Blk(nc, f"bC{hB}")
                    s1 = B.newsem()
                    t_w = B.dma("gpsimd", Wq[:], dram["wQB"][hB], s1)
                    q_ev = _proj_q(B, T, Wq, t_w)
                    groups = []
                    gi_tile = 0
                    for qt in range(8):
                        aset = qt % 2
                        acc = v3(psO[aset][:], 4)
                        tiles = []
                        first = True
                        for ct in range(qt // 4 + 1):
                            sb_ = SB4[gi_tile % 4]
                            pt_ = PT[gi_tile % 4]
                            pv = []
                            for qs in range(4):
                                pv.append((acc[:, qs, 0:65], pt_[:, qs * 128:(qs + 1) * 128], rcmp[:, ct, 64:129], first))
                                first = False
                            tiles.append(dict(
                                smm=[(sb_[:], kcT[:, ct * 128:(ct + 1) * 128], QA[0:64, qt * 512:(qt + 1) * 512])],
                                act=(sb_[:], pt_[:], None),
                                mask=(pt_[:], cmask[:, qt % 4, :]) if ct == qt // 4 else None,
                                pv=pv))
                            gi_tile += 1

                        def mk_epi(qt, aset, acc):
                            def epi(B, tk):
                                rd = rden[:, aset * 4:aset * 4 + 4].unsqueeze(2)
                                t1 = B.op("vector", lambda e: e.tensor_scalar(out=rd, in0=acc[:, :, 0:1], scalar1=1e-30, scalar2=None,
                                                                            op0=ALU.max), waits=[tk], sig=True)
                                t2 = B.op("vector", lambda e: e.reciprocal(out=rd, in_=rd), waits=[t1], sig=True)
                                dst = imp[:, 4 * qt:4 * qt + 4, :]
                                if hh == 0:
                                    t3 = B.op("vector", lambda e: e.tensor_tensor(out=dst, in0=acc[:, :, 1:65], in1=rd.to_broadcast([128, 4, 64]),
                                                                                op=ALU.mult), waits=[t2], sig=True)
                                else:
                                    t3 = B.op("vector", lambda e: e.tensor_tensor(out=tA[:], in0=acc[:, :, 1:65], in1=rd.to_broadcast([128, 4, 64]),
                                                                                op=ALU.mult), waits=[t2], sig=True)
                                    B.op("vector", lambda e: e.tensor_tensor(out=dst, in0=dst, in1=tA[:], op=ALU.add), waits=[t3], sig=True)
                                return t3
                            return epi
                        groups.append(dict(tiles=tiles, epi=mk_epi(qt, aset, acc), post=None))
                    groups[0]["tiles"][0]["extra_wait"] = q_ev
                    emit_attention(B, T, groups, filler=_mk_filler(lambda: psM[:, 0:128], ident, True), nS=4, nPT=4)
                    B.run()

                if stage < 4:
                    continue
                B = Blk(nc, f"bS{g}")
                B.op("vector", lambda e: e.memset(maskp[0][:], 0.0))
                B.op("vector", lambda e: e.memset(maskp[1][:], 0.0))
                t_a = B.op("vector", lambda e: e.tensor_tensor(out=imp[:], in0=imp[:], in1=T.addm_slc[:], op=ALU.add), sig=True)
                tk = t_a
                for tt in range(NT):
                    t1 = B.op("vector", lambda e, tt=tt: e.max(out=m16[:, tt, 0:8], in_=imp[:, tt, :]), waits=[tk], sig=True)
                    t2 = B.op("vector", lambda e, tt=tt: e.match_replace(out=wk[:], in_to_replace=m16[:, tt, 0:8], in_values=imp[:, tt, :],
                                                                       imm_value=-1e30), waits=[t1], sig=True)
                    tk = B.op("vector", lambda e, tt=tt: e.max(out=m16[:, tt, 8:16], in_=wk[:]), waits=[t2], sig=True)
                mp_free = [None, None]
                ps_free = [None, None]
                for c4 in range(8):
                    b = c4 % 2
                    t_m = B.op("vector", lambda e, c4=c4, b=b: e.tensor_tensor(
                        out=maskp[b][:, :, 64:128], in0=imp[:, 4 * c4:4 * c4 + 4, :],
                        in1=m16[:, 4 * c4:4 * c4 + 4, 15:16].to_broadcast([128, 4, 64]), op=ALU.is_lt), waits=[tk, mp_free[b]], sig=True)
                    for j in range(4):
                        t_tr = B.op("tensor", lambda e, b=b, j=j: e.matmul(psS[b][:, j * 128:(j + 1) * 128], maskp[b][:, j, :], ident[:],
                                                                         start=True, stop=True),
                                    waits=[t_m, ps_free[b]] if j == 0 else (), sig=(j == 3))
                    mp_free[b] = t_tr
                    ps_free[b] = B.op("scalar", lambda e, b=b, c4=c4: e.copy(out=KW[64:128, c4 * 512:(c4 + 1) * 512], in_=psS[b][64:128, :]),
                                      waits=[t_tr], sig=True)
                B.run()

            for hh in range(4 if stage >= 5 else 0):
                hB = g * 4 + hh
                pair, off = 4 + hB // 2, (hB % 2) * 64
                B = Blk(nc, f"bN{hB}")
                s1 = B.newsem()
                t_w = B.dma("gpsimd", Wq[:], dram["wQB"][hB], s1)
                q_ev = _proj_q(B, T, Wq, t_w)
                t_mc = B.op("vector", lambda e: e.tensor_copy(out=QA[64:128, :], in_=KW[64:128, :]), sig=True)
                groups = []
                gi_tile = 0
                for qt in range(8):
                    aset = qt % 2
                    accS = v3(psO[aset][:], 4)
                    accW = v3(psO2[0][:], 4)
                    accC = v3(psM[:], 4)
                    tiles = []
                    firstS, firstW, firstC = True, True, True
                    for dl in range(4 * qt + 3, -1, -1):
                        sb_ = SB3[gi_tile % 3]
                        pt_ = PT[gi_tile % 4]
                        qsv = [qs for qs in range(4) if 4 * qt + qs - dl >= 0]
                        smm, pv = [], []
                        for qs in qsv:
                            kt, tq = 4 * qt + qs - dl, 4 * qt + qs
                            smm.append((sb_[:, qs * 128:(qs + 1) * 128], KA[:, kt * 128:(kt + 1) * 128], QA[:, tq * 128:(tq + 1) * 128]))
                            pv.append((accS[:, qs, 0:65], pt_[:, qs * 128:(qs + 1) * 128], VA[:, kt, :], firstS))
                            firstS = False
                        q0, q1 = qsv[0], qsv[-1] + 1
                        tiles.append(dict(smm=smm, act=(sb_[:, q0 * 128:q1 * 128], pt_[:, q0 * 128:q1 * 128], btab[:, hB, dl:dl + 1]),
                                          mask=(v3(pt_[:], 4), tri[:, 0:1, :].to_broadcast([128, 4, 128])) if dl == 0 else None, pv=pv))
                        gi_tile += 1
                    for dl in range(4, -1, -1):
                        qsv = [qs for qs in range(4) if 4 * qt + qs - dl >= 0]
                        if not qsv:
                            continue
                        sb_ = SB3[gi_tile % 3]
                        pt_ = PT[gi_tile % 4]
                        smm, pv = [], []
                        for qs in qsv:
                            kt, tq = 4 * qt + qs - dl, 4 * qt + qs
                            smm.append((sb_[:, qs * 128:(qs + 1) * 128], KW[0:64, kt * 128:(kt + 1) * 128], QA[0:64, tq * 128:(tq + 1) * 128]))
                            pv.append((accW[:, qs, 0:65], pt_[:, qs * 128:(qs + 1) * 128], VW[:, kt, :], firstW))
                            firstW = False
                        q0, q1 = qsv[0], qsv[-1] + 1
                        mk = None
                        if dl == 0 or dl == 4:
                            mi = 0 if dl == 0 else 1
                            mk = (v3(pt_[:], 4)[:, q0:q1, :], tri[:, mi:mi + 1, :].to_broadcast([128, q1 - q0, 128]))
                        tiles.append(dict(smm=smm, act=(sb_[:, q0 * 128:q1 * 128], pt_[:, q0 * 128:q1 * 128], btab[:, hB, dl:dl + 1]),
                                          mask=mk, pv=pv, cmp_first=(dl == 0)))
                        gi_tile += 1
                    for ct in range(qt // 4 + 1):
                        sb_ = SB3[gi_tile % 3]
                        pt_ = PT[gi_tile % 4]
                        pv = []
                        for qs in range(4):
                            pv.append((accC[:, qs, 0:65], pt_[:, qs * 128:(qs + 1) * 128], rcmp[:, ct, 0:65], firstC))
                            firstC = False
                        tiles.append(dict(
                            smm=[(sb_[:], kcT[:, ct * 128:(ct + 1) * 128], QA[0:64, qt * 512:(qt + 1) * 512])],
                            act=(sb_[:], pt_[:], None),
                            mask=(pt_[:], cmask[:, qt % 4, :]) if ct == qt // 4 else None,
                            pv=pv, cmp_first=(ct == 0)))
                        gi_tile += 1
                    groups.append(dict(qt=qt, aset=aset, tiles=tiles, accs=(accC, accS, accW)))

                epi_tok = {}
                st_ost = [None, None]
                post_state = {"psT_free": None}

                def mk_epi(qt, aset, accs):
                    def epi(B, tk):
                        dd = d3[aset]
                        for br in range(3):
                            t1 = B.op("vector", lambda e, br=br: e.tensor_scalar(out=dd[:, :, br:br + 1], in0=accs[br][:, :, 64:65], scalar1=1e-30,
                                                                               scalar2=None, op0=ALU.max), waits=[tk], sig=(br == 2))
                        t2 = B.op("vector", lambda e: e.reciprocal(out=dd[:], in_=dd[:]), waits=[t1], sig=True)
                        t3 = B.op("vector", lambda e: e.tensor_tensor(out=dd[:], in0=dd[:], in1=gates[:, 4 * qt:4 * qt + 4, 3 * hB:3 * hB + 3],
                                                                    op=ALU.mult), waits=[t2], sig=True)
                        t4 = B.op("vector", lambda e: e.tensor_tensor(out=tA[:], in0=accs[0][:, :, 0:64],
                                                                    in1=dd[:, :, 0:1].to_broadcast([128, 4, 64]), op=ALU.mult), waits=[t3], sig=True)
                        t5 = B.op("vector", lambda e: e.tensor_tensor(out=tB[:], in0=accs[1][:, :, 0:64],
                                                                    in1=dd[:, :, 1:2].to_broadcast([128, 4, 64]), op=ALU.mult), waits=[t3], sig=True)
                        t6 = B.op("vector", lambda e: e.tensor_tensor(out=tA[:], in0=tA[:], in1=tB[:], op=ALU.add), waits=[t4, t5], sig=True)
                        t7 = B.op("vector", lambda e: e.tensor_tensor(out=tB[:], in0=accs[2][:, :, 0:64],
                                                                    in1=dd[:, :, 2:3].to_broadcast([128, 4, 64]), op=ALU.mult), waits=[t6], sig=True)
                        t8 = B.op("vector", lambda e: e.tensor_tensor(out=Ost[aset][:, :, off:off + 64], in0=tA[:], in1=tB[:], op=ALU.add),
                                  waits=[t7, st_ost[aset]], sig=True)
                        epi_tok[qt] = t8
                        return t7
                    return epi

                def mk_post(qt, aset):
                    def post(B):
                        for qs in range(4):
                            t_tr = B.op("tensor", lambda e, qs=qs: e.matmul(psT[:, qs * 128:(qs + 1) * 128], Ost[aset][:, qs, :], ident[:],
                                                                           start=True, stop=True),
                                        waits=[epi_tok[qt], post_state["psT_free"]] if qs == 0 else (), sig=(qs == 3))
                        st_ost[aset] = t_tr
                        post_state["psT_free"] = B.op("vector", lambda e: e.tensor_copy(
                            out=OT[off:off + 64, pair, qt * 512:(qt + 1) * 512], in_=psT[off:off + 64, :]), waits=[t_tr], sig=True)
                    return post

                for gr in groups:
                    gr["epi"] = mk_epi(gr["qt"], gr["aset"], gr["accs"])
                    gr["post"] = mk_post(gr["qt"], gr["aset"])
                groups[0]["tiles"][0]["extra_wait"] = [q_ev, t_mc]
                emit_attention(B, T, groups, single_acc_key="cmp_first", nS=3, nPT=4,
                               filler=_mk_filler(lambda: psT[:, 0:128], ident, True, nf=NFILL2,
                                                 wait_fn=lambda: post_state["psT_free"]))
                B.run()
                if hB % 2 == 1:
                    gating_block(f"bZ{pair}", pair, wz, zs)


NFILL = [4]


NFILL2 = "NFILL2"


def _mk_filler(dst_fn, ident, start, m=128, nf=None, wait_fn=None):
    import os
    nfill = int(os.environ.get("NFILL", NFILL[0]))
    if nf == NFILL2:
        nfill = int(os.environ.get("NFILL2", 3))
    if nfill <= 0:
        return None

    def filler(B):
        dst = dst_fn()
        n = dst.shape[-1]
        for k in range(nfill):
            w = [wait_fn()] if (wait_fn is not None and k == 0) else ()
            B.op("tensor", lambda e: e.matmul(dst, ident[:, 0:m], ident[:, 0:n], start=start, stop=start), waits=w)
    return filler


def _proj_q(B, T, Wq, t_w):
    banks4 = [T.psS[0], T.psS[1], T.psO[0], T.psO[1]]
    free = [None] * 4
    t_e = None
    for qt in range(8):
        bk = banks4[qt % 4]
        for c in range(8):
            t_mm = B.op("tensor", lambda e, bk=bk, c=c, qt=qt: e.matmul(
                bk[0:64, :], Wq[:, c, :], T.hT[:, c, qt * 512:(qt + 1) * 512], start=(c == 0), stop=(c == 7)),
                waits=[t_w, free[qt % 4]] if c == 0 else (), sig=(c == 7))
        t_e = B.op("scalar", lambda e, bk=bk, qt=qt: e.activation(
            out=T.QA[0:64, qt * 512:(qt + 1) * 512], in_=bk[0:64, :], func=AF.Copy, scale=0.125), waits=[t_mm], sig=True)
        free[qt % 4] = t_e
    return t_e


def _build_final(nc, hT, OT, psS, psO, dram, x, out, epsc, psO2=None, psM=None, psT=None):
    with ExitStack() as esF:
        def sbf(name, shape, dt):
            return esF.enter_context(nc.sbuf_tensor("f_" + name, list(shape), dt))
        wO = sbf("wO", [128, 8, D], BF16)
        gpost = sbf("gpost", [128, D], F32)
        xt = [sbf(f"xt{i}", [128, 2, D], F32) for i in range(3)]
        yt = [sbf(f"yt{i}", [128, 2, D], F32) for i in range(2)]
        junk = sbf("junk", [128, 512], BF16)
        ssq = sbf("ssq", [128, NT, 2], F32)
        rs = sbf("rs", [128, NT], F32)
        B = Blk(nc, "bF")
        sw, sg = B.newsem(), B.newsem("sync")
        t_w = [B.dma("gpsimd", wO[:, c, :], dram["wO"][:, c, :], sw) for c in range(8)]
        t_g = B.dma("sync", gpost[:], dram["gpost"][:], sg)
        xs = [B.newsem("sync") for _ in range(3)]
        os_ = [B.newsem("sync") for _ in range(2)]
        pY = [(psS[0], psS[1]), (psO[0], psO[1]), (psO2[0], psO2[1]), (psM, psT)]
        ps_free = [None] * 4
        xt_free = [None, None, None]
        yt_free = [None, None]
        t_xs = [None] * (NT // 2)
        for tp in range(NT // 2):
            b3 = tp % 3
            by = tp % 2
            xv = x[tp * 256:(tp + 1) * 256, :].rearrange("(n p) d -> p n d", p=128)
            ov = out[tp * 256:(tp + 1) * 256, :].rearrange("(n p) d -> p n d", p=128)
            if tp == 0:
                for t2 in range(2):
                    xv2 = x[t2 * 256:(t2 + 1) * 256, :].rearrange("(n p) d -> p n d", p=128)
                    t_xs[t2] = B.dma("sync", xt[t2 % 3][:], xv2, xs[t2 % 3])
            t_x = t_xs[tp]
            for j in range(2):
                tt = tp * 2 + j
                b = tt % 4
                for half in range(2):
                    pk = pY[b][half]
                    for c in range(8):
                        t_mm = B.op("tensor", lambda e, pk=pk, c=c, tt=tt, half=half: e.matmul(
                            pk[:], OT[:, c, tt * 128:(tt + 1) * 128], wO[:, c, half * 512:(half + 1) * 512], start=(c == 0), stop=(c == 7)),
                            waits=(t_w + [ps_free[b]]) if (c == 0 and half == 0) else (), sig=(c == 7 and half == 1))
                for half in range(2):
                    t_sq = B.op("scalar", lambda e, half=half, b=b, tt=tt: e.activation(
                        out=junk[:], in_=pY[b][half][:], func=AF.Square, accum_out=ssq[:, tt, half:half + 1]), waits=[t_mm], sig=True)
                t_a = B.op("vector", lambda e, tt=tt: e.tensor_tensor(out=rs[:, tt:tt + 1], in0=ssq[:, tt, 0:1], in1=ssq[:, tt, 1:2], op=ALU.add),
                           waits=[t_sq], sig=True)
                t_r1 = B.op("scalar", lambda e, tt=tt: e.activation(out=rs[:, tt:tt + 1], in_=rs[:, tt:tt + 1], func=AF.Sqrt,
                                                                  bias=epsc[:, 0:1], scale=1.0 / D), waits=[t_a], sig=True)
                t_r2 = B.op("vector", lambda e, tt=tt: e.reciprocal(out=rs[:, tt:tt + 1], in_=rs[:, tt:tt + 1]), waits=[t_r1], sig=True)
                for half in range(2):
                    t_y = B.op("vector", lambda e, half=half, b=b, tt=tt, by=by, j=j: e.scalar_tensor_tensor(
                        out=yt[by][:, j, half * 512:(half + 1) * 512], in0=pY[b][half][:], scalar=rs[:, tt:tt + 1],
                        in1=gpost[:, half * 512:(half + 1) * 512], op0=ALU.mult, op1=ALU.mult),
                        waits=[t_r2, t_g, yt_free[by]] if half == 0 else (), sig=True)
                ps_free[b] = t_y
            t_o = B.op("vector", lambda e, by=by, b3=b3: e.tensor_tensor(out=yt[by][:], in0=yt[by][:], in1=xt[b3][:], op=ALU.add),
                       waits=[t_y, t_x], sig=True)
            xt_free[b3] = t_o
            yt_free[by] = B.dma("sync", ov, yt[by][:], os_[by], waits=[t_o])
            if tp + 2 < NT // 2:
                t3 = tp + 2
                xv2 = x[t3 * 256:(t3 + 1) * 256, :].rearrange("(n p) d -> p n d", p=128)
                t_xs[t3] = B.dma("sync", xt[t3 % 3][:], xv2, xs[t3 % 3], waits=[xt_free[t3 % 3]])
        B.op("sync", lambda e: e.nop(), waits=[yt_free[0], yt_free[1]])
        B.run()


def build(debug=None, stop=None):
    nc = bass.Bass("TRN2", target_bir_lowering=False)
    dram = {}

    def din(name, shape):
        dram[name] = nc.dram_tensor(name, list(shape), F32, kind="ExternalInput").ap()
        return dram[name]

    x = din("x", [S, D])
    cshapes = {k: v.shape for k, v in host_consts().items()}
    for k, shp in cshapes.items():
        din(k, shp)
    wshapes = dict(wA=[8, 128, 3, 8, 64], wZ=[8, 128, 8, 128], wQB=[8, 128, 8, 64], wG=[128, 8, 24],
                   wKV=[2, 128, 8, 384], w1kv=[128, 32, 128], w2kv=[128, 2, 64], peT=[128, 32],
                   wO=[128, 8, 1024], gpre=[128, 8], gpost=[128, D])
    for k, shp in wshapes.items():
        din(k, shp)
    out = nc.dram_tensor("out", [S, D], F32, kind="ExternalOutput").ap()
    dbg = {}
    if debug:
        for name, shp in debug.items():
            dbg[name] = nc.dram_tensor("dbg_" + name, list(shp), F32, kind="ExternalOutput").ap()

    es = ExitStack()

    def sb(name, shape, dt):
        return es.enter_context(nc.sbuf_tensor("s_" + name, list(shape), dt))

    def ps(name, shape, dt=F32):
        return es.enter_context(nc.psum_tensor(name, list(shape), dt))

    with es:
        hT = sb("hT", [128, 8, S], BF16)
        OT = sb("OT", [128, 8, S], BF16)
        ident = sb("ident", [128, 128], BF16)
        addm_moba = sb("addm_moba", [128, NT, 16], F32)
        addm_slc = sb("addm_slc", [128, NT, 64], BF16)
        cmask = sb("cmask", [128, 4, 512], BF16)
        tri = sb("tri", [128, 2, 128], BF16)
        btab = sb("btab", [128, 8, 32], F32)
        gpre = sb("gpre", [128, 8], F32)
        epsc = sb("epsc", [128, 1], F32)
        gates = sb("gates", [128, NT, 24], F32)
        esW = ExitStack()

        def sbw(name, shape, dt):
            return esW.enter_context(nc.sbuf_tensor("w_" + name, list(shape), dt))
        QA = sbw("QA", [128, S], BF16)
        KA = sbw("KA", [128, S], BF16)
        KW = sbw("KW", [128, S], BF16)
        VA = sbw("VA", [128, NT, 65], BF16)
        VW = sbw("VW", [128, NT, 65], BF16)
        PT = [sbw(f"PT{i}", [128, 512], BF16) for i in range(4)]
        Ost = [sbw(f"Ost{i}", [128, 4, 128], BF16) for i in range(2)]
        rden = sbw("rden", [128, 16], F32)
        psS = [ps(f"psS{i}", [128, 512]) for i in range(2)]
        psO = [ps(f"psO{i}", [128, 512]) for i in range(2)]
        psO2 = [ps(f"psO2{i}", [128, 512]) for i in range(2)]
        psM = ps("psM", [128, 512])
        psT = ps("psT", [128, 512])

        POOL[0] = SemPool(nc, es)
        with nc.Block() as blk_clr:
            @blk_clr.gpsimd
            def _(g):
                for sm in POOL[0].all():
                    g.sem_clear(sm.h)

        B = Blk(nc, "b0")
        toks = []
        toks.append(B.dma("gpsimd", ident[:], dram["c_ident"][:], B.newsem()))
        toks.append(B.dma("sync", addm_moba[:], dram["c_addm_moba"][:], B.newsem("sync")))
        toks.append(B.dma("gpsimd", addm_slc[:], dram["c_addm_slc"][:], B.newsem()))
        toks.append(B.dma("gpsimd", cmask[:], dram["c_cmask"][:], B.newsem()))
        toks.append(B.dma("gpsimd", tri[:], dram["c_tri"][:], B.newsem()))
        toks.append(B.dma("sync", btab[:], dram["c_btab"][:], B.newsem("sync")))
        toks.append(B.dma("sync", gpre[:], dram["gpre"][:], B.newsem("sync")))
        B.op("vector", lambda e: e.memset(VA[:, :, 64:65], 1.0))
        B.op("vector", lambda e: e.memset(epsc[:], 1e-6))
        B.op("vector", lambda e: e.memset(VW[:, :, 64:65], 1.0))
        B.op("vector", lambda e: e.memset(QA[64:128, :], 0.0))
        t_ms = B.op("vector", lambda e: e.memset(KA[64:128, :], 0.0), sig=True)
        B.op("gpsimd", lambda e: e.memset(Ost[0][:], 0.0))
        B.op("gpsimd", lambda e: e.memset(Ost[1][:], 0.0))
        B.op("sync", lambda e: e.nop(), waits=toks)
        B.run()

        with ExitStack() as esA:
            xt = [esA.enter_context(nc.sbuf_tensor(f"xt{i}", [128, D], F32)) for i in range(4)]
            junk = esA.enter_context(nc.sbuf_tensor("junkA", [128, D], BF16))
            hb = [esA.enter_context(nc.sbuf_tensor(f"hb{i}", [128, D], BF16)) for i in range(2)]
            ss = esA.enter_context(nc.sbuf_tensor("ssA", [128, NT], F32))
            rs = esA.enter_context(nc.sbuf_tensor("rsA", [128, NT], F32))
            B = Blk(nc, "bA")
            xs = [B.newsem("sync") for _ in range(4)]
            xtok = [None] * NT
            hb_free = [None, None]
            xt_free = [None] * 4
            ps_free = [None, None]
            pA = [(psS[0], psS[1]), (psO[0], psO[1])]
            tr2 = [None] * NT
            tsq = [None] * NT

            def stage0(tt):
                b3 = tt % 4
                xtok[tt] = B.dma("sync", xt[b3][:], x[tt * 128:(tt + 1) * 128, :], xs[b3], waits=[xt_free[b3]])

            def stage1(tt):
                b3 = tt % 4
                t_sq = B.op("scalar", lambda e, b3=b3, tt=tt: e.activation(out=junk[:], in_=xt[b3][:], func=AF.Square,
                                                                     accum_out=ss[:, tt:tt + 1]),
                            waits=[xtok[tt]], sig=True)
                t_r1 = B.op("scalar", lambda e, tt=tt: e.activation(out=rs[:, tt:tt + 1], in_=ss[:, tt:tt + 1], func=AF.Sqrt,
                                                                  bias=epsc[:, 0:1], scale=1.0 / D),
                            waits=[t_sq], sig=True)
                tr2[tt] = B.op("vector", lambda e, tt=tt: e.reciprocal(out=rs[:, tt:tt + 1], in_=rs[:, tt:tt + 1]),
                               waits=[t_r1], sig=True)
                tsq[tt] = t_sq

            def stage2(tt):
                b3 = tt % 4
                b2 = tt % 2
                t_h = B.op("scalar", lambda e, tt=tt, b3=b3, b2=b2: e.activation(
                    out=hb[b2][:], in_=xt[b3][:], func=AF.Copy, scale=rs[:, tt:tt + 1]),
                    waits=[tr2[tt], hb_free[b2], tsq[tt]], sig=True)
                xt_free[b3] = t_h
                pa, pb = pA[b2]
                for c in range(8):
                    dst = (pa if c < 4 else pb)[:, (c % 4) * 128:(c % 4 + 1) * 128]
                    t_tr = B.op("tensor", lambda e, dst=dst, b2=b2, c=c: e.matmul(
                        dst, hb[b2][:, c * 128:(c + 1) * 128], ident[:], start=True, stop=True),
                        waits=[t_h, ps_free[b2]], sig=(c == 7))
                hb_free[b2] = t_tr
                B.op("vector", lambda e, tt=tt, pa=pa: e.tensor_tensor(
                    out=hT[:, 0:4, tt * 128:(tt + 1) * 128], in0=pa[:].rearrange("p (c t) -> p c t", c=4),
                    in1=gpre[:, 0:4].unsqueeze(2).to_broadcast([128, 4, 128]), op=ALU.mult), waits=[t_tr])
                ps_free[b2] = B.op("vector", lambda e, tt=tt, pb=pb: e.tensor_tensor(
                    out=hT[:, 4:8, tt * 128:(tt + 1) * 128], in0=pb[:].rearrange("p (c t) -> p c t", c=4),
                    in1=gpre[:, 4:8].unsqueeze(2).to_broadcast([128, 4, 128]), op=ALU.mult), waits=[t_tr], sig=True)

            for t0 in range(3):
                stage0(t0)
            stage1(0)
            stage1(1)
            for tt in range(NT):
                if tt + 3 < NT:
                    stage0(tt + 3)
                if tt + 2 < NT:
                    stage1(tt + 2)
                stage2(tt)
            B.run()


        if stop == "A":
            pass
        else:
            _build_rest(nc, locals(), debug, dbg, stop)
        esW.close()
        if stop is None or (isinstance(stop, dict) and stop.get("final")):
            _build_final(nc, hT, OT, psS, psO, dram, x, out, epsc, psO2, psM, psT)

        if debug and "hT" in debug:
            with ExitStack() as esD:
                tmp = esD.enter_context(nc.sbuf_tensor("dbgtmp", [128, 8, 512], F32))
                B = Blk(nc, "bD")
                s1 = B.newsem("sync")
                tk = None
                for q in range(8):
                    t1 = B.op("vector", lambda e, q=q: e.tensor_copy(out=tmp[:], in_=hT[:, :, q * 512:(q + 1) * 512]),
                              waits=[tk], sig=True)
                    tk = B.dma("sync", dbg["hT"][:, :, q * 512:(q + 1) * 512], tmp[:], s1, waits=[t1])
                B.op("sync", lambda e: e.nop(), waits=[tk])
                B.run()
    return nc


_CACHE = {}


def kernel(x, pre_norm_g, post_norm_g, w_in, cmp_pos_k, cmp_pos_v, w_cmp_k1, w_cmp_k2, w_cmp_v1, w_cmp_v2, w_out):
    x = np.asarray(x, np.float32)
    consts = host_consts()
    wts = host_weights(*(np.asarray(a, np.float32) for a in (pre_norm_g, post_norm_g, w_in, cmp_pos_k, cmp_pos_v,
                                                            w_cmp_k1, w_cmp_k2, w_cmp_v1, w_cmp_v2, w_out)))
    nc = build()
    in_maps = []
    for b in range(8):
        m = {"x": np.ascontiguousarray(x[b])}
        m.update(consts)
        m.update(wts)
        in_maps.append(m)
    res = run_bass_kernel_spmd(nc, in_maps, core_ids=list(range(8)))
    return np.stack([r["out"] for r in res.results], 0).astype(np.float32)
```

```python
import numpy as np
from contextlib import ExitStack
import concourse.bass as bass
import concourse.mybir as mybir
from concourse.bass_utils import run_bass_kernel_spmd

F32 = mybir.dt.float32
BF16 = mybir.dt.bfloat16
ALU = mybir.AluOpType
AF = mybir.ActivationFunctionType
AX = mybir.AxisListType

S = 4096
D = 1024
NT = 32
DIN = 3864
NEGM = -30000.0
ENG = ("sync", "scalar", "vector", "gpsimd", "tensor")


class Sem:
    def __init__(self, h):
        self.h = h
        self.n = 0


class SemPool:
    def __init__(self, nc, es, ndma=24):
        self.eng = {e: Sem(es.enter_context(nc.semaphore(f"pool_{e}"))) for e in ENG}
        self.dma = {"gpsimd": [Sem(es.enter_context(nc.semaphore(f"pool_g{i}"))) for i in range(ndma)],
                    "sync": [Sem(es.enter_context(nc.semaphore(f"pool_s{i}"))) for i in range(ndma)],
                    "scalar": [Sem(es.enter_context(nc.semaphore(f"pool_a{i}"))) for i in range(4)]}

    def all(self):
        return list(self.eng.values()) + self.dma["gpsimd"] + self.dma["sync"] + self.dma["scalar"]


POOL = [None]


class Blk:
    def __init__(self, nc, name):
        self.nc = nc
        self.name = name
        self.ops = {e: [] for e in ENG}
        self.esem = POOL[0].eng
        self.k = {"gpsimd": 0, "sync": 0, "scalar": 0}

    def newsem(self, kind="gpsimd"):
        lst = POOL[0].dma[kind]
        s_ = lst[self.k[kind] % len(lst)]
        self.k[kind] += 1
        s_.kind = kind
        return s_

    def op(self, eng, fn, waits=(), sig=False):
        tok = None
        s = None
        if sig:
            s = self.esem[eng]
            s.n += 1
            tok = (s.h, s.n)
        self.ops[eng].append((fn, tuple(w for w in waits if w is not None), s, 1))
        return tok

    def dma(self, eng, out, in_, sem, waits=()):
        assert sem.kind == eng, (sem.kind, eng)
        sem.n += 16
        tok = (sem.h, sem.n)
        self.ops[eng].append((lambda e: e.dma_start(out=out, in_=in_), tuple(w for w in waits if w is not None), sem, 16))
        return tok

    def last(self, eng):
        s = self.esem[eng]
        return (s.h, s.n) if s.n > 0 else None

    def run(self):
        with self.nc.Block() as block:
            for e in ENG:
                ops = self.ops[e]

                def body(eng, ops=ops):
                    seen = {}
                    for fn, waits, s, amt in ops:
                        for (h, v) in waits:
                            key = id(h)
                            if seen.get(key, 0) >= v:
                                continue
                            seen[key] = v
                            eng.wait_ge(h, v)
                        ins = fn(eng)
                        if s is not None:
                            ins.then_inc(s.h, amt)

                getattr(block, e)(body)


def _slopes():
    return [2.0 ** (-(i + 1)) for i in range(8)]


def host_consts():
    c = {}
    c["c_ident"] = np.eye(128, dtype=np.float32)
    k = np.arange(S)
    c["c_E16"] = (k[None, :] // 256 == np.arange(16)[:, None]).astype(np.float32)
    c["c_E64"] = (NEGM * (k[None, :] // 64 == np.arange(64)[:, None])).astype(np.float32)
    am = np.zeros((128, NT, 16), np.float32)
    for tt in range(NT):
        own = tt // 2
        am[:, tt, own] = 1e9
        am[:, tt, own + 1:] = -1e9
    c["c_addm_moba"] = am
    a2 = np.zeros((128, NT, 64), np.float32)
    t = (np.arange(NT)[None, :] * 128 + np.arange(128)[:, None])
    own = t // 64
    j = np.arange(64)[None, None, :]
    a2 = np.where(j > own[:, :, None], -1e9, 0.0).astype(np.float32)
    a2 = np.where(j == 0, 1e9, a2)
    a2 = np.where(j == own[:, :, None] - 1, 2e9, a2)
    a2 = np.where(j == own[:, :, None], 3e9, a2)
    c["c_addm_slc"] = a2.astype(np.float32)
    cr = np.arange(128)[:, None, None]
    r = np.arange(4)[None, :, None]
    tr = np.arange(512)[None, None, :]
    c["c_cmask"] = (16 * cr + 31 - 512 * r <= tr).astype(np.float32)
    kk = np.arange(128)[:, None]
    tq = np.arange(128)[None, :]
    c["c_tri"] = np.stack([(kk <= tq), (kk > tq)], axis=1).astype(np.float32)
    sl = np.array(_slopes(), np.float32)
    dl = np.arange(32)[None, None, :]
    c["c_btab"] = (sl[None, :, None] * (np.arange(128)[:, None, None] - 128.0 * dl - 64.0)).astype(np.float32)
    tmod = (np.arange(S) % 128).astype(np.float32)
    qrow = np.zeros((8, 2, S), np.float32)
    krow = np.zeros((8, 2, S), np.float32)
    for h in range(8):
        qrow[h, 0] = -sl[h] * tmod
        qrow[h, 1] = 1.0
        krow[h, 0] = 1.0
        krow[h, 1] = sl[h] * tmod
    c["c_qrow"] = qrow
    c["c_krow"] = krow
    rc = np.zeros((128, 2, 65), np.float32)
    cidx = np.arange(2)[None, :] * 128 + np.arange(128)[:, None]
    valid = cidx < 255
    rc[:, :, 0] = valid
    cst = cidx * 16
    js = np.arange(64)[None, None, :] * 64
    ov = (cst[:, :, None] < js + 64) & (cst[:, :, None] + 32 > js) & valid[:, :, None]
    rc[:, :, 1:] = ov
    c["c_rc"] = rc
    return c


def host_weights(pre_norm_g, post_norm_g, w_in, cmp_pos_k, cmp_pos_v, w_cmp_k1, w_cmp_k2, w_cmp_v1, w_cmp_v2, w_out):
    w = {}
    W = np.ascontiguousarray(w_in[0].reshape(8, 128, DIN).transpose(1, 0, 2))

    def cols(a, n=64):
        return W[:, :, a:a + n]

    w["wA"] = np.ascontiguousarray(np.stack(
        [np.stack([cols(0 + h * 64), cols(512 + h * 64), cols(1024 + h * 64)], axis=1) for h in range(8)], 0))
    zc = [1536 + p * 128 for p in range(4)] + [3352 + p * 128 for p in range(4)]
    w["wZ"] = np.ascontiguousarray(np.stack([cols(a, 128) for a in zc], 0))
    w["wQB"] = np.ascontiguousarray(np.stack([cols(2048 + h * 64) for h in range(8)], 0))
    w["wG"] = np.ascontiguousarray(cols(3328, 24))
    kv = []
    for g in range(2):
        kv.append(np.concatenate([cols(2560 + g * 64), cols(2688 + g * 64), cols(2816 + g * 64),
                                  cols(3072 + g * 64), cols(2944 + g * 64), cols(3200 + g * 64)], axis=2))
    w["wKV"] = np.ascontiguousarray(np.stack(kv, 0))
    k1 = w_cmp_k1[0].reshape(32, 64, 128).transpose(1, 0, 2)
    v1 = w_cmp_v1[0].reshape(32, 64, 128).transpose(1, 0, 2)
    w["w1kv"] = np.ascontiguousarray(np.concatenate([k1, v1], 0))
    w["w2kv"] = np.ascontiguousarray(np.stack([w_cmp_k2[0], w_cmp_k2[0], w_cmp_v2[0]], 1))
    w["peT"] = np.ascontiguousarray(np.concatenate([cmp_pos_k[0].T, cmp_pos_v[0].T], 0))
    w["wO"] = np.ascontiguousarray(w_out[0].reshape(8, 128, D).transpose(1, 0, 2))
    w["gpre"] = np.ascontiguousarray(pre_norm_g[0].reshape(8, 128).T)
    w["gpost"] = np.ascontiguousarray(np.broadcast_to(post_norm_g[0][None, :], (128, D)))
    return {k: np.asarray(v, np.float32) for k, v in w.items()}


def emit_attention(B, T, groups, single_acc_key=None, filler=None, nS=2, nPT=3, prev=None):
    psS, PT = T.psS, T.PT
    flat = []
    for gi, g in enumerate(groups):
        for ti, t in enumerate(g["tiles"]):
            flat.append((gi, ti, t))
    n = len(flat)
    act_done = [None] * n
    rdy = [None] * n
    s_done = [None] * n
    pv_done = [None] * n
    epi_done = [None] * len(groups)
    last_pv_of_group = [None] * len(groups)

    def emit_S(i):
        gi, ti, t = flat[i]
        w = [act_done[i - nS] if i >= nS else (prev[0] if prev else None)]
        ew = t.get("extra_wait")
        if ew is not None:
            w += list(ew) if isinstance(ew, (list, tuple)) and not (len(ew) == 2 and not isinstance(ew[0], tuple)) else [ew]
        m = len(t["smm"])
        for j, (o, l, r) in enumerate(t["smm"]):
            tk = B.op("tensor", lambda e, o=o, l=l, r=r: e.matmul(o, l, r, start=True, stop=True),
                      waits=w if j == 0 else (), sig=(j == m - 1))
        s_done[i] = tk

    def emit_act(i):
        gi, ti, t = flat[i]
        in_, o, bias = t["act"]
        w = [s_done[i], pv_done[i - nPT] if i >= nPT else (prev[1] if prev else None)]
        if bias is None:
            act_done[i] = B.op("scalar", lambda e, in_=in_, o=o: e.activation(out=o, in_=in_, func=AF.Exp), waits=w, sig=True)
        else:
            act_done[i] = B.op("scalar", lambda e, in_=in_, o=o, bias=bias: e.activation(out=o, in_=in_, func=AF.Exp, bias=bias),
                               waits=w, sig=True)
        rdy[i] = act_done[i]
        if t.get("mask") is not None:
            ap, mk = t["mask"]
            rdy[i] = B.op("vector", lambda e, ap=ap, mk=mk: e.tensor_tensor(out=ap, in0=ap, in1=mk, op=ALU.mult),
                          waits=[act_done[i]], sig=True)

    def emit_PV(i):
        gi, ti, t = flat[i]
        if filler is not None:
            filler(B)
        w = [rdy[i]]
        if ti == 0 and gi >= 2:
            w.append(epi_done[gi - 2])
        if ti == 0 and gi < 2 and prev:
            w.append(prev[2])
        if single_acc_key and t.get(single_acc_key) and gi >= 1:
            w.append(epi_done[gi - 1])
        m = len(t["pv"])
        for j, (o, l, r, st) in enumerate(t["pv"]):
            tk = B.op("tensor", lambda e, o=o, l=l, r=r, st=st: e.matmul(o, l, r, start=st, stop=False),
                      waits=w if j == 0 else (), sig=(j == m - 1))
        pv_done[i] = tk
        if ti == len(groups[gi]["tiles"]) - 1:
            last_pv_of_group[gi] = tk
            if gi >= 1 and groups[gi - 1].get("post"):
                groups[gi - 1]["post"](B)
            epi_done[gi] = groups[gi]["epi"](B, tk)

    dD = nS - 1
    for i in range(n + dD):
        if i < n:
            emit_S(i)
            emit_act(i)
        if i >= dD:
            emit_PV(i - dD)
    if groups and groups[-1].get("post"):
        groups[-1]["post"](B)
    return (act_done[-1], pv_done[-1], epi_done[-1])


def _build_rest(nc, L, debug, dbg, stop):
    import types
    T = types.SimpleNamespace(**{k: v for k, v in L.items() if k not in ("es", "B")})
    hT, OT, QA, KA, KW, VA, VW, PT, Ost = T.hT, T.OT, T.QA, T.KA, T.KW, T.VA, T.VW, T.PT, T.Ost
    psS, psO, psO2, psM, psT = T.psS, T.psO, T.psO2, T.psM, T.psT
    ident, btab, tri, cmask, rden = T.ident, T.btab, T.tri, T.cmask, T.rden
    dram = T.dram
    banks4 = [psS[0], psS[1], psO[0], psO[1]]
    SB4 = [psS[0], psS[1], psO2[0], psO2[1]]

    def v3(ap2d, a):
        return ap2d.rearrange("p (a b) -> p a b", a=a)

    def gating_block(name, p, wz, zs):
        B = Blk(nc, name)
        s1 = B.newsem()
        t_w = B.dma("gpsimd", wz[:], dram["wZ"][p], s1)
        z_free = [None, None]
        ps_free = [None, None]
        for qt in range(8):
            b = qt % 2
            for c in range(8):
                t_mm = B.op("tensor", lambda e, b=b, c=c, qt=qt: e.matmul(
                    psS[b][:], wz[:, c, :], hT[:, c, qt * 512:(qt + 1) * 512], start=(c == 0), stop=(c == 7)),
                    waits=[t_w, ps_free[b]] if c == 0 else (), sig=(c == 7))
            t_s = B.op("scalar", lambda e, b=b: e.activation(out=zs[b][:], in_=psS[b][:], func=AF.Silu),
                       waits=[t_mm, z_free[b]], sig=True)
            ps_free[b] = t_s
            z_free[b] = B.op("vector", lambda e, b=b, qt=qt: e.tensor_tensor(
                out=OT[:, p, qt * 512:(qt + 1) * 512], in0=OT[:, p, qt * 512:(qt + 1) * 512], in1=zs[b][:], op=ALU.mult),
                waits=[t_s], sig=True)
        B.run()

    with ExitStack() as esM:
        def sbm(name, shape, dt):
            return esM.enter_context(nc.sbuf_tensor("m_" + name, list(shape), dt))
        QA2 = sbm("QA2", [128, S], BF16)
        Wq2 = [sbm(f"Wqk{i}", [128, 2, 8, 128], BF16) for i in range(2)]
        Wv2 = [sbm(f"Wv{i}", [128, 8, 64], BF16) for i in range(2)]
        maskpad = sbm("maskpad", [128, NT, 16], BF16)
        scg = sbm("scg", [128, NT, 16], F32)
        m8 = sbm("m8", [128, NT, 8], F32)
        tmpf = sbm("tmpf", [128, NT, 16], F32)
        kmf = sbm("kmf", [64, 16], F32)
        kmb = sbm("kmb", [64, 16], BF16)
        wz = sbm("wz", [128, 8, 128], BF16)
        zs = [scg[:].rearrange("p a b -> p (a b)").bitcast(BF16)[:, 0:512], tmpf[:].rearrange("p a b -> p (a b)").bitcast(BF16)[:, 0:512]]
        QS = [QA, QA2]
        KS_ = [KA, KW]
        VS_ = [VA, VW]

        B = Blk(nc, "bM0")
        t1 = B.op("vector", lambda e: e.memset(maskpad[:], 0.0), sig=True)
        B.op("vector", lambda e: e.memset(QA2[64:128, :], 0.0))
        t1b = B.op("vector", lambda e: e.memset(KW[64:128, :], 0.0), sig=True)
        t2 = B.dma("gpsimd", KA[64:80, :], dram["c_E16"][:], B.newsem())
        t3 = B.dma("gpsimd", KW[64:80, :], dram["c_E16"][:], B.newsem(), waits=[t1b])
        B.op("sync", lambda e: e.nop(), waits=[t1, t2, t3])
        B.run()

        nheads = 8 if stop is None else int(stop.get("moba_heads", 8)) if isinstance(stop, dict) else 8

        def proj_ops(B, h, t_w, banks, bank_free):
            st = h % 2
            Wt, Wv, Qd, Kd, Vd = Wq2[st], Wv2[st], QS[st], KS_[st], VS_[st]
            ops = []
            state = {"bi": 0, "first": True}

            def mm_qk(which, qt, c):
                def f():
                    bi = state["bi"]
                    bk = banks[bi % len(banks)]
                    w = ()
                    if c == 0:
                        w = [bank_free[bi % len(banks)]] + ([t_w] if state["first"] else [])
                        state["first"] = False
                    tk = B.op("tensor", lambda e: e.matmul(bk[:, :], Wt[:, which, c, :], hT[:, c, qt * 512:(qt + 1) * 512],
                                                          start=(c == 0), stop=(c == 7)), waits=w, sig=(c == 7))
                    if c == 7:
                        if which == 0:
                            t_e = B.op("vector", lambda e: e.tensor_scalar(out=Qd[0:64, qt * 512:(qt + 1) * 512], in0=bk[0:64, :],
                                                                         scalar1=0.125, scalar2=None, op0=ALU.mult), waits=[tk], sig=True)
                        else:
                            t_e = B.op("vector", lambda e: e.tensor_copy(out=Kd[0:64, qt * 512:(qt + 1) * 512], in_=bk[0:64, :]),
                                       waits=[tk], sig=True)
                        bank_free[bi % len(banks)] = t_e
                        state["last_ev"] = t_e
                        state["bi"] += 1
                return f

            def mm_v(tg, j, c):
                def f():
                    bi = state["bi"]
                    bk = banks[bi % len(banks)]
                    tt = tg * 4 + j
                    w = [bank_free[bi % len(banks)]] if (c == 0 and j == 0) else ()
                    tk = B.op("tensor", lambda e: e.matmul(bk[:, j * 64:(j + 1) * 64], hT[:, c, tt * 128:(tt + 1) * 128], Wv[:, c, :],
                                                          start=(c == 0), stop=(c == 7)), waits=w, sig=(c == 7 and j == 3))
                    if c == 7 and j == 3:
                        t_e = B.op("vector", lambda e: e.tensor_copy(out=Vd[:, tg * 4:(tg + 1) * 4, 0:64], in_=v3(bk[:, 0:256], 4)),
                                   waits=[tk], sig=True)
                        bank_free[bi % len(banks)] = t_e
                        state["last_ev"] = t_e
                        state["bi"] += 1
                return f

            qk_groups = [[(mm_qk(which, qt, c), 1.0) for c in range(8)] for qt in range(8) for which in (0, 1)]
            v_groups = [[(mm_v(tg, j, c), 0.15) for j in range(4) for c in range(8)] for tg in range(8)]
            gi = 0
            for k in range(8):
                ops += qk_groups[2 * k]
                ops += v_groups[k]
                ops += qk_groups[2 * k + 1]
            return ops, state

        def load_head_consts(B, h):
            st = h % 2
            sw_ = B.newsem()
            for which in range(2):
                for half in range(2):
                    B.dma("gpsimd", Wq2[st][:, which, :, half * 64:(half + 1) * 64], dram["wA"][h][:, which], sw_)
            t_w = B.dma("gpsimd", Wv2[st][:], dram["wA"][h][:, 2], sw_)
            t_qr = B.dma("gpsimd", QS[st][80:82, :], dram["c_qrow"][h], B.newsem())
            t_kr = B.dma("gpsimd", KS_[st][80:82, :], dram["c_krow"][h], B.newsem())
            return t_w, t_qr, t_kr

        def selection_block(h, extra_waits=()):
            st = h % 2
            Qd, Kd = QS[st], KS_[st]
            B = Blk(nc, f"bS{h}")
            t_km = B.op("vector", lambda e: e.tensor_reduce(out=kmf[:], in_=Kd[0:64, :].rearrange("p (j k) -> p j k", k=256),
                                                          axis=AX.X, op=ALU.add), waits=list(extra_waits), sig=True)
            t_kb = B.op("vector", lambda e: e.tensor_scalar(out=kmb[:], in0=kmf[:], scalar1=1.0 / 256, scalar2=None, op0=ALU.mult),
                        waits=[t_km], sig=True)
            for tt in range(NT):
                t_g = B.op("tensor", lambda e, tt=tt: e.matmul(psM[:, tt * 16:(tt + 1) * 16], Qd[0:64, tt * 128:(tt + 1) * 128], kmb[:],
                                                              start=True, stop=True),
                           waits=[t_kb] if tt == 0 else (), sig=(tt == NT - 1))
            t_sc = B.op("vector", lambda e: e.tensor_tensor(out=scg[:], in0=v3(psM[:], NT), in1=T.addm_moba[:], op=ALU.add),
                        waits=[t_g], sig=True)
            for tt in range(NT):
                t_m8 = B.op("vector", lambda e, tt=tt: e.max(out=m8[:, tt, :], in_=scg[:, tt, :]),
                            waits=[t_sc] if tt == 0 else (), sig=(tt == NT - 1))
            t_c = B.op("vector", lambda e: e.tensor_tensor(out=tmpf[:], in0=scg[:], in1=m8[:, :, 3:4].to_broadcast([128, NT, 16]),
                                                         op=ALU.is_lt), waits=[t_m8], sig=True)
            t_mp = B.op("vector", lambda e: e.tensor_scalar(out=maskpad[:], in0=tmpf[:], scalar1=NEGM, scalar2=None,
                                                          op0=ALU.mult), waits=[t_c], sig=True)
            tb_free = [None, None]
            for g8 in range(8):
                bk = psS[g8 % 2]
                for j in range(4):
                    tt = g8 * 4 + j
                    t_tr = B.op("tensor", lambda e, bk=bk, j=j, tt=tt: e.matmul(
                        bk[64:80, j * 128:(j + 1) * 128], maskpad[:, tt, :], ident[:], start=True, stop=True),
                        waits=[t_mp, tb_free[g8 % 2]] if j == 0 else (), sig=(j == 3))
                tb_free[g8 % 2] = B.op("vector", lambda e, bk=bk, g8=g8: e.tensor_copy(
                    out=Qd[64:80, g8 * 512:(g8 + 1) * 512], in_=bk[64:80, :]), waits=[t_tr], sig=True)
            B.run()

        if nheads > 0:
            B = Blk(nc, "bP0")
            t_w, t_qr, t_kr = load_head_consts(B, 0)
            bfree = [None] * 4
            ops, stt = proj_ops(B, 0, t_w, banks4, bfree)
            for f, _c in ops:
                f()
            B.op("sync", lambda e: e.nop(), waits=[t_qr, t_kr, stt["last_ev"]])
            B.run()
            selection_block(0)

        for h in range(nheads):
            pair, off = h // 2, (h % 2) * 64
            st = h % 2
            Qd, Kd, Vd = QS[st], KS_[st], VS_[st]
            B = Blk(nc, f"bT{h}")
            nxt = h + 1 < nheads
            if nxt:
                t_w, t_qr, t_kr = load_head_consts(B, h + 1)
                pbfree = [None, None]
                pops, pstate = proj_ops(B, h + 1, t_w, [psO2[1], psM], pbfree)
            SBk = [psS[0], psS[1], psO2[0]] if nxt else SB4
            groups = []
            for qt in range(8):
                aset = qt % 2
                acc = v3(psO[aset][:], 4)
                tiles = []
                first = True
                for dl in range(4 * qt + 3, -1, -1):
                    smm, pv = [], []
                    qs_valid = [qs for qs in range(4) if 4 * qt + qs - dl >= 0]
                    for qs in qs_valid:
                        kt = 4 * qt + qs - dl
                        tq = 4 * qt + qs
                        smm.append(("S", qs, Kd[0:82, kt * 128:(kt + 1) * 128], Qd[0:82, tq * 128:(tq + 1) * 128]))
                        pv.append((acc[:, qs, 0:65], qs, Vd[:, kt, :], first))
                        first = False
                    tiles.append(dict(qs=qs_valid, smm=smm, pv=pv, bias=float(-_slopes()[h] * 128.0 * dl), mask=(dl == 0)))
                groups.append(dict(qt=qt, aset=aset, tiles=tiles))
            gi_tile = 0
            ntile = sum(len(g["tiles"]) for g in groups)
            for g in groups:
                for t in g["tiles"]:
                    sb_ = SBk[gi_tile % len(SBk)]
                    pt_ = PT[gi_tile % 4]
                    q0, q1 = t["qs"][0], t["qs"][-1] + 1
                    t["smm"] = [(sb_[:, qs * 128:(qs + 1) * 128], l, r) for (_, qs, l, r) in t["smm"]]
                    t["act"] = (sb_[:, q0 * 128:q1 * 128], pt_[:, q0 * 128:q1 * 128], t["bias"])
                    t["pv"] = [(o, pt_[:, qs * 128:(qs + 1) * 128], r, st_) for (o, qs, r, st_) in t["pv"]]
                    if t["mask"]:
                        t["mask"] = (v3(pt_[:], 4), tri[:, 0:1, :].to_broadcast([128, 4, 128]))
                    else:
                        t["mask"] = None
                    gi_tile += 1

            def mk_epi(qt, aset):
                acc = v3(psO[aset][:], 4)

                def epi(B, tk):
                    rd = rden[:, aset * 4:aset * 4 + 4].unsqueeze(2)
                    t_r = B.op("vector", lambda e: e.reciprocal(out=rd, in_=acc[:, :, 64:65]),
                               waits=[tk, st_ost[aset]], sig=True)
                    t_n = B.op("vector", lambda e: e.tensor_tensor(out=Ost[aset][:, :, off:off + 64], in0=acc[:, :, 0:64],
                                                                 in1=rd.to_broadcast([128, 4, 64]), op=ALU.mult),
                               waits=[t_r], sig=True)
                    epi_tok[qt] = t_n
                    return t_n
                return epi

            def mk_post(qt, aset):
                def post(B):
                    for qs in range(4):
                        t_tr = B.op("tensor", lambda e, qs=qs: e.matmul(psT[:, qs * 128:(qs + 1) * 128], Ost[aset][:, qs, :], ident[:],
                                                                       start=True, stop=True),
                                    waits=[epi_tok[qt], post_state["psT_free"]] if qs == 0 else (), sig=(qs == 3))
                    st_ost[aset] = t_tr
                    post_state["psT_free"] = B.op("vector", lambda e: e.tensor_copy(
                        out=OT[off:off + 64, pair, qt * 512:(qt + 1) * 512], in_=psT[off:off + 64, :]), waits=[t_tr], sig=True)
                return post

            epi_tok = {}
            st_ost = [None, None]
            post_state = {"psT_free": None}
            for g in groups:
                g["epi"] = mk_epi(g["qt"], g["aset"])
                g["post"] = mk_post(g["qt"], g["aset"])
            if nxt:
                budget = sum(c_ for _f, c_ in pops) / max(1, ntile - 8)
                pq = list(pops)
                fstate = {"acc": 0.0}

                def filler(B):
                    fstate["acc"] += budget
                    while pq and fstate["acc"] > 0:
                        f, c_ = pq.pop(0)
                        f()
                        fstate["acc"] -= c_
                emit_attention(B, T, groups, filler=filler, nS=3, nPT=4)
                while pq:
                    pq.pop(0)[0]()
                B.op("sync", lambda e: e.nop(), waits=[t_qr, t_kr, pstate["last_ev"]])
            else:
                emit_attention(B, T, groups, filler=_mk_filler(lambda: psM[:, 0:128], ident, True), nS=4, nPT=4)
            B.run()
            if nxt:
                selection_block(h + 1)
            if h % 2 == 1:
                gating_block(f"bZ{pair}", pair, wz, zs)

    only = stop.get("only") if isinstance(stop, dict) else None
    if only != "moba":
        _build_nsa(nc, T, gating_block, v3, stop)

    if debug and "OT" in debug:
        with ExitStack() as esD:
            tmp = esD.enter_context(nc.sbuf_tensor("dbgtmp2", [128, 8, 512], F32))
            B = Blk(nc, "bD2")
            s1 = B.newsem("sync")
            tk = None
            for q in range(8):
                t1 = B.op("vector", lambda e, q=q: e.tensor_copy(out=tmp[:], in_=OT[:, :, q * 512:(q + 1) * 512]),
                          waits=[tk], sig=True)
                tk = B.dma("sync", dbg["OT"][:, :, q * 512:(q + 1) * 512], tmp[:], s1, waits=[t1])
            B.op("sync", lambda e: e.nop(), waits=[tk])
            B.run()


def _build_nsa(nc, T, gating_block, v3, stop):
    hT, OT, QA, KA, KW, VA, VW, PT, Ost = T.hT, T.OT, T.QA, T.KA, T.KW, T.VA, T.VW, T.PT, T.Ost
    psS, psO, psO2, psM, psT = T.psS, T.psO, T.psO2, T.psM, T.psT
    ident, btab, tri, cmask, rden, gates = T.ident, T.btab, T.tri, T.cmask, T.rden, T.gates
    dram = T.dram
    banks4 = [psS[0], psS[1], psO[0], psO[1]]
    SB4 = [psS[0], psS[1], psO2[0], psO2[1]]
    SB3 = [psS[0], psS[1], psO2[1]]
    ngroups = int(stop.get("nsa_groups", 2)) if isinstance(stop, dict) else 2
    stage = int(stop.get("nsa_stage", 9)) if isinstance(stop, dict) else 9
    with ExitStack() as esN:
        def sbn(name, shape, dt):
            return esN.enter_context(nc.sbuf_tensor("n_" + name, list(shape), dt))
        kcT = sbn("kcT", [128, 256], BF16)
        rcmp = sbn("rcmp", [128, 2, 129], BF16)
        scrN = sbn("scrN", [128, 2048], BF16)
        WqD = [scrN[:, 0:1024].rearrange("p (c n) -> p c n", c=8), scrN[:, 1024:2048].rearrange("p (c n) -> p c n", c=8)]
        wz = WqD[0]
        m16 = sbn("m16", [128, NT, 16], F32)
        wk4 = sbn("wk4", [128, 4, 64], F32)
        maskp = [sbn(f"maskp{i}", [128, 4, 128], BF16) for i in range(2)]
        zs = [maskp[i][:].rearrange("p a b -> p (a b)") for i in range(2)]
        hs = [scrN[:, 1024:1280], scrN[:, 1280:1536]]
        d3 = [sbn(f"d3{i}", [128, 4, 3], F32) for i in range(2)]
        tA = sbn("tA", [128, 4, 64], F32)
        tB = sbn("tB", [128, 4, 64], F32)
        bcol = sbn("bcol", [128, 2], F32)

        with ExitStack() as es1:
            Wg = es1.enter_context(nc.sbuf_tensor("n_Wg", [128, 8, 24], BF16))
            B = Blk(nc, "bG")
            s1, s2, s3 = B.newsem(), B.newsem(), B.newsem()
            t_w = B.dma("gpsimd", Wg[:], dram["wG"][:], s1)
            t_e = B.dma("gpsimd", KA[64:128, :], dram["c_E64"][:], s2)
            t_rc = B.dma("gpsimd", rcmp[:, :, 64:129], dram["c_rc"][:], s3)
            t_z0 = B.op("vector", lambda e: e.memset(maskp[0][:], 0.0))
            t_z1 = B.op("vector", lambda e: e.memset(maskp[1][:], 0.0))
            t_z4 = B.op("vector", lambda e: e.memset(kcT[:], 0.0), sig=True)
            gT = es1.enter_context(nc.sbuf_tensor("n_gT", [24, S], BF16))
            gfree = [None, None]
            for qt in range(8):
                bk = psS[qt % 2]
                for c in range(8):
                    t_mm = B.op("tensor", lambda e, bk=bk, c=c, qt=qt: e.matmul(
                        bk[0:24, :], Wg[:, c, :], hT[:, c, qt * 512:(qt + 1) * 512], start=(c == 0), stop=(c == 7)),
                        waits=[t_w, gfree[qt % 2]] if c == 0 else (), sig=(c == 7))
                gfree[qt % 2] = B.op("scalar", lambda e, bk=bk, qt=qt: e.activation(
                    out=gT[:, qt * 512:(qt + 1) * 512], in_=bk[0:24, :], func=AF.Sigmoid), waits=[t_mm], sig=True)
            for half in range(2):
                bk = (psM, psT)[half]
                for j in range(16):
                    tt = half * 16 + j
                    t_mm = B.op("tensor", lambda e, bk=bk, j=j, tt=tt: e.matmul(
                        bk[:, j * 24:(j + 1) * 24], gT[:, tt * 128:(tt + 1) * 128], ident[0:24, 0:24], start=True, stop=True),
                        waits=[gfree[0], gfree[1]] if j == 0 else (), sig=(j == 15))
                B.op("scalar", lambda e, bk=bk, half=half: e.copy(
                    out=gates[:, half * 16:(half + 1) * 16, :], in_=bk[:, 0:384].rearrange("p (a b) -> p a b", a=16)),
                    waits=[t_mm], sig=True)
            B.op("sync", lambda e: e.nop(), waits=[t_e, t_rc, t_z4, B.last("scalar")])
            B.run()

        for g in range(ngroups if stage >= 2 else 0):
            with ExitStack() as es1:
                Wkv = es1.enter_context(nc.sbuf_tensor(f"n_Wkv{g}", [128, 8, 384], BF16))
                w1kv = es1.enter_context(nc.sbuf_tensor(f"n_w1kv{g}", [128, 32, 128], BF16))
                w2kv = es1.enter_context(nc.sbuf_tensor(f"n_w2kv{g}", [128, 3, 64], BF16))
                peT = es1.enter_context(nc.sbuf_tensor(f"n_peT{g}", [128, 32], BF16))
                B = Blk(nc, f"bK{g}")
                s1, s2, s3, s4 = B.newsem(), B.newsem(), B.newsem(), B.newsem()
                B.dma("gpsimd", Wkv[:, 0:4, :], dram["wKV"][g][:, 0:4, :], s1)
                t_w = B.dma("gpsimd", Wkv[:, 4:8, :], dram["wKV"][g][:, 4:8, :], s1)
                import os
                skip = os.environ.get("BK_SKIP", "")
                for i4 in range(0 if "w" in skip else 3):
                    B.dma("gpsimd", w1kv[:, i4 * 8:(i4 + 1) * 8, :], dram["w1kv"][:, i4 * 8:(i4 + 1) * 8, :], s2)
                t_w1a = B.dma("gpsimd", w1kv[:, 24:32, :], dram["w1kv"][:, 24:32, :], s2)
                t_w1b = t_w1a
                t_w2 = B.dma("gpsimd", w2kv[:], dram["w2kv"][:], s3)
                t_pe = B.dma("gpsimd", peT[:], dram["peT"][:], s4)
                bank_free = [None] * 4
                bi = 0
                evs = {"scalar": None, "vector": None}
                specs = [(0, 128, QA, 128), (128, 64, KA, 64), (192, 64, KW, 64)]
                if "a" in skip:
                    specs = specs[0:1]
                if "b" in skip:
                    specs = specs[1:2]
                if "c" in skip:
                    specs = specs[2:3]
                for qt in range(0 if "q" in skip else 8):
                    for si, (c0, m, dst, rows) in enumerate(specs):
                        bk = banks4[bi % 4]
                        for c in range(8):
                            t_mm = B.op("tensor", lambda e, bk=bk, c=c, qt=qt, c0=c0, m=m, rows=rows: e.matmul(
                                bk[0:rows, :], Wkv[:, c, c0:c0 + m], hT[:, c, qt * 512:(qt + 1) * 512], start=(c == 0), stop=(c == 7)),
                                waits=[t_w, bank_free[bi % 4]] if c == 0 else (), sig=(c == 7))
                        eng = "scalar" if si != 1 else "vector"
                        if eng == "scalar":
                            t_e = B.op("scalar", lambda e, bk=bk, dst=dst, rows=rows, qt=qt: e.copy(
                                out=dst[0:rows, qt * 512:(qt + 1) * 512], in_=bk[0:rows, :]), waits=[t_mm], sig=True)
                        else:
                            t_e = B.op("vector", lambda e, bk=bk, dst=dst, rows=rows, qt=qt: e.tensor_copy(
                                out=dst[0:rows, qt * 512:(qt + 1) * 512], in_=bk[0:rows, :]), waits=[t_mm], sig=True)
                        evs[eng] = t_e
                        bank_free[bi % 4] = t_e
                        bi += 1
                for tg in range(0 if "v" in skip else 8):
                    bk = banks4[bi % 4]
                    for j in range(4):
                        tt = tg * 4 + j
                        for c in range(8):
                            t_mm = B.op("tensor", lambda e, bk=bk, j=j, c=c, tt=tt: e.matmul(
                                bk[:, j * 128:(j + 1) * 128], hT[:, c, tt * 128:(tt + 1) * 128], Wkv[:, c, 256:384], start=(c == 0), stop=(c == 7)),
                                waits=[bank_free[bi % 4]] if (c == 0 and j == 0) else (), sig=(c == 7 and j == 3))
                    B.op("scalar", lambda e, bk=bk, tg=tg: e.copy(
                        out=VA[:, tg * 4:(tg + 1) * 4, 0:64], in_=v3(bk[:], 4)[:, :, 0:64]), waits=[t_mm])
                    t_e2 = B.op("scalar", lambda e, bk=bk, tg=tg: e.copy(
                        out=VW[:, tg * 4:(tg + 1) * 4, 0:64], in_=v3(bk[:], 4)[:, :, 64:128]), waits=[t_mm], sig=True)
                    bank_free[bi % 4] = t_e2
                    bi += 1
                B.op("vector", lambda e: e.memset(hs[0][:, 255:256], 0.0))
                t_hz = B.op("vector", lambda e: e.memset(hs[1][:, 255:256], 0.0), sig=True)
                kvv = QA[:, :].rearrange("p (c s) -> p c s", s=16)
                import os
                sub = int(os.environ.get("BK_SUB", "9"))
                for kv in range(2 if sub >= 2 else 0):
                    r0 = kv * 64
                    bkH = banks4[bi % 4]
                    bi += 1
                    for l in range(32):
                        a, b_ = l // 16, l % 16
                        t_h = B.op("tensor", lambda e, bkH=bkH, l=l, a=a, b_=b_, r0=r0: e.matmul(
                            bkH[:, 0:255], w1kv[r0:r0 + 64, l, :], kvv[r0:r0 + 64, a:a + 255, b_], start=(l == 0), stop=(l == 31)),
                            waits=[t_w1a, t_w1b, evs["scalar"], evs["vector"], bank_free[(bi - 1) % 4]] if l == 0 else (), sig=(l == 31))
                    if sub == 2:
                        continue
                    if kv == 1 and sub == 3:
                        continue
                    for l in range(32):
                        t_b = B.op("tensor", lambda e, bkH=bkH, l=l, r0=r0: e.matmul(
                            bkH[:, 256:257], w1kv[r0:r0 + 64, l, :], peT[r0:r0 + 64, l:l + 1], start=(l == 0), stop=(l == 31)),
                            waits=[t_pe] if l == 0 else (), sig=(l == 31))
                    t_bc = B.op("vector", lambda e, bkH=bkH, kv=kv: e.tensor_copy(out=bcol[:, kv:kv + 1], in_=bkH[:, 256:257]),
                                waits=[t_b], sig=True)
                    t_s = B.op("scalar", lambda e, bkH=bkH, kv=kv: e.activation(
                        out=hs[kv][:, 0:255], in_=bkH[:, 0:255], func=AF.Silu, bias=bcol[:, kv:kv + 1]), waits=[t_bc, t_h, t_hz], sig=True)
                    bank_free[(bi - 1) % 4] = t_s
                    if kv == 0:
                        t_k = B.op("tensor", lambda e: e.matmul(psM[:, 0:255], w2kv[:, 0:2, :].rearrange("p a b -> p (a b)"), hs[0][:, 0:255],
                                                                start=True, stop=True), waits=[t_s, t_w2], sig=True)
                        B.op("vector", lambda e: e.tensor_copy(out=kcT[:, 0:255], in_=psM[:, 0:255]), waits=[t_k], sig=True)
                    else:
                        for ct in range(2):
                            t_v = B.op("tensor", lambda e, ct=ct: e.matmul(psT[:, ct * 64:(ct + 1) * 64], hs[1][:, ct * 128:(ct + 1) * 128],
                                                                          w2kv[:, 2, :], start=True, stop=True),
                                       waits=[t_s, t_w2] if ct == 0 else (), sig=(ct == 1))
                        B.op("vector", lambda e: e.tensor_copy(out=rcmp[:, :, 0:64], in_=v3(psT[:, 0:128], 2)), waits=[t_v], sig=True)
                B.run()

            with ExitStack() as es2:
                imp = es2.enter_context(nc.sbuf_tensor(f"n_imp{g}", [128, NT, 64], F32))
                if stage >= 3:
                    B = Blk(nc, f"bC{g}")
                    QB = [(QA, 0), (KW, 64)]
                    t_w0 = _load_wq(B, T, WqD[0], g * 4)
                    bfree4 = [None] * 4
                    ops0, st0 = _proj_q_ops(B, T, WqD[0], t_w0, QA, 0, banks4, bfree4, evac="scalar")
                    for f, _c in ops0:
                        f()
                    q_ready = st0["last_ev"]
                    prev = None
                    pbfree = [None, None]
                    wq_last = [st0["last_mm"], None]
                    for hh in range(4):
                        hB = g * 4 + hh
                        Qb, r0 = QB[hh % 2]
                        pq = None
                        if hh < 3:
                            Qn, rn = QB[(hh + 1) % 2]
                            t_wn = _load_wq(B, T, WqD[(hh + 1) % 2], hB + 1, waits=[wq_last[(hh + 1) % 2]])
                            pq, stn = _proj_q_ops(B, T, WqD[(hh + 1) % 2], t_wn, Qn, rn, [psM, psT], pbfree, evac="vector")
                        groups = []
                        gi_tile = 0
                        for qt in range(8):
                            aset = qt % 2
                            acc = v3(psO[aset][:], 4)
                            tiles = []
                            first = True
                            for ct in range(qt // 4 + 1):
                                sb_ = SB4[gi_tile % 4]
                                pt_ = PT[gi_tile % 4]
                                pv = []
                                for qs in range(4):
                                    pv.append((acc[:, qs, 0:65], pt_[:, qs * 128:(qs + 1) * 128], rcmp[:, ct, 64:129], first))
                                    first = False
                                tiles.append(dict(
                                    smm=[(sb_[:], kcT[r0:r0 + 64, ct * 128:(ct + 1) * 128], Qb[r0:r0 + 64, qt * 512:(qt + 1) * 512])],
                                    act=(sb_[:], pt_[:], None),
                                    mask=(pt_[:], cmask[:, qt % 4, :]) if ct == qt // 4 else None,
                                    pv=pv))
                                gi_tile += 1

                            def mk_epi(qt, aset, acc, hh):
                                def epi(B, tk):
                                    rd = rden[:, aset * 4:aset * 4 + 4].unsqueeze(2)
                                    t1 = B.op("vector", lambda e: e.tensor_scalar(out=rd, in0=acc[:, :, 0:1], scalar1=1e-30, scalar2=None,
                                                                                op0=ALU.max), waits=[tk], sig=True)
                                    t2 = B.op("vector", lambda e: e.reciprocal(out=rd, in_=rd), waits=[t1], sig=True)
                                    dst = imp[:, 4 * qt:4 * qt + 4, :]
                                    if hh == 0:
                                        t3 = B.op("vector", lambda e: e.tensor_tensor(out=dst, in0=acc[:, :, 1:65], in1=rd.to_broadcast([128, 4, 64]),
                                                                                    op=ALU.mult), waits=[t2], sig=True)
                                    else:
                                        t3 = B.op("vector", lambda e: e.tensor_tensor(out=tA[:], in0=acc[:, :, 1:65], in1=rd.to_broadcast([128, 4, 64]),
                                                                                    op=ALU.mult), waits=[t2], sig=True)
                                        B.op("vector", lambda e: e.tensor_tensor(out=dst, in0=dst, in1=tA[:], op=ALU.add), waits=[t3], sig=True)
                                    return t3
                                return epi
                            groups.append(dict(tiles=tiles, epi=mk_epi(qt, aset, acc, hh), post=None))
                        groups[0]["tiles"][0]["extra_wait"] = q_ready
                        if pq is not None:
                            fl = _budget_filler(pq, sum(c_ for _f, c_ in pq) / max(1, gi_tile - 3))
                        else:
                            fl = None
                        prev = emit_attention(B, T, groups, filler=fl, nS=4, nPT=4, prev=prev)
                        if pq is not None:
                            while pq:
                                pq.pop(0)[0]()
                            q_ready = stn["last_ev"]
                            wq_last[(hh + 1) % 2] = stn["last_mm"]
                    B.run()

                if stage < 4:
                    continue
                B = Blk(nc, f"bS{g}")
                B.op("vector", lambda e: e.memset(maskp[0][:], 0.0))
                B.op("vector", lambda e: e.memset(maskp[1][:], 0.0))
                t_a = B.op("vector", lambda e: e.tensor_tensor(out=imp[:], in0=imp[:], in1=T.addm_slc[:], op=ALU.add), sig=True)
                tk = None
                for t4 in range(0, NT, 4):
                    t1s = [B.op("vector", lambda e, tt=tt: e.max(out=m16[:, tt, 0:8], in_=imp[:, tt, :]), waits=[t_a], sig=True)
                           for tt in range(t4, t4 + 4)]
                    t2s = [B.op("vector", lambda e, tt=tt: e.match_replace(out=wk4[:, tt % 4, :], in_to_replace=m16[:, tt, 0:8],
                                                                        in_values=imp[:, tt, :], imm_value=-1e30),
                                waits=[t1s[tt - t4]], sig=True) for tt in range(t4, t4 + 4)]
                    for tt in range(t4, t4 + 4):
                        tk = B.op("vector", lambda e, tt=tt: e.max(out=m16[:, tt, 8:16], in_=wk4[:, tt % 4, :]), waits=[t2s[tt - t4]], sig=True)
                mp_free = [None, None]
                ps_free = [None, None]
                for c4 in range(8):
                    b = c4 % 2
                    t_m = B.op("vector", lambda e, c4=c4, b=b: e.tensor_tensor(
                        out=maskp[b][:, :, 64:128], in0=imp[:, 4 * c4:4 * c4 + 4, :],
                        in1=m16[:, 4 * c4:4 * c4 + 4, 15:16].to_broadcast([128, 4, 64]), op=ALU.is_lt), waits=[tk, mp_free[b]], sig=True)
                    for j in range(4):
                        t_tr = B.op("tensor", lambda e, b=b, j=j: e.matmul(psS[b][:, j * 128:(j + 1) * 128], maskp[b][:, j, :], ident[:],
                                                                         start=True, stop=True),
                                    waits=[t_m, ps_free[b]] if j == 0 else (), sig=(j == 3))
                    mp_free[b] = t_tr
                    ps_free[b] = B.op("scalar", lambda e, b=b, c4=c4: e.copy(out=KW[64:128, c4 * 512:(c4 + 1) * 512], in_=psS[b][64:128, :]),
                                      waits=[t_tr], sig=True)
                B.run()

            for hh in range(4 if stage >= 5 else 0):
                hB = g * 4 + hh
                pair, off = 4 + hB // 2, (hB % 2) * 64
                B = Blk(nc, f"bN{hB}")
                s1 = B.newsem()
                t_w = _load_wq(B, T, WqD[1], hB)
                bfree4 = [None] * 4
                ops0, st0 = _proj_q_ops(B, T, WqD[1], t_w, QA, 0, banks4, bfree4, evac="scalar")
                for f, _c in ops0:
                    f()
                q_ev = st0["last_ev"]
                t_mc = B.op("vector", lambda e: e.tensor_copy(out=QA[64:128, :], in_=KW[64:128, :]), sig=True)
                groups = []
                gi_tile = 0
                for qt in range(8):
                    aset = qt % 2
                    accS = v3(psO[aset][:], 4)
                    accW = v3(psO2[0][:], 4)
                    accC = v3(psM[:], 4)
                    tiles = []
                    firstS, firstW, firstC = True, True, True
                    for dl in range(4 * qt + 3, -1, -1):
                        sb_ = SB3[gi_tile % 3]
                        pt_ = PT[gi_tile % 4]
                        qsv = [qs for qs in range(4) if 4 * qt + qs - dl >= 0]
                        smm, pv = [], []
                        for qs in qsv:
                            kt, tq = 4 * qt + qs - dl, 4 * qt + qs
                            smm.append((sb_[:, qs * 128:(qs + 1) * 128], KA[:, kt * 128:(kt + 1) * 128], QA[:, tq * 128:(tq + 1) * 128]))
                            pv.append((accS[:, qs, 0:65], pt_[:, qs * 128:(qs + 1) * 128], VA[:, kt, :], firstS))
                            firstS = False
                        q0, q1 = qsv[0], qsv[-1] + 1
                        tiles.append(dict(smm=smm, act=(sb_[:, q0 * 128:q1 * 128], pt_[:, q0 * 128:q1 * 128], btab[:, hB, dl:dl + 1]),
                                          mask=(v3(pt_[:], 4), tri[:, 0:1, :].to_broadcast([128, 4, 128])) if dl == 0 else None, pv=pv))
                        gi_tile += 1
                    for dl in range(4, -1, -1):
                        qsv = [qs for qs in range(4) if 4 * qt + qs - dl >= 0]
                        if not qsv:
                            continue
                        sb_ = SB3[gi_tile % 3]
                        pt_ = PT[gi_tile % 4]
                        smm, pv = [], []
                        for qs in qsv:
                            kt, tq = 4 * qt + qs - dl, 4 * qt + qs
                            smm.append((sb_[:, qs * 128:(qs + 1) * 128], KW[0:64, kt * 128:(kt + 1) * 128], QA[0:64, tq * 128:(tq + 1) * 128]))
                            pv.append((accW[:, qs, 0:65], pt_[:, qs * 128:(qs + 1) * 128], VW[:, kt, :], firstW))
                            firstW = False
                        q0, q1 = qsv[0], qsv[-1] + 1
                        mk = None
                        if dl == 0 or dl == 4:
                            mi = 0 if dl == 0 else 1
                            mk = (v3(pt_[:], 4)[:, q0:q1, :], tri[:, mi:mi + 1, :].to_broadcast([128, q1 - q0, 128]))
                        tiles.append(dict(smm=smm, act=(sb_[:, q0 * 128:q1 * 128], pt_[:, q0 * 128:q1 * 128], btab[:, hB, dl:dl + 1]),
                                          mask=mk, pv=pv, cmp_first=(dl == 0)))
                        gi_tile += 1
                    for ct in range(qt // 4 + 1):
                        sb_ = SB3[gi_tile % 3]
                        pt_ = PT[gi_tile % 4]
                        pv = []
                        for qs in range(4):
                            pv.append((accC[:, qs, 0:65], pt_[:, qs * 128:(qs + 1) * 128], rcmp[:, ct, 0:65], firstC))
                            firstC = False
                        tiles.append(dict(
                            smm=[(sb_[:], kcT[0:64, ct * 128:(ct + 1) * 128], QA[0:64, qt * 512:(qt + 1) * 512])],
                            act=(sb_[:], pt_[:], None),
                            mask=(pt_[:], cmask[:, qt % 4, :]) if ct == qt // 4 else None,
                            pv=pv, cmp_first=(ct == 0)))
                        gi_tile += 1
                    groups.append(dict(qt=qt, aset=aset, tiles=tiles, accs=(accC, accS, accW)))

                epi_tok = {}
                st_ost = [None, None]
                post_state = {"psT_free": None}

                def mk_epi(qt, aset, accs):
                    def epi(B, tk):
                        dd = d3[aset]
                        for br in range(3):
                            t1 = B.op("vector", lambda e, br=br: e.tensor_scalar(out=dd[:, :, br:br + 1], in0=accs[br][:, :, 64:65], scalar1=1e-30,
                                                                               scalar2=None, op0=ALU.max), waits=[tk], sig=(br == 2))
                        t2 = B.op("vector", lambda e: e.reciprocal(out=dd[:], in_=dd[:]), waits=[t1], sig=True)
                        t3 = B.op("vector", lambda e: e.tensor_tensor(out=dd[:], in0=dd[:], in1=gates[:, 4 * qt:4 * qt + 4, 3 * hB:3 * hB + 3],
                                                                    op=ALU.mult), waits=[t2], sig=True)
                        t4 = B.op("vector", lambda e: e.tensor_tensor(out=tA[:], in0=accs[0][:, :, 0:64],
                                                                    in1=dd[:, :, 0:1].to_broadcast([128, 4, 64]), op=ALU.mult), waits=[t3], sig=True)
                        t5 = B.op("vector", lambda e: e.tensor_tensor(out=tB[:], in0=accs[1][:, :, 0:64],
                                                                    in1=dd[:, :, 1:2].to_broadcast([128, 4, 64]), op=ALU.mult), waits=[t3], sig=True)
                        t6 = B.op("vector", lambda e: e.tensor_tensor(out=tA[:], in0=tA[:], in1=tB[:], op=ALU.add), waits=[t4, t5], sig=True)
                        t7 = B.op("vector", lambda e: e.tensor_tensor(out=tB[:], in0=accs[2][:, :, 0:64],
                                                                    in1=dd[:, :, 2:3].to_broadcast([128, 4, 64]), op=ALU.mult), waits=[t6], sig=True)
                        t8 = B.op("vector", lambda e: e.tensor_tensor(out=Ost[aset][:, :, off:off + 64], in0=tA[:], in1=tB[:], op=ALU.add),
                                  waits=[t7, st_ost[aset]], sig=True)
                        epi_tok[qt] = t8
                        return t7
                    return epi

                def mk_post(qt, aset):
                    def post(B):
                        for qs in range(4):
                            t_tr = B.op("tensor", lambda e, qs=qs: e.matmul(psT[:, qs * 128:(qs + 1) * 128], Ost[aset][:, qs, :], ident[:],
                                                                           start=True, stop=True),
                                        waits=[epi_tok[qt], post_state["psT_free"]] if qs == 0 else (), sig=(qs == 3))
                        st_ost[aset] = t_tr
                        post_state["psT_free"] = B.op("vector", lambda e: e.tensor_copy(
                            out=OT[off:off + 64, pair, qt * 512:(qt + 1) * 512], in_=psT[off:off + 64, :]), waits=[t_tr], sig=True)
                    return post

                for gr in groups:
                    gr["epi"] = mk_epi(gr["qt"], gr["aset"], gr["accs"])
                    gr["post"] = mk_post(gr["qt"], gr["aset"])
                groups[0]["tiles"][0]["extra_wait"] = [q_ev, t_mc]
                emit_attention(B, T, groups, single_acc_key="cmp_first", nS=3, nPT=4,
                               filler=_mk_filler(lambda: psT[:, 0:128], ident, True, nf=NFILL2,
                                                 wait_fn=lambda: post_state["psT_free"]))
                B.run()
                if hB % 2 == 1:
                    gating_block(f"bZ{pair}", pair, wz, zs)


NFILL = [4]


NFILL2 = "NFILL2"


def _mk_filler(dst_fn, ident, start, m=128, nf=None, wait_fn=None):
    import os
    nfill = int(os.environ.get("NFILL", NFILL[0]))
    if nf == NFILL2:
        nfill = int(os.environ.get("NFILL2", 3))
    if nfill <= 0:
        return None

    def filler(B):
        dst = dst_fn()
        n = dst.shape[-1]
        for k in range(nfill):
            w = [wait_fn()] if (wait_fn is not None and k == 0) else ()
            B.op("tensor", lambda e: e.matmul(dst, ident[:, 0:m], ident[:, 0:n], start=start, stop=start), waits=w)
    return filler


def _load_wq(B, T, WqD_i, hB, waits=()):
    sw_ = B.newsem()
    B.dma("gpsimd", WqD_i[:, :, 0:64], T.dram["wQB"][hB], sw_, waits=list(waits))
    return B.dma("gpsimd", WqD_i[:, :, 64:128], T.dram["wQB"][hB], sw_, waits=list(waits))


def _proj_q_ops(B, T, Wq, t_w, dst, r0, banks, bank_free, evac="vector"):
    state = {"bi": 0, "first": True, "last_ev": None}

    def mk(qt, c):
        def f():
            bi = state["bi"]
            bk = banks[bi % len(banks)]
            w = ()
            if c == 0:
                w = [bank_free[bi % len(banks)]] + ([t_w] if state["first"] else [])
                state["first"] = False
            tk = B.op("tensor", lambda e: e.matmul(bk[:, :], Wq[:, c, :], T.hT[:, c, qt * 512:(qt + 1) * 512],
                                                  start=(c == 0), stop=(c == 7)), waits=w, sig=(c == 7))
            if c == 7:
                if evac == "vector":
                    t_e = B.op("vector", lambda e: e.tensor_scalar(out=dst[r0:r0 + 64, qt * 512:(qt + 1) * 512], in0=bk[r0:r0 + 64, :],
                                                                 scalar1=0.125, scalar2=None, op0=ALU.mult), waits=[tk], sig=True)
                else:
                    t_e = B.op("scalar", lambda e: e.activation(out=dst[r0:r0 + 64, qt * 512:(qt + 1) * 512], in_=bk[r0:r0 + 64, :],
                                                              func=AF.Copy, scale=0.125), waits=[tk], sig=True)
                bank_free[bi % len(banks)] = t_e
                state["last_ev"] = t_e
                state["last_mm"] = tk
                state["bi"] += 1
        return f
    return [(mk(qt, c), 1.0) for qt in range(8) for c in range(8)], state


def _budget_filler(pq, budget):
    st = {"acc": 0.0}

    def filler(B):
        st["acc"] += budget
        while pq and st["acc"] > 0:
            f, c_ = pq.pop(0)
            f()
            st["acc"] -= c_
    return filler


def _build_final(nc, hT, OT, psS, psO, dram, x, out, epsc, psO2=None, psM=None, psT=None):
    with ExitStack() as esF:
        def sbf(name, shape, dt):
            return esF.enter_context(nc.sbuf_tensor("f_" + name, list(shape), dt))
        wO = sbf("wO", [128, 8, D], BF16)
        gpost = sbf("gpost", [128, D], F32)
        xt = [sbf(f"xt{i}", [128, 2, D], F32) for i in range(3)]
        yt = [sbf(f"yt{i}", [128, 2, D], F32) for i in range(2)]
        junk = sbf("junk", [128, 512], BF16)
        ssq = sbf("ssq", [128, NT, 2], F32)
        rs = sbf("rs", [128, NT], F32)
        B = Blk(nc, "bF")
        sw, sg = B.newsem(), B.newsem("sync")
        t_w = [B.dma("gpsimd", wO[:, c, :], dram["wO"][:, c, :], sw) for c in range(8)]
        t_g = B.dma("sync", gpost[:], dram["gpost"][:], sg)
        xs = [B.newsem("sync") for _ in range(3)]
        os_ = [B.newsem("sync") for _ in range(2)]
        pY = [(psS[0], psS[1]), (psO[0], psO[1]), (psO2[0], psO2[1]), (psM, psT)]
        ps_free = [None] * 4
        xt_free = [None, None, None]
        yt_free = [None, None]
        t_xs = [None] * (NT // 2)
        for tp in range(NT // 2):
            b3 = tp % 3
            by = tp % 2
            xv = x[tp * 256:(tp + 1) * 256, :].rearrange("(n p) d -> p n d", p=128)
            ov = out[tp * 256:(tp + 1) * 256, :].rearrange("(n p) d -> p n d", p=128)
            if tp == 0:
                for t2 in range(2):
                    xv2 = x[t2 * 256:(t2 + 1) * 256, :].rearrange("(n p) d -> p n d", p=128)
                    t_xs[t2] = B.dma("sync", xt[t2 % 3][:], xv2, xs[t2 % 3])
            t_x = t_xs[tp]
            for j in range(2):
                tt = tp * 2 + j
                b = tt % 4
                for half in range(2):
                    pk = pY[b][half]
                    for c in range(8):
                        t_mm = B.op("tensor", lambda e, pk=pk, c=c, tt=tt, half=half: e.matmul(
                            pk[:], OT[:, c, tt * 128:(tt + 1) * 128], wO[:, c, half * 512:(half + 1) * 512], start=(c == 0), stop=(c == 7)),
                            waits=(t_w + [ps_free[b]]) if (c == 0 and half == 0) else (), sig=(c == 7 and half == 1))
                for half in range(2):
                    t_sq = B.op("scalar", lambda e, half=half, b=b, tt=tt: e.activation(
                        out=junk[:], in_=pY[b][half][:], func=AF.Square, accum_out=ssq[:, tt, half:half + 1]), waits=[t_mm], sig=True)
                t_a = B.op("vector", lambda e, tt=tt: e.tensor_tensor(out=rs[:, tt:tt + 1], in0=ssq[:, tt, 0:1], in1=ssq[:, tt, 1:2], op=ALU.add),
                           waits=[t_sq], sig=True)
                t_r1 = B.op("scalar", lambda e, tt=tt: e.activation(out=rs[:, tt:tt + 1], in_=rs[:, tt:tt + 1], func=AF.Sqrt,
                                                                  bias=epsc[:, 0:1], scale=1.0 / D), waits=[t_a], sig=True)
                t_r2 = B.op("vector", lambda e, tt=tt: e.reciprocal(out=rs[:, tt:tt + 1], in_=rs[:, tt:tt + 1]), waits=[t_r1], sig=True)
                for half in range(2):
                    t_y = B.op("vector", lambda e, half=half, b=b, tt=tt, by=by, j=j: e.scalar_tensor_tensor(
                        out=yt[by][:, j, half * 512:(half + 1) * 512], in0=pY[b][half][:], scalar=rs[:, tt:tt + 1],
                        in1=gpost[:, half * 512:(half + 1) * 512], op0=ALU.mult, op1=ALU.mult),
                        waits=[t_r2, t_g, yt_free[by]] if half == 0 else (), sig=True)
                ps_free[b] = t_y
            t_o = B.op("vector", lambda e, by=by, b3=b3: e.tensor_tensor(out=yt[by][:], in0=yt[by][:], in1=xt[b3][:], op=ALU.add),
                       waits=[t_y, t_x], sig=True)
            xt_free[b3] = t_o
            yt_free[by] = B.dma("sync", ov, yt[by][:], os_[by], waits=[t_o])
            if tp + 2 < NT // 2:
                t3 = tp + 2
                xv2 = x[t3 * 256:(t3 + 1) * 256, :].rearrange("(n p) d -> p n d", p=128)
                t_xs[t3] = B.dma("sync", xt[t3 % 3][:], xv2, xs[t3 % 3], waits=[xt_free[t3 % 3]])
        B.op("sync", lambda e: e.nop(), waits=[yt_free[0], yt_free[1]])
        B.run()


def build(debug=None, stop=None):
    nc = bass.Bass("TRN2", target_bir_lowering=False)
    dram = {}

    def din(name, shape):
        dram[name] = nc.dram_tensor(name, list(shape), F32, kind="ExternalInput").ap()
        return dram[name]

    x = din("x", [S, D])
    cshapes = {k: v.shape for k, v in host_consts().items()}
    for k, shp in cshapes.items():
        din(k, shp)
    wshapes = dict(wA=[8, 128, 3, 8, 64], wZ=[8, 128, 8, 128], wQB=[8, 128, 8, 64], wG=[128, 8, 24],
                   wKV=[2, 128, 8, 384], w1kv=[128, 32, 128], w2kv=[128, 3, 64], peT=[128, 32],
                   wO=[128, 8, 1024], gpre=[128, 8], gpost=[128, D])
    for k, shp in wshapes.items():
        din(k, shp)
    out = nc.dram_tensor("out", [S, D], F32, kind="ExternalOutput").ap()
    dbg = {}
    if debug:
        for name, shp in debug.items():
            dbg[name] = nc.dram_tensor("dbg_" + name, list(shp), F32, kind="ExternalOutput").ap()

    es = ExitStack()

    def sb(name, shape, dt):
        return es.enter_context(nc.sbuf_tensor("s_" + name, list(shape), dt))

    def ps(name, shape, dt=F32):
        return es.enter_context(nc.psum_tensor(name, list(shape), dt))

    with es:
        hT = sb("hT", [128, 8, S], BF16)
        OT = sb("OT", [128, 8, S], BF16)
        ident = sb("ident", [128, 128], BF16)
        addm_moba = sb("addm_moba", [128, NT, 16], F32)
        addm_slc = sb("addm_slc", [128, NT, 64], BF16)
        cmask = sb("cmask", [128, 4, 512], BF16)
        tri = sb("tri", [128, 2, 128], BF16)
        btab = sb("btab", [128, 8, 32], F32)
        gpre = sb("gpre", [128, 8], F32)
        epsc = sb("epsc", [128, 1], F32)
        gates = sb("gates", [128, NT, 24], F32)
        esW = ExitStack()

        def sbw(name, shape, dt):
            return esW.enter_context(nc.sbuf_tensor("w_" + name, list(shape), dt))
        QA = sbw("QA", [128, S], BF16)
        KA = sbw("KA", [128, S], BF16)
        KW = sbw("KW", [128, S], BF16)
        VA = sbw("VA", [128, NT, 65], BF16)
        VW = sbw("VW", [128, NT, 65], BF16)
        PT = [sbw(f"PT{i}", [128, 512], BF16) for i in range(4)]
        Ost = [sbw(f"Ost{i}", [128, 4, 128], BF16) for i in range(2)]
        rden = sbw("rden", [128, 16], F32)
        psS = [ps(f"psS{i}", [128, 512]) for i in range(2)]
        psO = [ps(f"psO{i}", [128, 512]) for i in range(2)]
        psO2 = [ps(f"psO2{i}", [128, 512]) for i in range(2)]
        psM = ps("psM", [128, 512])
        psT = ps("psT", [128, 512])

        POOL[0] = SemPool(nc, es)
        with nc.Block() as blk_clr:
            @blk_clr.gpsimd
            def _(g):
                for sm in POOL[0].all():
                    g.sem_clear(sm.h)

        B = Blk(nc, "b0")
        toks = []
        toks.append(B.dma("gpsimd", ident[:], dram["c_ident"][:], B.newsem()))
        toks.append(B.dma("sync", addm_moba[:], dram["c_addm_moba"][:], B.newsem("sync")))
        toks.append(B.dma("gpsimd", addm_slc[:], dram["c_addm_slc"][:], B.newsem()))
        toks.append(B.dma("gpsimd", cmask[:], dram["c_cmask"][:], B.newsem()))
        toks.append(B.dma("gpsimd", tri[:], dram["c_tri"][:], B.newsem()))
        toks.append(B.dma("sync", btab[:], dram["c_btab"][:], B.newsem("sync")))
        toks.append(B.dma("sync", gpre[:], dram["gpre"][:], B.newsem("sync")))
        B.op("vector", lambda e: e.memset(VA[:, :, 64:65], 1.0))
        B.op("vector", lambda e: e.memset(epsc[:], 1e-6))
        B.op("vector", lambda e: e.memset(VW[:, :, 64:65], 1.0))
        B.op("vector", lambda e: e.memset(QA[64:128, :], 0.0))
        t_ms = B.op("vector", lambda e: e.memset(KA[64:128, :], 0.0), sig=True)
        B.op("gpsimd", lambda e: e.memset(Ost[0][:], 0.0))
        B.op("gpsimd", lambda e: e.memset(Ost[1][:], 0.0))
        B.op("sync", lambda e: e.nop(), waits=toks)
        B.run()

        with ExitStack() as esA:
            xt = [esA.enter_context(nc.sbuf_tensor(f"xt{i}", [128, D], F32)) for i in range(4)]
            junk = esA.enter_context(nc.sbuf_tensor("junkA", [128, D], BF16))
            hb = [esA.enter_context(nc.sbuf_tensor(f"hb{i}", [128, D], BF16)) for i in range(2)]
            ss = esA.enter_context(nc.sbuf_tensor("ssA", [128, NT], F32))
            rs = esA.enter_context(nc.sbuf_tensor("rsA", [128, NT], F32))
            B = Blk(nc, "bA")
            xs = [B.newsem("sync") for _ in range(4)]
            xtok = [None] * NT
            hb_free = [None, None]
            xt_free = [None] * 4
            ps_free = [None, None]
            pA = [(psS[0], psS[1]), (psO[0], psO[1])]
            tr2 = [None] * NT
            tsq = [None] * NT

            def stage0(tt):
                b3 = tt % 4
                xtok[tt] = B.dma("sync", xt[b3][:], x[tt * 128:(tt + 1) * 128, :], xs[b3], waits=[xt_free[b3]])

            def stage1(tt):
                b3 = tt % 4
                t_sq = B.op("scalar", lambda e, b3=b3, tt=tt: e.activation(out=junk[:], in_=xt[b3][:], func=AF.Square,
                                                                     accum_out=ss[:, tt:tt + 1]),
                            waits=[xtok[tt]], sig=True)
                t_r1 = B.op("scalar", lambda e, tt=tt: e.activation(out=rs[:, tt:tt + 1], in_=ss[:, tt:tt + 1], func=AF.Sqrt,
                                                                  bias=epsc[:, 0:1], scale=1.0 / D),
                            waits=[t_sq], sig=True)
                tr2[tt] = B.op("vector", lambda e, tt=tt: e.reciprocal(out=rs[:, tt:tt + 1], in_=rs[:, tt:tt + 1]),
                               waits=[t_r1], sig=True)
                tsq[tt] = t_sq

            def stage2(tt):
                b3 = tt % 4
                b2 = tt % 2
                t_h = B.op("scalar", lambda e, tt=tt, b3=b3, b2=b2: e.activation(
                    out=hb[b2][:], in_=xt[b3][:], func=AF.Copy, scale=rs[:, tt:tt + 1]),
                    waits=[tr2[tt], hb_free[b2], tsq[tt]], sig=True)
                xt_free[b3] = t_h
                pa, pb = pA[b2]
                for c in range(8):
                    dst = (pa if c < 4 else pb)[:, (c % 4) * 128:(c % 4 + 1) * 128]
                    t_tr = B.op("tensor", lambda e, dst=dst, b2=b2, c=c: e.matmul(
                        dst, hb[b2][:, c * 128:(c + 1) * 128], ident[:], start=True, stop=True),
                        waits=[t_h, ps_free[b2]], sig=(c == 7))
                hb_free[b2] = t_tr
                B.op("vector", lambda e, tt=tt, pa=pa: e.tensor_tensor(
                    out=hT[:, 0:4, tt * 128:(tt + 1) * 128], in0=pa[:].rearrange("p (c t) -> p c t", c=4),
                    in1=gpre[:, 0:4].unsqueeze(2).to_broadcast([128, 4, 128]), op=ALU.mult), waits=[t_tr])
                ps_free[b2] = B.op("vector", lambda e, tt=tt, pb=pb: e.tensor_tensor(
                    out=hT[:, 4:8, tt * 128:(tt + 1) * 128], in0=pb[:].rearrange("p (c t) -> p c t", c=4),
                    in1=gpre[:, 4:8].unsqueeze(2).to_broadcast([128, 4, 128]), op=ALU.mult), waits=[t_tr], sig=True)

            for t0 in range(3):
                stage0(t0)
            stage1(0)
            stage1(1)
            for tt in range(NT):
                if tt + 3 < NT:
                    stage0(tt + 3)
                if tt + 2 < NT:
                    stage1(tt + 2)
                stage2(tt)
            B.run()


        if stop == "A":
            pass
        else:
            _build_rest(nc, locals(), debug, dbg, stop)
        esW.close()
        if stop is None or (isinstance(stop, dict) and stop.get("final")):
            _build_final(nc, hT, OT, psS, psO, dram, x, out, epsc, psO2, psM, psT)

        if debug and "hT" in debug:
            with ExitStack() as esD:
                tmp = esD.enter_context(nc.sbuf_tensor("dbgtmp", [128, 8, 512], F32))
                B = Blk(nc, "bD")
                s1 = B.newsem("sync")
                tk = None
                for q in range(8):
                    t1 = B.op("vector", lambda e, q=q: e.tensor_copy(out=tmp[:], in_=hT[:, :, q * 512:(q + 1) * 512]),
                              waits=[tk], sig=True)
                    tk = B.dma("sync", dbg["hT"][:, :, q * 512:(q + 1) * 512], tmp[:], s1, waits=[t1])
                B.op("sync", lambda e: e.nop(), waits=[tk])
                B.run()
    return nc


_CACHE = {}


def kernel(x, pre_norm_g, post_norm_g, w_in, cmp_pos_k, cmp_pos_v, w_cmp_k1, w_cmp_k2, w_cmp_v1, w_cmp_v2, w_out):
    x = np.asarray(x, np.float32)
    consts = host_consts()
    wts = host_weights(*(np.asarray(a, np.float32) for a in (pre_norm_g, post_norm_g, w_in, cmp_pos_k, cmp_pos_v,
                                                            w_cmp_k1, w_cmp_k2, w_cmp_v1, w_cmp_v2, w_out)))
    nc = build()
    in_maps = []
    for b in range(8):
        m = {"x": np.ascontiguousarray(x[b])}
        m.update(consts)
        m.update(wts)
        in_maps.append(m)
    res = run_bass_kernel_spmd(nc, in_maps, core_ids=list(range(8)))
    return np.stack([r["out"] for r in res.results], 0).astype(np.float32)
```

```python
import numpy as np
from contextlib import ExitStack
import concourse.bass as bass
import concourse.mybir as mybir
from concourse.bass_utils import run_bass_kernel_spmd

F32 = mybir.dt.float32
BF16 = mybir.dt.bfloat16
ALU = mybir.AluOpType
AF = mybir.ActivationFunctionType
AX = mybir.AxisListType

S = 4096
D = 1024
NT = 32
DIN = 3864
NEGM = -30000.0
ENG = ("sync", "scalar", "vector", "gpsimd", "tensor")


class Sem:
    def __init__(self, h):
        self.h = h
        self.n = 0


class SemPool:
    def __init__(self, nc, es, ndma=24):
        self.eng = {e: Sem(es.enter_context(nc.semaphore(f"pool_{e}"))) for e in ENG}
        self.dma = {"gpsimd": [Sem(es.enter_context(nc.semaphore(f"pool_g{i}"))) for i in range(ndma)],
                    "sync": [Sem(es.enter_context(nc.semaphore(f"pool_s{i}"))) for i in range(ndma)],
                    "scalar": [Sem(es.enter_context(nc.semaphore(f"pool_a{i}"))) for i in range(4)]}

    def all(self):
        return list(self.eng.values()) + self.dma["gpsimd"] + self.dma["sync"] + self.dma["scalar"]


POOL = [None]


class Blk:
    def __init__(self, nc, name):
        self.nc = nc
        self.name = name
        self.ops = {e: [] for e in ENG}
        self.esem = POOL[0].eng
        self.k = {"gpsimd": 0, "sync": 0, "scalar": 0}

    def newsem(self, kind="gpsimd"):
        lst = POOL[0].dma[kind]
        s_ = lst[self.k[kind] % len(lst)]
        self.k[kind] += 1
        s_.kind = kind
        return s_

    def op(self, eng, fn, waits=(), sig=False):
        tok = None
        s = None
        if sig:
            s = self.esem[eng]
            s.n += 1
            tok = (s.h, s.n)
        self.ops[eng].append((fn, tuple(w for w in waits if w is not None), s, 1))
        return tok

    def dma(self, eng, out, in_, sem, waits=()):
        assert sem.kind == eng, (sem.kind, eng)
        sem.n += 16
        tok = (sem.h, sem.n)
        self.ops[eng].append((lambda e: e.dma_start(out=out, in_=in_), tuple(w for w in waits if w is not None), sem, 16))
        return tok

    def last(self, eng):
        s = self.esem[eng]
        return (s.h, s.n) if s.n > 0 else None

    def run(self):
        with self.nc.Block() as block:
            for e in ENG:
                ops = self.ops[e]

                def body(eng, ops=ops):
                    seen = {}
                    for fn, waits, s, amt in ops:
                        for (h, v) in waits:
                            key = id(h)
                            if seen.get(key, 0) >= v:
                                continue
                            seen[key] = v
                            eng.wait_ge(h, v)
                        ins = fn(eng)
                        if s is not None:
                            ins.then_inc(s.h, amt)

                getattr(block, e)(body)


def _slopes():
    return [2.0 ** (-(i + 1)) for i in range(8)]


def host_consts():
    c = {}
    c["c_ident"] = np.eye(128, dtype=np.float32)
    k = np.arange(S)
    c["c_E16"] = (k[None, :] // 256 == np.arange(16)[:, None]).astype(np.float32)
    c["c_E64"] = (NEGM * (k[None, :] // 64 == np.arange(64)[:, None])).astype(np.float32)
    am = np.zeros((128, NT, 16), np.float32)
    for tt in range(NT):
        own = tt // 2
        am[:, tt, own] = 1e9
        am[:, tt, own + 1:] = -1e9
    c["c_addm_moba"] = am
    a2 = np.zeros((128, NT, 64), np.float32)
    t = (np.arange(NT)[None, :] * 128 + np.arange(128)[:, None])
    own = t // 64
    j = np.arange(64)[None, None, :]
    a2 = np.where(j > own[:, :, None], -1e9, 0.0).astype(np.float32)
    a2 = np.where(j == 0, 1e9, a2)
    a2 = np.where(j == own[:, :, None] - 1, 2e9, a2)
    a2 = np.where(j == own[:, :, None], 3e9, a2)
    c["c_addm_slc"] = a2.astype(np.float32)
    cr = np.arange(128)[:, None, None]
    r = np.arange(4)[None, :, None]
    tr = np.arange(512)[None, None, :]
    c["c_cmask"] = (16 * cr + 31 - 512 * r <= tr).astype(np.float32)
    kk = np.arange(128)[:, None]
    tq = np.arange(128)[None, :]
    c["c_tri"] = np.stack([(kk <= tq), (kk > tq)], axis=1).astype(np.float32)
    sl = np.array(_slopes(), np.float32)
    dl = np.arange(32)[None, None, :]
    c["c_btab"] = (sl[None, :, None] * (np.arange(128)[:, None, None] - 128.0 * dl - 64.0)).astype(np.float32)
    tmod = (np.arange(S) % 128).astype(np.float32)
    qrow = np.zeros((8, 2, S), np.float32)
    krow = np.zeros((8, 2, S), np.float32)
    for h in range(8):
        qrow[h, 0] = -sl[h] * tmod
        qrow[h, 1] = 1.0
        krow[h, 0] = 1.0
        krow[h, 1] = sl[h] * tmod
    c["c_qrow"] = qrow
    c["c_krow"] = krow
    rc = np.zeros((128, 2, 65), np.float32)
    cidx = np.arange(2)[None, :] * 128 + np.arange(128)[:, None]
    valid = cidx < 255
    rc[:, :, 0] = valid
    cst = cidx * 16
    js = np.arange(64)[None, None, :] * 64
    ov = (cst[:, :, None] < js + 64) & (cst[:, :, None] + 32 > js) & valid[:, :, None]
    rc[:, :, 1:] = ov
    c["c_rc"] = rc
    return c


def host_weights(pre_norm_g, post_norm_g, w_in, cmp_pos_k, cmp_pos_v, w_cmp_k1, w_cmp_k2, w_cmp_v1, w_cmp_v2, w_out):
    w = {}
    W = np.ascontiguousarray(w_in[0].reshape(8, 128, DIN).transpose(1, 0, 2))

    def cols(a, n=64):
        return W[:, :, a:a + n]

    w["wA"] = np.ascontiguousarray(np.stack(
        [np.stack([cols(0 + h * 64), cols(512 + h * 64), cols(1024 + h * 64)], axis=1) for h in range(8)], 0))
    zc = [1536 + p * 128 for p in range(4)] + [3352 + p * 128 for p in range(4)]
    w["wZ"] = np.ascontiguousarray(np.stack([cols(a, 128) for a in zc], 0))
    w["wQB"] = np.ascontiguousarray(np.stack([cols(2048 + h * 64) for h in range(8)], 0))
    w["wG"] = np.ascontiguousarray(cols(3328, 24))
    kv = []
    for g in range(2):
        kv.append(np.concatenate([cols(2560 + g * 64), cols(2688 + g * 64), cols(2816 + g * 64),
                                  cols(3072 + g * 64), cols(2944 + g * 64), cols(3200 + g * 64)], axis=2))
    w["wKV"] = np.ascontiguousarray(np.stack(kv, 0))
    k1 = w_cmp_k1[0].reshape(32, 64, 128).transpose(1, 0, 2)
    v1 = w_cmp_v1[0].reshape(32, 64, 128).transpose(1, 0, 2)
    w["w1kv"] = np.ascontiguousarray(np.concatenate([k1, v1], 0))
    w["w2kv"] = np.ascontiguousarray(np.stack([w_cmp_k2[0], w_cmp_k2[0], w_cmp_v2[0]], 1))
    w["peT"] = np.ascontiguousarray(np.concatenate([cmp_pos_k[0].T, cmp_pos_v[0].T], 0))
    w["wO"] = np.ascontiguousarray(w_out[0].reshape(8, 128, D).transpose(1, 0, 2))
    w["gpre"] = np.ascontiguousarray(pre_norm_g[0].reshape(8, 128).T)
    w["gpost"] = np.ascontiguousarray(np.broadcast_to(post_norm_g[0][None, :], (128, D)))
    return {k: np.asarray(v, np.float32) for k, v in w.items()}


def emit_attention(B, T, groups, single_acc_key=None, filler=None, nS=2, nPT=3, prev=None):
    psS, PT = T.psS, T.PT
    flat = []
    for gi, g in enumerate(groups):
        for ti, t in enumerate(g["tiles"]):
            flat.append((gi, ti, t))
    n = len(flat)
    act_done = [None] * n
    rdy = [None] * n
    s_done = [None] * n
    pv_done = [None] * n
    epi_done = [None] * len(groups)
    last_pv_of_group = [None] * len(groups)

    def emit_S(i):
        gi, ti, t = flat[i]
        w = [act_done[i - nS] if i >= nS else (prev[0] if prev else None)]
        ew = t.get("extra_wait")
        if ew is not None:
            w += list(ew) if isinstance(ew, (list, tuple)) and not (len(ew) == 2 and not isinstance(ew[0], tuple)) else [ew]
        m = len(t["smm"])
        for j, (o, l, r) in enumerate(t["smm"]):
            tk = B.op("tensor", lambda e, o=o, l=l, r=r: e.matmul(o, l, r, start=True, stop=True),
                      waits=w if j == 0 else (), sig=(j == m - 1))
        s_done[i] = tk

    def emit_act(i):
        gi, ti, t = flat[i]
        in_, o, bias = t["act"]
        w = [s_done[i], pv_done[i - nPT] if i >= nPT else (prev[1] if prev else None)]
        if bias is None:
            act_done[i] = B.op("scalar", lambda e, in_=in_, o=o: e.activation(out=o, in_=in_, func=AF.Exp), waits=w, sig=True)
        else:
            act_done[i] = B.op("scalar", lambda e, in_=in_, o=o, bias=bias: e.activation(out=o, in_=in_, func=AF.Exp, bias=bias),
                               waits=w, sig=True)
        rdy[i] = act_done[i]
        if t.get("mask") is not None:
            ap, mk = t["mask"]
            rdy[i] = B.op("vector", lambda e, ap=ap, mk=mk: e.tensor_tensor(out=ap, in0=ap, in1=mk, op=ALU.mult),
                          waits=[act_done[i]], sig=True)

    def emit_PV(i):
        gi, ti, t = flat[i]
        if filler is not None:
            filler(B)
        w = [rdy[i]]
        if ti == 0 and gi >= 2:
            w.append(epi_done[gi - 2])
        if ti == 0 and gi < 2 and prev:
            w.append(prev[2])
        if single_acc_key and t.get(single_acc_key) and gi >= 1:
            w.append(epi_done[gi - 1])
        m = len(t["pv"])
        for j, (o, l, r, st) in enumerate(t["pv"]):
            tk = B.op("tensor", lambda e, o=o, l=l, r=r, st=st: e.matmul(o, l, r, start=st, stop=False),
                      waits=w if j == 0 else (), sig=(j == m - 1))
        pv_done[i] = tk
        if ti == len(groups[gi]["tiles"]) - 1:
            last_pv_of_group[gi] = tk
            if gi >= 1 and groups[gi - 1].get("post"):
                groups[gi - 1]["post"](B)
            epi_done[gi] = groups[gi]["epi"](B, tk)

    dD = nS - 1
    for i in range(n + dD):
        if i < n:
            emit_S(i)
            emit_act(i)
        if i >= dD:
            emit_PV(i - dD)
    if groups and groups[-1].get("post"):
        groups[-1]["post"](B)
    return (act_done[-1], pv_done[-1], epi_done[-1])


def _build_rest(nc, L, debug, dbg, stop):
    import types
    T = types.SimpleNamespace(**{k: v for k, v in L.items() if k not in ("es", "B")})
    hT, OT, QA, KA, KW, VA, VW, PT, Ost = T.hT, T.OT, T.QA, T.KA, T.KW, T.VA, T.VW, T.PT, T.Ost
    psS, psO, psO2, psM, psT = T.psS, T.psO, T.psO2, T.psM, T.psT
    ident, btab, tri, cmask, rden = T.ident, T.btab, T.tri, T.cmask, T.rden
    dram = T.dram
    banks4 = [psS[0], psS[1], psO[0], psO[1]]
    SB4 = [psS[0], psS[1], psO2[0], psO2[1]]

    def v3(ap2d, a):
        return ap2d.rearrange("p (a b) -> p a b", a=a)

    def gating_block(name, p, wz, zs):
        B = Blk(nc, name)
        s1 = B.newsem()
        t_w = B.dma("gpsimd", wz[:], dram["wZ"][p], s1)
        z_free = [None, None]
        ps_free = [None, None]
        for qt in range(8):
            b = qt % 2
            for c in range(8):
                t_mm = B.op("tensor", lambda e, b=b, c=c, qt=qt: e.matmul(
                    psS[b][:], wz[:, c, :], hT[:, c, qt * 512:(qt + 1) * 512], start=(c == 0), stop=(c == 7)),
                    waits=[t_w, ps_free[b]] if c == 0 else (), sig=(c == 7))
            t_s = B.op("scalar", lambda e, b=b: e.activation(out=zs[b][:], in_=psS[b][:], func=AF.Silu),
                       waits=[t_mm, z_free[b]], sig=True)
            ps_free[b] = t_s
            z_free[b] = B.op("vector", lambda e, b=b, qt=qt: e.tensor_tensor(
                out=OT[:, p, qt * 512:(qt + 1) * 512], in0=OT[:, p, qt * 512:(qt + 1) * 512], in1=zs[b][:], op=ALU.mult),
                waits=[t_s], sig=True)
        B.run()

    with ExitStack() as esM:
        def sbm(name, shape, dt):
            return esM.enter_context(nc.sbuf_tensor("m_" + name, list(shape), dt))
        QA2 = sbm("QA2", [128, S], BF16)
        Wq2 = [sbm(f"Wqk{i}", [128, 2, 8, 128], BF16) for i in range(2)]
        Wv2 = [sbm(f"Wv{i}", [128, 8, 64], BF16) for i in range(2)]
        maskpad = sbm("maskpad", [128, NT, 16], BF16)
        scg = sbm("scg", [128, NT, 16], F32)
        m8 = sbm("m8", [128, NT, 8], F32)
        tmpf = sbm("tmpf", [128, NT, 16], F32)
        kmf = sbm("kmf", [64, 16], F32)
        kmb = sbm("kmb", [64, 16], BF16)
        wz = sbm("wz", [128, 8, 128], BF16)
        zs = [scg[:].rearrange("p a b -> p (a b)").bitcast(BF16)[:, 0:512], tmpf[:].rearrange("p a b -> p (a b)").bitcast(BF16)[:, 0:512]]
        QS = [QA, QA2]
        KS_ = [KA, KW]
        VS_ = [VA, VW]

        B = Blk(nc, "bM0")
        t1 = B.op("vector", lambda e: e.memset(maskpad[:], 0.0), sig=True)
        B.op("vector", lambda e: e.memset(QA2[64:128, :], 0.0))
        t1b = B.op("vector", lambda e: e.memset(KW[64:128, :], 0.0), sig=True)
        t2 = B.dma("gpsimd", KA[64:80, :], dram["c_E16"][:], B.newsem())
        t3 = B.dma("gpsimd", KW[64:80, :], dram["c_E16"][:], B.newsem(), waits=[t1b])
        B.op("sync", lambda e: e.nop(), waits=[t1, t2, t3])
        B.run()

        nheads = 8 if stop is None else int(stop.get("moba_heads", 8)) if isinstance(stop, dict) else 8

        def proj_ops(B, h, t_w, banks, bank_free):
            st = h % 2
            Wt, Wv, Qd, Kd, Vd = Wq2[st], Wv2[st], QS[st], KS_[st], VS_[st]
            ops = []
            state = {"bi": 0, "first": True}

            def mm_qk(which, qt, c):
                def f():
                    bi = state["bi"]
                    bk = banks[bi % len(banks)]
                    w = ()
                    if c == 0:
                        w = [bank_free[bi % len(banks)]] + ([t_w] if state["first"] else [])
                        state["first"] = False
                    tk = B.op("tensor", lambda e: e.matmul(bk[:, :], Wt[:, which, c, :], hT[:, c, qt * 512:(qt + 1) * 512],
                                                          start=(c == 0), stop=(c == 7)), waits=w, sig=(c == 7))
                    if c == 7:
                        if which == 0:
                            t_e = B.op("vector", lambda e: e.tensor_scalar(out=Qd[0:64, qt * 512:(qt + 1) * 512], in0=bk[0:64, :],
                                                                         scalar1=0.125, scalar2=None, op0=ALU.mult), waits=[tk], sig=True)
                        else:
                            t_e = B.op("vector", lambda e: e.tensor_copy(out=Kd[0:64, qt * 512:(qt + 1) * 512], in_=bk[0:64, :]),
                                       waits=[tk], sig=True)
                        bank_free[bi % len(banks)] = t_e
                        state["last_ev"] = t_e
                        state["bi"] += 1
                return f

            def mm_v(tg, j, c):
                def f():
                    bi = state["bi"]
                    bk = banks[bi % len(banks)]
                    tt = tg * 4 + j
                    w = [bank_free[bi % len(banks)]] if (c == 0 and j == 0) else ()
                    tk = B.op("tensor", lambda e: e.matmul(bk[:, j * 64:(j + 1) * 64], hT[:, c, tt * 128:(tt + 1) * 128], Wv[:, c, :],
                                                          start=(c == 0), stop=(c == 7)), waits=w, sig=(c == 7 and j == 3))
                    if c == 7 and j == 3:
                        t_e = B.op("vector", lambda e: e.tensor_copy(out=Vd[:, tg * 4:(tg + 1) * 4, 0:64], in_=v3(bk[:, 0:256], 4)),
                                   waits=[tk], sig=True)
                        bank_free[bi % len(banks)] = t_e
                        state["last_ev"] = t_e
                        state["bi"] += 1
                return f

            qk_groups = [[(mm_qk(which, qt, c), 1.0) for c in range(8)] for qt in range(8) for which in (0, 1)]
            v_groups = [[(mm_v(tg, j, c), 0.15) for j in range(4) for c in range(8)] for tg in range(8)]
            gi = 0
            for k in range(8):
                ops += qk_groups[2 * k]
                ops += v_groups[k]
                ops += qk_groups[2 * k + 1]
            return ops, state

        def load_head_consts(B, h):
            st = h % 2
            sw_ = B.newsem()
            for which in range(2):
                for half in range(2):
                    B.dma("gpsimd", Wq2[st][:, which, :, half * 64:(half + 1) * 64], dram["wA"][h][:, which], sw_)
            t_w = B.dma("gpsimd", Wv2[st][:], dram["wA"][h][:, 2], sw_)
            t_qr = B.dma("gpsimd", QS[st][80:82, :], dram["c_qrow"][h], B.newsem())
            t_kr = B.dma("gpsimd", KS_[st][80:82, :], dram["c_krow"][h], B.newsem())
            return t_w, t_qr, t_kr

        def selection_block(h, extra_waits=()):
            st = h % 2
            Qd, Kd = QS[st], KS_[st]
            B = Blk(nc, f"bS{h}")
            t_km = B.op("vector", lambda e: e.tensor_reduce(out=kmf[:], in_=Kd[0:64, :].rearrange("p (j k) -> p j k", k=256),
                                                          axis=AX.X, op=ALU.add), waits=list(extra_waits), sig=True)
            t_kb = B.op("vector", lambda e: e.tensor_scalar(out=kmb[:], in0=kmf[:], scalar1=1.0 / 256, scalar2=None, op0=ALU.mult),
                        waits=[t_km], sig=True)
            for tt in range(NT):
                t_g = B.op("tensor", lambda e, tt=tt: e.matmul(psM[:, tt * 16:(tt + 1) * 16], Qd[0:64, tt * 128:(tt + 1) * 128], kmb[:],
                                                              start=True, stop=True),
                           waits=[t_kb] if tt == 0 else (), sig=(tt == NT - 1))
            t_sc = B.op("vector", lambda e: e.tensor_tensor(out=scg[:], in0=v3(psM[:], NT), in1=T.addm_moba[:], op=ALU.add),
                        waits=[t_g], sig=True)
            for tt in range(NT):
                t_m8 = B.op("vector", lambda e, tt=tt: e.max(out=m8[:, tt, :], in_=scg[:, tt, :]),
                            waits=[t_sc] if tt == 0 else (), sig=(tt == NT - 1))
            t_c = B.op("vector", lambda e: e.tensor_tensor(out=tmpf[:], in0=scg[:], in1=m8[:, :, 3:4].to_broadcast([128, NT, 16]),
                                                         op=ALU.is_lt), waits=[t_m8], sig=True)
            t_mp = B.op("vector", lambda e: e.tensor_scalar(out=maskpad[:], in0=tmpf[:], scalar1=NEGM, scalar2=None,
                                                          op0=ALU.mult), waits=[t_c], sig=True)
            tb_free = [None, None]
            for g8 in range(8):
                bk = psS[g8 % 2]
                for j in range(4):
                    tt = g8 * 4 + j
                    t_tr = B.op("tensor", lambda e, bk=bk, j=j, tt=tt: e.matmul(
                        bk[64:80, j * 128:(j + 1) * 128], maskpad[:, tt, :], ident[:], start=True, stop=True),
                        waits=[t_mp, tb_free[g8 % 2]] if j == 0 else (), sig=(j == 3))
                tb_free[g8 % 2] = B.op("vector", lambda e, bk=bk, g8=g8: e.tensor_copy(
                    out=Qd[64:80, g8 * 512:(g8 + 1) * 512], in_=bk[64:80, :]), waits=[t_tr], sig=True)
            B.run()

        if nheads > 0:
            B = Blk(nc, "bP0")
            t_w, t_qr, t_kr = load_head_consts(B, 0)
            bfree = [None] * 4
            ops, stt = proj_ops(B, 0, t_w, banks4, bfree)
            for f, _c in ops:
                f()
            B.op("sync", lambda e: e.nop(), waits=[t_qr, t_kr, stt["last_ev"]])
            B.run()
            selection_block(0)

        for h in range(nheads):
            pair, off = h // 2, (h % 2) * 64
            st = h % 2
            Qd, Kd, Vd = QS[st], KS_[st], VS_[st]
            B = Blk(nc, f"bT{h}")
            nxt = h + 1 < nheads
            if nxt:
                t_w, t_qr, t_kr = load_head_consts(B, h + 1)
                pbfree = [None, None]
                pops, pstate = proj_ops(B, h + 1, t_w, [psO2[1], psM], pbfree)
            SBk = [psS[0], psS[1], psO2[0]] if nxt else SB4
            groups = []
            for qt in range(8):
                aset = qt % 2
                acc = v3(psO[aset][:], 4)
                tiles = []
                first = True
                for dl in range(4 * qt + 3, -1, -1):
                    smm, pv = [], []
                    qs_valid = [qs for qs in range(4) if 4 * qt + qs - dl >= 0]
                    for qs in qs_valid:
                        kt = 4 * qt + qs - dl
                        tq = 4 * qt + qs
                        smm.append(("S", qs, Kd[0:82, kt * 128:(kt + 1) * 128], Qd[0:82, tq * 128:(tq + 1) * 128]))
                        pv.append((acc[:, qs, 0:65], qs, Vd[:, kt, :], first))
                        first = False
                    tiles.append(dict(qs=qs_valid, smm=smm, pv=pv, bias=float(-_slopes()[h] * 128.0 * dl), mask=(dl == 0)))
                groups.append(dict(qt=qt, aset=aset, tiles=tiles))
            gi_tile = 0
            ntile = sum(len(g["tiles"]) for g in groups)
            for g in groups:
                for t in g["tiles"]:
                    sb_ = SBk[gi_tile % len(SBk)]
                    pt_ = PT[gi_tile % 4]
                    q0, q1 = t["qs"][0], t["qs"][-1] + 1
                    t["smm"] = [(sb_[:, qs * 128:(qs + 1) * 128], l, r) for (_, qs, l, r) in t["smm"]]
                    t["act"] = (sb_[:, q0 * 128:q1 * 128], pt_[:, q0 * 128:q1 * 128], t["bias"])
                    t["pv"] = [(o, pt_[:, qs * 128:(qs + 1) * 128], r, st_) for (o, qs, r, st_) in t["pv"]]
                    if t["mask"]:
                        t["mask"] = (v3(pt_[:], 4), tri[:, 0:1, :].to_broadcast([128, 4, 128]))
                    else:
                        t["mask"] = None
                    gi_tile += 1

            def mk_epi(qt, aset):
                acc = v3(psO[aset][:], 4)

                def epi(B, tk):
                    rd = rden[:, aset * 4:aset * 4 + 4].unsqueeze(2)
                    t_r = B.op("vector", lambda e: e.reciprocal(out=rd, in_=acc[:, :, 64:65]),
                               waits=[tk, st_ost[aset]], sig=True)
                    t_n = B.op("vector", lambda e: e.tensor_tensor(out=Ost[aset][:, :, off:off + 64], in0=acc[:, :, 0:64],
                                                                 in1=rd.to_broadcast([128, 4, 64]), op=ALU.mult),
                               waits=[t_r], sig=True)
                    epi_tok[qt] = t_n
                    return t_n
                return epi

            def mk_post(qt, aset):
                def post(B):
                    for qs in range(4):
                        t_tr = B.op("tensor", lambda e, qs=qs: e.matmul(psT[:, qs * 128:(qs + 1) * 128], Ost[aset][:, qs, :], ident[:],
                                                                       start=True, stop=True),
                                    waits=[epi_tok[qt], post_state["psT_free"]] if qs == 0 else (), sig=(qs == 3))
                    st_ost[aset] = t_tr
                    post_state["psT_free"] = B.op("vector", lambda e: e.tensor_copy(
                        out=OT[off:off + 64, pair, qt * 512:(qt + 1) * 512], in_=psT[off:off + 64, :]), waits=[t_tr], sig=True)
                return post

            epi_tok = {}
            st_ost = [None, None]
            post_state = {"psT_free": None}
            for g in groups:
                g["epi"] = mk_epi(g["qt"], g["aset"])
                g["post"] = mk_post(g["qt"], g["aset"])
            if nxt:
                budget = sum(c_ for _f, c_ in pops) / max(1, ntile - 8)
                pq = list(pops)
                fstate = {"acc": 0.0}

                def filler(B):
                    fstate["acc"] += budget
                    while pq and fstate["acc"] > 0:
                        f, c_ = pq.pop(0)
                        f()
                        fstate["acc"] -= c_
                emit_attention(B, T, groups, filler=filler, nS=3, nPT=4)
                while pq:
                    pq.pop(0)[0]()
                B.op("sync", lambda e: e.nop(), waits=[t_qr, t_kr, pstate["last_ev"]])
            else:
                emit_attention(B, T, groups, filler=_mk_filler(lambda: psM[:, 0:128], ident, True), nS=4, nPT=4)
            B.run()
            if nxt:
                selection_block(h + 1)
            if h % 2 == 1:
                gating_block(f"bZ{pair}", pair, wz, zs)

    only = stop.get("only") if isinstance(stop, dict) else None
    if only != "moba":
        _build_nsa(nc, T, gating_block, v3, stop)

    if debug and "OT" in debug:
        with ExitStack() as esD:
            tmp = esD.enter_context(nc.sbuf_tensor("dbgtmp2", [128, 8, 512], F32))
            B = Blk(nc, "bD2")
            s1 = B.newsem("sync")
            tk = None
            for q in range(8):
                t1 = B.op("vector", lambda e, q=q: e.tensor_copy(out=tmp[:], in_=OT[:, :, q * 512:(q + 1) * 512]),
                          waits=[tk], sig=True)
                tk = B.dma("sync", dbg["OT"][:, :, q * 512:(q + 1) * 512], tmp[:], s1, waits=[t1])
            B.op("sync", lambda e: e.nop(), waits=[tk])
            B.run()


def _build_nsa(nc, T, gating_block, v3, stop):
    hT, OT, QA, KA, KW, VA, VW, PT, Ost = T.hT, T.OT, T.QA, T.KA, T.KW, T.VA, T.VW, T.PT, T.Ost
    psS, psO, psO2, psM, psT = T.psS, T.psO, T.psO2, T.psM, T.psT
    ident, btab, tri, cmask, rden, gates = T.ident, T.btab, T.tri, T.cmask, T.rden, T.gates
    dram = T.dram
    banks4 = [psS[0], psS[1], psO[0], psO[1]]
    SB4 = [psS[0], psS[1], psO2[0], psO2[1]]
    SB3 = [psS[0], psS[1], psO2[1]]
    ngroups = int(stop.get("nsa_groups", 2)) if isinstance(stop, dict) else 2
    stage = int(stop.get("nsa_stage", 9)) if isinstance(stop, dict) else 9
    with ExitStack() as esN:
        def sbn(name, shape, dt):
            return esN.enter_context(nc.sbuf_tensor("n_" + name, list(shape), dt))
        kcT = sbn("kcT", [128, 256], BF16)
        rcmp = sbn("rcmp", [128, 2, 129], BF16)
        scrN = sbn("scrN", [128, 2048], BF16)
        WqD = [scrN[:, 0:1024].rearrange("p (c n) -> p c n", c=8), scrN[:, 1024:2048].rearrange("p (c n) -> p c n", c=8)]
        wz = WqD[0]
        m16 = sbn("m16", [128, NT, 16], F32)
        wk4 = sbn("wk4", [128, 4, 64], F32)
        maskp = [sbn(f"maskp{i}", [128, 4, 128], BF16) for i in range(2)]
        zs = [maskp[i][:].rearrange("p a b -> p (a b)") for i in range(2)]
        hs = [scrN[:, 1024:1280], scrN[:, 1280:1536]]
        d3 = [sbn(f"d3{i}", [128, 4, 3], F32) for i in range(2)]
        tA = sbn("tA", [128, 4, 64], F32)
        tB = sbn("tB", [128, 4, 64], F32)
        bcol = sbn("bcol", [128, 2], F32)

        with ExitStack() as es1:
            Wg = es1.enter_context(nc.sbuf_tensor("n_Wg", [128, 8, 24], BF16))
            B = Blk(nc, "bG")
            s1, s2, s3 = B.newsem(), B.newsem(), B.newsem()
            t_w = B.dma("gpsimd", Wg[:], dram["wG"][:], s1)
            t_e = B.dma("gpsimd", KA[64:128, :], dram["c_E64"][:], s2)
            t_rc = B.dma("gpsimd", rcmp[:, :, 64:129], dram["c_rc"][:], s3)
            t_z0 = B.op("vector", lambda e: e.memset(maskp[0][:], 0.0))
            t_z1 = B.op("vector", lambda e: e.memset(maskp[1][:], 0.0))
            t_z4 = B.op("vector", lambda e: e.memset(kcT[:], 0.0), sig=True)
            gT = es1.enter_context(nc.sbuf_tensor("n_gT", [24, S], BF16))
            gfree = [None, None]
            for qt in range(8):
                bk = psS[qt % 2]
                for c in range(8):
                    t_mm = B.op("tensor", lambda e, bk=bk, c=c, qt=qt: e.matmul(
                        bk[0:24, :], Wg[:, c, :], hT[:, c, qt * 512:(qt + 1) * 512], start=(c == 0), stop=(c == 7)),
                        waits=[t_w, gfree[qt % 2]] if c == 0 else (), sig=(c == 7))
                gfree[qt % 2] = B.op("scalar", lambda e, bk=bk, qt=qt: e.activation(
                    out=gT[:, qt * 512:(qt + 1) * 512], in_=bk[0:24, :], func=AF.Sigmoid), waits=[t_mm], sig=True)
            for half in range(2):
                bk = (psM, psT)[half]
                for j in range(16):
                    tt = half * 16 + j
                    t_mm = B.op("tensor", lambda e, bk=bk, j=j, tt=tt: e.matmul(
                        bk[:, j * 24:(j + 1) * 24], gT[:, tt * 128:(tt + 1) * 128], ident[0:24, 0:24], start=True, stop=True),
                        waits=[gfree[0], gfree[1]] if j == 0 else (), sig=(j == 15))
                B.op("scalar", lambda e, bk=bk, half=half: e.copy(
                    out=gates[:, half * 16:(half + 1) * 16, :], in_=bk[:, 0:384].rearrange("p (a b) -> p a b", a=16)),
                    waits=[t_mm], sig=True)
            B.op("sync", lambda e: e.nop(), waits=[t_e, t_rc, t_z4, B.last("scalar")])
            B.run()

        for g in range(ngroups if stage >= 2 else 0):
            with ExitStack() as es1:
                Wkv = es1.enter_context(nc.sbuf_tensor(f"n_Wkv{g}", [128, 8, 384], BF16))
                w1kv = es1.enter_context(nc.sbuf_tensor(f"n_w1kv{g}", [128, 32, 128], BF16))
                w2kv = es1.enter_context(nc.sbuf_tensor(f"n_w2kv{g}", [128, 3, 64], BF16))
                peT = es1.enter_context(nc.sbuf_tensor(f"n_peT{g}", [128, 32], BF16))
                B = Blk(nc, f"bK{g}")
                s1, s2, s3, s4 = B.newsem(), B.newsem(), B.newsem(), B.newsem()
                B.dma("gpsimd", Wkv[:, 0:4, :], dram["wKV"][g][:, 0:4, :], s1)
                t_w = B.dma("gpsimd", Wkv[:, 4:8, :], dram["wKV"][g][:, 4:8, :], s1)
                import os
                skip = os.environ.get("BK_SKIP", "")
                for i4 in range(0 if "w" in skip else 3):
                    B.dma("gpsimd", w1kv[:, i4 * 8:(i4 + 1) * 8, :], dram["w1kv"][:, i4 * 8:(i4 + 1) * 8, :], s2)
                t_w1a = B.dma("gpsimd", w1kv[:, 24:32, :], dram["w1kv"][:, 24:32, :], s2)
                t_w1b = t_w1a
                t_w2 = B.dma("gpsimd", w2kv[:], dram["w2kv"][:], s3)
                t_pe = B.dma("gpsimd", peT[:], dram["peT"][:], s4)
                bank_free = [None] * 4
                bi = 0
                evs = {"scalar": None, "vector": None}
                specs = [(0, 128, QA, 128), (128, 64, KA, 64), (192, 64, KW, 64)]
                if "a" in skip:
                    specs = specs[0:1]
                if "b" in skip:
                    specs = specs[1:2]
                if "c" in skip:
                    specs = specs[2:3]
                for qt in range(0 if "q" in skip else 8):
                    for si, (c0, m, dst, rows) in enumerate(specs):
                        bk = banks4[bi % 4]
                        for c in range(8):
                            t_mm = B.op("tensor", lambda e, bk=bk, c=c, qt=qt, c0=c0, m=m, rows=rows: e.matmul(
                                bk[0:rows, :], Wkv[:, c, c0:c0 + m], hT[:, c, qt * 512:(qt + 1) * 512], start=(c == 0), stop=(c == 7)),
                                waits=[t_w, bank_free[bi % 4]] if c == 0 else (), sig=(c == 7))
                        eng = "scalar" if si != 1 else "vector"
                        if eng == "scalar":
                            t_e = B.op("scalar", lambda e, bk=bk, dst=dst, rows=rows, qt=qt: e.copy(
                                out=dst[0:rows, qt * 512:(qt + 1) * 512], in_=bk[0:rows, :]), waits=[t_mm], sig=True)
                        else:
                            t_e = B.op("vector", lambda e, bk=bk, dst=dst, rows=rows, qt=qt: e.tensor_copy(
                                out=dst[0:rows, qt * 512:(qt + 1) * 512], in_=bk[0:rows, :]), waits=[t_mm], sig=True)
                        evs[eng] = t_e
                        bank_free[bi % 4] = t_e
                        bi += 1
                for tg in range(0 if "v" in skip else 8):
                    bk = banks4[bi % 4]
                    for j in range(4):
                        tt = tg * 4 + j
                        for c in range(8):
                            t_mm = B.op("tensor", lambda e, bk=bk, j=j, c=c, tt=tt: e.matmul(
                                bk[:, j * 128:(j + 1) * 128], hT[:, c, tt * 128:(tt + 1) * 128], Wkv[:, c, 256:384], start=(c == 0), stop=(c == 7)),
                                waits=[bank_free[bi % 4]] if (c == 0 and j == 0) else (), sig=(c == 7 and j == 3))
                    B.op("scalar", lambda e, bk=bk, tg=tg: e.copy(
                        out=VA[:, tg * 4:(tg + 1) * 4, 0:64], in_=v3(bk[:], 4)[:, :, 0:64]), waits=[t_mm])
                    t_e2 = B.op("scalar", lambda e, bk=bk, tg=tg: e.copy(
                        out=VW[:, tg * 4:(tg + 1) * 4, 0:64], in_=v3(bk[:], 4)[:, :, 64:128]), waits=[t_mm], sig=True)
                    bank_free[bi % 4] = t_e2
                    bi += 1
                B.op("vector", lambda e: e.memset(hs[0][:, 255:256], 0.0))
                t_hz = B.op("vector", lambda e: e.memset(hs[1][:, 255:256], 0.0), sig=True)
                kvv = QA[:, :].rearrange("p (c s) -> p c s", s=16)
                import os
                sub = int(os.environ.get("BK_SUB", "9"))
                for kv in range(2 if sub >= 2 else 0):
                    r0 = kv * 64
                    bkH = banks4[bi % 4]
                    bi += 1
                    for l in range(32):
                        a, b_ = l // 16, l % 16
                        t_h = B.op("tensor", lambda e, bkH=bkH, l=l, a=a, b_=b_, r0=r0: e.matmul(
                            bkH[:, 0:255], w1kv[r0:r0 + 64, l, :], kvv[r0:r0 + 64, a:a + 255, b_], start=(l == 0), stop=(l == 31)),
                            waits=[t_w1a, t_w1b, evs["scalar"], evs["vector"], bank_free[(bi - 1) % 4]] if l == 0 else (), sig=(l == 31))
                    if sub == 2:
                        continue
                    if kv == 1 and sub == 3:
                        continue
                    for l in range(32):
                        t_b = B.op("tensor", lambda e, bkH=bkH, l=l, r0=r0: e.matmul(
                            bkH[:, 256:257], w1kv[r0:r0 + 64, l, :], peT[r0:r0 + 64, l:l + 1], start=(l == 0), stop=(l == 31)),
                            waits=[t_pe] if l == 0 else (), sig=(l == 31))
                    t_bc = B.op("vector", lambda e, bkH=bkH, kv=kv: e.tensor_copy(out=bcol[:, kv:kv + 1], in_=bkH[:, 256:257]),
                                waits=[t_b], sig=True)
                    t_s = B.op("scalar", lambda e, bkH=bkH, kv=kv: e.activation(
                        out=hs[kv][:, 0:255], in_=bkH[:, 0:255], func=AF.Silu, bias=bcol[:, kv:kv + 1]), waits=[t_bc, t_h, t_hz], sig=True)
                    bank_free[(bi - 1) % 4] = t_s
                    if kv == 0:
                        t_k = B.op("tensor", lambda e: e.matmul(psM[:, 0:255], w2kv[:, 0:2, :].rearrange("p a b -> p (a b)"), hs[0][:, 0:255],
                                                                start=True, stop=True), waits=[t_s, t_w2], sig=True)
                        B.op("vector", lambda e: e.tensor_copy(out=kcT[:, 0:255], in_=psM[:, 0:255]), waits=[t_k], sig=True)
                    else:
                        for ct in range(2):
                            t_v = B.op("tensor", lambda e, ct=ct: e.matmul(psT[:, ct * 64:(ct + 1) * 64], hs[1][:, ct * 128:(ct + 1) * 128],
                                                                          w2kv[:, 2, :], start=True, stop=True),
                                       waits=[t_s, t_w2] if ct == 0 else (), sig=(ct == 1))
                        B.op("vector", lambda e: e.tensor_copy(out=rcmp[:, :, 0:64], in_=v3(psT[:, 0:128], 2)), waits=[t_v], sig=True)
                B.run()

            with ExitStack() as es2:
                imp = es2.enter_context(nc.sbuf_tensor(f"n_imp{g}", [128, NT, 64], F32))
                if stage >= 3:
                    B = Blk(nc, f"bC{g}")
                    QB = [(QA, 0), (KW, 64)]
                    t_w0 = _load_wq(B, T, WqD[0], g * 4)
                    bfree4 = [None] * 4
                    ops0, st0 = _proj_q_ops(B, T, WqD[0], t_w0, QA, 0, banks4, bfree4, evac="scalar")
                    for f, _c in ops0:
                        f()
                    q_ready = st0["last_ev"]
                    prev = None
                    pbfree = [None, None]
                    wq_last = [st0["last_mm"], None]
                    for hh in range(4):
                        hB = g * 4 + hh
                        Qb, r0 = QB[hh % 2]
                        pq = None
                        if hh < 3:
                            Qn, rn = QB[(hh + 1) % 2]
                            t_wn = _load_wq(B, T, WqD[(hh + 1) % 2], hB + 1, waits=[wq_last[(hh + 1) % 2]])
                            pq, stn = _proj_q_ops(B, T, WqD[(hh + 1) % 2], t_wn, Qn, rn, [psM, psT], pbfree, evac="vector")
                        groups = []
                        gi_tile = 0
                        for qt in range(8):
                            aset = qt % 2
                            acc = v3(psO[aset][:], 4)
                            tiles = []
                            first = True
                            for ct in range(qt // 4 + 1):
                                sb_ = SB4[gi_tile % 4]
                                pt_ = PT[gi_tile % 4]
                                pv = []
                                for qs in range(4):
                                    pv.append((acc[:, qs, 0:65], pt_[:, qs * 128:(qs + 1) * 128], rcmp[:, ct, 64:129], first))
                                    first = False
                                tiles.append(dict(
                                    smm=[(sb_[:], kcT[r0:r0 + 64, ct * 128:(ct + 1) * 128], Qb[r0:r0 + 64, qt * 512:(qt + 1) * 512])],
                                    act=(sb_[:], pt_[:], None),
                                    mask=(pt_[:], cmask[:, qt % 4, :]) if ct == qt // 4 else None,
                                    pv=pv))
                                gi_tile += 1

                            def mk_epi(qt, aset, acc, hh):
                                def epi(B, tk):
                                    rd = rden[:, aset * 4:aset * 4 + 4].unsqueeze(2)
                                    t1 = B.op("vector", lambda e: e.tensor_scalar(out=rd, in0=acc[:, :, 0:1], scalar1=1e-30, scalar2=None,
                                                                                op0=ALU.max), waits=[tk], sig=True)
                                    t2 = B.op("vector", lambda e: e.reciprocal(out=rd, in_=rd), waits=[t1], sig=True)
                                    dst = imp[:, 4 * qt:4 * qt + 4, :]
                                    if hh == 0:
                                        t3 = B.op("vector", lambda e: e.tensor_tensor(out=dst, in0=acc[:, :, 1:65], in1=rd.to_broadcast([128, 4, 64]),
                                                                                    op=ALU.mult), waits=[t2], sig=True)
                                    else:
                                        t3 = B.op("vector", lambda e: e.tensor_tensor(out=tA[:], in0=acc[:, :, 1:65], in1=rd.to_broadcast([128, 4, 64]),
                                                                                    op=ALU.mult), waits=[t2], sig=True)
                                        B.op("vector", lambda e: e.tensor_tensor(out=dst, in0=dst, in1=tA[:], op=ALU.add), waits=[t3], sig=True)
                                    return t3
                                return epi
                            groups.append(dict(tiles=tiles, epi=mk_epi(qt, aset, acc, hh), post=None))
                        groups[0]["tiles"][0]["extra_wait"] = q_ready
                        if pq is not None:
                            fl = _budget_filler(pq, sum(c_ for _f, c_ in pq) / max(1, gi_tile - 3))
                        else:
                            fl = None
                        prev = emit_attention(B, T, groups, filler=fl, nS=4, nPT=4, prev=prev)
                        if pq is not None:
                            while pq:
                                pq.pop(0)[0]()
                            q_ready = stn["last_ev"]
                            wq_last[(hh + 1) % 2] = stn["last_mm"]
                    B.run()

                if stage < 4:
                    continue
                B = Blk(nc, f"bS{g}")
                B.op("vector", lambda e: e.memset(maskp[0][:], 0.0))
                B.op("vector", lambda e: e.memset(maskp[1][:], 0.0))
                t_a = B.op("vector", lambda e: e.tensor_tensor(out=imp[:], in0=imp[:], in1=T.addm_slc[:], op=ALU.add), sig=True)
                tk = None
                for t4 in range(0, NT, 4):
                    t1s = [B.op("vector", lambda e, tt=tt: e.max(out=m16[:, tt, 0:8], in_=imp[:, tt, :]), waits=[t_a], sig=True)
                           for tt in range(t4, t4 + 4)]
                    t2s = [B.op("vector", lambda e, tt=tt: e.match_replace(out=wk4[:, tt % 4, :], in_to_replace=m16[:, tt, 0:8],
                                                                        in_values=imp[:, tt, :], imm_value=-1e30),
                                waits=[t1s[tt - t4]], sig=True) for tt in range(t4, t4 + 4)]
                    for tt in range(t4, t4 + 4):
                        tk = B.op("vector", lambda e, tt=tt: e.max(out=m16[:, tt, 8:16], in_=wk4[:, tt % 4, :]), waits=[t2s[tt - t4]], sig=True)
                mp_free = [None, None]
                ps_free = [None, None]
                for c4 in range(8):
                    b = c4 % 2
                    t_m = B.op("vector", lambda e, c4=c4, b=b: e.tensor_tensor(
                        out=maskp[b][:, :, 64:128], in0=imp[:, 4 * c4:4 * c4 + 4, :],
                        in1=m16[:, 4 * c4:4 * c4 + 4, 15:16].to_broadcast([128, 4, 64]), op=ALU.is_lt), waits=[tk, mp_free[b]], sig=True)
                    for j in range(4):
                        t_tr = B.op("tensor", lambda e, b=b, j=j: e.matmul(psS[b][:, j * 128:(j + 1) * 128], maskp[b][:, j, :], ident[:],
                                                                         start=True, stop=True),
                                    waits=[t_m, ps_free[b]] if j == 0 else (), sig=(j == 3))
                    mp_free[b] = t_tr
                    ps_free[b] = B.op("scalar", lambda e, b=b, c4=c4: e.copy(out=KW[64:128, c4 * 512:(c4 + 1) * 512], in_=psS[b][64:128, :]),
                                      waits=[t_tr], sig=True)
                B.run()

            es3 = ExitStack()
            QB2 = es3.enter_context(nc.sbuf_tensor(f"n_QB2{g}", [128, S], BF16))
            QBUF = [QA, QB2]
            for hh in range(4 if stage >= 5 else 0):
                hB = g * 4 + hh
                pair, off = 4 + hB // 2, (hB % 2) * 64
                Qc = QBUF[hh % 2]
                B = Blk(nc, f"bN{hB}")
                if hh == 0:
                    t_w = _load_wq(B, T, WqD[0], hB)
                    bfree4 = [None] * 4
                    ops0, st0 = _proj_q_ops(B, T, WqD[0], t_w, Qc, 0, banks4, bfree4, evac="scalar")
                    for f, _c in ops0:
                        f()
                    q_ev = st0["last_ev"]
                    t_mc = B.op("vector", lambda e: e.tensor_copy(out=Qc[64:128, :], in_=KW[64:128, :]), sig=True)
                else:
                    q_ev, t_mc = None, None
                nxt_ops = None
                if hh < 3:
                    Qn = QBUF[(hh + 1) % 2]
                    t_wn = _load_wq(B, T, WqD[1], hB + 1)
                    t_mcn = B.op("gpsimd", lambda e, Qn=Qn: e.tensor_copy(out=Qn[64:128, :], in_=KW[64:128, :]), sig=True)
                    nfree = [None]
                    nxt_ops, nst = _proj_q_ops(B, T, WqD[1], t_wn, Qn, 0, [psT], nfree, evac="vector")
                groups = []
                gi_tile = 0
                for qt in range(8):
                    aset = qt % 2
                    accS = v3(psO[aset][:], 4)
                    accW = v3(psO2[0][:], 4)
                    accC = v3(psM[:], 4)
                    tiles = []
                    firstS, firstW, firstC = True, True, True
                    for dl in range(4 * qt + 3, -1, -1):
                        sb_ = SB3[gi_tile % 3]
                        pt_ = PT[gi_tile % 4]
                        qsv = [qs for qs in range(4) if 4 * qt + qs - dl >= 0]
                        smm, pv = [], []
                        for qs in qsv:
                            kt, tq = 4 * qt + qs - dl, 4 * qt + qs
                            smm.append((sb_[:, qs * 128:(qs + 1) * 128], KA[:, kt * 128:(kt + 1) * 128], Qc[:, tq * 128:(tq + 1) * 128]))
                            pv.append((accS[:, qs, 0:65], pt_[:, qs * 128:(qs + 1) * 128], VA[:, kt, :], firstS))
                            firstS = False
                        q0, q1 = qsv[0], qsv[-1] + 1
                        tiles.append(dict(smm=smm, act=(sb_[:, q0 * 128:q1 * 128], pt_[:, q0 * 128:q1 * 128], btab[:, hB, dl:dl + 1]),
                                          mask=(v3(pt_[:], 4), tri[:, 0:1, :].to_broadcast([128, 4, 128])) if dl == 0 else None, pv=pv))
                        gi_tile += 1
                    for dl in range(4, -1, -1):
                        qsv = [qs for qs in range(4) if 4 * qt + qs - dl >= 0]
                        if not qsv:
                            continue
                        sb_ = SB3[gi_tile % 3]
                        pt_ = PT[gi_tile % 4]
                        smm, pv = [], []
                        for qs in qsv:
                            kt, tq = 4 * qt + qs - dl, 4 * qt + qs
                            smm.append((sb_[:, qs * 128:(qs + 1) * 128], KW[0:64, kt * 128:(kt + 1) * 128], Qc[0:64, tq * 128:(tq + 1) * 128]))
                            pv.append((accW[:, qs, 0:65], pt_[:, qs * 128:(qs + 1) * 128], VW[:, kt, :], firstW))
                            firstW = False
                        q0, q1 = qsv[0], qsv[-1] + 1
                        mk = None
                        if dl == 0 or dl == 4:
                            mi = 0 if dl == 0 else 1
                            mk = (v3(pt_[:], 4)[:, q0:q1, :], tri[:, mi:mi + 1, :].to_broadcast([128, q1 - q0, 128]))
                        tiles.append(dict(smm=smm, act=(sb_[:, q0 * 128:q1 * 128], pt_[:, q0 * 128:q1 * 128], btab[:, hB, dl:dl + 1]),
                                          mask=mk, pv=pv, cmp_first=(dl == 0)))
                        gi_tile += 1
                    for ct in range(qt // 4 + 1):
                        sb_ = SB3[gi_tile % 3]
                        pt_ = PT[gi_tile % 4]
                        pv = []
                        for qs in range(4):
                            pv.append((accC[:, qs, 0:65], pt_[:, qs * 128:(qs + 1) * 128], rcmp[:, ct, 0:65], firstC))
                            firstC = False
                        tiles.append(dict(
                            smm=[(sb_[:], kcT[0:64, ct * 128:(ct + 1) * 128], Qc[0:64, qt * 512:(qt + 1) * 512])],
                            act=(sb_[:], pt_[:], None),
                            mask=(pt_[:], cmask[:, qt % 4, :]) if ct == qt // 4 else None,
                            pv=pv, cmp_first=(ct == 0)))
                        gi_tile += 1
                    groups.append(dict(qt=qt, aset=aset, tiles=tiles, accs=(accC, accS, accW)))

                epi_tok = {}
                st_ost = [None, None]
                post_state = {"psT_free": None}

                def mk_epi(qt, aset, accs):
                    def epi(B, tk):
                        dd = d3[aset]
                        for br in range(3):
                            t1 = B.op("vector", lambda e, br=br: e.tensor_scalar(out=dd[:, :, br:br + 1], in0=accs[br][:, :, 64:65], scalar1=1e-30,
                                                                               scalar2=None, op0=ALU.max), waits=[tk], sig=(br == 2))
                        t2 = B.op("vector", lambda e: e.reciprocal(out=dd[:], in_=dd[:]), waits=[t1], sig=True)
                        t3 = B.op("vector", lambda e: e.tensor_tensor(out=dd[:], in0=dd[:], in1=gates[:, 4 * qt:4 * qt + 4, 3 * hB:3 * hB + 3],
                                                                    op=ALU.mult), waits=[t2], sig=True)
                        t4 = B.op("vector", lambda e: e.tensor_tensor(out=tA[:], in0=accs[0][:, :, 0:64],
                                                                    in1=dd[:, :, 0:1].to_broadcast([128, 4, 64]), op=ALU.mult), waits=[t3], sig=True)
                        t5 = B.op("vector", lambda e: e.tensor_tensor(out=tB[:], in0=accs[1][:, :, 0:64],
                                                                    in1=dd[:, :, 1:2].to_broadcast([128, 4, 64]), op=ALU.mult), waits=[t3], sig=True)
                        t6 = B.op("vector", lambda e: e.tensor_tensor(out=tA[:], in0=tA[:], in1=tB[:], op=ALU.add), waits=[t4, t5], sig=True)
                        t7 = B.op("vector", lambda e: e.tensor_tensor(out=tB[:], in0=accs[2][:, :, 0:64],
                                                                    in1=dd[:, :, 2:3].to_broadcast([128, 4, 64]), op=ALU.mult), waits=[t6], sig=True)
                        t8 = B.op("vector", lambda e: e.tensor_tensor(out=Ost[aset][:, :, off:off + 64], in0=tA[:], in1=tB[:], op=ALU.add),
                                  waits=[t7, st_ost[aset]], sig=True)
                        epi_tok[qt] = t8
                        return t7
                    return epi

                def mk_post(qt, aset):
                    def post(B):
                        for qs in range(4):
                            t_tr = B.op("tensor", lambda e, qs=qs: e.matmul(psT[:, qs * 128:(qs + 1) * 128], Ost[aset][:, qs, :], ident[:],
                                                                           start=True, stop=True),
                                        waits=[epi_tok[qt], post_state["psT_free"]] if qs == 0 else (), sig=(qs == 3))
                        st_ost[aset] = t_tr
                        post_state["psT_free"] = B.op("vector", lambda e: e.tensor_copy(
                            out=OT[off:off + 64, pair, qt * 512:(qt + 1) * 512], in_=psT[off:off + 64, :]), waits=[t_tr], sig=True)
                    return post

                for gr in groups:
                    gr["epi"] = mk_epi(gr["qt"], gr["aset"], gr["accs"])
                    gr["post"] = mk_post(gr["qt"], gr["aset"])
                if hh == 0:
                    groups[0]["tiles"][0]["extra_wait"] = [q_ev, t_mc]
                junk = _mk_filler(lambda: psT[:, 0:128], ident, True, nf=NFILL2, wait_fn=lambda: post_state["psT_free"])
                if nxt_ops is not None:
                    ntl = sum(len(gr["tiles"]) for gr in groups)
                    every = max(1, (ntl - 10) // 8)
                    fst = {"n": 0}

                    def filler(B, nxt_ops=nxt_ops, nfree=nfree, nst=nst):
                        fst["n"] += 1
                        if nxt_ops and fst["n"] % every == 0:
                            nfree[0] = post_state["psT_free"]
                            for _ in range(8):
                                nxt_ops.pop(0)[0]()
                            post_state["psT_free"] = nst["last_ev"]
                        elif junk is not None:
                            junk(B)
                    emit_attention(B, T, groups, single_acc_key="cmp_first", nS=3, nPT=4, filler=filler)
                    while nxt_ops:
                        nfree[0] = post_state["psT_free"]
                        for _ in range(8):
                            nxt_ops.pop(0)[0]()
                        post_state["psT_free"] = nst["last_ev"]
                    B.op("sync", lambda e: e.nop(), waits=[nst["last_ev"], t_mcn])
                else:
                    emit_attention(B, T, groups, single_acc_key="cmp_first", nS=3, nPT=4, filler=junk)
                B.run()
                if hB % 2 == 1:
                    gating_block(f"bZ{pair}", pair, wz, zs)
            es3.close()


NFILL = [4]


NFILL2 = "NFILL2"


def _mk_filler(dst_fn, ident, start, m=128, nf=None, wait_fn=None):
    import os
    nfill = int(os.environ.get("NFILL", NFILL[0]))
    if nf == NFILL2:
        nfill = int(os.environ.get("NFILL2", 3))
    if nfill <= 0:
        return None

    def filler(B):
        dst = dst_fn()
        n = dst.shape[-1]
        for k in range(nfill):
            w = [wait_fn()] if (wait_fn is not None and k == 0) else ()
            B.op("tensor", lambda e: e.matmul(dst, ident[:, 0:m], ident[:, 0:n], start=start, stop=start), waits=w)
    return filler


def _load_wq(B, T, WqD_i, hB, waits=()):
    sw_ = B.newsem()
    B.dma("gpsimd", WqD_i[:, :, 0:64], T.dram["wQB"][hB], sw_, waits=list(waits))
    return B.dma("gpsimd", WqD_i[:, :, 64:128], T.dram["wQB"][hB], sw_, waits=list(waits))


def _proj_q_ops(B, T, Wq, t_w, dst, r0, banks, bank_free, evac="vector"):
    state = {"bi": 0, "first": True, "last_ev": None}

    def mk(qt, c):
        def f():
            bi = state["bi"]
            bk = banks[bi % len(banks)]
            w = ()
            if c == 0:
                w = [bank_free[bi % len(banks)]] + ([t_w] if state["first"] else [])
                state["first"] = False
            tk = B.op("tensor", lambda e: e.matmul(bk[:, :], Wq[:, c, :], T.hT[:, c, qt * 512:(qt + 1) * 512],
                                                  start=(c == 0), stop=(c == 7)), waits=w, sig=(c == 7))
            if c == 7:
                if evac == "vector":
                    t_e = B.op("vector", lambda e: e.tensor_scalar(out=dst[r0:r0 + 64, qt * 512:(qt + 1) * 512], in0=bk[r0:r0 + 64, :],
                                                                 scalar1=0.125, scalar2=None, op0=ALU.mult), waits=[tk], sig=True)
                else:
                    t_e = B.op("scalar", lambda e: e.activation(out=dst[r0:r0 + 64, qt * 512:(qt + 1) * 512], in_=bk[r0:r0 + 64, :],
                                                              func=AF.Copy, scale=0.125), waits=[tk], sig=True)
                bank_free[bi % len(banks)] = t_e
                state["last_ev"] = t_e
                state["last_mm"] = tk
                state["bi"] += 1
        return f
    return [(mk(qt, c), 1.0) for qt in range(8) for c in range(8)], state


def _budget_filler(pq, budget):
    st = {"acc": 0.0}

    def filler(B):
        st["acc"] += budget
        while pq and st["acc"] > 0:
            f, c_ = pq.pop(0)
            f()
            st["acc"] -= c_
    return filler


def _build_final(nc, hT, OT, psS, psO, dram, x, out, epsc, psO2=None, psM=None, psT=None):
    with ExitStack() as esF:
        def sbf(name, shape, dt):
            return esF.enter_context(nc.sbuf_tensor("f_" + name, list(shape), dt))
        wO = sbf("wO", [128, 8, D], BF16)
        gpost = sbf("gpost", [128, D], F32)
        xt = [sbf(f"xt{i}", [128, 2, D], F32) for i in range(3)]
        yt = [sbf(f"yt{i}", [128, 2, D], F32) for i in range(2)]
        junk = sbf("junk", [128, 512], BF16)
        ssq = sbf("ssq", [128, NT, 2], F32)
        rs = sbf("rs", [128, NT], F32)
        B = Blk(nc, "bF")
        sw, sg = B.newsem(), B.newsem("sync")
        t_w = [B.dma("gpsimd", wO[:, c, :], dram["wO"][:, c, :], sw) for c in range(8)]
        t_g = B.dma("sync", gpost[:], dram["gpost"][:], sg)
        xs = [B.newsem("sync") for _ in range(3)]
        os_ = [B.newsem("sync") for _ in range(2)]
        pY = [(psS[0], psS[1]), (psO[0], psO[1]), (psO2[0], psO2[1]), (psM, psT)]
        ps_free = [None] * 4
        xt_free = [None, None, None]
        yt_free = [None, None]
        t_xs = [None] * (NT // 2)
        for tp in range(NT // 2):
            b3 = tp % 3
            by = tp % 2
            xv = x[tp * 256:(tp + 1) * 256, :].rearrange("(n p) d -> p n d", p=128)
            ov = out[tp * 256:(tp + 1) * 256, :].rearrange("(n p) d -> p n d", p=128)
            if tp == 0:
                for t2 in range(2):
                    xv2 = x[t2 * 256:(t2 + 1) * 256, :].rearrange("(n p) d -> p n d", p=128)
                    t_xs[t2] = B.dma("sync", xt[t2 % 3][:], xv2, xs[t2 % 3])
            t_x = t_xs[tp]
            for j in range(2):
                tt = tp * 2 + j
                b = tt % 4
                for half in range(2):
                    pk = pY[b][half]
                    for c in range(8):
                        t_mm = B.op("tensor", lambda e, pk=pk, c=c, tt=tt, half=half: e.matmul(
                            pk[:], OT[:, c, tt * 128:(tt + 1) * 128], wO[:, c, half * 512:(half + 1) * 512], start=(c == 0), stop=(c == 7)),
                            waits=(t_w + [ps_free[b]]) if (c == 0 and half == 0) else (), sig=(c == 7 and half == 1))
                for half in range(2):
                    t_sq = B.op("scalar", lambda e, half=half, b=b, tt=tt: e.activation(
                        out=junk[:], in_=pY[b][half][:], func=AF.Square, accum_out=ssq[:, tt, half:half + 1]), waits=[t_mm], sig=True)
                t_a = B.op("vector", lambda e, tt=tt: e.tensor_tensor(out=rs[:, tt:tt + 1], in0=ssq[:, tt, 0:1], in1=ssq[:, tt, 1:2], op=ALU.add),
                           waits=[t_sq], sig=True)
                t_r1 = B.op("scalar", lambda e, tt=tt: e.activation(out=rs[:, tt:tt + 1], in_=rs[:, tt:tt + 1], func=AF.Sqrt,
                                                                  bias=epsc[:, 0:1], scale=1.0 / D), waits=[t_a], sig=True)
                t_r2 = B.op("vector", lambda e, tt=tt: e.reciprocal(out=rs[:, tt:tt + 1], in_=rs[:, tt:tt + 1]), waits=[t_r1], sig=True)
                for half in range(2):
                    t_y = B.op("vector", lambda e, half=half, b=b, tt=tt, by=by, j=j: e.scalar_tensor_tensor(
                        out=yt[by][:, j, half * 512:(half + 1) * 512], in0=pY[b][half][:], scalar=rs[:, tt:tt + 1],
                        in1=gpost[:, half * 512:(half + 1) * 512], op0=ALU.mult, op1=ALU.mult),
                        waits=[t_r2, t_g, yt_free[by]] if half == 0 else (), sig=True)
                ps_free[b] = t_y
            t_o = B.op("vector", lambda e, by=by, b3=b3: e.tensor_tensor(out=yt[by][:], in0=yt[by][:], in1=xt[b3][:], op=ALU.add),
                       waits=[t_y, t_x], sig=True)
            xt_free[b3] = t_o
            yt_free[by] = B.dma("sync", ov, yt[by][:], os_[by], waits=[t_o])
            if tp + 2 < NT // 2:
                t3 = tp + 2
                xv2 = x[t3 * 256:(t3 + 1) * 256, :].rearrange("(n p) d -> p n d", p=128)
                t_xs[t3] = B.dma("sync", xt[t3 % 3][:], xv2, xs[t3 % 3], waits=[xt_free[t3 % 3]])
        B.op("sync", lambda e: e.nop(), waits=[yt_free[0], yt_free[1]])
        B.run()


def build(debug=None, stop=None):
    nc = bass.Bass("TRN2", target_bir_lowering=False)
    dram = {}

    def din(name, shape):
        dram[name] = nc.dram_tensor(name, list(shape), F32, kind="ExternalInput").ap()
        return dram[name]

    x = din("x", [S, D])
    cshapes = {k: v.shape for k, v in host_consts().items()}
    for k, shp in cshapes.items():
        din(k, shp)
    wshapes = dict(wA=[8, 128, 3, 8, 64], wZ=[8, 128, 8, 128], wQB=[8, 128, 8, 64], wG=[128, 8, 24],
                   wKV=[2, 128, 8, 384], w1kv=[128, 32, 128], w2kv=[128, 3, 64], peT=[128, 32],
                   wO=[128, 8, 1024], gpre=[128, 8], gpost=[128, D])
    for k, shp in wshapes.items():
        din(k, shp)
    out = nc.dram_tensor("out", [S, D], F32, kind="ExternalOutput").ap()
    dbg = {}
    if debug:
        for name, shp in debug.items():
            dbg[name] = nc.dram_tensor("dbg_" + name, list(shp), F32, kind="ExternalOutput").ap()

    es = ExitStack()

    def sb(name, shape, dt):
        return es.enter_context(nc.sbuf_tensor("s_" + name, list(shape), dt))

    def ps(name, shape, dt=F32):
        return es.enter_context(nc.psum_tensor(name, list(shape), dt))

    with es:
        hT = sb("hT", [128, 8, S], BF16)
        OT = sb("OT", [128, 8, S], BF16)
        ident = sb("ident", [128, 128], BF16)
        addm_moba = sb("addm_moba", [128, NT, 16], F32)
        addm_slc = sb("addm_slc", [128, NT, 64], BF16)
        cmask = sb("cmask", [128, 4, 512], BF16)
        tri = sb("tri", [128, 2, 128], BF16)
        btab = sb("btab", [128, 8, 32], F32)
        gpre = sb("gpre", [128, 8], F32)
        epsc = sb("epsc", [128, 1], F32)
        gates = sb("gates", [128, NT, 24], F32)
        esW = ExitStack()

        def sbw(name, shape, dt):
            return esW.enter_context(nc.sbuf_tensor("w_" + name, list(shape), dt))
        QA = sbw("QA", [128, S], BF16)
        KA = sbw("KA", [128, S], BF16)
        KW = sbw("KW", [128, S], BF16)
        VA = sbw("VA", [128, NT, 65], BF16)
        VW = sbw("VW", [128, NT, 65], BF16)
        PT = [sbw(f"PT{i}", [128, 512], BF16) for i in range(4)]
        Ost = [sbw(f"Ost{i}", [128, 4, 128], BF16) for i in range(2)]
        rden = sbw("rden", [128, 16], F32)
        psS = [ps(f"psS{i}", [128, 512]) for i in range(2)]
        psO = [ps(f"psO{i}", [128, 512]) for i in range(2)]
        psO2 = [ps(f"psO2{i}", [128, 512]) for i in range(2)]
        psM = ps("psM", [128, 512])
        psT = ps("psT", [128, 512])

        POOL[0] = SemPool(nc, es)
        with nc.Block() as blk_clr:
            @blk_clr.gpsimd
            def _(g):
                for sm in POOL[0].all():
                    g.sem_clear(sm.h)

        B = Blk(nc, "b0")
        toks = []
        toks.append(B.dma("gpsimd", ident[:], dram["c_ident"][:], B.newsem()))
        toks.append(B.dma("sync", addm_moba[:], dram["c_addm_moba"][:], B.newsem("sync")))
        toks.append(B.dma("gpsimd", addm_slc[:], dram["c_addm_slc"][:], B.newsem()))
        toks.append(B.dma("gpsimd", cmask[:], dram["c_cmask"][:], B.newsem()))
        toks.append(B.dma("gpsimd", tri[:], dram["c_tri"][:], B.newsem()))
        toks.append(B.dma("sync", btab[:], dram["c_btab"][:], B.newsem("sync")))
        toks.append(B.dma("sync", gpre[:], dram["gpre"][:], B.newsem("sync")))
        B.op("vector", lambda e: e.memset(VA[:, :, 64:65], 1.0))
        B.op("vector", lambda e: e.memset(epsc[:], 1e-6))
        B.op("vector", lambda e: e.memset(VW[:, :, 64:65], 1.0))
        B.op("vector", lambda e: e.memset(QA[64:128, :], 0.0))
        t_ms = B.op("vector", lambda e: e.memset(KA[64:128, :], 0.0), sig=True)
        B.op("gpsimd", lambda e: e.memset(Ost[0][:], 0.0))
        B.op("gpsimd", lambda e: e.memset(Ost[1][:], 0.0))
        B.op("sync", lambda e: e.nop(), waits=toks)
        B.run()

        with ExitStack() as esA:
            xt = [esA.enter_context(nc.sbuf_tensor(f"xt{i}", [128, D], F32)) for i in range(4)]
            junk = esA.enter_context(nc.sbuf_tensor("junkA", [128, D], BF16))
            hb = [esA.enter_context(nc.sbuf_tensor(f"hb{i}", [128, D], BF16)) for i in range(2)]
            ss = esA.enter_context(nc.sbuf_tensor("ssA", [128, NT], F32))
            rs = esA.enter_context(nc.sbuf_tensor("rsA", [128, NT], F32))
            B = Blk(nc, "bA")
            xs = [B.newsem("sync") for _ in range(4)]
            xtok = [None] * NT
            hb_free = [None, None]
            xt_free = [None] * 4
            ps_free = [None, None]
            pA = [(psS[0], psS[1]), (psO[0], psO[1])]
            tr2 = [None] * NT
            tsq = [None] * NT

            def stage0(tt):
                b3 = tt % 4
                xtok[tt] = B.dma("sync", xt[b3][:], x[tt * 128:(tt + 1) * 128, :], xs[b3], waits=[xt_free[b3]])

            def stage1(tt):
                b3 = tt % 4
                t_sq = B.op("scalar", lambda e, b3=b3, tt=tt: e.activation(out=junk[:], in_=xt[b3][:], func=AF.Square,
                                                                     accum_out=ss[:, tt:tt + 1]),
                            waits=[xtok[tt]], sig=True)
                t_r1 = B.op("scalar", lambda e, tt=tt: e.activation(out=rs[:, tt:tt + 1], in_=ss[:, tt:tt + 1], func=AF.Sqrt,
                                                                  bias=epsc[:, 0:1], scale=1.0 / D),
                            waits=[t_sq], sig=True)
                tr2[tt] = B.op("vector", lambda e, tt=tt: e.reciprocal(out=rs[:, tt:tt + 1], in_=rs[:, tt:tt + 1]),
                               waits=[t_r1], sig=True)
                tsq[tt] = t_sq

            def stage2(tt):
                b3 = tt % 4
                b2 = tt % 2
                t_h = B.op("scalar", lambda e, tt=tt, b3=b3, b2=b2: e.activation(
                    out=hb[b2][:], in_=xt[b3][:], func=AF.Copy, scale=rs[:, tt:tt + 1]),
                    waits=[tr2[tt], hb_free[b2], tsq[tt]], sig=True)
                xt_free[b3] = t_h
                pa, pb = pA[b2]
                for c in range(8):
                    dst = (pa if c < 4 else pb)[:, (c % 4) * 128:(c % 4 + 1) * 128]
                    t_tr = B.op("tensor", lambda e, dst=dst, b2=b2, c=c: e.matmul(
                        dst, hb[b2][:, c * 128:(c + 1) * 128], ident[:], start=True, stop=True),
                        waits=[t_h, ps_free[b2]], sig=(c == 7))
                hb_free[b2] = t_tr
                B.op("vector", lambda e, tt=tt, pa=pa: e.tensor_tensor(
                    out=hT[:, 0:4, tt * 128:(tt + 1) * 128], in0=pa[:].rearrange("p (c t) -> p c t", c=4),
                    in1=gpre[:, 0:4].unsqueeze(2).to_broadcast([128, 4, 128]), op=ALU.mult), waits=[t_tr])
                ps_free[b2] = B.op("vector", lambda e, tt=tt, pb=pb: e.tensor_tensor(
                    out=hT[:, 4:8, tt * 128:(tt + 1) * 128], in0=pb[:].rearrange("p (c t) -> p c t", c=4),
                    in1=gpre[:, 4:8].unsqueeze(2).to_broadcast([128, 4, 128]), op=ALU.mult), waits=[t_tr], sig=True)

            for t0 in range(3):
                stage0(t0)
            stage1(0)
            stage1(1)
            for tt in range(NT):
                if tt + 3 < NT:
                    stage0(tt + 3)
                if tt + 2 < NT:
                    stage1(tt + 2)
                stage2(tt)
            B.run()


        if stop == "A":
            pass
        else:
            _build_rest(nc, locals(), debug, dbg, stop)
        esW.close()
        if stop is None or (isinstance(stop, dict) and stop.get("final")):
            _build_final(nc, hT, OT, psS, psO, dram, x, out, epsc, psO2, psM, psT)

        if debug and "hT" in debug:
            with ExitStack() as esD:
                tmp = esD.enter_context(nc.sbuf_tensor("dbgtmp", [128, 8, 512], F32))
                B = Blk(nc, "bD")
                s1 = B.newsem("sync")
                tk = None
                for q in range(8):
                    t1 = B.op("vector", lambda e, q=q: e.tensor_copy(out=tmp[:], in_=hT[:, :, q * 512:(q + 1) * 512]),
                              waits=[tk], sig=True)
                    tk = B.dma("sync", dbg["hT"][:, :, q * 512:(q + 1) * 512], tmp[:], s1, waits=[t1])
                B.op("sync", lambda e: e.nop(), waits=[tk])
                B.run()
    return nc


_CACHE = {}


def kernel(x, pre_norm_g, post_norm_g, w_in, cmp_pos_k, cmp_pos_v, w_cmp_k1, w_cmp_k2, w_cmp_v1, w_cmp_v2, w_out):
    x = np.asarray(x, np.float32)
    consts = host_consts()
    wts = host_weights(*(np.asarray(a, np.float32) for a in (pre_norm_g, post_norm_g, w_in, cmp_pos_k, cmp_pos_v,
                                                            w_cmp_k1, w_cmp_k2, w_cmp_v1, w_cmp_v2, w_out)))
    nc = build()
    in_maps = []
    for b in range(8):
        m = {"x": np.ascontiguousarray(x[b])}
        m.update(consts)
        m.update(wts)
        in_maps.append(m)
    res = run_bass_kernel_spmd(nc, in_maps, core_ids=list(range(8)))
    return np.stack([r["out"] for r in res.results], 0).astype(np.float32)
```

```python
import os
import numpy as np
from contextlib import ExitStack
import concourse.bass as bass
import concourse.mybir as mybir
from concourse.bass_utils import run_bass_kernel_spmd

F32 = mybir.dt.float32
BF16 = mybir.dt.bfloat16
ALU = mybir.AluOpType
AF = mybir.ActivationFunctionType
AX = mybir.AxisListType

S = 4096
D = 1024
NT = 32
DIN = 3864
NEGM = -30000.0
ENG = ("sync", "scalar", "vector", "gpsimd", "tensor")


class Sem:
    def __init__(self, h):
        self.h = h
        self.n = 0


class SemPool:
    def __init__(self, nc, es, ndma=24):
        self.eng = {e: Sem(es.enter_context(nc.semaphore(f"pool_{e}"))) for e in ENG}
        self.dma = {"gpsimd": [Sem(es.enter_context(nc.semaphore(f"pool_g{i}"))) for i in range(ndma)],
                    "sync": [Sem(es.enter_context(nc.semaphore(f"pool_s{i}"))) for i in range(ndma)],
                    "scalar": [Sem(es.enter_context(nc.semaphore(f"pool_a{i}"))) for i in range(4)]}

    def all(self):
        return list(self.eng.values()) + self.dma["gpsimd"] + self.dma["sync"] + self.dma["scalar"]


POOL = [None]


class Blk:
    def __init__(self, nc, name):
        self.nc = nc
        self.name = name
        self.ops = {e: [] for e in ENG}
        self.esem = POOL[0].eng
        self.k = {"gpsimd": 0, "sync": 0, "scalar": 0}

    def newsem(self, kind="gpsimd"):
        lst = POOL[0].dma[kind]
        s_ = lst[self.k[kind] % len(lst)]
        self.k[kind] += 1
        s_.kind = kind
        return s_

    def op(self, eng, fn, waits=(), sig=False):
        tok = None
        s = None
        if sig:
            s = self.esem[eng]
            s.n += 1
            tok = (s.h, s.n)
        self.ops[eng].append((fn, tuple(w for w in waits if w is not None), s, 1))
        return tok

    def dma(self, eng, out, in_, sem, waits=()):
        assert sem.kind == eng, (sem.kind, eng)
        sem.n += 16
        tok = (sem.h, sem.n)
        self.ops[eng].append((lambda e: e.dma_start(out=out, in_=in_), tuple(w for w in waits if w is not None), sem, 16))
        return tok

    def last(self, eng):
        s = self.esem[eng]
        return (s.h, s.n) if s.n > 0 else None

    def run(self):
        with self.nc.Block() as block:
            for e in ENG:
                ops = self.ops[e]

                def body(eng, ops=ops):
                    seen = {}
                    for fn, waits, s, amt in ops:
                        for (h, v) in waits:
                            key = id(h)
                            if seen.get(key, 0) >= v:
                                continue
                            seen[key] = v
                            eng.wait_ge(h, v)
                        ins = fn(eng)
                        if s is not None:
                            ins.then_inc(s.h, amt)

                getattr(block, e)(body)


def _slopes():
    return [2.0 ** (-(i + 1)) for i in range(8)]


def host_consts():
    c = {}
    c["c_ident"] = np.eye(128, dtype=np.float32)
    k = np.arange(S)
    c["c_E16"] = (k[None, :] // 256 == np.arange(16)[:, None]).astype(np.float32)
    c["c_E64"] = (NEGM * (k[None, :] // 64 == np.arange(64)[:, None])).astype(np.float32)
    am = np.zeros((128, NT, 16), np.float32)
    for tt in range(NT):
        own = tt // 2
        am[:, tt, own] = 1e9
        am[:, tt, own + 1:] = -1e9
    c["c_addm_moba"] = am
    a2 = np.zeros((128, NT, 64), np.float32)
    t = (np.arange(NT)[None, :] * 128 + np.arange(128)[:, None])
    own = t // 64
    j = np.arange(64)[None, None, :]
    a2 = np.where(j > own[:, :, None], -1e9, 0.0).astype(np.float32)
    a2 = np.where(j == 0, 1e9, a2)
    a2 = np.where(j == own[:, :, None] - 1, 2e9, a2)
    a2 = np.where(j == own[:, :, None], 3e9, a2)
    c["c_addm_slc"] = a2.astype(np.float32)
    cr = np.arange(128)[:, None, None]
    r = np.arange(4)[None, :, None]
    tr = np.arange(512)[None, None, :]
    c["c_cmask"] = (16 * cr + 31 - 512 * r <= tr).astype(np.float32)
    kk = np.arange(128)[:, None]
    tq = np.arange(128)[None, :]
    c["c_tri"] = np.stack([(kk <= tq), (kk > tq)], axis=1).astype(np.float32)
    sl = np.array(_slopes(), np.float32)
    dl = np.arange(32)[None, None, :]
    c["c_btab"] = (sl[None, :, None] * (np.arange(128)[:, None, None] - 128.0 * dl - 64.0)).astype(np.float32)
    tmod = (np.arange(S) % 128).astype(np.float32)
    qrow = np.zeros((8, 2, S), np.float32)
    krow = np.zeros((8, 2, S), np.float32)
    for h in range(8):
        qrow[h, 0] = -sl[h] * tmod
        qrow[h, 1] = 1.0
        krow[h, 0] = 1.0
        krow[h, 1] = sl[h] * tmod
    c["c_qrow"] = qrow
    c["c_krow"] = krow
    rc = np.zeros((128, 2, 65), np.float32)
    cidx = np.arange(2)[None, :] * 128 + np.arange(128)[:, None]
    valid = cidx < 255
    rc[:, :, 0] = valid
    cst = cidx * 16
    js = np.arange(64)[None, None, :] * 64
    ov = (cst[:, :, None] < js + 64) & (cst[:, :, None] + 32 > js) & valid[:, :, None]
    rc[:, :, 1:] = ov
    c["c_rc"] = rc
    return c


def host_weights(pre_norm_g, post_norm_g, w_in, cmp_pos_k, cmp_pos_v, w_cmp_k1, w_cmp_k2, w_cmp_v1, w_cmp_v2, w_out):
    w = {}
    W = np.ascontiguousarray(w_in[0].reshape(8, 128, DIN).transpose(1, 0, 2))

    def cols(a, n=64):
        return W[:, :, a:a + n]

    w["wA"] = np.ascontiguousarray(np.stack(
        [np.stack([cols(0 + h * 64), cols(512 + h * 64), cols(1024 + h * 64)], axis=1) for h in range(8)], 0))
    zc = [1536 + p * 128 for p in range(4)] + [3352 + p * 128 for p in range(4)]
    w["wZ"] = np.ascontiguousarray(np.stack([cols(a, 128) for a in zc], 0))
    w["wQB"] = np.ascontiguousarray(np.stack([cols(2048 + h * 64) for h in range(8)], 0))
    w["wG"] = np.ascontiguousarray(cols(3328, 24))
    kv = []
    for g in range(2):
        kv.append(np.concatenate([cols(2560 + g * 64), cols(2688 + g * 64), cols(2816 + g * 64),
                                  cols(3072 + g * 64), cols(2944 + g * 64), cols(3200 + g * 64)], axis=2))
    w["wKV"] = np.ascontiguousarray(np.stack(kv, 0))
    k1 = w_cmp_k1[0].reshape(32, 64, 128).transpose(1, 0, 2)
    v1 = w_cmp_v1[0].reshape(32, 64, 128).transpose(1, 0, 2)
    w["w1kv"] = np.ascontiguousarray(np.concatenate([k1, v1], 0))
    w["w2kv"] = np.ascontiguousarray(np.stack([w_cmp_k2[0], w_cmp_k2[0], w_cmp_v2[0]], 1))
    w["peT"] = np.ascontiguousarray(np.concatenate([cmp_pos_k[0].T, cmp_pos_v[0].T], 0))
    w["wO"] = np.ascontiguousarray(w_out[0].reshape(8, 128, D).transpose(1, 0, 2))
    w["gpre"] = np.ascontiguousarray(pre_norm_g[0].reshape(8, 128).T)
    w["gpost"] = np.ascontiguousarray(np.broadcast_to(post_norm_g[0][None, :], (128, D)))
    return {k: np.asarray(v, np.float32) for k, v in w.items()}


def emit_attention(B, T, groups, single_acc_key=None, filler=None, nS=2, nPT=3, prev=None):
    psS, PT = T.psS, T.PT
    flat = []
    for gi, g in enumerate(groups):
        for ti, t in enumerate(g["tiles"]):
            flat.append((gi, ti, t))
    n = len(flat)
    act_done = [None] * n
    rdy = [None] * n
    s_done = [None] * n
    pv_done = [None] * n
    epi_done = [None] * len(groups)
    last_pv_of_group = [None] * len(groups)

    def emit_S(i):
        gi, ti, t = flat[i]
        w = [act_done[i - nS] if i >= nS else (prev[0] if prev else None)]
        ew = t.get("extra_wait")
        if ew is not None:
            w += list(ew) if isinstance(ew, (list, tuple)) and not (len(ew) == 2 and not isinstance(ew[0], tuple)) else [ew]
        m = len(t["smm"])
        for j, (o, l, r) in enumerate(t["smm"]):
            tk = B.op("tensor", lambda e, o=o, l=l, r=r: e.matmul(o, l, r, start=True, stop=True),
                      waits=w if j == 0 else (), sig=(j == m - 1))
        s_done[i] = tk

    def emit_act(i):
        gi, ti, t = flat[i]
        in_, o, bias = t["act"]
        w = [s_done[i], pv_done[i - nPT] if i >= nPT else (prev[1] if prev else None)]
        if bias is None:
            act_done[i] = B.op("scalar", lambda e, in_=in_, o=o: e.activation(out=o, in_=in_, func=AF.Exp), waits=w, sig=True)
        else:
            act_done[i] = B.op("scalar", lambda e, in_=in_, o=o, bias=bias: e.activation(out=o, in_=in_, func=AF.Exp, bias=bias),
                               waits=w, sig=True)
        rdy[i] = act_done[i]
        if t.get("mask") is not None:
            ap, mk = t["mask"]
            rdy[i] = B.op("vector", lambda e, ap=ap, mk=mk: e.tensor_tensor(out=ap, in0=ap, in1=mk, op=ALU.mult),
                          waits=[act_done[i]], sig=True)

    def emit_PV(i):
        gi, ti, t = flat[i]
        if filler is not None:
            filler(B)
        w = [rdy[i]]
        if ti == 0 and gi >= 2:
            w.append(epi_done[gi - 2])
        if ti == 0 and gi < 2 and prev:
            w.append(prev[2])
        if single_acc_key and t.get(single_acc_key) and gi >= 1:
            w.append(epi_done[gi - 1])
        m = len(t["pv"])
        for j, (o, l, r, st) in enumerate(t["pv"]):
            tk = B.op("tensor", lambda e, o=o, l=l, r=r, st=st: e.matmul(o, l, r, start=st, stop=False),
                      waits=w if j == 0 else (), sig=(j == m - 1))
        pv_done[i] = tk
        if ti == len(groups[gi]["tiles"]) - 1:
            last_pv_of_group[gi] = tk
            if gi >= 1 and groups[gi - 1].get("post"):
                groups[gi - 1]["post"](B)
            epi_done[gi] = groups[gi]["epi"](B, tk)

    dD = nS - 1
    for i in range(n + dD):
        if i < n:
            emit_S(i)
            emit_act(i)
        if i >= dD:
            emit_PV(i - dD)
    if groups and groups[-1].get("post"):
        groups[-1]["post"](B)
    return (act_done[-1], pv_done[-1], epi_done[-1])


def _build_rest(nc, L, debug, dbg, stop):
    import types
    T = types.SimpleNamespace(**{k: v for k, v in L.items() if k not in ("es", "B")})
    hT, OT, QA, KA, KW, VA, VW, PT, Ost = T.hT, T.OT, T.QA, T.KA, T.KW, T.VA, T.VW, T.PT, T.Ost
    psS, psO, psO2, psM, psT = T.psS, T.psO, T.psO2, T.psM, T.psT
    ident, btab, tri, cmask, rden = T.ident, T.btab, T.tri, T.cmask, T.rden
    dram = T.dram
    banks4 = [psS[0], psS[1], psO[0], psO[1]]
    SB4 = [psS[0], psS[1], psO2[0], psO2[1]]

    def v3(ap2d, a):
        return ap2d.rearrange("p (a b) -> p a b", a=a)

    def gating_block(name, p, wz, zs):
        B = Blk(nc, name)
        s1 = B.newsem()
        t_w = B.dma("gpsimd", wz[:], dram["wZ"][p], s1)
        z_free = [None, None]
        ps_free = [None, None]
        for qt in range(8):
            b = qt % 2
            for c in range(8):
                t_mm = B.op("tensor", lambda e, b=b, c=c, qt=qt: e.matmul(
                    psS[b][:], wz[:, c, :], hT[:, c, qt * 512:(qt + 1) * 512], start=(c == 0), stop=(c == 7)),
                    waits=[t_w, ps_free[b]] if c == 0 else (), sig=(c == 7))
            t_s = B.op("scalar", lambda e, b=b: e.activation(out=zs[b][:], in_=psS[b][:], func=AF.Silu),
                       waits=[t_mm, z_free[b]], sig=True)
            ps_free[b] = t_s
            z_free[b] = B.op("vector", lambda e, b=b, qt=qt: e.tensor_tensor(
                out=OT[:, p, qt * 512:(qt + 1) * 512], in0=OT[:, p, qt * 512:(qt + 1) * 512], in1=zs[b][:], op=ALU.mult),
                waits=[t_s], sig=True)
        B.run()

    with ExitStack() as esM:
        def sbm(name, shape, dt):
            return esM.enter_context(nc.sbuf_tensor("m_" + name, list(shape), dt))
        QA2 = sbm("QA2", [128, S], BF16)
        Wq2 = [sbm(f"Wqk{i}", [128, 2, 8, 128], BF16) for i in range(2)]
        Wv2 = [sbm(f"Wv{i}", [128, 8, 64], BF16) for i in range(2)]
        maskpad = sbm("maskpad", [128, NT, 16], BF16)
        scg = sbm("scg", [128, NT, 16], F32)
        m8 = sbm("m8", [128, NT, 8], F32)
        tmpf = sbm("tmpf", [128, NT, 16], F32)
        kmf = sbm("kmf", [64, 16], F32)
        kmb = sbm("kmb", [64, 16], BF16)
        wz = sbm("wz", [128, 8, 128], BF16)
        zs = [scg[:].rearrange("p a b -> p (a b)").bitcast(BF16)[:, 0:512], tmpf[:].rearrange("p a b -> p (a b)").bitcast(BF16)[:, 0:512]]
        QS = [QA, QA2]
        KS_ = [KA, KW]
        VS_ = [VA, VW]

        B = Blk(nc, "bM0")
        t1 = B.op("vector", lambda e: e.memset(maskpad[:], 0.0), sig=True)
        B.op("vector", lambda e: e.memset(QA2[64:128, :], 0.0))
        t1b = B.op("vector", lambda e: e.memset(KW[64:128, :], 0.0), sig=True)
        t2 = B.dma("gpsimd", KA[64:80, :], dram["c_E16"][:], B.newsem())
        t3 = B.dma("gpsimd", KW[64:80, :], dram["c_E16"][:], B.newsem(), waits=[t1b])
        B.op("sync", lambda e: e.nop(), waits=[t1, t2, t3])
        B.run()

        nheads = 8 if stop is None else int(stop.get("moba_heads", 8)) if isinstance(stop, dict) else 8

        def proj_ops(B, h, t_w, banks, bank_free):
            st = h % 2
            Wt, Wv, Qd, Kd, Vd = Wq2[st], Wv2[st], QS[st], KS_[st], VS_[st]
            ops = []
            state = {"bi": 0, "first": True}

            def mm_qk(which, qt, c):
                def f():
                    bi = state["bi"]
                    bk = banks[bi % len(banks)]
                    w = ()
                    if c == 0:
                        w = [bank_free[bi % len(banks)]] + ([t_w] if state["first"] else [])
                        state["first"] = False
                    tk = B.op("tensor", lambda e: e.matmul(bk[:, :], Wt[:, which, c, :], hT[:, c, qt * 512:(qt + 1) * 512],
                                                          start=(c == 0), stop=(c == 7)), waits=w, sig=(c == 7))
                    if c == 7:
                        if which == 0:
                            t_e = B.op("vector", lambda e: e.tensor_scalar(out=Qd[0:64, qt * 512:(qt + 1) * 512], in0=bk[0:64, :],
                                                                         scalar1=0.125, scalar2=None, op0=ALU.mult), waits=[tk], sig=True)
                        else:
                            t_e = B.op("vector", lambda e: e.tensor_copy(out=Kd[0:64, qt * 512:(qt + 1) * 512], in_=bk[0:64, :]),
                                       waits=[tk], sig=True)
                        bank_free[bi % len(banks)] = t_e
                        state["last_ev"] = t_e
                        state["bi"] += 1
                return f

            def mm_v(tg, j, c):
                def f():
                    bi = state["bi"]
                    bk = banks[bi % len(banks)]
                    tt = tg * 4 + j
                    w = [bank_free[bi % len(banks)]] if (c == 0 and j == 0) else ()
                    tk = B.op("tensor", lambda e: e.matmul(bk[:, j * 64:(j + 1) * 64], hT[:, c, tt * 128:(tt + 1) * 128], Wv[:, c, :],
                                                          start=(c == 0), stop=(c == 7)), waits=w, sig=(c == 7 and j == 3))
                    if c == 7 and j == 3:
                        t_e = B.op("vector", lambda e: e.tensor_copy(out=Vd[:, tg * 4:(tg + 1) * 4, 0:64], in_=v3(bk[:, 0:256], 4)),
                                   waits=[tk], sig=True)
                        bank_free[bi % len(banks)] = t_e
                        state["last_ev"] = t_e
                        state["bi"] += 1
                return f

            qk_groups = [[(mm_qk(which, qt, c), 1.0) for c in range(8)] for qt in range(8) for which in (0, 1)]
            v_groups = [[(mm_v(tg, j, c), 0.15) for j in range(4) for c in range(8)] for tg in range(8)]
            gi = 0
            for k in range(8):
                ops += qk_groups[2 * k]
                ops += v_groups[k]
                ops += qk_groups[2 * k + 1]
            return ops, state

        def load_head_consts(B, h):
            st = h % 2
            sw_ = B.newsem()
            for which in range(2):
                for half in range(2):
                    B.dma("gpsimd", Wq2[st][:, which, :, half * 64:(half + 1) * 64], dram["wA"][h][:, which], sw_)
            t_w = B.dma("gpsimd", Wv2[st][:], dram["wA"][h][:, 2], sw_)
            t_qr = B.dma("gpsimd", QS[st][80:82, :], dram["c_qrow"][h], B.newsem())
            t_kr = B.dma("gpsimd", KS_[st][80:82, :], dram["c_krow"][h], B.newsem())
            return t_w, t_qr, t_kr

        def selection_block(h, extra_waits=()):
            st = h % 2
            Qd, Kd = QS[st], KS_[st]
            B = Blk(nc, f"bS{h}")
            t_km = B.op("vector", lambda e: e.tensor_reduce(out=kmf[:], in_=Kd[0:64, :].rearrange("p (j k) -> p j k", k=256),
                                                          axis=AX.X, op=ALU.add), waits=list(extra_waits), sig=True)
            t_kb = B.op("vector", lambda e: e.tensor_scalar(out=kmb[:], in0=kmf[:], scalar1=1.0 / 256, scalar2=None, op0=ALU.mult),
                        waits=[t_km], sig=True)
            for tt in range(NT):
                t_g = B.op("tensor", lambda e, tt=tt: e.matmul(psM[:, tt * 16:(tt + 1) * 16], Qd[0:64, tt * 128:(tt + 1) * 128], kmb[:],
                                                              start=True, stop=True),
                           waits=[t_kb] if tt == 0 else (), sig=(tt == NT - 1))
            t_sc = B.op("vector", lambda e: e.tensor_tensor(out=scg[:], in0=v3(psM[:], NT), in1=T.addm_moba[:], op=ALU.add),
                        waits=[t_g], sig=True)
            for tt in range(NT):
                t_m8 = B.op("vector", lambda e, tt=tt: e.max(out=m8[:, tt, :], in_=scg[:, tt, :]),
                            waits=[t_sc] if tt == 0 else (), sig=(tt == NT - 1))
            t_c = B.op("vector", lambda e: e.tensor_tensor(out=tmpf[:], in0=scg[:], in1=m8[:, :, 3:4].to_broadcast([128, NT, 16]),
                                                         op=ALU.is_lt), waits=[t_m8], sig=True)
            t_mp = B.op("vector", lambda e: e.tensor_scalar(out=maskpad[:], in0=tmpf[:], scalar1=NEGM, scalar2=None,
                                                          op0=ALU.mult), waits=[t_c], sig=True)
            tb_free = [None, None]
            for g8 in range(8):
                bk = psS[g8 % 2]
                for j in range(4):
                    tt = g8 * 4 + j
                    t_tr = B.op("tensor", lambda e, bk=bk, j=j, tt=tt: e.matmul(
                        bk[64:80, j * 128:(j + 1) * 128], maskpad[:, tt, :], ident[:], start=True, stop=True),
                        waits=[t_mp, tb_free[g8 % 2]] if j == 0 else (), sig=(j == 3))
                tb_free[g8 % 2] = B.op("vector", lambda e, bk=bk, g8=g8: e.tensor_copy(
                    out=Qd[64:80, g8 * 512:(g8 + 1) * 512], in_=bk[64:80, :]), waits=[t_tr], sig=True)
            B.run()

        if nheads > 0:
            B = Blk(nc, "bP0")
            t_w, t_qr, t_kr = load_head_consts(B, 0)
            bfree = [None] * 4
            ops, stt = proj_ops(B, 0, t_w, banks4, bfree)
            for f, _c in ops:
                f()
            B.op("sync", lambda e: e.nop(), waits=[t_qr, t_kr, stt["last_ev"]])
            B.run()
            selection_block(0)

        for h in range(nheads):
            pair, off = h // 2, (h % 2) * 64
            st = h % 2
            Qd, Kd, Vd = QS[st], KS_[st], VS_[st]
            B = Blk(nc, f"bT{h}")
            nxt = h + 1 < nheads
            if nxt:
                t_w, t_qr, t_kr = load_head_consts(B, h + 1)
                pbfree = [None, None]
                pops, pstate = proj_ops(B, h + 1, t_w, [psO2[1], psM], pbfree)
            SBk = [psS[0], psS[1], psO2[0]] if nxt else SB4
            groups = []
            for qt in range(8):
                aset = qt % 2
                acc = v3(psO[aset][:], 4)
                tiles = []
                first = True
                for dl in range(4 * qt + 3, -1, -1):
                    smm, pv = [], []
                    qs_valid = [qs for qs in range(4) if 4 * qt + qs - dl >= 0]
                    for qs in qs_valid:
                        kt = 4 * qt + qs - dl
                        tq = 4 * qt + qs
                        smm.append(("S", qs, Kd[0:82, kt * 128:(kt + 1) * 128], Qd[0:82, tq * 128:(tq + 1) * 128]))
                        pv.append((acc[:, qs, 0:65], qs, Vd[:, kt, :], first))
                        first = False
                    tiles.append(dict(qs=qs_valid, smm=smm, pv=pv, bias=float(-_slopes()[h] * 128.0 * dl), mask=(dl == 0)))
                groups.append(dict(qt=qt, aset=aset, tiles=tiles))
            gi_tile = 0
            ntile = sum(len(g["tiles"]) for g in groups)
            for g in groups:
                for t in g["tiles"]:
                    sb_ = SBk[gi_tile % len(SBk)]
                    pt_ = PT[gi_tile % 4]
                    q0, q1 = t["qs"][0], t["qs"][-1] + 1
                    t["smm"] = [(sb_[:, qs * 128:(qs + 1) * 128], l, r) for (_, qs, l, r) in t["smm"]]
                    t["act"] = (sb_[:, q0 * 128:q1 * 128], pt_[:, q0 * 128:q1 * 128], t["bias"])
                    t["pv"] = [(o, pt_[:, qs * 128:(qs + 1) * 128], r, st_) for (o, qs, r, st_) in t["pv"]]
                    if t["mask"]:
                        t["mask"] = (v3(pt_[:], 4), tri[:, 0:1, :].to_broadcast([128, 4, 128]))
                    else:
                        t["mask"] = None
                    gi_tile += 1

            def mk_epi(qt, aset):
                acc = v3(psO[aset][:], 4)

                def epi(B, tk):
                    rd = rden[:, aset * 4:aset * 4 + 4].unsqueeze(2)
                    t_r = B.op("vector", lambda e: e.reciprocal(out=rd, in_=acc[:, :, 64:65]),
                               waits=[tk, st_ost[aset]], sig=True)
                    t_n = B.op("vector", lambda e: e.tensor_tensor(out=Ost[aset][:, :, off:off + 64], in0=acc[:, :, 0:64],
                                                                 in1=rd.to_broadcast([128, 4, 64]), op=ALU.mult),
                               waits=[t_r], sig=True)
                    epi_tok[qt] = t_n
                    return t_n
                return epi

            def mk_post(qt, aset):
                def post(B):
                    for qs in range(4):
                        t_tr = B.op("tensor", lambda e, qs=qs: e.matmul(psT[:, qs * 128:(qs + 1) * 128], Ost[aset][:, qs, :], ident[:],
                                                                       start=True, stop=True),
                                    waits=[epi_tok[qt], post_state["psT_free"]] if qs == 0 else (), sig=(qs == 3))
                    st_ost[aset] = t_tr
                    post_state["psT_free"] = B.op("vector", lambda e: e.tensor_copy(
                        out=OT[off:off + 64, pair, qt * 512:(qt + 1) * 512], in_=psT[off:off + 64, :]), waits=[t_tr], sig=True)
                return post

            epi_tok = {}
            st_ost = [None, None]
            post_state = {"psT_free": None}
            for g in groups:
                g["epi"] = mk_epi(g["qt"], g["aset"])
                g["post"] = mk_post(g["qt"], g["aset"])
            if nxt:
                budget = sum(c_ for _f, c_ in pops) / max(1, ntile - 8)
                pq = list(pops)
                fstate = {"acc": 0.0}

                def filler(B):
                    fstate["acc"] += budget
                    while pq and fstate["acc"] > 0:
                        f, c_ = pq.pop(0)
                        f()
                        fstate["acc"] -= c_
                emit_attention(B, T, groups, filler=filler, nS=3, nPT=4)
                while pq:
                    pq.pop(0)[0]()
                B.op("sync", lambda e: e.nop(), waits=[t_qr, t_kr, pstate["last_ev"]])
            else:
                emit_attention(B, T, groups, filler=_mk_filler(lambda: psM[:, 0:128], ident, True), nS=4, nPT=4)
            B.run()
            if nxt:
                selection_block(h + 1)
            if h % 2 == 1:
                gating_block(f"bZ{pair}", pair, wz, zs)

    only = stop.get("only") if isinstance(stop, dict) else None
    if only != "moba":
        _build_nsa(nc, T, gating_block, v3, stop)

    if debug and "OT" in debug:
        with ExitStack() as esD:
            tmp = esD.enter_context(nc.sbuf_tensor("dbgtmp2", [128, 8, 512], F32))
            B = Blk(nc, "bD2")
            s1 = B.newsem("sync")
            tk = None
            for q in range(8):
                t1 = B.op("vector", lambda e, q=q: e.tensor_copy(out=tmp[:], in_=OT[:, :, q * 512:(q + 1) * 512]),
                          waits=[tk], sig=True)
                tk = B.dma("sync", dbg["OT"][:, :, q * 512:(q + 1) * 512], tmp[:], s1, waits=[t1])
            B.op("sync", lambda e: e.nop(), waits=[tk])
            B.run()


def _build_nsa(nc, T, gating_block, v3, stop):
    hT, OT, QA, KA, KW, VA, VW, PT, Ost = T.hT, T.OT, T.QA, T.KA, T.KW, T.VA, T.VW, T.PT, T.Ost
    psS, psO, psO2, psM, psT = T.psS, T.psO, T.psO2, T.psM, T.psT
    ident, btab, tri, cmask, rden, gates = T.ident, T.btab, T.tri, T.cmask, T.rden, T.gates
    dram = T.dram
    banks4 = [psS[0], psS[1], psO[0], psO[1]]
    SB4 = [psS[0], psS[1], psO2[0], psO2[1]]
    SB3 = [psS[0], psS[1], psO2[1]]
    ngroups = int(stop.get("nsa_groups", 2)) if isinstance(stop, dict) else 2
    stage = int(stop.get("nsa_stage", 9)) if isinstance(stop, dict) else 9
    with ExitStack() as esN:
        def sbn(name, shape, dt):
            return esN.enter_context(nc.sbuf_tensor("n_" + name, list(shape), dt))
        kcT = sbn("kcT", [128, 256], BF16)
        rcmp = sbn("rcmp", [128, 2, 129], BF16)
        scrN = sbn("scrN", [128, 2048], BF16)
        WqD = [scrN[:, 0:1024].rearrange("p (c n) -> p c n", c=8), scrN[:, 1024:2048].rearrange("p (c n) -> p c n", c=8)]
        wz = WqD[0]
        m16 = sbn("m16", [128, NT, 16], F32)
        wk4 = sbn("wk4", [128, 4, 64], F32)
        maskp = [sbn(f"maskp{i}", [128, 4, 128], BF16) for i in range(2)]
        zs = [maskp[i][:].rearrange("p a b -> p (a b)") for i in range(2)]
        hs = [scrN[:, 1024:1280], scrN[:, 1280:1536]]
        d3 = [sbn(f"d3{i}", [128, 4, 3], F32) for i in range(2)]
        tA = sbn("tA", [128, 4, 64], F32)
        tB = sbn("tB", [128, 4, 64], F32)
        bcol = sbn("bcol", [128, 2], F32)

        with ExitStack() as es1:
            Wg = es1.enter_context(nc.sbuf_tensor("n_Wg", [128, 8, 24], BF16))
            B = Blk(nc, "bG")
            s1, s2, s3 = B.newsem(), B.newsem(), B.newsem()
            t_w = B.dma("gpsimd", Wg[:], dram["wG"][:], s1)
            t_e = B.dma("gpsimd", KA[64:128, :], dram["c_E64"][:], s2)
            t_rc = B.dma("gpsimd", rcmp[:, :, 64:129], dram["c_rc"][:], s3)
            t_z0 = B.op("vector", lambda e: e.memset(maskp[0][:], 0.0))
            t_z1 = B.op("vector", lambda e: e.memset(maskp[1][:], 0.0))
            t_z4 = B.op("vector", lambda e: e.memset(kcT[:], 0.0), sig=True)
            gT = es1.enter_context(nc.sbuf_tensor("n_gT", [24, S], BF16))
            gfree = [None, None]
            for qt in range(8):
                bk = psS[qt % 2]
                for c in range(8):
                    t_mm = B.op("tensor", lambda e, bk=bk, c=c, qt=qt: e.matmul(
                        bk[0:24, :], Wg[:, c, :], hT[:, c, qt * 512:(qt + 1) * 512], start=(c == 0), stop=(c == 7)),
                        waits=[t_w, gfree[qt % 2]] if c == 0 else (), sig=(c == 7))
                gfree[qt % 2] = B.op("scalar", lambda e, bk=bk, qt=qt: e.activation(
                    out=gT[:, qt * 512:(qt + 1) * 512], in_=bk[0:24, :], func=AF.Sigmoid), waits=[t_mm], sig=True)
            for half in range(2):
                bk = (psM, psT)[half]
                for j in range(16):
                    tt = half * 16 + j
                    t_mm = B.op("tensor", lambda e, bk=bk, j=j, tt=tt: e.matmul(
                        bk[:, j * 24:(j + 1) * 24], gT[:, tt * 128:(tt + 1) * 128], ident[0:24, 0:24], start=True, stop=True),
                        waits=[gfree[0], gfree[1]] if j == 0 else (), sig=(j == 15))
                B.op("scalar", lambda e, bk=bk, half=half: e.copy(
                    out=gates[:, half * 16:(half + 1) * 16, :], in_=bk[:, 0:384].rearrange("p (a b) -> p a b", a=16)),
                    waits=[t_mm], sig=True)
            B.op("sync", lambda e: e.nop(), waits=[t_e, t_rc, t_z4, B.last("scalar")])
            B.run()

        for g in range(ngroups if stage >= 2 else 0):
            with ExitStack() as es1:
                Wkv = es1.enter_context(nc.sbuf_tensor(f"n_Wkv{g}", [128, 8, 384], BF16))
                w1kv = es1.enter_context(nc.sbuf_tensor(f"n_w1kv{g}", [128, 32, 128], BF16))
                w2kv = es1.enter_context(nc.sbuf_tensor(f"n_w2kv{g}", [128, 3, 64], BF16))
                peT = es1.enter_context(nc.sbuf_tensor(f"n_peT{g}", [128, 32], BF16))
                B = Blk(nc, f"bK{g}")
                s1, s2, s3, s4 = B.newsem(), B.newsem(), B.newsem(), B.newsem()
                B.dma("gpsimd", Wkv[:, 0:4, :], dram["wKV"][g][:, 0:4, :], s1)
                t_w = B.dma("gpsimd", Wkv[:, 4:8, :], dram["wKV"][g][:, 4:8, :], s1)
                import os
                skip = os.environ.get("BK_SKIP", "")
                for i4 in range(0 if "w" in skip else 3):
                    B.dma("gpsimd", w1kv[:, i4 * 8:(i4 + 1) * 8, :], dram["w1kv"][:, i4 * 8:(i4 + 1) * 8, :], s2)
                t_w1a = B.dma("gpsimd", w1kv[:, 24:32, :], dram["w1kv"][:, 24:32, :], s2)
                t_w1b = t_w1a
                t_w2 = B.dma("gpsimd", w2kv[:], dram["w2kv"][:], s3)
                t_pe = B.dma("gpsimd", peT[:], dram["peT"][:], s4)
                bank_free = [None] * 4
                bi = 0
                evs = {"scalar": None, "vector": None}
                specs = [(0, 128, QA, 128), (128, 64, KA, 64), (192, 64, KW, 64)]
                if "a" in skip:
                    specs = specs[0:1]
                if "b" in skip:
                    specs = specs[1:2]
                if "c" in skip:
                    specs = specs[2:3]
                for qt in range(0 if "q" in skip else 8):
                    for si, (c0, m, dst, rows) in enumerate(specs):
                        bk = banks4[bi % 4]
                        for c in range(8):
                            t_mm = B.op("tensor", lambda e, bk=bk, c=c, qt=qt, c0=c0, m=m, rows=rows: e.matmul(
                                bk[0:rows, :], Wkv[:, c, c0:c0 + m], hT[:, c, qt * 512:(qt + 1) * 512], start=(c == 0), stop=(c == 7)),
                                waits=[t_w, bank_free[bi % 4]] if c == 0 else (), sig=(c == 7))
                        eng = "scalar" if si != 1 else "vector"
                        if eng == "scalar":
                            t_e = B.op("scalar", lambda e, bk=bk, dst=dst, rows=rows, qt=qt: e.copy(
                                out=dst[0:rows, qt * 512:(qt + 1) * 512], in_=bk[0:rows, :]), waits=[t_mm], sig=True)
                        else:
                            t_e = B.op("vector", lambda e, bk=bk, dst=dst, rows=rows, qt=qt: e.tensor_copy(
                                out=dst[0:rows, qt * 512:(qt + 1) * 512], in_=bk[0:rows, :]), waits=[t_mm], sig=True)
                        evs[eng] = t_e
                        bank_free[bi % 4] = t_e
                        bi += 1
                for tg in range(0 if "v" in skip else 8):
                    bk = banks4[bi % 4]
                    for j in range(4):
                        tt = tg * 4 + j
                        for c in range(8):
                            t_mm = B.op("tensor", lambda e, bk=bk, j=j, c=c, tt=tt: e.matmul(
                                bk[:, j * 128:(j + 1) * 128], hT[:, c, tt * 128:(tt + 1) * 128], Wkv[:, c, 256:384], start=(c == 0), stop=(c == 7)),
                                waits=[bank_free[bi % 4]] if (c == 0 and j == 0) else (), sig=(c == 7 and j == 3))
                    B.op("scalar", lambda e, bk=bk, tg=tg: e.copy(
                        out=VA[:, tg * 4:(tg + 1) * 4, 0:64], in_=v3(bk[:], 4)[:, :, 0:64]), waits=[t_mm])
                    t_e2 = B.op("scalar", lambda e, bk=bk, tg=tg: e.copy(
                        out=VW[:, tg * 4:(tg + 1) * 4, 0:64], in_=v3(bk[:], 4)[:, :, 64:128]), waits=[t_mm], sig=True)
                    bank_free[bi % 4] = t_e2
                    bi += 1
                B.op("vector", lambda e: e.memset(hs[0][:, 255:256], 0.0))
                t_hz = B.op("vector", lambda e: e.memset(hs[1][:, 255:256], 0.0), sig=True)
                kvv = QA[:, :].rearrange("p (c s) -> p c s", s=16)
                import os
                sub = int(os.environ.get("BK_SUB", "9"))
                for kv in range(2 if sub >= 2 else 0):
                    r0 = kv * 64
                    bkH = banks4[bi % 4]
                    bi += 1
                    for l in range(32):
                        a, b_ = l // 16, l % 16
                        t_h = B.op("tensor", lambda e, bkH=bkH, l=l, a=a, b_=b_, r0=r0: e.matmul(
                            bkH[:, 0:255], w1kv[r0:r0 + 64, l, :], kvv[r0:r0 + 64, a:a + 255, b_], start=(l == 0), stop=(l == 31)),
                            waits=[t_w1a, t_w1b, evs["scalar"], evs["vector"], bank_free[(bi - 1) % 4]] if l == 0 else (), sig=(l == 31))
                    if sub == 2:
                        continue
                    if kv == 1 and sub == 3:
                        continue
                    for l in range(32):
                        t_b = B.op("tensor", lambda e, bkH=bkH, l=l, r0=r0: e.matmul(
                            bkH[:, 256:257], w1kv[r0:r0 + 64, l, :], peT[r0:r0 + 64, l:l + 1], start=(l == 0), stop=(l == 31)),
                            waits=[t_pe] if l == 0 else (), sig=(l == 31))
                    t_bc = B.op("vector", lambda e, bkH=bkH, kv=kv: e.tensor_copy(out=bcol[:, kv:kv + 1], in_=bkH[:, 256:257]),
                                waits=[t_b], sig=True)
                    t_s = B.op("scalar", lambda e, bkH=bkH, kv=kv: e.activation(
                        out=hs[kv][:, 0:255], in_=bkH[:, 0:255], func=AF.Silu, bias=bcol[:, kv:kv + 1]), waits=[t_bc, t_h, t_hz], sig=True)
                    bank_free[(bi - 1) % 4] = t_s
                    if kv == 0:
                        t_k = B.op("tensor", lambda e: e.matmul(psM[:, 0:255], w2kv[:, 0:2, :].rearrange("p a b -> p (a b)"), hs[0][:, 0:255],
                                                                start=True, stop=True), waits=[t_s, t_w2], sig=True)
                        B.op("vector", lambda e: e.tensor_copy(out=kcT[:, 0:255], in_=psM[:, 0:255]), waits=[t_k], sig=True)
                    else:
                        for ct in range(2):
                            t_v = B.op("tensor", lambda e, ct=ct: e.matmul(psT[:, ct * 64:(ct + 1) * 64], hs[1][:, ct * 128:(ct + 1) * 128],
                                                                          w2kv[:, 2, :], start=True, stop=True),
                                       waits=[t_s, t_w2] if ct == 0 else (), sig=(ct == 1))
                        B.op("vector", lambda e: e.tensor_copy(out=rcmp[:, :, 0:64], in_=v3(psT[:, 0:128], 2)), waits=[t_v], sig=True)
                B.run()

            with ExitStack() as es2:
                imp = es2.enter_context(nc.sbuf_tensor(f"n_imp{g}", [128, NT, 64], F32))
                if stage >= 3:
                    B = Blk(nc, f"bC{g}")
                    QB = [(QA, 0), (KW, 64)]
                    t_w0 = _load_wq(B, T, WqD[0], g * 4)
                    bfree4 = [None] * 4
                    ops0, st0 = _proj_q_ops(B, T, WqD[0], t_w0, QA, 0, banks4, bfree4, evac="scalar")
                    for f, _c in ops0:
                        f()
                    q_ready = st0["last_ev"]
                    prev = None
                    pbfree = [None, None]
                    wq_last = [st0["last_mm"], None]
                    for hh in range(4):
                        hB = g * 4 + hh
                        Qb, r0 = QB[hh % 2]
                        pq = None
                        if hh < 3:
                            Qn, rn = QB[(hh + 1) % 2]
                            t_wn = _load_wq(B, T, WqD[(hh + 1) % 2], hB + 1, waits=[wq_last[(hh + 1) % 2]])
                            pq, stn = _proj_q_ops(B, T, WqD[(hh + 1) % 2], t_wn, Qn, rn, [psM, psT], pbfree, evac="vector")
                        groups = []
                        gi_tile = 0
                        for qt in range(8):
                            aset = qt % 2
                            acc = v3(psO[aset][:], 4)
                            tiles = []
                            first = True
                            for ct in range(qt // 4 + 1):
                                sb_ = SB4[gi_tile % 4]
                                pt_ = PT[gi_tile % 4]
                                pv = []
                                for qs in range(4):
                                    pv.append((acc[:, qs, 0:65], pt_[:, qs * 128:(qs + 1) * 128], rcmp[:, ct, 64:129], first))
                                    first = False
                                tiles.append(dict(
                                    smm=[(sb_[:], kcT[r0:r0 + 64, ct * 128:(ct + 1) * 128], Qb[r0:r0 + 64, qt * 512:(qt + 1) * 512])],
                                    act=(sb_[:], pt_[:], None),
                                    mask=(pt_[:], cmask[:, qt % 4, :]) if ct == qt // 4 else None,
                                    pv=pv))
                                gi_tile += 1

                            def mk_epi(qt, aset, acc, hh):
                                def epi(B, tk):
                                    rd = rden[:, aset * 4:aset * 4 + 4].unsqueeze(2)
                                    t1 = B.op("vector", lambda e: e.tensor_scalar(out=rd, in0=acc[:, :, 0:1], scalar1=1e-30, scalar2=None,
                                                                                op0=ALU.max), waits=[tk], sig=True)
                                    t2 = B.op("vector", lambda e: e.reciprocal(out=rd, in_=rd), waits=[t1], sig=True)
                                    dst = imp[:, 4 * qt:4 * qt + 4, :]
                                    if hh == 0:
                                        t3 = B.op("vector", lambda e: e.tensor_tensor(out=dst, in0=acc[:, :, 1:65], in1=rd.to_broadcast([128, 4, 64]),
                                                                                    op=ALU.mult), waits=[t2], sig=True)
                                    else:
                                        t3 = B.op("vector", lambda e: e.tensor_tensor(out=tA[:], in0=acc[:, :, 1:65], in1=rd.to_broadcast([128, 4, 64]),
                                                                                    op=ALU.mult), waits=[t2], sig=True)
                                        B.op("vector", lambda e: e.tensor_tensor(out=dst, in0=dst, in1=tA[:], op=ALU.add), waits=[t3], sig=True)
                                    return t3
                                return epi
                            groups.append(dict(tiles=tiles, epi=mk_epi(qt, aset, acc, hh), post=None))
                        groups[0]["tiles"][0]["extra_wait"] = q_ready
                        if pq is not None:
                            fl = _budget_filler(pq, sum(c_ for _f, c_ in pq) / max(1, gi_tile - 3))
                        else:
                            fl = None
                        prev = emit_attention(B, T, groups, filler=fl, nS=4, nPT=4, prev=prev)
                        if pq is not None:
                            while pq:
                                pq.pop(0)[0]()
                            q_ready = stn["last_ev"]
                            wq_last[(hh + 1) % 2] = stn["last_mm"]
                    B.run()

                if stage < 4:
                    continue
                B = Blk(nc, f"bS{g}")
                B.op("vector", lambda e: e.memset(maskp[0][:], 0.0))
                B.op("vector", lambda e: e.memset(maskp[1][:], 0.0))
                t_a = B.op("vector", lambda e: e.tensor_tensor(out=imp[:], in0=imp[:], in1=T.addm_slc[:], op=ALU.add), sig=True)
                tk = None
                for t4 in range(0, NT, 4):
                    t1s = [B.op("vector", lambda e, tt=tt: e.max(out=m16[:, tt, 0:8], in_=imp[:, tt, :]), waits=[t_a], sig=True)
                           for tt in range(t4, t4 + 4)]
                    t2s = [B.op("vector", lambda e, tt=tt: e.match_replace(out=wk4[:, tt % 4, :], in_to_replace=m16[:, tt, 0:8],
                                                                        in_values=imp[:, tt, :], imm_value=-1e30),
                                waits=[t1s[tt - t4]], sig=True) for tt in range(t4, t4 + 4)]
                    for tt in range(t4, t4 + 4):
                        tk = B.op("vector", lambda e, tt=tt: e.max(out=m16[:, tt, 8:16], in_=wk4[:, tt % 4, :]), waits=[t2s[tt - t4]], sig=True)
                mp_free = [None, None]
                ps_free = [None, None]
                for c4 in range(8):
                    b = c4 % 2
                    t_m = B.op("vector", lambda e, c4=c4, b=b: e.tensor_tensor(
                        out=maskp[b][:, :, 64:128], in0=imp[:, 4 * c4:4 * c4 + 4, :],
                        in1=m16[:, 4 * c4:4 * c4 + 4, 15:16].to_broadcast([128, 4, 64]), op=ALU.is_lt), waits=[tk, mp_free[b]], sig=True)
                    for j in range(4):
                        t_tr = B.op("tensor", lambda e, b=b, j=j: e.matmul(psS[b][:, j * 128:(j + 1) * 128], maskp[b][:, j, :], ident[:],
                                                                         start=True, stop=True),
                                    waits=[t_m, ps_free[b]] if j == 0 else (), sig=(j == 3))
                    mp_free[b] = t_tr
                    ps_free[b] = B.op("scalar", lambda e, b=b, c4=c4: e.copy(out=KW[64:128, c4 * 512:(c4 + 1) * 512], in_=psS[b][64:128, :]),
                                      waits=[t_tr], sig=True)
                B.run()

            es3 = ExitStack()
            QB2 = es3.enter_context(nc.sbuf_tensor(f"n_QB2{g}", [128, S], BF16))
            QBUF = [QA, QB2]
            for hh in range(4 if stage >= 5 else 0):
                hB = g * 4 + hh
                pair, off = 4 + hB // 2, (hB % 2) * 64
                Qc = QBUF[hh % 2]
                B = Blk(nc, f"bN{hB}")
                if hh == 0:
                    t_w = _load_wq(B, T, WqD[0], hB)
                    bfree4 = [None] * 4
                    ops0, st0 = _proj_q_ops(B, T, WqD[0], t_w, Qc, 0, banks4, bfree4, evac="scalar")
                    for f, _c in ops0:
                        f()
                    q_ev = st0["last_ev"]
                    t_mc = B.op("vector", lambda e: e.tensor_copy(out=Qc[64:128, :], in_=KW[64:128, :]), sig=True)
                else:
                    q_ev, t_mc = None, None
                nxt_ops = None
                if hh < 3:
                    Qn = QBUF[(hh + 1) % 2]
                    t_wn = _load_wq(B, T, WqD[1], hB + 1)
                    t_mcn = B.op("gpsimd", lambda e, Qn=Qn: e.tensor_copy(out=Qn[64:128, :], in_=KW[64:128, :]), sig=True)
                    nfree = [None]
                    nxt_ops, nst = _proj_q_ops(B, T, WqD[1], t_wn, Qn, 0, [psT], nfree, evac="vector")
                groups = []
                gi_tile = 0
                for qt in range(8):
                    aset = qt % 2
                    accS = v3(psO[aset][:], 4)
                    accW = v3(psO2[0][:], 4)
                    accC = v3(psM[:], 4)
                    tiles = []
                    firstS, firstW, firstC = True, True, True
                    for dl in range(4 * qt + 3, -1, -1):
                        sb_ = SB3[gi_tile % 3]
                        pt_ = PT[gi_tile % 4]
                        qsv = [qs for qs in range(4) if 4 * qt + qs - dl >= 0]
                        smm, pv = [], []
                        for qs in qsv:
                            kt, tq = 4 * qt + qs - dl, 4 * qt + qs
                            smm.append((sb_[:, qs * 128:(qs + 1) * 128], KA[:, kt * 128:(kt + 1) * 128], Qc[:, tq * 128:(tq + 1) * 128]))
                            pv.append((accS[:, qs, 0:65], pt_[:, qs * 128:(qs + 1) * 128], VA[:, kt, :], firstS))
                            firstS = False
                        q0, q1 = qsv[0], qsv[-1] + 1
                        tiles.append(dict(smm=smm, act=(sb_[:, q0 * 128:q1 * 128], pt_[:, q0 * 128:q1 * 128], btab[:, hB, dl:dl + 1]),
                                          mask=(v3(pt_[:], 4), tri[:, 0:1, :].to_broadcast([128, 4, 128])) if dl == 0 else None, pv=pv))
                        gi_tile += 1
                    for dl in range(4, -1, -1):
                        qsv = [qs for qs in range(4) if 4 * qt + qs - dl >= 0]
                        if not qsv:
                            continue
                        sb_ = SB3[gi_tile % 3]
                        pt_ = PT[gi_tile % 4]
                        smm, pv = [], []
                        for qs in qsv:
                            kt, tq = 4 * qt + qs - dl, 4 * qt + qs
                            smm.append((sb_[:, qs * 128:(qs + 1) * 128], KW[0:64, kt * 128:(kt + 1) * 128], Qc[0:64, tq * 128:(tq + 1) * 128]))
                            pv.append((accW[:, qs, 0:65], pt_[:, qs * 128:(qs + 1) * 128], VW[:, kt, :], firstW))
                            firstW = False
                        q0, q1 = qsv[0], qsv[-1] + 1
                        mk = None
                        if dl == 0 or dl == 4:
                            mi = 0 if dl == 0 else 1
                            mk = (v3(pt_[:], 4)[:, q0:q1, :], tri[:, mi:mi + 1, :].to_broadcast([128, q1 - q0, 128]))
                        tiles.append(dict(smm=smm, act=(sb_[:, q0 * 128:q1 * 128], pt_[:, q0 * 128:q1 * 128], btab[:, hB, dl:dl + 1]),
                                          mask=mk, pv=pv, cmp_first=(dl == 0)))
                        gi_tile += 1
                    for ct in range(qt // 4 + 1):
                        sb_ = SB3[gi_tile % 3]
                        pt_ = PT[gi_tile % 4]
                        pv = []
                        for qs in range(4):
                            pv.append((accC[:, qs, 0:65], pt_[:, qs * 128:(qs + 1) * 128], rcmp[:, ct, 0:65], firstC))
                            firstC = False
                        tiles.append(dict(
                            smm=[(sb_[:], kcT[0:64, ct * 128:(ct + 1) * 128], Qc[0:64, qt * 512:(qt + 1) * 512])],
                            act=(sb_[:], pt_[:], None),
                            mask=(pt_[:], cmask[:, qt % 4, :]) if ct == qt // 4 else None,
                            pv=pv, cmp_first=(ct == 0)))
                        gi_tile += 1
                    groups.append(dict(qt=qt, aset=aset, tiles=tiles, accs=(accC, accS, accW)))

                epi_tok = {}
                st_ost = [None, None]
                post_state = {"psT_free": None}

                def mk_epi(qt, aset, accs):
                    def epi(B, tk):
                        dd = d3[aset]
                        for br in range(3):
                            t1 = B.op("vector", lambda e, br=br: e.tensor_scalar(out=dd[:, :, br:br + 1], in0=accs[br][:, :, 64:65], scalar1=1e-30,
                                                                               scalar2=None, op0=ALU.max), waits=[tk], sig=(br == 2))
                        t2 = B.op("vector", lambda e: e.reciprocal(out=dd[:], in_=dd[:]), waits=[t1], sig=True)
                        t3 = B.op("vector", lambda e: e.tensor_tensor(out=dd[:], in0=dd[:], in1=gates[:, 4 * qt:4 * qt + 4, 3 * hB:3 * hB + 3],
                                                                    op=ALU.mult), waits=[t2], sig=True)
                        t4 = B.op("vector", lambda e: e.tensor_tensor(out=tA[:], in0=accs[0][:, :, 0:64],
                                                                    in1=dd[:, :, 0:1].to_broadcast([128, 4, 64]), op=ALU.mult), waits=[t3], sig=True)
                        t5 = B.op("vector", lambda e: e.tensor_tensor(out=tB[:], in0=accs[1][:, :, 0:64],
                                                                    in1=dd[:, :, 1:2].to_broadcast([128, 4, 64]), op=ALU.mult), waits=[t3], sig=True)
                        t6 = B.op("vector", lambda e: e.tensor_tensor(out=tA[:], in0=tA[:], in1=tB[:], op=ALU.add), waits=[t4, t5], sig=True)
                        t7 = B.op("vector", lambda e: e.tensor_tensor(out=tB[:], in0=accs[2][:, :, 0:64],
                                                                    in1=dd[:, :, 2:3].to_broadcast([128, 4, 64]), op=ALU.mult), waits=[t6], sig=True)
                        t8 = B.op("vector", lambda e: e.tensor_tensor(out=Ost[aset][:, :, off:off + 64], in0=tA[:], in1=tB[:], op=ALU.add),
                                  waits=[t7, st_ost[aset]], sig=True)
                        epi_tok[qt] = t8
                        return t7
                    return epi

                def mk_post(qt, aset):
                    def post(B):
                        for qs in range(4):
                            t_tr = B.op("tensor", lambda e, qs=qs: e.matmul(psT[:, qs * 128:(qs + 1) * 128], Ost[aset][:, qs, :], ident[:],
                                                                           start=True, stop=True),
                                        waits=[epi_tok[qt], post_state["psT_free"]] if qs == 0 else (), sig=(qs == 3))
                        st_ost[aset] = t_tr
                        post_state["psT_free"] = B.op("vector", lambda e: e.tensor_copy(
                            out=OT[off:off + 64, pair, qt * 512:(qt + 1) * 512], in_=psT[off:off + 64, :]), waits=[t_tr], sig=True)
                    return post

                for gr in groups:
                    gr["epi"] = mk_epi(gr["qt"], gr["aset"], gr["accs"])
                    gr["post"] = mk_post(gr["qt"], gr["aset"])
                if hh == 0:
                    groups[0]["tiles"][0]["extra_wait"] = [q_ev, t_mc]
                junk = _mk_filler(lambda: psT[:, 0:128], ident, True, nf=NFILL2, wait_fn=lambda: post_state["psT_free"])
                if nxt_ops is not None:
                    ntl = sum(len(gr["tiles"]) for gr in groups)
                    every = max(1, (ntl - 10) // 8)
                    fst = {"n": 0}

                    def filler(B, nxt_ops=nxt_ops, nfree=nfree, nst=nst):
                        fst["n"] += 1
                        if nxt_ops and fst["n"] % every == 0:
                            nfree[0] = post_state["psT_free"]
                            for _ in range(8):
                                nxt_ops.pop(0)[0]()
                            post_state["psT_free"] = nst["last_ev"]
                        elif junk is not None and os.environ.get("JUNK2", "0") == "1":
                            junk(B)
                    emit_attention(B, T, groups, single_acc_key="cmp_first", nS=3, nPT=4, filler=filler)
                    while nxt_ops:
                        nfree[0] = post_state["psT_free"]
                        for _ in range(8):
                            nxt_ops.pop(0)[0]()
                        post_state["psT_free"] = nst["last_ev"]
                    B.op("sync", lambda e: e.nop(), waits=[nst["last_ev"], t_mcn])
                else:
                    emit_attention(B, T, groups, single_acc_key="cmp_first", nS=3, nPT=4, filler=junk)
                B.run()
                if hB % 2 == 1:
                    gating_block(f"bZ{pair}", pair, wz, zs)
            es3.close()


NFILL = [4]


NFILL2 = "NFILL2"


def _mk_filler(dst_fn, ident, start, m=128, nf=None, wait_fn=None):
    import os
    nfill = int(os.environ.get("NFILL", NFILL[0]))
    if nf == NFILL2:
        nfill = int(os.environ.get("NFILL2", 3))
    if nfill <= 0:
        return None

    def filler(B):
        dst = dst_fn()
        n = dst.shape[-1]
        for k in range(nfill):
            w = [wait_fn()] if (wait_fn is not None and k == 0) else ()
            B.op("tensor", lambda e: e.matmul(dst, ident[:, 0:m], ident[:, 0:n], start=start, stop=start), waits=w)
    return filler


def _load_wq(B, T, WqD_i, hB, waits=()):
    sw_ = B.newsem()
    B.dma("gpsimd", WqD_i[:, :, 0:64], T.dram["wQB"][hB], sw_, waits=list(waits))
    return B.dma("gpsimd", WqD_i[:, :, 64:128], T.dram["wQB"][hB], sw_, waits=list(waits))


def _proj_q_ops(B, T, Wq, t_w, dst, r0, banks, bank_free, evac="vector"):
    state = {"bi": 0, "first": True, "last_ev": None}

    def mk(qt, c):
        def f():
            bi = state["bi"]
            bk = banks[bi % len(banks)]
            w = ()
            if c == 0:
                w = [bank_free[bi % len(banks)]] + ([t_w] if state["first"] else [])
                state["first"] = False
            tk = B.op("tensor", lambda e: e.matmul(bk[:, :], Wq[:, c, :], T.hT[:, c, qt * 512:(qt + 1) * 512],
                                                  start=(c == 0), stop=(c == 7)), waits=w, sig=(c == 7))
            if c == 7:
                if evac == "vector":
                    t_e = B.op("vector", lambda e: e.tensor_scalar(out=dst[r0:r0 + 64, qt * 512:(qt + 1) * 512], in0=bk[r0:r0 + 64, :],
                                                                 scalar1=0.125, scalar2=None, op0=ALU.mult), waits=[tk], sig=True)
                else:
                    t_e = B.op("scalar", lambda e: e.activation(out=dst[r0:r0 + 64, qt * 512:(qt + 1) * 512], in_=bk[r0:r0 + 64, :],
                                                              func=AF.Copy, scale=0.125), waits=[tk], sig=True)
                bank_free[bi % len(banks)] = t_e
                state["last_ev"] = t_e
                state["last_mm"] = tk
                state["bi"] += 1
        return f
    return [(mk(qt, c), 1.0) for qt in range(8) for c in range(8)], state


def _budget_filler(pq, budget):
    st = {"acc": 0.0}

    def filler(B):
        st["acc"] += budget
        while pq and st["acc"] > 0:
            f, c_ = pq.pop(0)
            f()
            st["acc"] -= c_
    return filler


def _build_final(nc, hT, OT, psS, psO, dram, x, out, epsc, psO2=None, psM=None, psT=None):
    with ExitStack() as esF:
        def sbf(name, shape, dt):
            return esF.enter_context(nc.sbuf_tensor("f_" + name, list(shape), dt))
        wO = sbf("wO", [128, 8, D], BF16)
        gpost = sbf("gpost", [128, D], F32)
        xt = [sbf(f"xt{i}", [128, 2, D], F32) for i in range(3)]
        yt = [sbf(f"yt{i}", [128, 2, D], F32) for i in range(2)]
        junk = sbf("junk", [128, 512], BF16)
        ssq = sbf("ssq", [128, NT, 2], F32)
        rs = sbf("rs", [128, NT], F32)
        B = Blk(nc, "bF")
        sw, sg = B.newsem(), B.newsem("sync")
        t_w = [B.dma("gpsimd", wO[:, c, :], dram["wO"][:, c, :], sw) for c in range(8)]
        t_g = B.dma("sync", gpost[:], dram["gpost"][:], sg)
        xs = [B.newsem("sync") for _ in range(3)]
        os_ = [B.newsem("sync") for _ in range(2)]
        pY = [(psS[0], psS[1]), (psO[0], psO[1]), (psO2[0], psO2[1]), (psM, psT)]
        ps_free = [None] * 4
        xt_free = [None, None, None]
        yt_free = [None, None]
        t_xs = [None] * (NT // 2)
        for tp in range(NT // 2):
            b3 = tp % 3
            by = tp % 2
            xv = x[tp * 256:(tp + 1) * 256, :].rearrange("(n p) d -> p n d", p=128)
            ov = out[tp * 256:(tp + 1) * 256, :].rearrange("(n p) d -> p n d", p=128)
            if tp == 0:
                for t2 in range(2):
                    xv2 = x[t2 * 256:(t2 + 1) * 256, :].rearrange("(n p) d -> p n d", p=128)
                    t_xs[t2] = B.dma("sync", xt[t2 % 3][:], xv2, xs[t2 % 3])
            t_x = t_xs[tp]
            for j in range(2):
                tt = tp * 2 + j
                b = tt % 4
                for half in range(2):
                    pk = pY[b][half]
                    for c in range(8):
                        t_mm = B.op("tensor", lambda e, pk=pk, c=c, tt=tt, half=half: e.matmul(
                            pk[:], OT[:, c, tt * 128:(tt + 1) * 128], wO[:, c, half * 512:(half + 1) * 512], start=(c == 0), stop=(c == 7)),
                            waits=(t_w + [ps_free[b]]) if (c == 0 and half == 0) else (), sig=(c == 7 and half == 1))
                for half in range(2):
                    t_sq = B.op("scalar", lambda e, half=half, b=b, tt=tt: e.activation(
                        out=junk[:], in_=pY[b][half][:], func=AF.Square, accum_out=ssq[:, tt, half:half + 1]), waits=[t_mm], sig=True)
                t_a = B.op("vector", lambda e, tt=tt: e.tensor_tensor(out=rs[:, tt:tt + 1], in0=ssq[:, tt, 0:1], in1=ssq[:, tt, 1:2], op=ALU.add),
                           waits=[t_sq], sig=True)
                t_r1 = B.op("scalar", lambda e, tt=tt: e.activation(out=rs[:, tt:tt + 1], in_=rs[:, tt:tt + 1], func=AF.Sqrt,
                                                                  bias=epsc[:, 0:1], scale=1.0 / D), waits=[t_a], sig=True)
                t_r2 = B.op("vector", lambda e, tt=tt: e.reciprocal(out=rs[:, tt:tt + 1], in_=rs[:, tt:tt + 1]), waits=[t_r1], sig=True)
                for half in range(2):
                    t_y = B.op("vector", lambda e, half=half, b=b, tt=tt, by=by, j=j: e.scalar_tensor_tensor(
                        out=yt[by][:, j, half * 512:(half + 1) * 512], in0=pY[b][half][:], scalar=rs[:, tt:tt + 1],
                        in1=gpost[:, half * 512:(half + 1) * 512], op0=ALU.mult, op1=ALU.mult),
                        waits=[t_r2, t_g, yt_free[by]] if half == 0 else (), sig=True)
                ps_free[b] = t_y
            t_o = B.op("vector", lambda e, by=by, b3=b3: e.tensor_tensor(out=yt[by][:], in0=yt[by][:], in1=xt[b3][:], op=ALU.add),
                       waits=[t_y, t_x], sig=True)
            xt_free[b3] = t_o
            yt_free[by] = B.dma("sync", ov, yt[by][:], os_[by], waits=[t_o])
            if tp + 2 < NT // 2:
                t3 = tp + 2
                xv2 = x[t3 * 256:(t3 + 1) * 256, :].rearrange("(n p) d -> p n d", p=128)
                t_xs[t3] = B.dma("sync", xt[t3 % 3][:], xv2, xs[t3 % 3], waits=[xt_free[t3 % 3]])
        B.op("sync", lambda e: e.nop(), waits=[yt_free[0], yt_free[1]])
        B.run()


def build(debug=None, stop=None):
    nc = bass.Bass("TRN2", target_bir_lowering=False)
    dram = {}

    def din(name, shape):
        dram[name] = nc.dram_tensor(name, list(shape), F32, kind="ExternalInput").ap()
        return dram[name]

    x = din("x", [S, D])
    cshapes = {k: v.shape for k, v in host_consts().items()}
    for k, shp in cshapes.items():
        din(k, shp)
    wshapes = dict(wA=[8, 128, 3, 8, 64], wZ=[8, 128, 8, 128], wQB=[8, 128, 8, 64], wG=[128, 8, 24],
                   wKV=[2, 128, 8, 384], w1kv=[128, 32, 128], w2kv=[128, 3, 64], peT=[128, 32],
                   wO=[128, 8, 1024], gpre=[128, 8], gpost=[128, D])
    for k, shp in wshapes.items():
        din(k, shp)
    out = nc.dram_tensor("out", [S, D], F32, kind="ExternalOutput").ap()
    dbg = {}
    if debug:
        for name, shp in debug.items():
            dbg[name] = nc.dram_tensor("dbg_" + name, list(shp), F32, kind="ExternalOutput").ap()

    es = ExitStack()

    def sb(name, shape, dt):
        return es.enter_context(nc.sbuf_tensor("s_" + name, list(shape), dt))

    def ps(name, shape, dt=F32):
        return es.enter_context(nc.psum_tensor(name, list(shape), dt))

    with es:
        hT = sb("hT", [128, 8, S], BF16)
        OT = sb("OT", [128, 8, S], BF16)
        ident = sb("ident", [128, 128], BF16)
        addm_moba = sb("addm_moba", [128, NT, 16], F32)
        addm_slc = sb("addm_slc", [128, NT, 64], BF16)
        cmask = sb("cmask", [128, 4, 512], BF16)
        tri = sb("tri", [128, 2, 128], BF16)
        btab = sb("btab", [128, 8, 32], F32)
        gpre = sb("gpre", [128, 8], F32)
        epsc = sb("epsc", [128, 1], F32)
        gates = sb("gates", [128, NT, 24], F32)
        esW = ExitStack()

        def sbw(name, shape, dt):
            return esW.enter_context(nc.sbuf_tensor("w_" + name, list(shape), dt))
        QA = sbw("QA", [128, S], BF16)
        KA = sbw("KA", [128, S], BF16)
        KW = sbw("KW", [128, S], BF16)
        VA = sbw("VA", [128, NT, 65], BF16)
        VW = sbw("VW", [128, NT, 65], BF16)
        PT = [sbw(f"PT{i}", [128, 512], BF16) for i in range(4)]
        Ost = [sbw(f"Ost{i}", [128, 4, 128], BF16) for i in range(2)]
        rden = sbw("rden", [128, 16], F32)
        psS = [ps(f"psS{i}", [128, 512]) for i in range(2)]
        psO = [ps(f"psO{i}", [128, 512]) for i in range(2)]
        psO2 = [ps(f"psO2{i}", [128, 512]) for i in range(2)]
        psM = ps("psM", [128, 512])
        psT = ps("psT", [128, 512])

        POOL[0] = SemPool(nc, es)
        with nc.Block() as blk_clr:
            @blk_clr.gpsimd
            def _(g):
                for sm in POOL[0].all():
                    g.sem_clear(sm.h)

        B = Blk(nc, "b0")
        toks = []
        toks.append(B.dma("gpsimd", ident[:], dram["c_ident"][:], B.newsem()))
        toks.append(B.dma("sync", addm_moba[:], dram["c_addm_moba"][:], B.newsem("sync")))
        toks.append(B.dma("gpsimd", addm_slc[:], dram["c_addm_slc"][:], B.newsem()))
        toks.append(B.dma("gpsimd", cmask[:], dram["c_cmask"][:], B.newsem()))
        toks.append(B.dma("gpsimd", tri[:], dram["c_tri"][:], B.newsem()))
        toks.append(B.dma("sync", btab[:], dram["c_btab"][:], B.newsem("sync")))
        toks.append(B.dma("sync", gpre[:], dram["gpre"][:], B.newsem("sync")))
        B.op("vector", lambda e: e.memset(VA[:, :, 64:65], 1.0))
        B.op("vector", lambda e: e.memset(epsc[:], 1e-6))
        B.op("vector", lambda e: e.memset(VW[:, :, 64:65], 1.0))
        B.op("vector", lambda e: e.memset(QA[64:128, :], 0.0))
        t_ms = B.op("vector", lambda e: e.memset(KA[64:128, :], 0.0), sig=True)
        B.op("gpsimd", lambda e: e.memset(Ost[0][:], 0.0))
        B.op("gpsimd", lambda e: e.memset(Ost[1][:], 0.0))
        B.op("sync", lambda e: e.nop(), waits=toks)
        B.run()

        with ExitStack() as esA:
            xt = [esA.enter_context(nc.sbuf_tensor(f"xt{i}", [128, D], F32)) for i in range(4)]
            junk = esA.enter_context(nc.sbuf_tensor("junkA", [128, D], BF16))
            hb = [esA.enter_context(nc.sbuf_tensor(f"hb{i}", [128, D], BF16)) for i in range(2)]
            ss = esA.enter_context(nc.sbuf_tensor("ssA", [128, NT], F32))
            rs = esA.enter_context(nc.sbuf_tensor("rsA", [128, NT], F32))
            B = Blk(nc, "bA")
            xs = [B.newsem("sync") for _ in range(4)]
            xtok = [None] * NT
            hb_free = [None, None]
            xt_free = [None] * 4
            ps_free = [None, None]
            pA = [(psS[0], psS[1]), (psO[0], psO[1])]
            tr2 = [None] * NT
            tsq = [None] * NT

            def stage0(tt):
                b3 = tt % 4
                xtok[tt] = B.dma("sync", xt[b3][:], x[tt * 128:(tt + 1) * 128, :], xs[b3], waits=[xt_free[b3]])

            def stage1(tt):
                b3 = tt % 4
                t_sq = B.op("scalar", lambda e, b3=b3, tt=tt: e.activation(out=junk[:], in_=xt[b3][:], func=AF.Square,
                                                                     accum_out=ss[:, tt:tt + 1]),
                            waits=[xtok[tt]], sig=True)
                t_r1 = B.op("scalar", lambda e, tt=tt: e.activation(out=rs[:, tt:tt + 1], in_=ss[:, tt:tt + 1], func=AF.Sqrt,
                                                                  bias=epsc[:, 0:1], scale=1.0 / D),
                            waits=[t_sq], sig=True)
                tr2[tt] = B.op("vector", lambda e, tt=tt: e.reciprocal(out=rs[:, tt:tt + 1], in_=rs[:, tt:tt + 1]),
                               waits=[t_r1], sig=True)
                tsq[tt] = t_sq

            def stage2(tt):
                b3 = tt % 4
                b2 = tt % 2
                t_h = B.op("scalar", lambda e, tt=tt, b3=b3, b2=b2: e.activation(
                    out=hb[b2][:], in_=xt[b3][:], func=AF.Copy, scale=rs[:, tt:tt + 1]),
                    waits=[tr2[tt], hb_free[b2], tsq[tt]], sig=True)
                xt_free[b3] = t_h
                pa, pb = pA[b2]
                for c in range(8):
                    dst = (pa if c < 4 else pb)[:, (c % 4) * 128:(c % 4 + 1) * 128]
                    t_tr = B.op("tensor", lambda e, dst=dst, b2=b2, c=c: e.matmul(
                        dst, hb[b2][:, c * 128:(c + 1) * 128], ident[:], start=True, stop=True),
                        waits=[t_h, ps_free[b2]], sig=(c == 7))
                hb_free[b2] = t_tr
                B.op("vector", lambda e, tt=tt, pa=pa: e.tensor_tensor(
                    out=hT[:, 0:4, tt * 128:(tt + 1) * 128], in0=pa[:].rearrange("p (c t) -> p c t", c=4),
                    in1=gpre[:, 0:4].unsqueeze(2).to_broadcast([128, 4, 128]), op=ALU.mult), waits=[t_tr])
                ps_free[b2] = B.op("vector", lambda e, tt=tt, pb=pb: e.tensor_tensor(
                    out=hT[:, 4:8, tt * 128:(tt + 1) * 128], in0=pb[:].rearrange("p (c t) -> p c t", c=4),
                    in1=gpre[:, 4:8].unsqueeze(2).to_broadcast([128, 4, 128]), op=ALU.mult), waits=[t_tr], sig=True)

            for t0 in range(3):
                stage0(t0)
            stage1(0)
            stage1(1)
            for tt in range(NT):
                if tt + 3 < NT:
                    stage0(tt + 3)
                if tt + 2 < NT:
                    stage1(tt + 2)
                stage2(tt)
            B.run()


        if stop == "A":
            pass
        else:
            _build_rest(nc, locals(), debug, dbg, stop)
        esW.close()
        if stop is None or (isinstance(stop, dict) and stop.get("final")):
            _build_final(nc, hT, OT, psS, psO, dram, x, out, epsc, psO2, psM, psT)

        if debug and "hT" in debug:
            with ExitStack() as esD:
                tmp = esD.enter_context(nc.sbuf_tensor("dbgtmp", [128, 8, 512], F32))
                B = Blk(nc, "bD")
                s1 = B.newsem("sync")
                tk = None
                for q in range(8):
                    t1 = B.op("vector", lambda e, q=q: e.tensor_copy(out=tmp[:], in_=hT[:, :, q * 512:(q + 1) * 512]),
                              waits=[tk], sig=True)
                    tk = B.dma("sync", dbg["hT"][:, :, q * 512:(q + 1) * 512], tmp[:], s1, waits=[t1])
                B.op("sync", lambda e: e.nop(), waits=[tk])
                B.run()
    return nc


_CACHE = {}


def kernel(x, pre_norm_g, post_norm_g, w_in, cmp_pos_k, cmp_pos_v, w_cmp_k1, w_cmp_k2, w_cmp_v1, w_cmp_v2, w_out):
    x = np.asarray(x, np.float32)
    consts = host_consts()
    wts = host_weights(*(np.asarray(a, np.float32) for a in (pre_norm_g, post_norm_g, w_in, cmp_pos_k, cmp_pos_v,
                                                            w_cmp_k1, w_cmp_k2, w_cmp_v1, w_cmp_v2, w_out)))
    nc = build()
    in_maps = []
    for b in range(8):
        m = {"x": np.ascontiguousarray(x[b])}
        m.update(consts)
        m.update(wts)
        in_maps.append(m)
    res = run_bass_kernel_spmd(nc, in_maps, core_ids=list(range(8)))
    return np.stack([r["out"] for r in res.results], 0).astype(np.float32)
```
